# Optimizing a Trainium2 kernel written in Bass

```python
import math
import jax
import jax.numpy as jnp
from jax import lax
import numpy as np


D_MODEL = 1024
BATCH = 8
SEQ = 4096
DEPTH = 4

GRID_W = 64
CTX_LEN = 256
HEAD_DIM = 64
ROPE_THETA = 10000.0
Q_BLOCK = 128
EPS = 1e-6
ADA_CHUNKS = 6

GQA_HEADS = 8
GQA_KV_HEADS = 2
DIFF_HEADS = 4
DIFF_V_DIM = 2 * HEAD_DIM
CONV_CH = 512
CONV_WIDTH = 31
HGRN_HEADS = 8
HGRN_DK = 64
HGRN_DV = 64
HGRN_CHUNK = 64
N_BRANCH = 4
BRANCH_W = 512

N_EXPERTS = 8
TOP_K = 2
D_FF = 3584
N_DENSE = (DEPTH + 1) // 2
N_MOE = DEPTH // 2

SPLIT_SIZES = (
    GQA_HEADS * HEAD_DIM,
    GQA_KV_HEADS * HEAD_DIM,
    GQA_KV_HEADS * HEAD_DIM,
    DIFF_HEADS * 2 * HEAD_DIM,
    DIFF_HEADS * 2 * HEAD_DIM,
    DIFF_HEADS * DIFF_V_DIM,
    2 * CONV_CH,
    HGRN_HEADS * HGRN_DK,
    HGRN_HEADS * HGRN_DK,
    HGRN_HEADS * HGRN_DK,
    HGRN_HEADS * HGRN_DV,
    HGRN_HEADS * HGRN_DV,
    N_BRANCH * D_MODEL,
)
SPLIT_POINTS = tuple(int(v) for v in np.cumsum(SPLIT_SIZES)[:-1])
D_IN = int(sum(SPLIT_SIZES))

kernel_name = 'hybrid_gated_branch_dit_moe'


def _rmsnorm(x, g):
    xf = x.astype(jnp.float32)
    y = xf * lax.rsqrt(jnp.mean(jnp.square(xf), axis=-1, keepdims=True) + EPS)
    return (y * g.astype(jnp.float32)).astype(x.dtype)


def _layernorm(x, g, b):
    xf = x.astype(jnp.float32)
    mu = jnp.mean(xf, axis=-1, keepdims=True)
    var = jnp.mean(jnp.square(xf - mu), axis=-1, keepdims=True)
    y = (xf - mu) * lax.rsqrt(var + EPS) * g.astype(jnp.float32) + b.astype(jnp.float32)
    return y.astype(x.dtype)


def _modulate(h, shift, scale):
    return h * (1 + scale) + shift


def _swiglu(h, w1, w3, w2):
    return (jax.nn.silu(h @ w1) * (h @ w3)) @ w2


def _axial_rope_tables(n_tokens, rows):
    row = jnp.repeat(jnp.arange(rows, dtype=jnp.float32), GRID_W)
    col = (jnp.arange(n_tokens) % GRID_W).astype(jnp.float32)
    axis_dim = HEAD_DIM // 2
    inv_freq = ROPE_THETA ** (-jnp.arange(0, axis_dim, 2, dtype=jnp.float32) / axis_dim)
    ang_r = row[:, None] * inv_freq
    ang_c = col[:, None] * inv_freq
    return (jnp.cos(ang_r), jnp.sin(ang_r), jnp.cos(ang_c), jnp.sin(ang_c))


def _rope_1d(x, cos, sin):
    x1, x2 = jnp.split(x, 2, axis=-1)
    cos = cos[:, None, :].astype(x.dtype)
    sin = sin[:, None, :].astype(x.dtype)
    return jnp.concatenate([x1 * cos - x2 * sin, x2 * cos + x1 * sin], axis=-1)


def _rope_axial(x, rope):
    cos_r, sin_r, cos_c, sin_c = rope
    xr, xc = jnp.split(x, 2, axis=-1)
    return jnp.concatenate([_rope_1d(xr, cos_r, sin_r), _rope_1d(xc, cos_c, sin_c)], axis=-1)


def _scores(q, k):
    return jnp.einsum('bhgqd,bhkd->bhgqk', q, k).astype(jnp.float32) * (q.shape[-1] ** -0.5)


def _dense_attend(q, k, v):
    p = jax.nn.softmax(_scores(q, k), axis=-1).astype(v.dtype)
    return jnp.einsum('bhgqk,bhkd->bhgqd', p, v)


def _block_attend(q, k_x, v_x, k_c, v_c):
    b, h, g, s, d = q.shape
    n_blk = s // Q_BLOCK
    q_blocks = jnp.moveaxis(q.reshape(b, h, g, n_blk, Q_BLOCK, d), 3, 0)
    n_x = k_x.shape[2]

    def one_block(qi):
        sc = jnp.concatenate([_scores(qi, k_x), _scores(qi, k_c)], axis=-1)
        p = jax.nn.softmax(sc, axis=-1).astype(v_x.dtype)
        return (jnp.einsum('bhgqk,bhkd->bhgqd', p[..., :n_x], v_x)
                + jnp.einsum('bhgqk,bhkd->bhgqd', p[..., n_x:], v_c))

    o = lax.map(one_block, q_blocks)
    return jnp.moveaxis(o, 0, 3).reshape(b, h, g, s, v_x.shape[-1])


def _gqa_branch(aq, ak, av, g_q, g_k, rope, n_ctx, need_ctx):
    b, t, _ = aq.shape
    q = _rmsnorm(aq.reshape(b, t, GQA_HEADS, HEAD_DIM), g_q)
    k = _rmsnorm(ak.reshape(b, t, GQA_KV_HEADS, HEAD_DIM), g_k)
    v = av.reshape(b, t, GQA_KV_HEADS, HEAD_DIM)
    q_x = _rope_axial(q[:, n_ctx:], rope)
    k_x = _rope_axial(k[:, n_ctx:], rope)
    grp = GQA_HEADS // GQA_KV_HEADS

    def q_heads(u):
        return u.reshape(b, u.shape[1], GQA_KV_HEADS, grp, HEAD_DIM).transpose(0, 2, 3, 1, 4)

    def kv_heads(u):
        return u.transpose(0, 2, 1, 3)

    k_c, v_c = kv_heads(k[:, :n_ctx]), kv_heads(v[:, :n_ctx])
    o = _block_attend(q_heads(q_x), kv_heads(k_x), kv_heads(v[:, n_ctx:]), k_c, v_c)
    if need_ctx:
        o = jnp.concatenate([_dense_attend(q_heads(q[:, :n_ctx]), k_c, v_c), o], axis=3)
    return o.transpose(0, 3, 1, 2, 4).reshape(b, o.shape[3], BRANCH_W)


def _diff_branch(bq, bk, bv, g_q, g_k, lam_p, subln_g, lam_init, rope, n_ctx, need_ctx):
    b, t, _ = bq.shape
    q = _rmsnorm(bq.reshape(b, t, 2 * DIFF_HEADS, HEAD_DIM), g_q)
    k = _rmsnorm(bk.reshape(b, t, 2 * DIFF_HEADS, HEAD_DIM), g_k)
    v = bv.reshape(b, t, DIFF_HEADS, DIFF_V_DIM).transpose(0, 2, 1, 3)
    q_x = _rope_axial(q[:, n_ctx:], rope)
    k_x = _rope_axial(k[:, n_ctx:], rope)
    q_c, k_c = q[:, :n_ctx], k[:, :n_ctx]
    v_c, v_x = v[:, :, :n_ctx], v[:, :, n_ctx:]

    def sub(u, i):
        return u.reshape(b, u.shape[1], DIFF_HEADS, 2, HEAD_DIM)[:, :, :, i].transpose(0, 2, 1, 3)

    lp = lam_p.astype(jnp.float32)
    lam = (jnp.exp(jnp.sum(lp[0] * lp[1])) - jnp.exp(jnp.sum(lp[2] * lp[3])) + lam_init).astype(bv.dtype)

    def latent_map(i):
        return _block_attend(sub(q_x, i)[:, :, None], sub(k_x, i), v_x, sub(k_c, i), v_c)

    o = latent_map(0) - lam * latent_map(1)
    if need_ctx:
        o_c = (_dense_attend(sub(q_c, 0)[:, :, None], sub(k_c, 0), v_c)
               - lam * _dense_attend(sub(q_c, 1)[:, :, None], sub(k_c, 1), v_c))
        o = jnp.concatenate([o_c, o], axis=3)
    o = o[:, :, 0].transpose(0, 2, 1, 3)
    o = _rmsnorm(o, subln_g) * (1.0 - lam_init)
    return o.reshape(b, o.shape[1], BRANCH_W)


def _depthwise_conv(u, w, bias):
    pad = CONV_WIDTH // 2
    y = lax.conv_general_dilated(u, w[:, None, :].astype(u.dtype), window_strides=(1,),
                                 padding=[(pad, pad)], dimension_numbers=('NWC', 'WIO', 'NWC'),
                                 feature_group_count=u.shape[-1])
    return y + bias


def _conv_branch(cglu, w, bias, ln_g, ln_b, n_ctx, need_ctx):
    a, gate = jnp.split(cglu, 2, axis=-1)
    u = a * jax.nn.sigmoid(gate)
    y = _depthwise_conv(u[:, n_ctx:], w, bias)
    if need_ctx:
        y = jnp.concatenate([_depthwise_conv(u[:, :n_ctx], w, bias), y], axis=1)
    return jax.nn.silu(_layernorm(y, ln_g, ln_b))


def _log_forget(z, lb):
    return jnp.logaddexp(jnp.log(lb), jnp.log1p(-lb) + jax.nn.log_sigmoid(z.astype(jnp.float32)))


def _gla_chunk_scan(q, k, logf, v, s0):
    b, h, t, _ = q.shape
    n_chunk = t // HGRN_CHUNK

    def chunks(u):
        return jnp.moveaxis(u.reshape(b, h, n_chunk, HGRN_CHUNK, u.shape[-1]), 2, 0)

    causal = jnp.tril(jnp.ones((HGRN_CHUNK, HGRN_CHUNK), dtype=bool))[:, :, None]

    def step(state, inp):
        qc, kc, gc, vc = inp
        cum = jnp.cumsum(gc, axis=2)
        rel = jnp.where(causal, cum[:, :, :, None, :] - cum[:, :, None, :, :], -jnp.inf)
        att = jnp.einsum('bhtd,bhsd,bhtsd->bhts', qc, kc, jnp.exp(rel))
        o = (jnp.einsum('bhts,bhse->bhte', att, vc)
             + jnp.einsum('bhtd,bhde->bhte', qc * jnp.exp(cum), state))
        end = cum[:, :, -1:, :]
        state = (jnp.exp(end)[:, :, 0, :, None] * state
                 + jnp.einsum('bhsd,bhse->bhde', kc * jnp.exp(end - cum), vc))
        return state, o

    s_final, o = lax.scan(step, s0, (chunks(q), chunks(k), chunks(logf), chunks(v)))
    return jnp.moveaxis(o, 0, 2).reshape(b, h, t, v.shape[-1]), s_final


def _gla_final_state(k, logf, v):
    cum = jnp.cumsum(logf, axis=2)
    return jnp.einsum('bhsd,bhse->bhde', k * jnp.exp(cum[:, :, -1:] - cum), v)


def _take(u, start, stop, rev):
    u = u[:, :, start:stop]
    return jnp.flip(u, axis=2) if rev else u


def _hgrn_branch(dq, df_fwd, df_bwd, di, dg, lb_l, norm_g, n_ctx, need_ctx):
    b, t, _ = dq.shape

    def heads(u):
        return u.reshape(b, t, HGRN_HEADS, -1).transpose(0, 2, 1, 3).astype(jnp.float32)

    q = heads(jax.nn.silu(dq)) * (HGRN_DK ** -0.5)
    v = heads(di)
    outs_c, outs_x = [], []
    for d_idx, fz in enumerate((df_fwd, df_bwd)):
        rev = d_idx == 1
        logf = heads(_log_forget(fz, lb_l[d_idx]))
        k = -jnp.expm1(logf)
        ctx_in = [_take(u, 0, n_ctx, rev) for u in (q, k, logf, v)]
        lat_in = [_take(u, n_ctx, None, rev) for u in (q, k, logf, v)]
        if need_ctx:
            o_c, s_c = _gla_chunk_scan(*ctx_in, jnp.zeros((b, HGRN_HEADS, HGRN_DK, HGRN_DV), jnp.float32))
            outs_c.append(jnp.flip(o_c, axis=2) if rev else o_c)
        else:
            s_c = _gla_final_state(*ctx_in[1:])
        o_x, _ = _gla_chunk_scan(*lat_in, s_c)
        outs_x.append(jnp.flip(o_x, axis=2) if rev else o_x)
    o = outs_x[0] + outs_x[1]
    if need_ctx:
        o = jnp.concatenate([outs_c[0] + outs_c[1], o], axis=2)
    o = o.transpose(0, 2, 1, 3)
    t_out = o.shape[1]
    gate = dg[:, t - t_out:].reshape(b, t_out, HGRN_HEADS, HGRN_DV).astype(jnp.float32)
    o = _rmsnorm(o, norm_g) * jax.nn.silu(gate)
    return o.reshape(b, t_out, BRANCH_W).astype(dq.dtype)


def _token_mixer(hc, hx, layer, w_in_l, qk_g, lam_p, subln_g, conv_w_l, conv_b_l, ln_g, ln_b,
                 lb_l, hgrn_g, w_branch_l, w_out_l, rope, need_ctx):
    n_ctx = hc.shape[1]
    p = jnp.concatenate([hc, hx], axis=1) @ w_in_l
    aq, ak, av, bq, bk, bv, cglu, dq, df_fwd, df_bwd, di, dg, gates = jnp.split(p, SPLIT_POINTS, axis=-1)
    lam_init = 0.8 - 0.6 * math.exp(-0.3 * layer)
    branches = (
        _gqa_branch(aq, ak, av, qk_g[0], qk_g[1], rope, n_ctx, need_ctx),
        _diff_branch(bq, bk, bv, qk_g[2], qk_g[3], lam_p, subln_g, lam_init, rope, n_ctx, need_ctx),
        _conv_branch(cglu, conv_w_l, conv_b_l, ln_g, ln_b, n_ctx, need_ctx),
        _hgrn_branch(dq, df_fwd, df_bwd, di, dg, lb_l, hgrn_g, n_ctx, need_ctx),
    )
    start = 0 if need_ctx else n_ctx
    gate_parts = jnp.split(jax.nn.sigmoid(gates[:, start:]), N_BRANCH, axis=-1)
    merged = sum(g * (br @ w_branch_l[i]) for i, (g, br) in enumerate(zip(gate_parts, branches)))
    return merged @ w_out_l


def _moe(h, router, w1, w3, w2):
    logits = (h @ router).astype(jnp.float32)
    top_val, top_idx = lax.top_k(logits, TOP_K)
    weights = jax.nn.softmax(top_val, axis=-1)
    combine = jnp.sum(jax.nn.one_hot(top_idx, N_EXPERTS, dtype=jnp.float32) * weights[..., None],
                      axis=-2).astype(h.dtype)
    out = jnp.zeros_like(h)
    for e in range(N_EXPERTS):
        out = out + combine[..., e:e + 1] * _swiglu(h, w1[e], w3[e], w2[e])
    return out


def setup_inputs(seed: int = 0) -> dict:
    key = jax.random.key(seed)
    ks = iter(jax.random.split(key, 32))

    def nrm(shape, scale):
        return scale * jax.random.normal(next(ks), shape, jnp.float32)

    def gain(shape):
        return 1.0 + nrm(shape, 0.05)

    return {
        'x': nrm((BATCH, SEQ, D_MODEL), 1.0),
        'c': nrm((BATCH, D_MODEL), 1.0),
        'ctx': nrm((BATCH, CTX_LEN, D_MODEL), 1.0),
        'c_ctx': nrm((D_MODEL,), 1.0),
        'ada_w': nrm((DEPTH, D_MODEL, ADA_CHUNKS * D_MODEL), 0.5 * D_MODEL ** -0.5),
        'ada_b': nrm((DEPTH, ADA_CHUNKS * D_MODEL), 0.02),
        'norm1_g': gain((DEPTH, D_MODEL)),
        'norm2_g': gain((DEPTH, D_MODEL)),
        'w_in': nrm((DEPTH, D_MODEL, D_IN), D_MODEL ** -0.5),
        'qk_norm_g': gain((DEPTH, 4, HEAD_DIM)),
        'diff_lambda': nrm((DEPTH, 4, HEAD_DIM), 0.1),
        'diff_subln_g': gain((DEPTH, DIFF_V_DIM)),
        'conv_w': nrm((DEPTH, CONV_WIDTH, CONV_CH), CONV_WIDTH ** -0.5),
        'conv_b': nrm((DEPTH, CONV_CH), 0.02),
        'conv_ln_g': gain((DEPTH, CONV_CH)),
        'conv_ln_b': nrm((DEPTH, CONV_CH), 0.02),
        'hgrn_lb_logits': nrm((DEPTH, 2, HGRN_HEADS * HGRN_DK), 0.1),
        'hgrn_norm_g': gain((DEPTH, HGRN_DV)),
        'w_branch': nrm((DEPTH, N_BRANCH, BRANCH_W, D_MODEL), BRANCH_W ** -0.5),
        'w_out': nrm((DEPTH, D_MODEL, D_MODEL), D_MODEL ** -0.5),
        'ffn_w1': nrm((N_DENSE, D_MODEL, D_FF), D_MODEL ** -0.5),
        'ffn_w3': nrm((N_DENSE, D_MODEL, D_FF), D_MODEL ** -0.5),
        'ffn_w2': nrm((N_DENSE, D_FF, D_MODEL), D_FF ** -0.5),
        'moe_router': nrm((N_MOE, D_MODEL, N_EXPERTS), D_MODEL ** -0.5),
        'moe_w1': nrm((N_MOE, N_EXPERTS, D_MODEL, D_FF), D_MODEL ** -0.5),
        'moe_w3': nrm((N_MOE, N_EXPERTS, D_MODEL, D_FF), D_MODEL ** -0.5),
        'moe_w2': nrm((N_MOE, N_EXPERTS, D_FF, D_MODEL), D_FF ** -0.5),
    }


def reference(x, c, ctx, c_ctx, ada_w, ada_b, norm1_g, norm2_g, w_in, qk_norm_g, diff_lambda,
              diff_subln_g, conv_w, conv_b, conv_ln_g, conv_ln_b, hgrn_lb_logits, hgrn_norm_g,
              w_branch, w_out, ffn_w1, ffn_w3, ffn_w2, moe_router, moe_w1, moe_w3, moe_w2):
    seq = x.shape[1]
    n_ctx = ctx.shape[1]
    rows = seq // GRID_W
    rope = _axial_rope_tables(seq, rows)
    lb = jnp.cumsum(jax.nn.softmax(hgrn_lb_logits.astype(jnp.float32), axis=0), axis=0)
    lb = lb - lb[:1]
    silu_c = jax.nn.silu(c)
    silu_cc = jax.nn.silu(c_ctx)
    for l in range(DEPTH):
        need_ctx = l < DEPTH - 1
        mod_x = (silu_c @ ada_w[l] + ada_b[l])[:, None, :]
        mod_c = silu_cc @ ada_w[l] + ada_b[l]
        sh1x, sc1x, gt1x, sh2x, sc2x, gt2x = jnp.split(mod_x, ADA_CHUNKS, axis=-1)
        sh1c, sc1c, gt1c, sh2c, sc2c, gt2c = jnp.split(mod_c, ADA_CHUNKS, axis=-1)
        hx = _modulate(_rmsnorm(x, norm1_g[l]), sh1x, sc1x)
        hc = _modulate(_rmsnorm(ctx, norm1_g[l]), sh1c, sc1c)
        mix = _token_mixer(hc, hx, l, w_in[l], qk_norm_g[l], diff_lambda[l], diff_subln_g[l],
                           conv_w[l], conv_b[l], conv_ln_g[l], conv_ln_b[l], lb[l], hgrn_norm_g[l],
                           w_branch[l], w_out[l], rope, need_ctx)
        x = x + gt1x * mix[:, mix.shape[1] - seq:]
        h2 = _modulate(_rmsnorm(x, norm2_g[l]), sh2x, sc2x)
        if need_ctx:
            ctx = ctx + gt1c * mix[:, :n_ctx]
            h2 = jnp.concatenate([_modulate(_rmsnorm(ctx, norm2_g[l]), sh2c, sc2c), h2], axis=1)
        if l % 2 == 0:
            f = _swiglu(h2, ffn_w1[l // 2], ffn_w3[l // 2], ffn_w2[l // 2])
        else:
            f = _moe(h2, moe_router[l // 2], moe_w1[l // 2], moe_w3[l // 2], moe_w2[l // 2])
        x = x + gt2x * f[:, f.shape[1] - seq:]
        if need_ctx:
            ctx = ctx + gt2c * f[:, :n_ctx]
    return x
```

```python
import math
from contextlib import ExitStack, contextmanager
import numpy as np
import ml_dtypes
import concourse.bass as bass
import concourse.mybir as mybir
from concourse.bass_utils import run_bass_kernel_spmd

F32 = mybir.dt.float32
BF16 = mybir.dt.bfloat16
AF = mybir.ActivationFunctionType
ALU = mybir.AluOpType
AX = mybir.AxisListType

D = 1024
SEQ = 4096
NCTX = 256
T = SEQ + NCTX
NT = T // 128
DEPTH = 4
D_IN = 9984
NOC = D_IN // 128
DFF = 3584
NEXP = 8
EPS = 1e-6
BLK = [(0, 256, 1)] + [(256 + 512 * i, 512, 0) for i in range(8)]
VCH = {5: 0, 14: 1, 15: 2, 16: 3, 17: 4, 38: 5, 39: 6, 40: 7, 41: 8}
QKCH = [0, 1, 2, 3, 4, 6, 7, 8, 9, 10, 11, 12, 13]
UPAD = 15
ULEN = UPAD + 256 + UPAD + UPAD + 4096 + UPAD
UOFF = [UPAD, UPAD + 256 + 2 * UPAD]


class Buf:
    __slots__ = ("t", "name", "key", "w", "r", "psum")

    def __init__(self, t, name, key=None):
        self.psum = False
        self.t = t
        self.name = name
        self.key = key or name
        self.w = None
        self.r = {}

    def __getitem__(self, idx):
        return self.t[idx]


class FW:
    def __init__(self, nc, root):
        self.nc = nc
        self.root = root
        self.stack = root
        self.engs = {"pe": nc.tensor, "act": nc.scalar, "dve": nc.vector, "pool": nc.gpsimd, "sp": nc.sync}
        self.sem = {k: root.enter_context(nc.semaphore("s_" + k)) for k in self.engs}
        self.cnt = {k: 0 for k in self.engs}
        self.seen = {k: {} for k in self.engs}
        self.dsem = {}
        self.nbuf = 0
        self.ninst = 0

    @contextmanager
    def scope(self):
        old = self.stack
        with ExitStack() as s:
            self.stack = s
            try:
                yield
            finally:
                self.barrier()
                self.stack = old

    def sb(self, shape, dt, name=None):
        self.nbuf += 1
        key = name or 'sb'
        name = f"{key}_{self.nbuf}"
        t = self.stack.enter_context(self.nc.sbuf_tensor(name, list(shape), dt))
        return Buf(t, name, key)

    def ps(self, shape, dt=F32, name=None):
        self.nbuf += 1
        name = f"{name or 'ps'}_{self.nbuf}"
        t = self.root.enter_context(self.nc.psum_tensor(name, list(shape), dt))
        b = Buf(t, name)
        b.psum = True
        return b

    def dram(self, name, shape, dt, kind="Internal"):
        t = self.nc.dram_tensor(name, list(shape), dt, kind=kind)
        return Buf(t.ap(), name)

    def _semof(self, key):
        return self.sem[key] if key in self.sem else self.dsem[key][0]

    def _wait(self, E, ev):
        if ev is None:
            return
        key, val = ev
        if key not in self.sem:
            val = 16 * self.dsem[key][1]
        if key == "pe" and E == "pe":
            return
        if key == E and val > self.cnt[E]:
            return
        if self.seen[E].get(key, 0) >= val:
            return
        self.seen[E][key] = val
        self.engs[E].wait_ge(self._semof(key), val)
        self.ninst += 1

    def _deps(self, E, reads, writes):
        for b in reads:
            self._wait(E, b.w)
            if b.psum:
                for k, v in b.r.items():
                    if k != E:
                        self._wait(E, (k, v))
        for b in writes:
            self._wait(E, b.w)
            for k, v in b.r.items():
                self._wait(E, (k, v))

    def _record(self, ev, reads, writes):
        k, v = ev
        for b in reads:
            if b.r.get(k, 0) < v:
                b.r[k] = v
        for b in writes:
            b.w = ev
            b.r = {}

    def op(self, E, fn, reads=(), writes=(), sig=True):
        self._deps(E, reads, writes)
        ins = fn(self.engs[E])
        self.ninst += 1
        if sig:
            self.cnt[E] += 1
            ins.then_inc(self.sem[E], 1)
            ev = (E, self.cnt[E])
        else:
            ev = (E, self.cnt[E] + 1)
        self._record(ev, reads, writes)
        return ins

    def dma(self, Q, out, in_, reads=(), writes=(), key=None, **kw):
        self._deps(Q, reads, writes)
        if key is None:
            key = "d_" + (writes[0].key if writes else reads[0].key)
        if key not in self.dsem:
            s = self.root.enter_context(self.nc.semaphore("q_" + str(len(self.dsem))))
            self.dsem[key] = [s, 0]
        ent = self.dsem[key]
        ent[1] += 1
        ins = self.engs[Q].dma_start(out=out, in_=in_, **kw)
        ins.then_inc(ent[0], 16)
        self.ninst += 1
        self._record((key, 16 * ent[1]), reads, writes)
        return ins

    def barrier(self, engines=("pe", "act", "dve", "pool", "sp")):
        for E in engines:
            for k in ("pe", "act", "dve", "pool", "sp"):
                if k != E and self.cnt[k]:
                    self._wait(E, (k, self.cnt[k]))
            for key, (s, c) in self.dsem.items():
                if c:
                    self._wait(E, (key, 16 * c))


def _const_tables():
    c = {}
    c["ones"] = np.ones((128, 128), np.float32)
    bd = np.zeros((128, 128), np.float32)
    bd[:64, :64] = 1
    bd[64:, 64:] = 1
    c["bd64"] = bd
    c["ident"] = np.eye(128, dtype=np.float32)
    R = np.zeros((128, 128), np.float32)
    for p in range(128):
        d = p % 64
        j = d % 32
        if j < 16:
            R[p + 16, p] = -1.0
        else:
            R[p - 16, p] = 1.0
    c["rot"] = R
    inv_freq = (10000.0 ** (-np.arange(0, 32, 2, dtype=np.float32) / 32)).astype(np.float32)
    tl = np.arange(SEQ)
    row = (tl // 64).astype(np.float32)
    col = (tl % 64).astype(np.float32)
    cos = np.ones((128, T), np.float32)
    sin = np.zeros((128, T), np.float32)
    for p in range(128):
        d = p % 64
        pos = row if d < 32 else col
        f = inv_freq[(d % 32) % 16]
        ang = (pos * f).astype(np.float32)
        cos[p, NCTX:] = np.cos(ang)
        sin[p, NCTX:] = np.sin(ang)
    c["cos"] = cos
    c["sin"] = sin
    s = np.arange(128)[:, None]
    t = np.arange(128)[None, :]
    same = (s // 64) == (t // 64)
    c["mask_f"] = np.tile((same & (s <= t)).astype(np.float32), (1, 4))
    c["mask_b"] = np.tile((same & (s >= t)).astype(np.float32), (1, 4))
    return c


CONST_SPECS = [("ones", [128, 128]), ("bd64", [128, 128]), ("ident", [128, 128]), ("rot", [128, 128]),
               ("cos", [128, T]), ("sin", [128, T]), ("mask_f", [128, 512]), ("mask_b", [128, 512])]

IN_SPECS = [
    ("xT0", [D, T]), ("cvec", [128, 8, 2]), ("ada_w", [DEPTH, D, 6 * D]), ("ada_bT", [128, DEPTH, 48]),
    ("g1T", [128, DEPTH, 8]), ("g2T", [128, DEPTH, 8]), ("w_in", [DEPTH, D, D_IN]),
    ("qkgT", [128, DEPTH, 4]), ("lamB", [128, DEPTH, 256]), ("sublnT", [128, DEPTH]),
    ("convwT", [128, DEPTH, 4, 31]), ("convbT", [128, DEPTH, 4]), ("clngT", [128, DEPTH, 4]),
    ("clnbT", [128, DEPTH, 4]), ("lblT", [128, DEPTH, 2, 4]), ("hgT", [64, DEPTH]),
    ("w_branch", [DEPTH, 4, 512, D]), ("w_out", [DEPTH, D, D]),
    ("ffn_w1", [2, D, DFF]), ("ffn_w3", [2, D, DFF]), ("ffn_w2", [2, DFF, D]),
    ("routerT", [128, 2, 8, 8]), ("moe_w1", [2, NEXP, D, DFF]), ("moe_w3", [2, NEXP, D, DFF]),
    ("moe_w2", [2, NEXP, DFF, D]),
]


class MK:
    def __init__(self, n_layers=DEPTH, debug=(), stop_after=None, ext_in=()):
        self.n_layers = n_layers
        self.debug = set(debug)
        self.ext_in = set(ext_in)
        self.stop_after = stop_after
        self.nc = bass.Bass("TRN2", target_bir_lowering=False)
        self.rr = 0

    def scratch(self, name, shape, dt):
        kind = "Internal"
        if name in self.debug:
            kind = "ExternalOutput"
        if name in self.ext_in:
            kind = "ExternalInput"
        return self.fw.dram(name, shape, dt, kind=kind)

    def build(self):
        nc = self.nc
        with ExitStack() as root:
            fw = self.fw = FW(nc, root)
            big = ("ada_w", "w_in", "w_branch", "w_out", "ffn_w1", "ffn_w3", "ffn_w2", "moe_w1", "moe_w3", "moe_w2")
            tiny = getattr(self, "tiny", False)
            self.I = {n: fw.dram(n, ([1] * len(s) if (tiny and n in big) else s), F32, kind="ExternalInput") for n, s in IN_SPECS}
            self.CI = {n: fw.dram("c_" + n, s, F32, kind="ExternalInput") for n, s in CONST_SPECS}
            self.xs = fw.dram("xs", [D, T], F32, kind="ExternalOutput")
            self.PT = [self.scratch(f"PT{i}", [128, T], BF16) for i in range(NOC)]
            self.PTall = self.scratch("PTg", [32, 128, T], BF16)
            self.VT = self.scratch("VT", [9, T, 128], BF16)
            self.QK = {oc: self.scratch(f"QK{oc}", [128, T], BF16) for oc in QKCH}
            self.BR = self.scratch("BR", [16, 128, T], BF16)
            self.OD = [self.scratch(f"OD{d}", [8, 64, T], F32) for d in range(2)]
            self.CB = self.scratch("CB", [NEXP, 128, T], BF16)
            self.psb = [fw.ps([128, 512], F32, f"bank{i}") for i in range(7)]
            self.psT = fw.ps([128, 1024], BF16, "bankT")
            self.ones = fw.sb([128, 128], BF16, "ones")
            self.bd64 = fw.sb([128, 128], BF16, "bd64")
            self.identb = fw.sb([128, 128], BF16, "identb")
            self.identf = fw.sb([128, 128], F32, "identf")
            self.rot = fw.sb([128, 128], BF16, "rot")
            for nm, b in (("ones", self.ones), ("bd64", self.bd64), ("ident", self.identb), ("rot", self.rot)):
                fw.dma("pool", b[:, :], self.CI[nm][:, :], reads=[self.CI[nm]], writes=[b])
            fw.dma("sp", self.identf[:, :], self.CI["ident"][:, :], reads=[self.CI["ident"]], writes=[self.identf])
            self.eps = fw.sb([128, 1], F32, "eps")
            fw.op("dve", lambda e: e.memset(self.eps[:, :], EPS), writes=[self.eps])
            self.small_params()
            self.modt = fw.sb([128, 48, 2], F32, "mod")
            self.gs = fw.sb([128, 2, 8, 2], F32, "gs")
            for l in (getattr(self, "layers", None) or range(self.n_layers)):
                self.layer(l)
                if self.stop_after is not None and self.stop_after[0] == l and self.done:
                    break
            fw.barrier(engines=("sp",))
        return nc

    def small_params(self):
        fw, I = self.fw, self.I
        def ld(name, shape):
            b = fw.sb(shape, F32, name)
            src = I[name]
            fw.dma("sp", b.t[tuple(slice(None) for _ in shape)], src.t[tuple(slice(None) for _ in shape)], reads=[src], writes=[b])
            return b
        self.cvec = ld("cvec", [128, 8, 2])
        self.ada_bT = ld("ada_bT", [128, DEPTH, 48])
        self.g1T = ld("g1T", [128, DEPTH, 8])
        self.g2T = ld("g2T", [128, DEPTH, 8])
        self.qkgT = ld("qkgT", [128, DEPTH, 4])
        self.lamB = ld("lamB", [128, DEPTH, 256])
        self.sublnT = ld("sublnT", [128, DEPTH])
        self.convwT = ld("convwT", [128, DEPTH, 4, 31])
        self.convbT = ld("convbT", [128, DEPTH, 4])
        self.clngT = ld("clngT", [128, DEPTH, 4])
        self.clnbT = ld("clnbT", [128, DEPTH, 4])
        self.lblT = ld("lblT", [128, DEPTH, 2, 4])
        self.hgT = ld("hgT", [64, DEPTH])
        self.routerT = ld("routerT", [128, 2, 8, 8])
        self.siluc = fw.sb([128, 8, 2], BF16, "siluc")
        fw.op("act", lambda e: e.activation(out=self.siluc[:, :, :], in_=self.cvec[:, :, :], func=AF.Silu),
              reads=[self.cvec], writes=[self.siluc])
        ex = fw.sb([128, DEPTH, 8], F32, "lbex")
        fw.op("act", lambda e: e.activation(out=ex[:, :, :], in_=self.lblT.t.rearrange("p l d c -> p l (d c)"), func=AF.Exp),
              reads=[self.lblT], writes=[ex])
        ssum = fw.sb([128, 8], F32, "lbsum")
        fw.op("dve", lambda e: e.tensor_tensor(out=ssum[:, :], in0=ex[:, 0, :], in1=ex[:, 1, :], op=ALU.add), reads=[ex], writes=[ssum])
        for l in range(2, DEPTH):
            fw.op("dve", lambda e, l=l: e.tensor_tensor(out=ssum[:, :], in0=ssum[:, :], in1=ex[:, l, :], op=ALU.add), reads=[ex, ssum], writes=[ssum])
        fw.op("dve", lambda e: e.reciprocal(out=ssum[:, :], in_=ssum[:, :]), reads=[ssum], writes=[ssum])
        self.lbT = fw.sb([128, DEPTH, 8], F32, "lbT")
        self.omlT = fw.sb([128, DEPTH, 8], F32, "omlT")
        fw.op("dve", lambda e: e.memset(self.lbT[:, 0, :], 0.0), writes=[self.lbT])
        for l in range(1, DEPTH):
            fw.op("dve", lambda e, l=l: e.tensor_tensor(out=ex[:, l, :], in0=ex[:, l, :], in1=ssum[:, :], op=ALU.mult), reads=[ex, ssum], writes=[ex])
            fw.op("dve", lambda e, l=l: e.tensor_tensor(out=self.lbT[:, l, :], in0=self.lbT[:, l - 1, :], in1=ex[:, l, :], op=ALU.add),
                  reads=[ex, self.lbT], writes=[self.lbT])
        fw.op("dve", lambda e: e.tensor_scalar(out=self.omlT[:, :, :], in0=self.lbT[:, :, :], scalar1=-1.0, scalar2=1.0, op0=ALU.mult, op1=ALU.add),
              reads=[self.lbT], writes=[self.omlT])
        self.neglam = fw.sb([128, DEPTH], F32, "neglam")
        self.subg = fw.sb([128, DEPTH], F32, "subg")
        pr = fw.sb([128, DEPTH, 2, 64], F32, "lampr")
        s12 = fw.sb([128, DEPTH, 2], F32, "lams")
        lam4 = self.lamB.t.rearrange("p l (i d) -> p l i d", i=4)
        for l in range(DEPTH):
            for j in range(2):
                fw.op("dve", lambda e, l=l, j=j: e.tensor_tensor(out=pr[:, l, j, :], in0=lam4[:, l, 2 * j, :], in1=lam4[:, l, 2 * j + 1, :], op=ALU.mult),
                      reads=[self.lamB], writes=[pr])
                fw.op("dve", lambda e, l=l, j=j: e.reduce_sum(out=s12[:, l, j:j + 1], in_=pr[:, l, j, :], axis=AX.X), reads=[pr], writes=[s12])
        fw.op("act", lambda e: e.activation(out=s12[:, :, :], in_=s12[:, :, :], func=AF.Exp), reads=[s12], writes=[s12])
        for l in range(DEPTH):
            li = 0.8 - 0.6 * math.exp(-0.3 * l)
            fw.op("dve", lambda e, l=l, li=li: e.scalar_tensor_tensor(out=self.neglam[:, l:l + 1], in0=s12[:, l, 1:2], scalar=-li, in1=s12[:, l, 0:1],
                                                                     op0=ALU.add, op1=ALU.subtract), reads=[s12], writes=[self.neglam])
            fw.op("dve", lambda e, l=l, li=li: e.tensor_scalar(out=self.subg[:, l:l + 1], in0=self.sublnT[:, l:l + 1], scalar1=1.0 - li, scalar2=None, op0=ALU.mult),
                  reads=[self.sublnT], writes=[self.subg])

    def bank(self):
        self.rr = (self.rr + 1) % 7
        return self.psb[self.rr]

    def layer(self, l):
        self.done = False
        need_ctx = l < DEPTH - 1
        self.l = l
        self.need_ctx = need_ctx
        steps = [self.ph_mod, self.ph_inproj, self.ph_qk, self.ph_gqa, self.ph_diff, self.ph_conv, self.ph_hgrn,
                 self.ph_merge, self.ph_ffn]
        for i, s in enumerate(steps):
            if getattr(self, "only", None) is not None and i not in self.only:
                continue
            s()
            if self.stop_after is not None and self.stop_after == (l, i):
                self.done = True
                return

    def xsrc(self):
        return self.I["xT0"] if (self.l == 0 and not self.x_written) else self.xs

    def ph_mod(self):
        fw, l = self.fw, self.l
        self.x_written = (l > 0)
        with fw.scope():
            ps = self.psb[0]
            wts = [fw.sb([128, 8, 512], BF16, f"adaw{i}") for i in range(2)]
            aw = self.I["ada_w"].t[l].rearrange("(k p) c -> p k c", p=128)
            for g in range(12):
                w = wts[g % 2]
                fw.dma("pool", w[:, :, :], aw[:, :, g * 512:(g + 1) * 512], reads=[self.I["ada_w"]], writes=[w], key=f"adaw{g % 2}")
                for cc in range(4):
                    j = g * 4 + cc
                    for k in range(8):
                        fw.op("pe", lambda e, w=w, cc=cc, k=k, j=j: e.matmul(ps[:, 2 * j:2 * j + 2], w[:, k, cc * 128:(cc + 1) * 128], self.siluc[:, k, :],
                                                                       start=(k == 0), stop=(k == 7)),
                              reads=[w, self.siluc], writes=[ps], sig=(k == 7))
            mod = self.modt
            for i in range(2):
                fw.op("dve", lambda e, i=i: e.tensor_tensor(out=mod[:, :, i], in0=ps.t[:, 0:96].rearrange("p (j i) -> p j i", i=2)[:, :, i],
                                                            in1=self.ada_bT[:, l, :], op=ALU.add), reads=[ps, self.ada_bT], writes=[mod])
            for s, (gT, c0) in enumerate(((self.g1T, 8), (self.g2T, 32))):
                for i in range(2):
                    fw.op("dve", lambda e, s=s, gT=gT, c0=c0, i=i: e.scalar_tensor_tensor(out=self.gs[:, s, :, i], in0=mod[:, c0:c0 + 8, i], scalar=1.0, in1=gT[:, l, :],
                                                                                     op0=ALU.add, op1=ALU.mult), reads=[mod, gT], writes=[self.gs])

    def norm_block(self, s, t0, n, ic, hT, hcol, xb, sq, rstd, tmp, h32=None):
        fw = self.fw
        src = self.xsrc()
        shc = 0 if s == 0 else 24
        fw.dma("sp", xb[:, :, 0:n], src.t.rearrange("(k p) t -> p k t", p=128)[:, :, t0:t0 + n], reads=[src], writes=[xb])
        fw.op("act", lambda e: e.activation(out=sq[:, :, 0:n], in_=xb[:, :, 0:n], func=AF.Square), reads=[xb], writes=[sq])
        ps = self.bank()
        for k in range(8):
            fw.op("pe", lambda e, k=k: e.matmul(ps[:, 0:n], self.ones[:, :], sq[:, k, 0:n], start=(k == 0), stop=(k == 7)),
                  reads=[self.ones, sq], writes=[ps], sig=(k == 7))
        fw.op("act", lambda e: e.activation(out=rstd[:, 0:n], in_=ps[:, 0:n], func=AF.Sqrt, bias=self.eps[:, 0:1], scale=1.0 / D), reads=[ps, self.eps], writes=[rstd])
        fw.op("dve", lambda e: e.reciprocal(out=rstd[:, 0:n], in_=rstd[:, 0:n]), reads=[rstd], writes=[rstd])
        for k in range(8):
            fw.op("dve", lambda e, k=k: e.scalar_tensor_tensor(out=tmp[:, k, 0:n], in0=xb[:, k, 0:n], scalar=self.gs[:, s, k, ic:ic + 1], in1=rstd[:, 0:n],
                                                              op0=ALU.mult, op1=ALU.mult), reads=[xb, self.gs, rstd], writes=[tmp])
            fw.op("act", lambda e, k=k: e.activation(out=hT[:, k, hcol:hcol + n], in_=tmp[:, k, 0:n], func=AF.Identity, bias=self.modt[:, shc + k, ic:ic + 1], scale=1.0),
                  reads=[tmp, self.modt], writes=[hT])
            if h32 is not None:
                fw.op("pool", lambda e, k=k: e.tensor_scalar(out=h32[:, k, 0:n], in0=tmp[:, k, 0:n], scalar1=self.modt[:, shc + k, ic:ic + 1], scalar2=None, op0=ALU.add),
                      reads=[tmp, self.modt], writes=[h32])

    def ph_inproj(self):
        fw, l = self.fw, self.l
        with fw.scope():
            hT = fw.sb([128, 8, T], BF16, "hT")
            with fw.scope():
                xbs = [fw.sb([128, 8, 512], F32, f"xb{i}") for i in range(2)]
                sq = fw.sb([128, 8, 512], BF16, "sq")
                rstd = fw.sb([128, 512], F32, "rstd")
                tmp = fw.sb([128, 8, 512], F32, "tmp")
                for bi, (t0, n, ic) in enumerate(BLK):
                    self.norm_block(0, t0, n, ic, hT, t0, xbs[bi % 2], sq, rstd, tmp)
            wts = [fw.sb([128, 8, 512], BF16, f"win{i}") for i in range(2)]
            stg = [fw.sb([128, T], BF16, f"stg{i}") for i in range(2)]
            vst = [fw.sb([128, NT, 128], BF16, f"vst{i}") for i in range(2)]
            wv = self.I["w_in"].t[l].rearrange("(k p) c -> p k c", p=128)
            ns = 0
            for g in range(20):
                w = wts[g % 2]
                gc = min(512, D_IN - g * 512)
                fw.dma("pool", w[:, :, 0:gc], wv[:, :, g * 512:g * 512 + gc], reads=[self.I["w_in"]], writes=[w], key=f"win{g % 2}")
                for cc in range(gc // 128):
                    oc = g * 4 + cc
                    if oc in VCH:
                        v = vst[VCH[oc] % 2]
                        for tg in range(0, NT, 4):
                            ps = self.bank()
                            nt_ = min(4, NT - tg)
                            for ti in range(nt_):
                                tt = tg + ti
                                for k in range(8):
                                    fw.op("pe", lambda e, k=k, tt=tt, ti=ti, ps=ps, w=w, cc=cc: e.matmul(
                                        ps[:, ti * 128:(ti + 1) * 128], hT[:, k, tt * 128:(tt + 1) * 128], w[:, k, cc * 128:(cc + 1) * 128],
                                        start=(k == 0), stop=(k == 7)), reads=[hT, w], writes=[ps], sig=(k == 7))
                            fw.op("dve", lambda e, ps=ps, v=v, tg=tg, nt_=nt_: e.tensor_copy(out=v[:, tg:tg + nt_, :], in_=ps.t[:, 0:nt_ * 128].rearrange("p (a c) -> p a c", c=128)),
                                  reads=[ps], writes=[v])
                        fw.dma("sp", self.VT.t[VCH[oc]].rearrange("(n p) c -> p n c", p=128), v[:, :, :], reads=[v], writes=[self.VT], key="vt_st")
                        continue
                    so = stg[ns % 2]
                    ns += 1
                    for bi, (t0, n, ic) in enumerate(BLK):
                        ps = self.bank()
                        for k in range(8):
                            fw.op("pe", lambda e, k=k, ps=ps, w=w, cc=cc, t0=t0, n=n: e.matmul(ps[:, 0:n], w[:, k, cc * 128:(cc + 1) * 128], hT[:, k, t0:t0 + n],
                                                                                   start=(k == 0), stop=(k == 7)), reads=[hT, w], writes=[ps], sig=(k == 7))
                        if oc >= 46:
                            fw.op("act", lambda e, ps=ps, so=so, t0=t0, n=n: e.activation(out=so[:, t0:t0 + n], in_=ps[:, 0:n], func=AF.Sigmoid), reads=[ps], writes=[so])
                        elif bi % 2 == 0:
                            fw.op("dve", lambda e, ps=ps, so=so, t0=t0, n=n: e.tensor_copy(out=so[:, t0:t0 + n], in_=ps[:, 0:n]), reads=[ps], writes=[so])
                        else:
                            fw.op("act", lambda e, ps=ps, so=so, t0=t0, n=n: e.activation(out=so[:, t0:t0 + n], in_=ps[:, 0:n], func=AF.Copy), reads=[ps], writes=[so])
                    if oc >= 46:
                        fw.dma("sp", self.PTall.t[oc - 46], so[:, :], reads=[so], writes=[self.PTall], key="ptg_st")
                    else:
                        fw.dma("sp", self.PT[oc][:, :], so[:, :], reads=[so], writes=[self.PT[oc]], key=f"pt_st{ns % 2}")

    def ph_qk(self):
        fw, l = self.fw, self.l
        with fw.scope():
            cos = fw.sb([128, T], F32, "cos")
            sin = fw.sb([128, T], F32, "sin")
            fw.dma("sp", cos[:, :], self.CI["cos"][:, :], reads=[self.CI["cos"]], writes=[cos])
            fw.dma("sp", sin[:, :], self.CI["sin"][:, :], reads=[self.CI["sin"]], writes=[sin])
            qin = [fw.sb([128, T], BF16, f"qin{i}") for i in range(2)]
            qout = [fw.sb([128, T], BF16, f"qout{i}") for i in range(2)]
            sq = [fw.sb([128, 512], BF16, f"qsq{i}") for i in range(2)]
            rstd = [fw.sb([128, 512], F32, f"qrstd{i}") for i in range(2)]
            qn = [fw.sb([128, 512], BF16, f"qn{i}") for i in range(2)]
            t1 = [fw.sb([128, 512], F32, f"qt1{i}") for i in range(2)]
            t2 = [fw.sb([128, 512], F32, f"qt2{i}") for i in range(2)]
            it = 0
            for ci, oc in enumerate(QKCH):
                gi = 0 if oc < 4 else 1 if oc == 4 else 2 if oc < 10 else 3
                qi, qo = qin[ci % 2], qout[ci % 2]
                fw.dma("sp", qi[:, :], self.PT[oc][:, :], reads=[self.PT[oc]], writes=[qi], key=f"qk_ld{ci % 2}")
                for (t0, n, ic) in BLK:
                    i2 = it % 2
                    it += 1
                    fw.op("act", lambda e, qi=qi, i2=i2, t0=t0, n=n: e.activation(out=sq[i2][:, 0:n], in_=qi[:, t0:t0 + n], func=AF.Square), reads=[qi], writes=[sq[i2]])
                    ps = self.bank()
                    fw.op("pe", lambda e, ps=ps, i2=i2, n=n: e.matmul(ps[:, 0:n], self.bd64[:, :], sq[i2][:, 0:n], start=True, stop=True), reads=[self.bd64, sq[i2]], writes=[ps])
                    fw.op("act", lambda e, ps=ps, i2=i2, n=n: e.activation(out=rstd[i2][:, 0:n], in_=ps[:, 0:n], func=AF.Sqrt, bias=self.eps[:, 0:1], scale=1.0 / 64),
                          reads=[ps, self.eps], writes=[rstd[i2]])
                    fw.op("dve", lambda e, i2=i2, n=n: e.reciprocal(out=rstd[i2][:, 0:n], in_=rstd[i2][:, 0:n]), reads=[rstd[i2]], writes=[rstd[i2]])
                    fw.op("dve", lambda e, qi=qi, i2=i2, t0=t0, n=n, gi=gi: e.scalar_tensor_tensor(out=qn[i2][:, 0:n], in0=qi[:, t0:t0 + n], scalar=self.qkgT[:, l, gi:gi + 1],
                                                                                          in1=rstd[i2][:, 0:n], op0=ALU.mult, op1=ALU.mult),
                          reads=[qi, self.qkgT, rstd[i2]], writes=[qn[i2]])
                    ps2 = self.bank()
                    fw.op("pe", lambda e, ps2=ps2, i2=i2, n=n: e.matmul(ps2[:, 0:n], self.rot[:, :], qn[i2][:, 0:n], start=True, stop=True), reads=[self.rot, qn[i2]], writes=[ps2])
                    fw.op("pool", lambda e, i2=i2, t0=t0, n=n: e.tensor_tensor(out=t1[i2][:, 0:n], in0=qn[i2][:, 0:n], in1=cos[:, t0:t0 + n], op=ALU.mult),
                          reads=[qn[i2], cos], writes=[t1[i2]])
                    fw.op("dve", lambda e, ps2=ps2, i2=i2, t0=t0, n=n: e.tensor_tensor(out=t2[i2][:, 0:n], in0=ps2[:, 0:n], in1=sin[:, t0:t0 + n], op=ALU.mult),
                          reads=[ps2, sin], writes=[t2[i2]])
                    fw.op("dve", lambda e, qo=qo, i2=i2, t0=t0, n=n: e.tensor_tensor(out=qo[:, t0:t0 + n], in0=t1[i2][:, 0:n], in1=t2[i2][:, 0:n], op=ALU.add),
                          reads=[t1[i2], t2[i2]], writes=[qo])
                fw.dma("sp", self.QK[oc][:, :], qo[:, :], reads=[qo], writes=[self.QK[oc]], key=f"qk_st{ci % 2}")

    def attend(self, qh, kh, vaug, lsep, accs, qblocks, pbufs, finish):
        fw = self.fw
        for (t0, n, ic) in qblocks:
            kts = list(range(2)) if ic else list(range(NT))
            pend = None
            for idx, kt in enumerate(kts):
                ps = self.bank_s()
                fw.op("pe", lambda e, ps=ps, kt=kt, t0=t0, n=n: e.matmul(ps[:, 0:n], kh[0:64, kt * 128:(kt + 1) * 128], qh[0:64, t0:t0 + n], start=True, stop=True),
                      reads=[kh, qh], writes=[ps])
                pb = pbufs[idx % len(pbufs)]
                fw.op("act", lambda e, ps=ps, pb=pb, n=n: e.activation(out=pb[:, 0:n], in_=ps[:, 0:n], func=AF.Exp, scale=0.125), reads=[ps], writes=[pb])
                if pend is not None:
                    self._pv(pend, accs, vaug, lsep, n, kts)
                pend = (idx, kt, pb)
            self._pv(pend, accs, vaug, lsep, n, kts)
            finish(t0, n, ic)

    def _pv(self, pend, accs, vaug, lsep, n, kts):
        fw = self.fw
        idx, kt, pb = pend
        first, last = idx == 0, idx == len(kts) - 1
        fw.op("pe", lambda e: e.matmul(accs[0][:, 0:n], vaug[:, kt, :], pb[:, 0:n], start=first, stop=last), reads=[vaug, pb], writes=[accs[0]], sig=(last and not lsep))
        if lsep:
            fw.op("pe", lambda e: e.matmul(accs[1][:, 0:n], self.ones[:, :], pb[:, 0:n], start=first, stop=last), reads=[self.ones, pb], writes=[accs[1]], sig=last)

    def bank_s(self):
        self.rs = (getattr(self, "rs", 0) + 1) % 3
        return self.psb[self.rs]

    def qblocks(self):
        return BLK if self.need_ctx else BLK[1:]

    def ph_gqa(self):
        fw, l = self.fw, self.l
        with fw.scope():
            qh = [fw.sb([64, T], BF16, f"gq{i}") for i in range(2)]
            kh = [fw.sb([64, T], BF16, f"gk{i}") for i in range(2)]
            vaug = [fw.sb([128, NT, 128], BF16, f"gv{i}") for i in range(2)]
            pbufs = [fw.sb([128, 512], BF16, f"gp{i}") for i in range(3)]
            rl = fw.sb([64, 512], F32, "grl")
            ob = [fw.sb([64, 512], BF16, f"gob{i}") for i in range(2)]
            for kv in range(2):
                fw.op("pool", lambda e, kv=kv: e.memset(vaug[kv][:, :, 64:128], 1.0), writes=[vaug[kv]])
                fw.dma("sp", vaug[kv][:, :, 0:64], self.VT.t[0].rearrange("(n p) c -> p n c", p=128)[:, :, kv * 64:(kv + 1) * 64],
                       reads=[self.VT], writes=[vaug[kv]], key=f"g_v{kv}")
                fw.dma("sp", kh[kv][:, :], self.QK[4].t[kv * 64:(kv + 1) * 64, :], reads=[self.QK[4]], writes=[kh[kv]], key=f"g_k{kv}")
            acc = self.psb[3]
            cnt = [0]
            for h in range(8):
                q = qh[h % 2]
                fw.dma("sp", q[:, :], self.QK[h // 2].t[(h % 2) * 64:(h % 2) * 64 + 64, :], reads=[self.QK[h // 2]], writes=[q], key=f"g_q{h % 2}")

                def fin(t0, n, ic, h=h):
                    o = ob[cnt[0] % 2]
                    cnt[0] += 1
                    fw.op("dve", lambda e: e.reciprocal(out=rl[0:64, 0:n], in_=acc[64:128, 0:n]), reads=[acc], writes=[rl])
                    fw.op("dve", lambda e: e.tensor_tensor(out=o[0:64, 0:n], in0=acc[0:64, 0:n], in1=rl[0:64, 0:n], op=ALU.mult), reads=[acc, rl], writes=[o])
                    fw.dma("sp", self.BR.t[h // 2, (h % 2) * 64:(h % 2) * 64 + 64, t0:t0 + n], o[0:64, 0:n], reads=[o], writes=[self.BR], key=f"g_o{cnt[0] % 2}")
                self.attend(q, kh[h // 4], vaug[h // 4], False, [acc], self.qblocks(), pbufs, fin)

    def ph_diff(self):
        fw, l = self.fw, self.l
        with fw.scope():
            qh = [fw.sb([64, T], BF16, f"dq{i}") for i in range(2)]
            kh = [fw.sb([64, T], BF16, f"dk{i}") for i in range(2)]
            vv = [fw.sb([128, NT, 128], BF16, f"dv{i}") for i in range(2)]
            pbufs = [fw.sb([128, 512], BF16, f"dp{i}") for i in range(3)]
            r0 = fw.sb([128, 512], F32, "dr0")
            a0 = fw.sb([128, 512], F32, "da0")
            a1 = fw.sb([128, 512], F32, "da1")
            sq = fw.sb([128, 512], BF16, "dsq")
            rstd = fw.sb([128, 512], F32, "drstd")
            ob = [fw.sb([128, 512], BF16, f"dob{i}") for i in range(2)]
            accs = [[self.psb[3], self.psb[4]], [self.psb[5], self.psb[6]]]
            cnt = [0]
            for h in range(4):
                v = vv[h % 2]
                fw.dma("sp", v[:, :, :], self.VT.t[1 + h].rearrange("(n p) c -> p n c", p=128), reads=[self.VT], writes=[v], key=f"d_v{h % 2}")
                for (t0, n, ic) in self.qblocks():
                    for i in range(2):
                        m = h * 2 + i
                        q, k = qh[i], kh[i]
                        if t0 == self.qblocks()[0][0]:
                            fw.dma("sp", q[:, :], self.QK[6 + m // 2].t[(m % 2) * 64:(m % 2) * 64 + 64, :], reads=[self.QK[6 + m // 2]], writes=[q], key=f"d_q{i}")
                            fw.dma("sp", k[:, :], self.QK[10 + m // 2].t[(m % 2) * 64:(m % 2) * 64 + 64, :], reads=[self.QK[10 + m // 2]], writes=[k], key=f"d_k{i}")
                        self.attend(q, k, v, True, accs[i], [(t0, n, ic)], pbufs, lambda *a: None)
                    o = ob[cnt[0] % 2]
                    cnt[0] += 1
                    A0, L0 = accs[0]
                    A1, L1 = accs[1]
                    fw.op("dve", lambda e: e.reciprocal(out=r0[:, 0:n], in_=L0[:, 0:n]), reads=[L0], writes=[r0])
                    fw.op("dve", lambda e: e.tensor_tensor(out=a0[:, 0:n], in0=A0[:, 0:n], in1=r0[:, 0:n], op=ALU.mult), reads=[A0, r0], writes=[a0])
                    fw.op("dve", lambda e: e.reciprocal(out=r0[:, 0:n], in_=L1[:, 0:n]), reads=[L1], writes=[r0])
                    fw.op("dve", lambda e: e.tensor_tensor(out=a1[:, 0:n], in0=A1[:, 0:n], in1=r0[:, 0:n], op=ALU.mult), reads=[A1, r0], writes=[a1])
                    fw.op("dve", lambda e: e.scalar_tensor_tensor(out=a0[:, 0:n], in0=a1[:, 0:n], scalar=self.neglam[:, l:l + 1], in1=a0[:, 0:n], op0=ALU.mult, op1=ALU.add),
                          reads=[a1, a0, self.neglam], writes=[a0])
                    fw.op("act", lambda e: e.activation(out=sq[:, 0:n], in_=a0[:, 0:n], func=AF.Square), reads=[a0], writes=[sq])
                    ps = self.psb[0]
                    fw.op("pe", lambda e: e.matmul(ps[:, 0:n], self.ones[:, :], sq[:, 0:n], start=True, stop=True), reads=[self.ones, sq], writes=[ps])
                    fw.op("act", lambda e: e.activation(out=rstd[:, 0:n], in_=ps[:, 0:n], func=AF.Sqrt, bias=self.eps[:, 0:1], scale=1.0 / 128), reads=[ps, self.eps], writes=[rstd])
                    fw.op("dve", lambda e: e.reciprocal(out=rstd[:, 0:n], in_=rstd[:, 0:n]), reads=[rstd], writes=[rstd])
                    fw.op("dve", lambda e: e.scalar_tensor_tensor(out=o[:, 0:n], in0=a0[:, 0:n], scalar=self.subg[:, l:l + 1], in1=rstd[:, 0:n], op0=ALU.mult, op1=ALU.mult),
                          reads=[a0, self.subg, rstd], writes=[o])
                    fw.dma("sp", self.BR.t[4 + h, :, t0:t0 + n], o[:, 0:n], reads=[o], writes=[self.BR], key=f"d_o{cnt[0] % 2}")

    def ph_conv(self):
        fw, l = self.fw, self.l
        with fw.scope():
            upad = fw.sb([128, 4, ULEN], BF16, "upad")
            upo = fw.sb([128, 4, ULEN], BF16, "upo")
            for (a_, b_) in ((0, UOFF[0]), (UOFF[0] + 256, UOFF[1]), (UOFF[1] + 4096, ULEN)):
                fw.op("dve", lambda e: e.memset(upad[:, :, a_:b_], 0.0), writes=[upad])
                fw.op("dve", lambda e: e.memset(upo[:, :, max(a_ - 1, 0):b_ - 1 if b_ < ULEN else ULEN], 0.0), writes=[upo])
            diag = fw.sb([128, 4, 31, 128], BF16, "diag")
            ab = [fw.sb([128, T], BF16, f"ca{i}") for i in range(2)]
            gb = [fw.sb([128, T], BF16, f"cg{i}") for i in range(2)]
            for cc in range(4):
                a, g = ab[cc % 2], gb[cc % 2]
                fw.dma("sp", a[:, :], self.PT[18 + cc][:, :], reads=[self.PT[18 + cc]], writes=[a], key=f"c_a{cc % 2}")
                fw.dma("sp", g[:, :], self.PT[22 + cc][:, :], reads=[self.PT[22 + cc]], writes=[g], key=f"c_g{cc % 2}")
                fw.op("act", lambda e, g=g: e.activation(out=g[:, :], in_=g[:, :], func=AF.Sigmoid), reads=[g], writes=[g])
                fw.op("dve", lambda e, a=a, g=g, cc=cc: e.tensor_tensor(out=upad[:, cc, UOFF[0]:UOFF[0] + 256], in0=a[:, 0:256], in1=g[:, 0:256], op=ALU.mult), reads=[a, g], writes=[upad])
                fw.op("dve", lambda e, a=a, g=g, cc=cc: e.tensor_tensor(out=upad[:, cc, UOFF[1]:UOFF[1] + 4096], in0=a[:, 256:T], in1=g[:, 256:T], op=ALU.mult), reads=[a, g], writes=[upad])
                fw.op("pool", lambda e, a=a, g=g, cc=cc: e.tensor_tensor(out=upo[:, cc, UOFF[0] - 1:UOFF[0] - 1 + 256], in0=a[:, 0:256], in1=g[:, 0:256], op=ALU.mult), reads=[a, g], writes=[upo])
                fw.op("pool", lambda e, a=a, g=g, cc=cc: e.tensor_tensor(out=upo[:, cc, UOFF[1] - 1:UOFF[1] - 1 + 4096], in0=a[:, 256:T], in1=g[:, 256:T], op=ALU.mult), reads=[a, g], writes=[upo])
                for j in range(31):
                    fw.op("dve", lambda e, cc=cc, j=j: e.tensor_scalar(out=diag[:, cc, j, :], in0=self.identf[:, :], scalar1=self.convwT[:, l, cc, j:j + 1], scalar2=None, op0=ALU.mult),
                          reads=[self.identf, self.convwT], writes=[diag])
            import os as _os
            cut = int(_os.environ.get("CONV_CUT", "99"))
            if cut <= 0:
                return
            y32 = fw.sb([128, 4, 512], F32, "cy32")
            ybf = fw.sb([128, 4, 512], BF16, "cybf")
            mean = fw.sb([128, 512], F32, "cmean")
            sq = fw.sb([128, 4, 512], BF16, "csq")
            rstd = fw.sb([128, 512], F32, "crstd")
            ob = [fw.sb([128, 4, 512], BF16, f"cob{i}") for i in range(2)]
            for bi, (t0, n, ic) in enumerate(BLK):
                u0 = (UOFF[0] + t0 - UPAD) if ic else (UOFF[1] + (t0 - 256) - UPAD)
                for cc in range(4):
                    ps = self.bank()
                    for j in range(31):
                        usrc, uo = (upad, u0 + j) if (u0 + j) % 2 == 0 else (upo, u0 + j - 1)
                        fw.op("pe", lambda e, ps=ps, cc=cc, j=j: e.matmul(ps[:, 0:n], diag[:, cc, j, :], usrc[:, cc, uo:uo + n], start=(j == 0), stop=(j == 30)),
                              reads=[diag, usrc], writes=[ps], sig=(j == 30))
                    fw.op("act", lambda e, ps=ps, cc=cc: e.activation(out=y32[:, cc, 0:n], in_=ps[:, 0:n], func=AF.Identity, bias=self.convbT[:, l, cc:cc + 1], scale=1.0),
                          reads=[ps, self.convbT], writes=[y32])
                    fw.op("dve", lambda e, ps=ps, cc=cc: e.tensor_scalar(out=ybf[:, cc, 0:n], in0=ps[:, 0:n], scalar1=self.convbT[:, l, cc:cc + 1], scalar2=None, op0=ALU.add),
                          reads=[ps, self.convbT], writes=[ybf])
                if cut <= 1:
                    continue
                pm = self.bank()
                for cc in range(4):
                    fw.op("pe", lambda e, cc=cc: e.matmul(pm[:, 0:n], self.ones[:, :], ybf[:, cc, 0:n], start=(cc == 0), stop=(cc == 3)), reads=[self.ones, ybf], writes=[pm], sig=(cc == 3))
                fw.op("act", lambda e: e.activation(out=mean[:, 0:n], in_=pm[:, 0:n], func=AF.Copy, scale=1.0 / 512), reads=[pm], writes=[mean])
                for cc in range(4):
                    fw.op("dve", lambda e, cc=cc: e.tensor_tensor(out=y32[:, cc, 0:n], in0=y32[:, cc, 0:n], in1=mean[:, 0:n], op=ALU.subtract), reads=[y32, mean], writes=[y32])
                fw.op("act", lambda e: e.activation(out=sq[:, :, 0:n], in_=y32[:, :, 0:n], func=AF.Square), reads=[y32], writes=[sq])
                pv = self.bank()
                for cc in range(4):
                    fw.op("pe", lambda e, cc=cc: e.matmul(pv[:, 0:n], self.ones[:, :], sq[:, cc, 0:n], start=(cc == 0), stop=(cc == 3)), reads=[self.ones, sq], writes=[pv], sig=(cc == 3))
                fw.op("act", lambda e: e.activation(out=rstd[:, 0:n], in_=pv[:, 0:n], func=AF.Sqrt, bias=self.eps[:, 0:1], scale=1.0 / 512), reads=[pv, self.eps], writes=[rstd])
                fw.op("dve", lambda e: e.reciprocal(out=rstd[:, 0:n], in_=rstd[:, 0:n]), reads=[rstd], writes=[rstd])
                if cut <= 2:
                    continue
                o = ob[bi % 2]
                for cc in range(4):
                    fw.op("dve", lambda e, cc=cc: e.scalar_tensor_tensor(out=y32[:, cc, 0:n], in0=y32[:, cc, 0:n], scalar=self.clngT[:, l, cc:cc + 1], in1=rstd[:, 0:n], op0=ALU.mult, op1=ALU.mult),
                          reads=[y32, self.clngT, rstd], writes=[y32])
                    fw.op("act", lambda e, cc=cc: e.activation(out=o[:, cc, 0:n], in_=y32[:, cc, 0:n], func=AF.Silu, bias=self.clnbT[:, l, cc:cc + 1], scale=1.0),
                          reads=[y32, self.clnbT], writes=[o])
                fw.dma("sp", self.BR.t[8:12, :, t0:t0 + n].rearrange("c p t -> p c t"), o[:, :, 0:n], reads=[o], writes=[self.BR], key=f"c_o{bi % 2}")

    def ph_hgrn(self):
        fw, l = self.fw, self.l
        NCH = T // 64
        for d in range(2):
            for hp in range(4):
                with fw.scope():
                    vtok = fw.sb([128, NT, 128], BF16, "hv")
                    fw.dma("sp", vtok[:, :, :], self.VT.t[5 + hp].rearrange("(n p) c -> p n c", p=128), reads=[self.VT], writes=[vtok], key="h_v")
                    mask = fw.sb([128, 256], F32, "hmask")
                    mname = "mask_f" if d == 0 else "mask_b"
                    fw.dma("sp", mask[:, :], self.CI[mname][:, 0:256], reads=[self.CI[mname]], writes=[mask], key="h_m")
                    maski = mask.t[:, :].bitcast(mybir.dt.int32)
                    qT = [fw.sb([64, T], BF16, f"hq{i}") for i in range(2)]
                    qpT = [fw.sb([64, T], BF16, f"hqp{i}") for i in range(2)]
                    kT = [fw.sb([64, T], BF16, f"hk{i}") for i in range(2)]
                    dec2 = fw.sb([64, 2, NCH], F32, "hdec2")
                    ktok = fw.sb([128, NT, 128], BF16, "hkt")
                    dec = fw.sb([128, NCH], F32, "hdec")
                    with fw.scope():
                        onesf = fw.sb([128, 64], F32, "h1")
                        fw.op("pool", lambda e: e.memset(onesf[:, :], 1.0), writes=[onesf])
                        zin = fw.sb([128, T], BF16, "hz")
                        qin = fw.sb([128, T], BF16, "hqi")
                        qs = fw.sb([128, T], F32, "hqs")
                        f = fw.sb([128, T], F32, "hf")
                        lf = fw.sb([128, T], F32, "hlf")
                        cum = fw.sb([128, T], F32, "hcum")
                        kk = fw.sb([128, T], F32, "hkk")
                        ex = f
                        khT = fw.sb([128, T], BF16, "hkh")
                        c3 = cum.t.rearrange("p (c s) -> p c s", s=64)
                        e3 = ex.t.rearrange("p (c s) -> p c s", s=64)
                        iend, imid = (63, 31) if d == 0 else (0, 32)
                        fw.dma("sp", qin[:, :], self.PT[26 + hp][:, :], reads=[self.PT[26 + hp]], writes=[qin], key="h_qi")
                        fw.dma("sp", zin[:, :], self.PT[30 + 4 * d + hp][:, :], reads=[self.PT[30 + 4 * d + hp]], writes=[zin], key="h_zi")
                        fw.op("act", lambda e: e.activation(out=qs[:, :], in_=qin[:, :], func=AF.Silu), reads=[qin], writes=[qs])
                        fw.op("act", lambda e: e.activation(out=f[:, :], in_=zin[:, :], func=AF.Sigmoid), reads=[zin], writes=[f])
                        li = d * 4 + hp
                        fw.op("dve", lambda e: e.tensor_scalar(out=f[:, :], in0=f[:, :], scalar1=self.omlT[:, l, li:li + 1], scalar2=self.lbT[:, l, li:li + 1], op0=ALU.mult, op1=ALU.add),
                              reads=[f, self.omlT, self.lbT], writes=[f])
                        fw.op("act", lambda e: e.activation(out=lf[:, :], in_=f[:, :], func=AF.Ln), reads=[f], writes=[lf])
                        fw.op("pool", lambda e: e.tensor_scalar(out=kk[:, :], in0=f[:, :], scalar1=-1.0, scalar2=1.0, op0=ALU.mult, op1=ALU.add), reads=[f], writes=[kk])
                        for c in range(NCH):
                            fw.op("dve", lambda e: e.tensor_tensor_scan(out=cum[:, c * 64:(c + 1) * 64], data0=onesf[:, :], data1=lf[:, c * 64:(c + 1) * 64], initial=0.0,
                                                                       op0=ALU.mult, op1=ALU.add), reads=[onesf, lf], writes=[cum], sig=(c == NCH - 1))
                        if d == 1:
                            fw.op("dve", lambda e: e.tensor_tensor(out=e3, in0=c3[:, :, 63:64].broadcast_to([128, NCH, 64]), in1=c3, op=ALU.subtract), reads=[cum], writes=[ex])
                            fw.op("dve", lambda e: e.tensor_tensor(out=cum[:, :], in0=ex[:, :], in1=lf[:, :], op=ALU.add), reads=[ex, lf], writes=[cum])
                        fw.op("act", lambda e: e.activation(out=dec[:, :], in_=c3[:, :, iend], func=AF.Exp), reads=[cum], writes=[dec])
                        for hh in range(2):
                            fw.op("dve", lambda e: e.tensor_copy(out=dec2[0:64, hh, :], in_=dec[hh * 64:(hh + 1) * 64, :]), reads=[dec], writes=[dec2])
                        fw.op("act", lambda e: e.activation(out=ex[:, :], in_=cum[:, :], func=AF.Exp), reads=[cum], writes=[ex])
                        for hh in range(2):
                            fw.op("dve", lambda e: e.scalar_tensor_tensor(out=qpT[hh][0:64, :], in0=qs[hh * 64:(hh + 1) * 64, :], scalar=0.125, in1=ex[hh * 64:(hh + 1) * 64, :], op0=ALU.mult, op1=ALU.mult),
                                  reads=[qs, ex], writes=[qpT[hh]])
                        fw.op("dve", lambda e: e.tensor_tensor(out=e3, in0=c3[:, :, iend:iend + 1].broadcast_to([128, NCH, 64]), in1=c3, op=ALU.subtract), reads=[cum], writes=[ex])
                        fw.op("act", lambda e: e.activation(out=ex[:, :], in_=ex[:, :], func=AF.Exp), reads=[ex], writes=[ex])
                        fw.op("dve", lambda e: e.tensor_tensor(out=khT[:, :], in0=kk[:, :], in1=ex[:, :], op=ALU.mult), reads=[kk, ex], writes=[khT])
                        fw.op("dve", lambda e: e.tensor_tensor(out=e3, in0=c3, in1=c3[:, :, imid:imid + 1].broadcast_to([128, NCH, 64]), op=ALU.subtract), reads=[cum], writes=[ex])
                        fw.op("act", lambda e: e.activation(out=lf[:, :], in_=ex[:, :], func=AF.Exp), reads=[ex], writes=[lf])
                        for hh in range(2):
                            fw.op("dve", lambda e: e.scalar_tensor_tensor(out=qT[hh][0:64, :], in0=qs[hh * 64:(hh + 1) * 64, :], scalar=0.125, in1=lf[hh * 64:(hh + 1) * 64, :], op0=ALU.mult, op1=ALU.mult),
                                  reads=[qs, lf], writes=[qT[hh]])
                        fw.op("act", lambda e: e.activation(out=lf[:, :], in_=ex[:, :], func=AF.Exp, scale=-1.0), reads=[ex], writes=[lf])
                        for hh in range(2):
                            fw.op("dve", lambda e: e.tensor_tensor(out=kT[hh][0:64, :], in0=kk[hh * 64:(hh + 1) * 64, :], in1=lf[hh * 64:(hh + 1) * 64, :], op=ALU.mult), reads=[kk, lf], writes=[kT[hh]])
                        import os as _os
                        hcut = int(_os.environ.get("HG_CUT", "99"))
                        for tt in range(NT if hcut > 1 else 0):
                            fw.op("pe", lambda e: e.transpose(self.psT[:, (tt % 8) * 128:(tt % 8) * 128 + 128], khT[:, tt * 128:(tt + 1) * 128], self.identb[:, :]),
                                  reads=[khT, self.identb], writes=[self.psT], sig=(tt % 8 == 7 or tt == NT - 1))
                            if tt % 8 == 7 or tt == NT - 1:
                                t8 = tt - (tt % 8)
                                nn = tt - t8 + 1
                                fw.op("act", lambda e: e.activation(out=ktok[:, t8:t8 + nn, :], in_=self.psT.t[:, 0:nn * 128].rearrange("p (a c) -> p a c", c=128), func=AF.Copy),
                                      reads=[self.psT], writes=[ktok])
                    S = fw.sb([64, 2, 64], F32, "hS")
                    Sb = [fw.sb([64, 2, 64], BF16, f"hSb{i}") for i in range(2)]
                    fw.op("dve", lambda e: e.memset(S[:, :, :], 0.0), writes=[S])
                    fw.op("dve", lambda e: e.memset(Sb[0][:, :, :], 0.0), writes=[Sb[0]])
                    attm = [fw.sb([128, 256], BF16, f"hatt{i}") for i in range(2)]
                    for a_ in attm:
                        fw.op("dve", lambda e: e.memset(a_[:, :], 0.0), writes=[a_])
                    ost = fw.sb([64, 2, T], F32, "host")
                    tiles = list(range(NT)) if d == 0 else [1, 0] + list(range(NT - 1, 1, -1))
                    if hcut <= 2:
                        tiles = []
                    sbi = 0
                    for ti, tt in enumerate(tiles):
                        am = attm[ti % 2]
                        pa = self.psb[1 + ti % 2]
                        for hh in range(2):
                            r0 = hh * 64
                            fw.op("pe", lambda e: e.matmul(pa[:, hh * 128:hh * 128 + 128], kT[hh][0:64, tt * 128:(tt + 1) * 128], qT[hh][0:64, tt * 128:(tt + 1) * 128], start=True, stop=True),
                                  reads=[kT[hh], qT[hh]], writes=[pa], sig=(hh == 1))
                        fw.op("dve", lambda e: e.copy_predicated(out=am[:, :], mask=maski, data=pa[:, 0:256]), reads=[pa, mask], writes=[am])
                        if hcut <= 3:
                            continue
                        po = self.psb[3 + ti % 2]
                        chunks = [0, 1] if d == 0 else [1, 0]
                        if hcut <= 4:
                            chunks = []
                        for hh in range(2):
                            fw.op("pe", lambda e: e.matmul(po[0:64, hh * 128:hh * 128 + 128], vtok[:, tt, hh * 64:(hh + 1) * 64], am[:, hh * 128:(hh + 1) * 128],
                                                           start=(hh == 0), stop=False, skip_group_check=True), reads=[vtok, am], writes=[po], sig=False)
                        for ci, cj in enumerate(chunks):
                            c = tt * 2 + cj
                            sb_cur = Sb[sbi % 2]
                            sb_nxt = Sb[(sbi + 1) % 2]
                            sbi += 1
                            for hh in range(2):
                                r0 = hh * 64
                                fw.op("pe", lambda e: e.matmul(po[0:64, hh * 128 + cj * 64:hh * 128 + cj * 64 + 64], sb_cur[0:64, hh, :], qpT[hh][0:64, c * 64:(c + 1) * 64],
                                                               start=False, stop=(ci == 1), skip_group_check=True), reads=[sb_cur, qpT[hh]], writes=[po], sig=(hh == 1))
                            pS = self.psb[5 + sbi % 2]
                            for hh in range(2):
                                fw.op("pe", lambda e: e.matmul(pS[0:64, hh * 64:(hh + 1) * 64], ktok[cj * 64:cj * 64 + 64, tt, hh * 64:(hh + 1) * 64], vtok[cj * 64:cj * 64 + 64, tt, hh * 64:(hh + 1) * 64],
                                                               start=(hh == 0), stop=(hh == 1), skip_group_check=True), reads=[ktok, vtok], writes=[pS], sig=(hh == 1))
                            for hh in range(2):
                                fw.op("dve", lambda e: e.scalar_tensor_tensor(out=S[0:64, hh, :], in0=S[0:64, hh, :], scalar=dec2[0:64, hh, c:c + 1], in1=pS[0:64, hh * 64:(hh + 1) * 64], op0=ALU.mult, op1=ALU.add),
                                      reads=[S, dec2, pS], writes=[S])
                            fw.op("act", lambda e: e.activation(out=sb_nxt[:, :, :], in_=S[:, :, :], func=AF.Copy), reads=[S], writes=[sb_nxt])
                        fw.op("act", lambda e: e.activation(out=ost[:, :, tt * 128:(tt + 1) * 128], in_=po.t[0:64, 0:256].rearrange("p (a c) -> p a c", c=128), func=AF.Copy),
                              reads=[po], writes=[ost])
                    fw.dma("sp", self.OD[d].t[2 * hp:2 * hp + 2].rearrange("h e t -> e h t"), ost[:, :, :], reads=[ost], writes=[self.OD[d]], key="h_o")
        with fw.scope():
            of = [fw.sb([64, T], F32, f"hof{i}") for i in range(2)]
            obw = [fw.sb([64, T], F32, f"hobw{i}") for i in range(2)]
            gt = [fw.sb([64, T], BF16, f"hgt{i}") for i in range(2)]
            sq = fw.sb([64, 512], BF16, "hsq")
            rstd = fw.sb([64, 512], F32, "hrstd")
            sg = fw.sb([64, 512], F32, "hsg")
            ob = [fw.sb([64, T], BF16, f"hob{i}") for i in range(2)]
            for h in range(8):
                a, b, g, o = of[h % 2], obw[h % 2], gt[h % 2], ob[h % 2]
                fw.dma("sp", a[:, :], self.OD[0].t[h], reads=[self.OD[0]], writes=[a], key=f"hn_a{h % 2}")
                fw.dma("sp", b[:, :], self.OD[1].t[h], reads=[self.OD[1]], writes=[b], key=f"hn_b{h % 2}")
                fw.dma("sp", g[:, :], self.PT[42 + h // 2].t[(h % 2) * 64:(h % 2) * 64 + 64, :], reads=[self.PT[42 + h // 2]], writes=[g], key=f"hn_g{h % 2}")
                fw.op("pool", lambda e, a=a, b=b: e.tensor_tensor(out=a[:, :], in0=a[:, :], in1=b[:, :], op=ALU.add), reads=[a, b], writes=[a])
                for (t0, n, ic) in BLK:
                    fw.op("act", lambda e, a=a: e.activation(out=sq[:, 0:n], in_=a[:, t0:t0 + n], func=AF.Square), reads=[a], writes=[sq])
                    ps = self.bank()
                    fw.op("pe", lambda e, ps=ps: e.matmul(ps[0:64, 0:n], self.ones[0:64, 0:64], sq[0:64, 0:n], start=True, stop=True), reads=[self.ones, sq], writes=[ps])
                    fw.op("act", lambda e, ps=ps: e.activation(out=rstd[:, 0:n], in_=ps[0:64, 0:n], func=AF.Sqrt, bias=self.eps[0:64, 0:1], scale=1.0 / 64), reads=[ps, self.eps], writes=[rstd])
                    fw.op("dve", lambda e: e.reciprocal(out=rstd[:, 0:n], in_=rstd[:, 0:n]), reads=[rstd], writes=[rstd])
                    fw.op("act", lambda e, g=g: e.activation(out=sg[:, 0:n], in_=g[:, t0:t0 + n], func=AF.Silu), reads=[g], writes=[sg])
                    fw.op("dve", lambda e: e.scalar_tensor_tensor(out=rstd[:, 0:n], in0=rstd[:, 0:n], scalar=self.hgT[:, l:l + 1], in1=sg[:, 0:n], op0=ALU.mult, op1=ALU.mult),
                          reads=[rstd, self.hgT, sg], writes=[rstd])
                    fw.op("dve", lambda e, a=a, o=o: e.tensor_tensor(out=o[:, t0:t0 + n], in0=a[:, t0:t0 + n], in1=rstd[:, 0:n], op=ALU.mult), reads=[a, rstd], writes=[o])
                fw.dma("sp", self.BR.t[12 + h // 2, (h % 2) * 64:(h % 2) * 64 + 64, :], o[:, :], reads=[o], writes=[self.BR], key=f"hn_o{h % 2}")

    def ph_merge(self):
        fw, l = self.fw, self.l
        with fw.scope():
            wb = fw.sb([128, 16, D], BF16, "wbr")
            wo = fw.sb([128, 8, D], BF16, "wout")
            for i in range(4):
                fw.dma("pool", wb[:, i * 4:(i + 1) * 4, :], self.I["w_branch"].t[l, i].rearrange("(k p) c -> p k c", p=128), reads=[self.I["w_branch"]], writes=[wb], key="m_wb")
            fw.dma("pool", wo[:, :, :], self.I["w_out"].t[l].rearrange("(k p) c -> p k c", p=128), reads=[self.I["w_out"]], writes=[wo], key="m_wo")
            brs = [fw.sb([128, 16, 512], BF16, f"mbr{i}") for i in range(1)]
            gts = [fw.sb([128, 32, 512], BF16, f"mgt{i}") for i in range(1)]
            xbs = [fw.sb([128, 8, 512], F32, f"mxb{i}") for i in range(1)]
            mg = fw.sb([128, 512], F32, "mmg")
            tmp = fw.sb([128, 512], F32, "mtmp")
            mgb = fw.sb([128, 8, 512], BF16, "mmgb")
            xo = [fw.sb([128, 8, 512], F32, f"mxo{i}") for i in range(2)]
            src = self.xsrc()
            for bi, (t0, n, ic) in enumerate(self.qblocks()):
                br, gt, xb, xn = brs[0], gts[0], xbs[0], xo[bi % 2]
                fw.dma("sp", br[:, :, 0:n], self.BR.t[:, :, t0:t0 + n].rearrange("c p t -> p c t"), reads=[self.BR], writes=[br], key="m_br")
                fw.dma("sp", gt[:, :, 0:n], self.PTall.t[:, :, t0:t0 + n].rearrange("c p t -> p c t"), reads=[self.PTall], writes=[gt], key="m_gt")
                fw.dma("sp", xb[:, :, 0:n], src.t.rearrange("(k p) t -> p k t", p=128)[:, :, t0:t0 + n], reads=[src], writes=[xb], key="m_xb")
                for oc in range(8):
                    for i in range(4):
                        ps = self.bank()
                        for k in range(4):
                            fw.op("pe", lambda e, ps=ps, i=i, k=k, oc=oc: e.matmul(ps[:, 0:n], wb[:, i * 4 + k, oc * 128:(oc + 1) * 128], br[:, i * 4 + k, 0:n], start=(k == 0), stop=(k == 3)),
                                  reads=[wb, br], writes=[ps], sig=(k == 3))
                        if i == 0:
                            fw.op("dve", lambda e, ps=ps, oc=oc: e.tensor_tensor(out=mg[:, 0:n], in0=ps[:, 0:n], in1=gt[:, oc, 0:n], op=ALU.mult), reads=[ps, gt], writes=[mg])
                        else:
                            fw.op("dve", lambda e, ps=ps, oc=oc, i=i: e.tensor_tensor(out=tmp[:, 0:n], in0=ps[:, 0:n], in1=gt[:, i * 8 + oc, 0:n], op=ALU.mult), reads=[ps, gt], writes=[tmp])
                            if i < 3:
                                fw.op("pool", lambda e: e.tensor_tensor(out=mg[:, 0:n], in0=mg[:, 0:n], in1=tmp[:, 0:n], op=ALU.add), reads=[mg, tmp], writes=[mg])
                            else:
                                fw.op("pool", lambda e, oc=oc: e.tensor_tensor(out=mgb[:, oc, 0:n], in0=mg[:, 0:n], in1=tmp[:, 0:n], op=ALU.add), reads=[mg, tmp], writes=[mgb])
                for o2 in range(8):
                    ps = self.bank()
                    for k in range(8):
                        fw.op("pe", lambda e, ps=ps, k=k, o2=o2: e.matmul(ps[:, 0:n], wo[:, k, o2 * 128:(o2 + 1) * 128], mgb[:, k, 0:n], start=(k == 0), stop=(k == 7)),
                              reads=[wo, mgb], writes=[ps], sig=(k == 7))
                    fw.op("dve", lambda e, ps=ps, o2=o2: e.scalar_tensor_tensor(out=xn[:, o2, 0:n], in0=ps[:, 0:n], scalar=self.modt[:, 16 + o2, ic:ic + 1], in1=xb[:, o2, 0:n],
                                                                         op0=ALU.mult, op1=ALU.add), reads=[ps, self.modt, xb], writes=[xn])
                fw.dma("sp", self.xs.t.rearrange("(k p) t -> p k t", p=128)[:, :, t0:t0 + n], xn[:, :, 0:n], reads=[xn], writes=[self.xs], key=f"m_xo{bi % 2}")
        self.x_written = True

    def ph_ffn(self):
        fw, l = self.fw, self.l
        moe = (l % 2 == 1)
        li = l // 2
        JG = 512
        NJG = DFF // JG
        blocks = self.qblocks()
        halves = [blocks[:len(blocks) - 4], blocks[len(blocks) - 4:]]
        for half in halves:
            if not half:
                continue
            hb0 = half[0][0]
            hlen = sum(b[1] for b in half)
            with fw.scope():
                hT = fw.sb([128, 8, hlen], BF16, "fh")
                with fw.scope():
                    xbs = [fw.sb([128, 8, 512], F32, f"fxb{i}") for i in range(2)]
                    sq = fw.sb([128, 8, 512], BF16, "fsq")
                    rstd = fw.sb([128, 512], F32, "frstd")
                    tmp = fw.sb([128, 8, 512], F32, "ftmp")
                    h32 = fw.sb([128, 8, 512], F32, "fh32") if moe else None
                    if moe:
                        lg = fw.sb([128, 8], F32, "flg")
                        m1 = fw.sb([128, 1], F32, "fm1")
                        m2 = fw.sb([128, 1], F32, "fm2")
                        k1 = fw.sb([128, 8], F32, "fk1")
                        k2 = fw.sb([128, 8], F32, "fk2")
                        l2 = fw.sb([128, 8], F32, "fl2")
                        w1 = fw.sb([128, 1], F32, "fw1")
                        w2 = fw.sb([128, 1], F32, "fw2")
                        cmb = fw.sb([128, 8], F32, "fcmb")
                        cbm = fw.sb([128, 8, 128], F32, "fcbm")
                        cbo = [fw.sb([128, 8, 512], BF16, f"fcbo{i}") for i in range(2)]
                    for bi, (t0, n, ic) in enumerate(half):
                        self.norm_block(1, t0, n, ic, hT, t0 - hb0, xbs[bi % 2], sq, rstd, tmp, h32)
                        if not moe:
                            continue
                        co = cbo[bi % 2]
                        for ti in range(n // 128):
                            ps = self.bank()
                            for k in range(8):
                                fw.op("pe", lambda e, ps=ps, k=k, ti=ti: e.matmul(ps[:, 0:8], h32[:, k, ti * 128:(ti + 1) * 128], self.routerT[:, li, k, :], start=(k == 0), stop=(k == 7)),
                                      reads=[h32, self.routerT], writes=[ps], sig=(k == 7))
                            fw.op("dve", lambda e, ps=ps: e.tensor_copy(out=lg[:, :], in_=ps[:, 0:8]), reads=[ps], writes=[lg])
                            fw.op("dve", lambda e: e.reduce_max(out=m1[:, :], in_=lg[:, :], axis=AX.X), reads=[lg], writes=[m1])
                            fw.op("dve", lambda e: e.tensor_scalar(out=k1[:, :], in0=lg[:, :], scalar1=m1[:, 0:1], scalar2=None, op0=ALU.is_ge), reads=[lg, m1], writes=[k1])
                            fw.op("dve", lambda e: e.scalar_tensor_tensor(out=l2[:, :], in0=k1[:, :], scalar=-1e30, in1=lg[:, :], op0=ALU.mult, op1=ALU.add), reads=[k1, lg], writes=[l2])
                            fw.op("dve", lambda e: e.reduce_max(out=m2[:, :], in_=l2[:, :], axis=AX.X), reads=[l2], writes=[m2])
                            fw.op("dve", lambda e: e.tensor_scalar(out=k2[:, :], in0=l2[:, :], scalar1=m2[:, 0:1], scalar2=None, op0=ALU.is_ge), reads=[l2, m2], writes=[k2])
                            fw.op("dve", lambda e: e.tensor_tensor(out=w2[:, :], in0=m2[:, :], in1=m1[:, :], op=ALU.subtract), reads=[m1, m2], writes=[w2])
                            fw.op("act", lambda e: e.activation(out=w2[:, :], in_=w2[:, :], func=AF.Exp), reads=[w2], writes=[w2])
                            fw.op("dve", lambda e: e.tensor_scalar(out=w1[:, :], in0=w2[:, :], scalar1=1.0, scalar2=None, op0=ALU.add), reads=[w2], writes=[w1])
                            fw.op("dve", lambda e: e.reciprocal(out=w1[:, :], in_=w1[:, :]), reads=[w1], writes=[w1])
                            fw.op("dve", lambda e: e.tensor_scalar(out=w2[:, :], in0=w1[:, :], scalar1=-1.0, scalar2=1.0, op0=ALU.mult, op1=ALU.add), reads=[w1], writes=[w2])
                            fw.op("dve", lambda e: e.tensor_scalar(out=cmb[:, :], in0=k1[:, :], scalar1=w1[:, 0:1], scalar2=None, op0=ALU.mult), reads=[k1, w1], writes=[cmb])
                            fw.op("dve", lambda e: e.scalar_tensor_tensor(out=cmb[:, :], in0=k2[:, :], scalar=w2[:, 0:1], in1=cmb[:, :], op0=ALU.mult, op1=ALU.add), reads=[k2, w2, cmb], writes=[cmb])
                            fw.op("dve", lambda e: e.tensor_copy(out=cbm[:, :, :], in_=cmb.t[:, :].unsqueeze(2).broadcast_to([128, 8, 128])), reads=[cmb], writes=[cbm])
                            for eh in range(2):
                                pc = self.bank()
                                for e4 in range(4):
                                    ex = eh * 4 + e4
                                    fw.op("pe", lambda e, pc=pc, e4=e4, ex=ex: e.matmul(pc[:, e4 * 128:(e4 + 1) * 128], cbm[:, ex, :], self.identf[:, :], start=True, stop=True),
                                          reads=[cbm, self.identf], writes=[pc], sig=(e4 == 3))
                                fw.op("act", lambda e, pc=pc, eh=eh, ti=ti, co=co: e.activation(out=co[:, eh * 4:eh * 4 + 4, ti * 128:(ti + 1) * 128],
                                                                                     in_=pc.t.rearrange("p (a c) -> p a c", c=128), func=AF.Copy), reads=[pc], writes=[co])
                        fw.dma("sp", self.CB.t[:, :, t0:t0 + n].rearrange("e p t -> p e t"), co[:, :, 0:n], reads=[co], writes=[self.CB], key=f"f_cb{bi % 2}")
                acc = fw.sb([128, 8, hlen], F32, "facc")
                self._ffn_experts(moe, li, half, hb0, hT, acc)
                self._ffn_resid(half, hb0, acc)

    def _ffn_experts(self, moe, li, half, hb0, hT, acc):
        fw, l = self.fw, self.l
        JG = 512
        NJG = DFF // JG
        with fw.scope():
            if True:
                w1s = [fw.sb([128, 8, JG], BF16, f"fw1_{i}") for i in range(2)]
                w3s = [fw.sb([128, 8, JG], BF16, f"fw3_{i}") for i in range(2)]
                w2s = [fw.sb([128, JG // 128, D], BF16, f"fw2_{i}") for i in range(2)]
                sa = [fw.sb([128, 512], BF16, f"fsa{i}") for i in range(2)]
                gb = [fw.sb([128, JG // 128, 512], BF16, f"fgb{i}") for i in range(2)]
                cbl = [fw.sb([128, 512], BF16, f"fcl{i}") for i in range(2)]
                ng = 0
                nb = 0
                first_acc = True
                for ex in range(NEXP if moe else 1):
                    for jg in range(NJG):
                        wa, wc, wd = w1s[ng % 2], w3s[ng % 2], w2s[ng % 2]
                        if moe:
                            s1, s3, s2 = self.I["moe_w1"].t[li, ex], self.I["moe_w3"].t[li, ex], self.I["moe_w2"].t[li, ex]
                            r1, r3, r2 = self.I["moe_w1"], self.I["moe_w3"], self.I["moe_w2"]
                        else:
                            s1, s3, s2 = self.I["ffn_w1"].t[li], self.I["ffn_w3"].t[li], self.I["ffn_w2"].t[li]
                            r1, r3, r2 = self.I["ffn_w1"], self.I["ffn_w3"], self.I["ffn_w2"]
                        fw.dma("pool", wa[:, :, :], s1.rearrange("(k p) c -> p k c", p=128)[:, :, jg * JG:(jg + 1) * JG], reads=[r1], writes=[wa], key=f"f_w1{ng % 2}")
                        fw.dma("pool", wc[:, :, :], s3.rearrange("(k p) c -> p k c", p=128)[:, :, jg * JG:(jg + 1) * JG], reads=[r3], writes=[wc], key=f"f_w3{ng % 2}")
                        fw.dma("pool", wd[:, :, :], s2[jg * JG:(jg + 1) * JG, :].rearrange("(k p) c -> p k c", p=128), reads=[r2], writes=[wd], key=f"f_w2{ng % 2}")
                        ng += 1
                        for (t0, n, ic) in half:
                            c0 = t0 - hb0
                            g = gb[nb % 2]
                            cl = cbl[nb % 2]
                            nb += 1
                            if moe:
                                fw.dma("sp", cl[:, 0:n], self.CB.t[ex, :, t0:t0 + n], reads=[self.CB], writes=[cl], key=f"f_cl{nb % 2}")
                            for jc in range(JG // 128):
                                p1 = self.psb[1 + (jc % 2) * 2]
                                p3 = self.psb[2 + (jc % 2) * 2]
                                for k in range(8):
                                    fw.op("pe", lambda e, p1=p1, k=k, jc=jc: e.matmul(p1[:, 0:n], wa[:, k, jc * 128:(jc + 1) * 128], hT[:, k, c0:c0 + n], start=(k == 0), stop=(k == 7)),
                                          reads=[wa, hT], writes=[p1], sig=(k == 7))
                                for k in range(8):
                                    fw.op("pe", lambda e, p3=p3, k=k, jc=jc: e.matmul(p3[:, 0:n], wc[:, k, jc * 128:(jc + 1) * 128], hT[:, k, c0:c0 + n], start=(k == 0), stop=(k == 7)),
                                          reads=[wc, hT], writes=[p3], sig=(k == 7))
                                s = sa[jc % 2]
                                fw.op("act", lambda e, p1=p1, s=s: e.activation(out=s[:, 0:n], in_=p1[:, 0:n], func=AF.Silu), reads=[p1], writes=[s])
                                fw.op("dve", lambda e, p3=p3, s=s, g=g, jc=jc: e.tensor_tensor(out=g[:, jc, 0:n], in0=p3[:, 0:n], in1=s[:, 0:n], op=ALU.mult), reads=[p3, s], writes=[g])
                                if moe:
                                    fw.op("pool", lambda e, g=g, jc=jc, cl=cl: e.tensor_tensor(out=g[:, jc, 0:n], in0=g[:, jc, 0:n], in1=cl[:, 0:n], op=ALU.mult), reads=[g, cl], writes=[g])
                            for o in range(8):
                                pf = self.psb[5 + (o % 2)]
                                for jc in range(JG // 128):
                                    fw.op("pe", lambda e, pf=pf, jc=jc, o=o, g=g: e.matmul(pf[:, 0:n], wd[:, jc, o * 128:(o + 1) * 128], g[:, jc, 0:n], start=(jc == 0), stop=(jc == JG // 128 - 1)),
                                          reads=[wd, g], writes=[pf], sig=(jc == JG // 128 - 1))
                                if first_acc:
                                    fw.op("act", lambda e, pf=pf, o=o: e.activation(out=acc[:, o, c0:c0 + n], in_=pf[:, 0:n], func=AF.Copy), reads=[pf], writes=[acc])
                                else:
                                    fw.op("dve", lambda e, pf=pf, o=o: e.tensor_tensor(out=acc[:, o, c0:c0 + n], in0=acc[:, o, c0:c0 + n], in1=pf[:, 0:n], op=ALU.add), reads=[pf, acc], writes=[acc])
                        first_acc = False

    def _ffn_resid(self, half, hb0, acc):
        fw, l = self.fw, self.l
        with fw.scope():
            if True:
                xbs = [fw.sb([128, 8, 512], F32, f"fxr{i}") for i in range(2)]
                for bi, (t0, n, ic) in enumerate(half):
                    c0 = t0 - hb0
                    xb = xbs[bi % 2]
                    fw.dma("sp", xb[:, :, 0:n], self.xs.t.rearrange("(k p) t -> p k t", p=128)[:, :, t0:t0 + n], reads=[self.xs], writes=[xb], key=f"f_xr{bi % 2}")
                    for k in range(8):
                        fw.op("dve", lambda e, k=k: e.scalar_tensor_tensor(out=xb[:, k, 0:n], in0=acc[:, k, c0:c0 + n], scalar=self.modt[:, 40 + k, ic:ic + 1], in1=xb[:, k, 0:n],
                                                                          op0=ALU.mult, op1=ALU.add), reads=[acc, self.modt, xb], writes=[xb])
                    fw.dma("sp", self.xs.t.rearrange("(k p) t -> p k t", p=128)[:, :, t0:t0 + n], xb[:, :, 0:n], reads=[xb], writes=[self.xs], key=f"f_xw{bi % 2}")


def host_inputs(inp, b):
    f = np.float32
    def colT(v):
        v = np.asarray(v, f)
        sh = v.shape
        v = v.reshape(sh[:-1] + (sh[-1] // 128, 128))
        return np.ascontiguousarray(np.moveaxis(v, -1, 0))
    m = {}
    m["xT0"] = np.ascontiguousarray(np.concatenate([inp["ctx"][b], inp["x"][b]], axis=0).T.astype(f))
    m["cvec"] = np.ascontiguousarray(np.stack([colT(inp["c"][b]), colT(inp["c_ctx"])], axis=-1))
    m["ada_w"] = np.asarray(inp["ada_w"], f)
    m["ada_bT"] = colT(inp["ada_b"])
    m["g1T"] = colT(inp["norm1_g"])
    m["g2T"] = colT(inp["norm2_g"])
    m["w_in"] = np.asarray(inp["w_in"], f)
    qkg = np.asarray(inp["qk_norm_g"], f)
    m["qkgT"] = np.ascontiguousarray(np.tile(np.transpose(qkg, (2, 0, 1)), (2, 1, 1)))
    m["lamB"] = np.ascontiguousarray(np.broadcast_to(np.asarray(inp["diff_lambda"], f).reshape(1, DEPTH, 256), (128, DEPTH, 256)))
    m["sublnT"] = np.ascontiguousarray(np.asarray(inp["diff_subln_g"], f).T)
    cw = np.asarray(inp["conv_w"], f)
    m["convwT"] = np.ascontiguousarray(np.transpose(cw.reshape(DEPTH, 31, 4, 128), (3, 0, 2, 1)))
    m["convbT"] = colT(inp["conv_b"])
    m["clngT"] = colT(inp["conv_ln_g"])
    m["clnbT"] = colT(inp["conv_ln_b"])
    m["lblT"] = colT(inp["hgrn_lb_logits"])
    m["hgT"] = np.ascontiguousarray(np.asarray(inp["hgrn_norm_g"], f).T)
    m["w_branch"] = np.asarray(inp["w_branch"], f)
    m["w_out"] = np.asarray(inp["w_out"], f)
    for k in ("ffn_w1", "ffn_w3", "ffn_w2", "moe_w1", "moe_w3", "moe_w2"):
        m[k] = np.asarray(inp[k], f)
    r = np.asarray(inp["moe_router"], f)
    m["routerT"] = np.ascontiguousarray(np.transpose(r.reshape(2, 8, 128, 8), (2, 0, 1, 3)))
    for k, v in _const_tables().items():
        m["c_" + k] = v
    return m


_CACHE = {}


def kernel(**inputs):
    if "nc" not in _CACHE:
        _CACHE["nc"] = MK().build()
    nc = _CACHE["nc"]
    in_maps = [host_inputs(inputs, b) for b in range(8)]
    res = run_bass_kernel_spmd(nc, in_maps, core_ids=list(range(8)))
    out = np.stack([np.ascontiguousarray(res.results[b]["xs"][:, NCTX:].T) for b in range(8)], axis=0)
    return out.astype(np.float32)
```

```python
import math
from contextlib import ExitStack, contextmanager
import numpy as np
import ml_dtypes
import concourse.bass as bass
import concourse.mybir as mybir
from concourse.bass_utils import run_bass_kernel_spmd

F32 = mybir.dt.float32
BF16 = mybir.dt.bfloat16
AF = mybir.ActivationFunctionType
ALU = mybir.AluOpType
AX = mybir.AxisListType

D = 1024
SEQ = 4096
NCTX = 256
T = SEQ + NCTX
NT = T // 128
DEPTH = 4
D_IN = 9984
NOC = D_IN // 128
DFF = 3584
NEXP = 8
EPS = 1e-6
BLK = [(0, 256, 1)] + [(256 + 512 * i, 512, 0) for i in range(8)]
VCH = {5: 0, 14: 1, 15: 2, 16: 3, 17: 4, 38: 5, 39: 6, 40: 7, 41: 8}
QKCH = [0, 1, 2, 3, 4, 6, 7, 8, 9, 10, 11, 12, 13]
UPAD = 15
ULEN = UPAD + 256 + UPAD + UPAD + 4096 + UPAD
UOFF = [UPAD, UPAD + 256 + 2 * UPAD]


class Buf:
    __slots__ = ("t", "name", "key", "w", "r", "psum")

    def __init__(self, t, name, key=None):
        self.psum = False
        self.t = t
        self.name = name
        self.key = key or name
        self.w = None
        self.r = {}

    def __getitem__(self, idx):
        return self.t[idx]


class FW:
    def __init__(self, nc, root):
        self.nc = nc
        self.root = root
        self.stack = root
        self.engs = {"pe": nc.tensor, "act": nc.scalar, "dve": nc.vector, "pool": nc.gpsimd, "sp": nc.sync}
        self.sem = {k: root.enter_context(nc.semaphore("s_" + k)) for k in self.engs}
        self.cnt = {k: 0 for k in self.engs}
        self.seen = {k: {} for k in self.engs}
        self.dsem = {}
        self.nbuf = 0
        self.ninst = 0

    @contextmanager
    def scope(self):
        old = self.stack
        with ExitStack() as s:
            self.stack = s
            try:
                yield
            finally:
                self.barrier()
                self.stack = old

    def sb(self, shape, dt, name=None):
        self.nbuf += 1
        key = name or 'sb'
        name = f"{key}_{self.nbuf}"
        t = self.stack.enter_context(self.nc.sbuf_tensor(name, list(shape), dt))
        return Buf(t, name, key)

    def ps(self, shape, dt=F32, name=None):
        self.nbuf += 1
        name = f"{name or 'ps'}_{self.nbuf}"
        t = self.root.enter_context(self.nc.psum_tensor(name, list(shape), dt))
        b = Buf(t, name)
        b.psum = True
        return b

    def dram(self, name, shape, dt, kind="Internal"):
        t = self.nc.dram_tensor(name, list(shape), dt, kind=kind)
        return Buf(t.ap(), name)

    def _semof(self, key):
        return self.sem[key] if key in self.sem else self.dsem[key][0]

    def _wait(self, E, ev):
        if ev is None:
            return
        key, val = ev
        if key not in self.sem:
            val = 16 * self.dsem[key][1]
        if key == "pe" and E == "pe":
            return
        if key == E and val > self.cnt[E]:
            return
        if self.seen[E].get(key, 0) >= val:
            return
        self.seen[E][key] = val
        self.engs[E].wait_ge(self._semof(key), val)
        self.ninst += 1

    def _deps(self, E, reads, writes):
        for b in reads:
            self._wait(E, b.w)
            if b.psum:
                for k, v in b.r.items():
                    if k != E:
                        self._wait(E, (k, v))
        for b in writes:
            self._wait(E, b.w)
            for k, v in b.r.items():
                self._wait(E, (k, v))

    def _record(self, ev, reads, writes):
        k, v = ev
        for b in reads:
            if b.r.get(k, 0) < v:
                b.r[k] = v
        for b in writes:
            b.w = ev
            b.r = {}

    def op(self, E, fn, reads=(), writes=(), sig=True):
        self._deps(E, reads, writes)
        ins = fn(self.engs[E])
        self.ninst += 1
        if sig:
            self.cnt[E] += 1
            ins.then_inc(self.sem[E], 1)
            ev = (E, self.cnt[E])
        else:
            ev = (E, self.cnt[E] + 1)
        self._record(ev, reads, writes)
        return ins

    def dma(self, Q, out, in_, reads=(), writes=(), key=None, **kw):
        self._deps(Q, reads, writes)
        if key is None:
            key = "d_" + (writes[0].key if writes else reads[0].key)
        if key not in self.dsem:
            s = self.root.enter_context(self.nc.semaphore("q_" + str(len(self.dsem))))
            self.dsem[key] = [s, 0]
        ent = self.dsem[key]
        ent[1] += 1
        ins = self.engs[Q].dma_start(out=out, in_=in_, **kw)
        ins.then_inc(ent[0], 16)
        self.ninst += 1
        self._record((key, 16 * ent[1]), reads, writes)
        return ins

    def barrier(self, engines=("pe", "act", "dve", "pool", "sp")):
        for E in engines:
            for k in ("pe", "act", "dve", "pool", "sp"):
                if k != E and self.cnt[k]:
                    self._wait(E, (k, self.cnt[k]))
            for key, (s, c) in self.dsem.items():
                if c:
                    self._wait(E, (key, 16 * c))


def _const_tables():
    c = {}
    c["ones"] = np.ones((128, 128), np.float32)
    bd = np.zeros((128, 128), np.float32)
    bd[:64, :64] = 1
    bd[64:, 64:] = 1
    c["bd64"] = bd
    c["ident"] = np.eye(128, dtype=np.float32)
    R = np.zeros((128, 128), np.float32)
    for p in range(128):
        d = p % 64
        j = d % 32
        if j < 16:
            R[p + 16, p] = -1.0
        else:
            R[p - 16, p] = 1.0
    c["rot"] = R
    inv_freq = (10000.0 ** (-np.arange(0, 32, 2, dtype=np.float32) / 32)).astype(np.float32)
    tl = np.arange(SEQ)
    row = (tl // 64).astype(np.float32)
    col = (tl % 64).astype(np.float32)
    cos = np.ones((128, T), np.float32)
    sin = np.zeros((128, T), np.float32)
    for p in range(128):
        d = p % 64
        pos = row if d < 32 else col
        f = inv_freq[(d % 32) % 16]
        ang = (pos * f).astype(np.float32)
        cos[p, NCTX:] = np.cos(ang)
        sin[p, NCTX:] = np.sin(ang)
    c["cos"] = cos
    c["sin"] = sin
    s = np.arange(128)[:, None]
    t = np.arange(128)[None, :]
    same = (s // 64) == (t // 64)
    c["mask_f"] = np.tile((same & (s <= t)).astype(np.float32), (1, 4))
    c["mask_b"] = np.tile((same & (s >= t)).astype(np.float32), (1, 4))
    return c


CONST_SPECS = [("ones", [128, 128]), ("bd64", [128, 128]), ("ident", [128, 128]), ("rot", [128, 128]),
               ("cos", [128, T]), ("sin", [128, T]), ("mask_f", [128, 512]), ("mask_b", [128, 512])]

IN_SPECS = [
    ("xT0", [D, T]), ("cvec", [128, 8, 2]), ("ada_w", [DEPTH, D, 6 * D]), ("ada_bT", [128, DEPTH, 48]),
    ("g1T", [128, DEPTH, 8]), ("g2T", [128, DEPTH, 8]), ("w_in", [DEPTH, D, D_IN]),
    ("qkgT", [128, DEPTH, 4]), ("lamB", [128, DEPTH, 256]), ("sublnT", [128, DEPTH]),
    ("convwT", [128, DEPTH, 4, 31]), ("convbT", [128, DEPTH, 4]), ("clngT", [128, DEPTH, 4]),
    ("clnbT", [128, DEPTH, 4]), ("lblT", [128, DEPTH, 2, 4]), ("hgT", [64, DEPTH]),
    ("w_branch", [DEPTH, 4, 512, D]), ("w_out", [DEPTH, D, D]),
    ("ffn_w1", [2, D, DFF]), ("ffn_w3", [2, D, DFF]), ("ffn_w2", [2, DFF, D]),
    ("routerT", [128, 2, 8, 8]), ("moe_w1", [2, NEXP, D, DFF]), ("moe_w3", [2, NEXP, D, DFF]),
    ("moe_w2", [2, NEXP, DFF, D]),
]


class MK:
    def __init__(self, n_layers=DEPTH, debug=(), stop_after=None, ext_in=()):
        self.n_layers = n_layers
        self.debug = set(debug)
        self.ext_in = set(ext_in)
        self.stop_after = stop_after
        self.nc = bass.Bass("TRN2", target_bir_lowering=False)
        self.rr = 0

    def scratch(self, name, shape, dt):
        kind = "Internal"
        if name in self.debug:
            kind = "ExternalOutput"
        if name in self.ext_in:
            kind = "ExternalInput"
        return self.fw.dram(name, shape, dt, kind=kind)

    def build(self):
        nc = self.nc
        with ExitStack() as root:
            fw = self.fw = FW(nc, root)
            big = ("ada_w", "w_in", "w_branch", "w_out", "ffn_w1", "ffn_w3", "ffn_w2", "moe_w1", "moe_w3", "moe_w2")
            tiny = getattr(self, "tiny", False)
            self.I = {n: fw.dram(n, ([1] * len(s) if (tiny and n in big) else s), F32, kind="ExternalInput") for n, s in IN_SPECS}
            self.CI = {n: fw.dram("c_" + n, s, F32, kind="ExternalInput") for n, s in CONST_SPECS}
            self.xs = fw.dram("xs", [D, T], F32, kind="ExternalOutput")
            self.PT = [self.scratch(f"PT{i}", [128, T], BF16) for i in range(NOC)]
            self.PTall = self.scratch("PTg", [32, 128, T], BF16)
            self.VT = self.scratch("VT", [9, T, 128], BF16)
            self.QK = {oc: self.scratch(f"QK{oc}", [128, T], BF16) for oc in QKCH}
            self.BR = self.scratch("BR", [16, 128, T], BF16)
            self.OD = [self.scratch(f"OD{d}", [8, 64, T], F32) for d in range(2)]
            self.CB = self.scratch("CB", [NEXP, 128, T], BF16)
            self.psb = [fw.ps([128, 512], F32, f"bank{i}") for i in range(7)]
            self.psT = fw.ps([128, 1024], BF16, "bankT")
            self.ones = fw.sb([128, 128], BF16, "ones")
            self.bd64 = fw.sb([128, 128], BF16, "bd64")
            self.identb = fw.sb([128, 128], BF16, "identb")
            self.identf = fw.sb([128, 128], F32, "identf")
            self.rot = fw.sb([128, 128], BF16, "rot")
            for nm, b in (("ones", self.ones), ("bd64", self.bd64), ("ident", self.identb), ("rot", self.rot)):
                fw.dma("pool", b[:, :], self.CI[nm][:, :], reads=[self.CI[nm]], writes=[b])
            fw.dma("sp", self.identf[:, :], self.CI["ident"][:, :], reads=[self.CI["ident"]], writes=[self.identf])
            self.eps = fw.sb([128, 1], F32, "eps")
            fw.op("dve", lambda e: e.memset(self.eps[:, :], EPS), writes=[self.eps])
            self.small_params()
            self.modt = fw.sb([128, 48, 2], F32, "mod")
            self.gs = fw.sb([128, 2, 8, 2], F32, "gs")
            for l in (getattr(self, "layers", None) or range(self.n_layers)):
                self.layer(l)
                if self.stop_after is not None and self.stop_after[0] == l and self.done:
                    break
            fw.barrier(engines=("sp",))
        return nc

    def small_params(self):
        fw, I = self.fw, self.I
        def ld(name, shape):
            b = fw.sb(shape, F32, name)
            src = I[name]
            fw.dma("sp", b.t[tuple(slice(None) for _ in shape)], src.t[tuple(slice(None) for _ in shape)], reads=[src], writes=[b])
            return b
        self.cvec = ld("cvec", [128, 8, 2])
        self.ada_bT = ld("ada_bT", [128, DEPTH, 48])
        self.g1T = ld("g1T", [128, DEPTH, 8])
        self.g2T = ld("g2T", [128, DEPTH, 8])
        self.qkgT = ld("qkgT", [128, DEPTH, 4])
        self.lamB = ld("lamB", [128, DEPTH, 256])
        self.sublnT = ld("sublnT", [128, DEPTH])
        self.convwT = ld("convwT", [128, DEPTH, 4, 31])
        self.convbT = ld("convbT", [128, DEPTH, 4])
        self.clngT = ld("clngT", [128, DEPTH, 4])
        self.clnbT = ld("clnbT", [128, DEPTH, 4])
        self.lblT = ld("lblT", [128, DEPTH, 2, 4])
        self.hgT = ld("hgT", [64, DEPTH])
        self.routerT = ld("routerT", [128, 2, 8, 8])
        self.siluc = fw.sb([128, 8, 2], BF16, "siluc")
        fw.op("act", lambda e: e.activation(out=self.siluc[:, :, :], in_=self.cvec[:, :, :], func=AF.Silu),
              reads=[self.cvec], writes=[self.siluc])
        ex = fw.sb([128, DEPTH, 8], F32, "lbex")
        fw.op("act", lambda e: e.activation(out=ex[:, :, :], in_=self.lblT.t.rearrange("p l d c -> p l (d c)"), func=AF.Exp),
              reads=[self.lblT], writes=[ex])
        ssum = fw.sb([128, 8], F32, "lbsum")
        fw.op("dve", lambda e: e.tensor_tensor(out=ssum[:, :], in0=ex[:, 0, :], in1=ex[:, 1, :], op=ALU.add), reads=[ex], writes=[ssum])
        for l in range(2, DEPTH):
            fw.op("dve", lambda e, l=l: e.tensor_tensor(out=ssum[:, :], in0=ssum[:, :], in1=ex[:, l, :], op=ALU.add), reads=[ex, ssum], writes=[ssum])
        fw.op("dve", lambda e: e.reciprocal(out=ssum[:, :], in_=ssum[:, :]), reads=[ssum], writes=[ssum])
        self.lbT = fw.sb([128, DEPTH, 8], F32, "lbT")
        self.omlT = fw.sb([128, DEPTH, 8], F32, "omlT")
        fw.op("dve", lambda e: e.memset(self.lbT[:, 0, :], 0.0), writes=[self.lbT])
        for l in range(1, DEPTH):
            fw.op("dve", lambda e, l=l: e.tensor_tensor(out=ex[:, l, :], in0=ex[:, l, :], in1=ssum[:, :], op=ALU.mult), reads=[ex, ssum], writes=[ex])
            fw.op("dve", lambda e, l=l: e.tensor_tensor(out=self.lbT[:, l, :], in0=self.lbT[:, l - 1, :], in1=ex[:, l, :], op=ALU.add),
                  reads=[ex, self.lbT], writes=[self.lbT])
        fw.op("dve", lambda e: e.tensor_scalar(out=self.omlT[:, :, :], in0=self.lbT[:, :, :], scalar1=-1.0, scalar2=1.0, op0=ALU.mult, op1=ALU.add),
              reads=[self.lbT], writes=[self.omlT])
        self.neglam = fw.sb([128, DEPTH], F32, "neglam")
        self.subg = fw.sb([128, DEPTH], F32, "subg")
        pr = fw.sb([128, DEPTH, 2, 64], F32, "lampr")
        s12 = fw.sb([128, DEPTH, 2], F32, "lams")
        lam4 = self.lamB.t.rearrange("p l (i d) -> p l i d", i=4)
        for l in range(DEPTH):
            for j in range(2):
                fw.op("dve", lambda e, l=l, j=j: e.tensor_tensor(out=pr[:, l, j, :], in0=lam4[:, l, 2 * j, :], in1=lam4[:, l, 2 * j + 1, :], op=ALU.mult),
                      reads=[self.lamB], writes=[pr])
                fw.op("dve", lambda e, l=l, j=j: e.reduce_sum(out=s12[:, l, j:j + 1], in_=pr[:, l, j, :], axis=AX.X), reads=[pr], writes=[s12])
        fw.op("act", lambda e: e.activation(out=s12[:, :, :], in_=s12[:, :, :], func=AF.Exp), reads=[s12], writes=[s12])
        for l in range(DEPTH):
            li = 0.8 - 0.6 * math.exp(-0.3 * l)
            fw.op("dve", lambda e, l=l, li=li: e.scalar_tensor_tensor(out=self.neglam[:, l:l + 1], in0=s12[:, l, 1:2], scalar=-li, in1=s12[:, l, 0:1],
                                                                     op0=ALU.add, op1=ALU.subtract), reads=[s12], writes=[self.neglam])
            fw.op("dve", lambda e, l=l, li=li: e.tensor_scalar(out=self.subg[:, l:l + 1], in0=self.sublnT[:, l:l + 1], scalar1=1.0 - li, scalar2=None, op0=ALU.mult),
                  reads=[self.sublnT], writes=[self.subg])

    def bank(self):
        self.rr = (self.rr + 1) % 7
        return self.psb[self.rr]

    def layer(self, l):
        self.done = False
        need_ctx = l < DEPTH - 1
        self.l = l
        self.need_ctx = need_ctx
        steps = [self.ph_mod, self.ph_inproj, self.ph_qk, self.ph_gqa, self.ph_diff, self.ph_conv, self.ph_hgrn,
                 self.ph_merge, self.ph_ffn]
        for i, s in enumerate(steps):
            if getattr(self, "only", None) is not None and i not in self.only:
                continue
            s()
            if self.stop_after is not None and self.stop_after == (l, i):
                self.done = True
                return

    def xsrc(self):
        return self.I["xT0"] if (self.l == 0 and not self.x_written) else self.xs

    def ph_mod(self):
        fw, l = self.fw, self.l
        self.x_written = (l > 0)
        with fw.scope():
            ps = self.psb[0]
            wts = [fw.sb([128, 8, 512], BF16, f"adaw{i}") for i in range(2)]
            aw = self.I["ada_w"].t[l].rearrange("(k p) c -> p k c", p=128)
            for g in range(12):
                w = wts[g % 2]
                fw.dma("pool", w[:, :, :], aw[:, :, g * 512:(g + 1) * 512], reads=[self.I["ada_w"]], writes=[w], key=f"adaw{g % 2}")
                for cc in range(4):
                    j = g * 4 + cc
                    for k in range(8):
                        fw.op("pe", lambda e, w=w, cc=cc, k=k, j=j: e.matmul(ps[:, 2 * j:2 * j + 2], w[:, k, cc * 128:(cc + 1) * 128], self.siluc[:, k, :],
                                                                       start=(k == 0), stop=(k == 7)),
                              reads=[w, self.siluc], writes=[ps], sig=(k == 7))
            mod = self.modt
            for i in range(2):
                fw.op("dve", lambda e, i=i: e.tensor_tensor(out=mod[:, :, i], in0=ps.t[:, 0:96].rearrange("p (j i) -> p j i", i=2)[:, :, i],
                                                            in1=self.ada_bT[:, l, :], op=ALU.add), reads=[ps, self.ada_bT], writes=[mod])
            for s, (gT, c0) in enumerate(((self.g1T, 8), (self.g2T, 32))):
                for i in range(2):
                    fw.op("dve", lambda e, s=s, gT=gT, c0=c0, i=i: e.scalar_tensor_tensor(out=self.gs[:, s, :, i], in0=mod[:, c0:c0 + 8, i], scalar=1.0, in1=gT[:, l, :],
                                                                                     op0=ALU.add, op1=ALU.mult), reads=[mod, gT], writes=[self.gs])

    def norm_block(self, s, t0, n, ic, hT, hcol, xb, sq, rstd, tmp, h32=None):
        fw = self.fw
        src = self.xsrc()
        shc = 0 if s == 0 else 24
        fw.dma("sp", xb[:, :, 0:n], src.t.rearrange("(k p) t -> p k t", p=128)[:, :, t0:t0 + n], reads=[src], writes=[xb])
        fw.op("act", lambda e: e.activation(out=sq[:, :, 0:n], in_=xb[:, :, 0:n], func=AF.Square), reads=[xb], writes=[sq])
        ps = self.bank()
        for k in range(8):
            fw.op("pe", lambda e, k=k: e.matmul(ps[:, 0:n], self.ones[:, :], sq[:, k, 0:n], start=(k == 0), stop=(k == 7)),
                  reads=[self.ones, sq], writes=[ps], sig=(k == 7))
        fw.op("act", lambda e: e.activation(out=rstd[:, 0:n], in_=ps[:, 0:n], func=AF.Sqrt, bias=self.eps[:, 0:1], scale=1.0 / D), reads=[ps, self.eps], writes=[rstd])
        fw.op("dve", lambda e: e.reciprocal(out=rstd[:, 0:n], in_=rstd[:, 0:n]), reads=[rstd], writes=[rstd])
        for k in range(8):
            fw.op("dve", lambda e, k=k: e.scalar_tensor_tensor(out=tmp[:, k, 0:n], in0=xb[:, k, 0:n], scalar=self.gs[:, s, k, ic:ic + 1], in1=rstd[:, 0:n],
                                                              op0=ALU.mult, op1=ALU.mult), reads=[xb, self.gs, rstd], writes=[tmp])
            fw.op("act", lambda e, k=k: e.activation(out=hT[:, k, hcol:hcol + n], in_=tmp[:, k, 0:n], func=AF.Identity, bias=self.modt[:, shc + k, ic:ic + 1], scale=1.0),
                  reads=[tmp, self.modt], writes=[hT])
            if h32 is not None:
                fw.op("pool", lambda e, k=k: e.tensor_scalar(out=h32[:, k, 0:n], in0=tmp[:, k, 0:n], scalar1=self.modt[:, shc + k, ic:ic + 1], scalar2=None, op0=ALU.add),
                      reads=[tmp, self.modt], writes=[h32])

    def ph_inproj(self):
        fw, l = self.fw, self.l
        with fw.scope():
            hT = fw.sb([128, 8, T], BF16, "hT")
            with fw.scope():
                xbs = [fw.sb([128, 8, 512], F32, f"xb{i}") for i in range(2)]
                sq = fw.sb([128, 8, 512], BF16, "sq")
                rstd = fw.sb([128, 512], F32, "rstd")
                tmp = fw.sb([128, 8, 512], F32, "tmp")
                for bi, (t0, n, ic) in enumerate(BLK):
                    self.norm_block(0, t0, n, ic, hT, t0, xbs[bi % 2], sq, rstd, tmp)
            wts = [fw.sb([128, 8, 512], BF16, f"win{i}") for i in range(2)]
            stg = [fw.sb([128, T], BF16, f"stg{i}") for i in range(2)]
            vst = [fw.sb([128, NT, 128], BF16, f"vst{i}") for i in range(2)]
            wv = self.I["w_in"].t[l].rearrange("(k p) c -> p k c", p=128)
            ns = 0
            for g in range(20):
                w = wts[g % 2]
                gc = min(512, D_IN - g * 512)
                fw.dma("pool", w[:, :, 0:gc], wv[:, :, g * 512:g * 512 + gc], reads=[self.I["w_in"]], writes=[w], key=f"win{g % 2}")
                for cc in range(gc // 128):
                    oc = g * 4 + cc
                    if oc in VCH:
                        v = vst[VCH[oc] % 2]
                        for tg in range(0, NT, 4):
                            ps = self.bank()
                            nt_ = min(4, NT - tg)
                            for ti in range(nt_):
                                tt = tg + ti
                                for k in range(8):
                                    fw.op("pe", lambda e, k=k, tt=tt, ti=ti, ps=ps, w=w, cc=cc: e.matmul(
                                        ps[:, ti * 128:(ti + 1) * 128], hT[:, k, tt * 128:(tt + 1) * 128], w[:, k, cc * 128:(cc + 1) * 128],
                                        start=(k == 0), stop=(k == 7)), reads=[hT, w], writes=[ps], sig=(k == 7))
                            fw.op("dve", lambda e, ps=ps, v=v, tg=tg, nt_=nt_: e.tensor_copy(out=v[:, tg:tg + nt_, :], in_=ps.t[:, 0:nt_ * 128].rearrange("p (a c) -> p a c", c=128)),
                                  reads=[ps], writes=[v])
                        fw.dma("sp", self.VT.t[VCH[oc]].rearrange("(n p) c -> p n c", p=128), v[:, :, :], reads=[v], writes=[self.VT], key="vt_st")
                        continue
                    so = stg[ns % 2]
                    ns += 1
                    for bi, (t0, n, ic) in enumerate(BLK):
                        ps = self.bank()
                        for k in range(8):
                            fw.op("pe", lambda e, k=k, ps=ps, w=w, cc=cc, t0=t0, n=n: e.matmul(ps[:, 0:n], w[:, k, cc * 128:(cc + 1) * 128], hT[:, k, t0:t0 + n],
                                                                                   start=(k == 0), stop=(k == 7)), reads=[hT, w], writes=[ps], sig=(k == 7))
                        if oc >= 46:
                            fw.op("act", lambda e, ps=ps, so=so, t0=t0, n=n: e.activation(out=so[:, t0:t0 + n], in_=ps[:, 0:n], func=AF.Sigmoid), reads=[ps], writes=[so])
                        elif bi % 2 == 0:
                            fw.op("dve", lambda e, ps=ps, so=so, t0=t0, n=n: e.tensor_copy(out=so[:, t0:t0 + n], in_=ps[:, 0:n]), reads=[ps], writes=[so])
                        else:
                            fw.op("act", lambda e, ps=ps, so=so, t0=t0, n=n: e.activation(out=so[:, t0:t0 + n], in_=ps[:, 0:n], func=AF.Copy), reads=[ps], writes=[so])
                    if oc >= 46:
                        fw.dma("sp", self.PTall.t[oc - 46], so[:, :], reads=[so], writes=[self.PTall], key="ptg_st")
                    else:
                        fw.dma("sp", self.PT[oc][:, :], so[:, :], reads=[so], writes=[self.PT[oc]], key=f"pt_st{ns % 2}")

    def ph_qk(self):
        fw, l = self.fw, self.l
        with fw.scope():
            cos = fw.sb([128, T], F32, "cos")
            sin = fw.sb([128, T], F32, "sin")
            fw.dma("sp", cos[:, :], self.CI["cos"][:, :], reads=[self.CI["cos"]], writes=[cos])
            fw.dma("sp", sin[:, :], self.CI["sin"][:, :], reads=[self.CI["sin"]], writes=[sin])
            qin = [fw.sb([128, T], BF16, f"qin{i}") for i in range(2)]
            qout = [fw.sb([128, T], BF16, f"qout{i}") for i in range(2)]
            sq = [fw.sb([128, 512], BF16, f"qsq{i}") for i in range(2)]
            rstd = [fw.sb([128, 512], F32, f"qrstd{i}") for i in range(2)]
            qn = [fw.sb([128, 512], BF16, f"qn{i}") for i in range(2)]
            t1 = [fw.sb([128, 512], F32, f"qt1{i}") for i in range(2)]
            t2 = [fw.sb([128, 512], F32, f"qt2{i}") for i in range(2)]
            it = 0
            for ci, oc in enumerate(QKCH):
                gi = 0 if oc < 4 else 1 if oc == 4 else 2 if oc < 10 else 3
                qi, qo = qin[ci % 2], qout[ci % 2]
                fw.dma("sp", qi[:, :], self.PT[oc][:, :], reads=[self.PT[oc]], writes=[qi], key=f"qk_ld{ci % 2}")
                for (t0, n, ic) in BLK:
                    i2 = it % 2
                    it += 1
                    fw.op("act", lambda e, qi=qi, i2=i2, t0=t0, n=n: e.activation(out=sq[i2][:, 0:n], in_=qi[:, t0:t0 + n], func=AF.Square), reads=[qi], writes=[sq[i2]])
                    ps = self.bank()
                    fw.op("pe", lambda e, ps=ps, i2=i2, n=n: e.matmul(ps[:, 0:n], self.bd64[:, :], sq[i2][:, 0:n], start=True, stop=True), reads=[self.bd64, sq[i2]], writes=[ps])
                    fw.op("act", lambda e, ps=ps, i2=i2, n=n: e.activation(out=rstd[i2][:, 0:n], in_=ps[:, 0:n], func=AF.Sqrt, bias=self.eps[:, 0:1], scale=1.0 / 64),
                          reads=[ps, self.eps], writes=[rstd[i2]])
                    fw.op("dve", lambda e, i2=i2, n=n: e.reciprocal(out=rstd[i2][:, 0:n], in_=rstd[i2][:, 0:n]), reads=[rstd[i2]], writes=[rstd[i2]])
                    fw.op("dve", lambda e, qi=qi, i2=i2, t0=t0, n=n, gi=gi: e.scalar_tensor_tensor(out=qn[i2][:, 0:n], in0=qi[:, t0:t0 + n], scalar=self.qkgT[:, l, gi:gi + 1],
                                                                                          in1=rstd[i2][:, 0:n], op0=ALU.mult, op1=ALU.mult),
                          reads=[qi, self.qkgT, rstd[i2]], writes=[qn[i2]])
                    ps2 = self.bank()
                    fw.op("pe", lambda e, ps2=ps2, i2=i2, n=n: e.matmul(ps2[:, 0:n], self.rot[:, :], qn[i2][:, 0:n], start=True, stop=True), reads=[self.rot, qn[i2]], writes=[ps2])
                    fw.op("pool", lambda e, i2=i2, t0=t0, n=n: e.tensor_tensor(out=t1[i2][:, 0:n], in0=qn[i2][:, 0:n], in1=cos[:, t0:t0 + n], op=ALU.mult),
                          reads=[qn[i2], cos], writes=[t1[i2]])
                    fw.op("dve", lambda e, ps2=ps2, i2=i2, t0=t0, n=n: e.tensor_tensor(out=t2[i2][:, 0:n], in0=ps2[:, 0:n], in1=sin[:, t0:t0 + n], op=ALU.mult),
                          reads=[ps2, sin], writes=[t2[i2]])
                    fw.op("dve", lambda e, qo=qo, i2=i2, t0=t0, n=n: e.tensor_tensor(out=qo[:, t0:t0 + n], in0=t1[i2][:, 0:n], in1=t2[i2][:, 0:n], op=ALU.add),
                          reads=[t1[i2], t2[i2]], writes=[qo])
                fw.dma("sp", self.QK[oc][:, :], qo[:, :], reads=[qo], writes=[self.QK[oc]], key=f"qk_st{ci % 2}")

    def attend(self, qh, kh, vaug, lsep, accs, qblocks, pbufs, finish, sbanks=None, LA=2):
        fw = self.fw
        sbanks = sbanks or self.psb[0:3]
        assert len(sbanks) >= LA + 1 and len(pbufs) >= LA + 2
        for (t0, n, ic) in qblocks:
            kts = list(range(2)) if ic else list(range(NT))
            pend = []
            for idx, kt in enumerate(kts):
                self.rs = (getattr(self, "rs", 0) + 1) % len(sbanks)
                ps = sbanks[self.rs]
                fw.op("pe", lambda e: e.matmul(ps[:, 0:n], kh[0:64, kt * 128:(kt + 1) * 128], qh[0:64, t0:t0 + n], start=True, stop=True),
                      reads=[kh, qh], writes=[ps])
                self.rp = (getattr(self, "rp", 0) + 1) % len(pbufs)
                pb = pbufs[self.rp]
                fw.op("act", lambda e: e.activation(out=pb[:, 0:n], in_=ps[:, 0:n], func=AF.Exp, scale=0.125), reads=[ps], writes=[pb])
                pend.append((idx, kt, pb))
                if len(pend) > LA:
                    self._pv(pend.pop(0), accs, vaug, lsep, n, kts)
            while pend:
                self._pv(pend.pop(0), accs, vaug, lsep, n, kts)
            finish(t0, n, ic)

    def _pv(self, pend, accs, vaug, lsep, n, kts):
        fw = self.fw
        idx, kt, pb = pend
        first, last = idx == 0, idx == len(kts) - 1
        fw.op("pe", lambda e: e.matmul(accs[0][:, 0:n], vaug[:, kt, :], pb[:, 0:n], start=first, stop=last), reads=[vaug, pb], writes=[accs[0]], sig=(last and not lsep))
        if lsep:
            fw.op("pe", lambda e: e.matmul(accs[1][:, 0:n], self.ones[:, :], pb[:, 0:n], start=first, stop=last), reads=[self.ones, pb], writes=[accs[1]], sig=last)

    def bank_s(self):
        self.rs = (getattr(self, "rs", 0) + 1) % 3
        return self.psb[self.rs]

    def qblocks(self):
        return BLK if self.need_ctx else BLK[1:]

    def ph_gqa(self):
        fw, l = self.fw, self.l
        with fw.scope():
            qh = [fw.sb([64, T], BF16, f"gq{i}") for i in range(2)]
            kh = [fw.sb([64, T], BF16, f"gk{i}") for i in range(2)]
            vaug = [fw.sb([128, NT, 128], BF16, f"gv{i}") for i in range(2)]
            pbufs = [fw.sb([128, 512], BF16, f"gp{i}") for i in range(5)]
            rl = fw.sb([64, 512], F32, "grl")
            ob = [fw.sb([64, 512], BF16, f"gob{i}") for i in range(2)]
            for kv in range(2):
                fw.op("pool", lambda e, kv=kv: e.memset(vaug[kv][:, :, 64:128], 1.0), writes=[vaug[kv]])
                fw.dma("sp", vaug[kv][:, :, 0:64], self.VT.t[0].rearrange("(n p) c -> p n c", p=128)[:, :, kv * 64:(kv + 1) * 64],
                       reads=[self.VT], writes=[vaug[kv]], key=f"g_v{kv}")
                fw.dma("sp", kh[kv][:, :], self.QK[4].t[kv * 64:(kv + 1) * 64, :], reads=[self.QK[4]], writes=[kh[kv]], key=f"g_k{kv}")
            acc = self.psb[3]
            cnt = [0]
            for h in range(8):
                q = qh[h % 2]
                fw.dma("sp", q[:, :], self.QK[h // 2].t[(h % 2) * 64:(h % 2) * 64 + 64, :], reads=[self.QK[h // 2]], writes=[q], key=f"g_q{h % 2}")

                def fin(t0, n, ic, h=h):
                    o = ob[cnt[0] % 2]
                    cnt[0] += 1
                    fw.op("dve", lambda e: e.reciprocal(out=rl[0:64, 0:n], in_=acc[64:128, 0:n]), reads=[acc], writes=[rl])
                    fw.op("dve", lambda e: e.tensor_tensor(out=o[0:64, 0:n], in0=acc[0:64, 0:n], in1=rl[0:64, 0:n], op=ALU.mult), reads=[acc, rl], writes=[o])
                    fw.dma("sp", self.BR.t[h // 2, (h % 2) * 64:(h % 2) * 64 + 64, t0:t0 + n], o[0:64, 0:n], reads=[o], writes=[self.BR], key=f"g_o{cnt[0] % 2}")
                self.attend(q, kh[h // 4], vaug[h // 4], False, [acc], self.qblocks(), pbufs, fin,
                            sbanks=[self.psb[0], self.psb[1], self.psb[2], self.psb[4]], LA=3)

    def ph_diff(self):
        fw, l = self.fw, self.l
        with fw.scope():
            qh = [fw.sb([64, T], BF16, f"dq{i}") for i in range(2)]
            kh = [fw.sb([64, T], BF16, f"dk{i}") for i in range(2)]
            vv = [fw.sb([128, NT, 128], BF16, f"dv{i}") for i in range(2)]
            pbufs = [fw.sb([128, 512], BF16, f"dp{i}") for i in range(4)]
            r0 = fw.sb([128, 512], F32, "dr0")
            a0 = fw.sb([128, 512], F32, "da0")
            a1 = fw.sb([128, 512], F32, "da1")
            sq = fw.sb([128, 512], BF16, "dsq")
            rstd = fw.sb([128, 512], F32, "drstd")
            ob = [fw.sb([128, 512], BF16, f"dob{i}") for i in range(2)]
            accs = [[self.psb[3], self.psb[4]], [self.psb[5], self.psb[6]]]
            cnt = [0]
            for h in range(4):
                v = vv[h % 2]
                fw.dma("sp", v[:, :, :], self.VT.t[1 + h].rearrange("(n p) c -> p n c", p=128), reads=[self.VT], writes=[v], key=f"d_v{h % 2}")
                for (t0, n, ic) in self.qblocks():
                    for i in range(2):
                        m = h * 2 + i
                        q, k = qh[i], kh[i]
                        if t0 == self.qblocks()[0][0]:
                            fw.dma("sp", q[:, :], self.QK[6 + m // 2].t[(m % 2) * 64:(m % 2) * 64 + 64, :], reads=[self.QK[6 + m // 2]], writes=[q], key=f"d_q{i}")
                            fw.dma("sp", k[:, :], self.QK[10 + m // 2].t[(m % 2) * 64:(m % 2) * 64 + 64, :], reads=[self.QK[10 + m // 2]], writes=[k], key=f"d_k{i}")
                        self.attend(q, k, v, True, accs[i], [(t0, n, ic)], pbufs, lambda *a: None)
                    o = ob[cnt[0] % 2]
                    cnt[0] += 1
                    A0, L0 = accs[0]
                    A1, L1 = accs[1]
                    fw.op("dve", lambda e: e.reciprocal(out=r0[:, 0:n], in_=L0[:, 0:n]), reads=[L0], writes=[r0])
                    fw.op("dve", lambda e: e.tensor_tensor(out=a0[:, 0:n], in0=A0[:, 0:n], in1=r0[:, 0:n], op=ALU.mult), reads=[A0, r0], writes=[a0])
                    fw.op("dve", lambda e: e.reciprocal(out=r0[:, 0:n], in_=L1[:, 0:n]), reads=[L1], writes=[r0])
                    fw.op("dve", lambda e: e.tensor_tensor(out=a1[:, 0:n], in0=A1[:, 0:n], in1=r0[:, 0:n], op=ALU.mult), reads=[A1, r0], writes=[a1])
                    fw.op("dve", lambda e: e.scalar_tensor_tensor(out=a0[:, 0:n], in0=a1[:, 0:n], scalar=self.neglam[:, l:l + 1], in1=a0[:, 0:n], op0=ALU.mult, op1=ALU.add),
                          reads=[a1, a0, self.neglam], writes=[a0])
                    fw.op("act", lambda e: e.activation(out=sq[:, 0:n], in_=a0[:, 0:n], func=AF.Square), reads=[a0], writes=[sq])
                    ps = self.psb[0]
                    fw.op("pe", lambda e: e.matmul(ps[:, 0:n], self.ones[:, :], sq[:, 0:n], start=True, stop=True), reads=[self.ones, sq], writes=[ps])
                    fw.op("act", lambda e: e.activation(out=rstd[:, 0:n], in_=ps[:, 0:n], func=AF.Sqrt, bias=self.eps[:, 0:1], scale=1.0 / 128), reads=[ps, self.eps], writes=[rstd])
                    fw.op("dve", lambda e: e.reciprocal(out=rstd[:, 0:n], in_=rstd[:, 0:n]), reads=[rstd], writes=[rstd])
                    fw.op("dve", lambda e: e.scalar_tensor_tensor(out=o[:, 0:n], in0=a0[:, 0:n], scalar=self.subg[:, l:l + 1], in1=rstd[:, 0:n], op0=ALU.mult, op1=ALU.mult),
                          reads=[a0, self.subg, rstd], writes=[o])
                    fw.dma("sp", self.BR.t[4 + h, :, t0:t0 + n], o[:, 0:n], reads=[o], writes=[self.BR], key=f"d_o{cnt[0] % 2}")

    def ph_conv(self):
        fw, l = self.fw, self.l
        with fw.scope():
            upad = fw.sb([128, 4, ULEN], BF16, "upad")
            upo = fw.sb([128, 4, ULEN], BF16, "upo")
            for (a_, b_) in ((0, UOFF[0]), (UOFF[0] + 256, UOFF[1]), (UOFF[1] + 4096, ULEN)):
                fw.op("dve", lambda e: e.memset(upad[:, :, a_:b_], 0.0), writes=[upad])
                fw.op("dve", lambda e: e.memset(upo[:, :, max(a_ - 1, 0):b_ - 1 if b_ < ULEN else ULEN], 0.0), writes=[upo])
            diag = fw.sb([128, 4, 31, 128], BF16, "diag")
            ab = [fw.sb([128, T], BF16, f"ca{i}") for i in range(2)]
            gb = [fw.sb([128, T], BF16, f"cg{i}") for i in range(2)]
            for cc in range(4):
                a, g = ab[cc % 2], gb[cc % 2]
                fw.dma("sp", a[:, :], self.PT[18 + cc][:, :], reads=[self.PT[18 + cc]], writes=[a], key=f"c_a{cc % 2}")
                fw.dma("sp", g[:, :], self.PT[22 + cc][:, :], reads=[self.PT[22 + cc]], writes=[g], key=f"c_g{cc % 2}")
                fw.op("act", lambda e, g=g: e.activation(out=g[:, :], in_=g[:, :], func=AF.Sigmoid), reads=[g], writes=[g])
                fw.op("dve", lambda e, a=a, g=g, cc=cc: e.tensor_tensor(out=upad[:, cc, UOFF[0]:UOFF[0] + 256], in0=a[:, 0:256], in1=g[:, 0:256], op=ALU.mult), reads=[a, g], writes=[upad])
                fw.op("dve", lambda e, a=a, g=g, cc=cc: e.tensor_tensor(out=upad[:, cc, UOFF[1]:UOFF[1] + 4096], in0=a[:, 256:T], in1=g[:, 256:T], op=ALU.mult), reads=[a, g], writes=[upad])
                fw.op("pool", lambda e, a=a, g=g, cc=cc: e.tensor_tensor(out=upo[:, cc, UOFF[0] - 1:UOFF[0] - 1 + 256], in0=a[:, 0:256], in1=g[:, 0:256], op=ALU.mult), reads=[a, g], writes=[upo])
                fw.op("pool", lambda e, a=a, g=g, cc=cc: e.tensor_tensor(out=upo[:, cc, UOFF[1] - 1:UOFF[1] - 1 + 4096], in0=a[:, 256:T], in1=g[:, 256:T], op=ALU.mult), reads=[a, g], writes=[upo])
                for j in range(31):
                    fw.op("dve", lambda e, cc=cc, j=j: e.tensor_scalar(out=diag[:, cc, j, :], in0=self.identf[:, :], scalar1=self.convwT[:, l, cc, j:j + 1], scalar2=None, op0=ALU.mult),
                          reads=[self.identf, self.convwT], writes=[diag])
            import os as _os
            cut = int(_os.environ.get("CONV_CUT", "99"))
            if cut <= 0:
                return
            y32 = fw.sb([128, 4, 512], F32, "cy32")
            ybf = fw.sb([128, 4, 512], BF16, "cybf")
            mean = fw.sb([128, 512], F32, "cmean")
            sq = fw.sb([128, 4, 512], BF16, "csq")
            rstd = fw.sb([128, 512], F32, "crstd")
            ob = [fw.sb([128, 4, 512], BF16, f"cob{i}") for i in range(2)]
            for bi, (t0, n, ic) in enumerate(BLK):
                u0 = (UOFF[0] + t0 - UPAD) if ic else (UOFF[1] + (t0 - 256) - UPAD)
                for cc in range(4):
                    ps = self.bank()
                    for j in range(31):
                        usrc, uo = (upad, u0 + j) if (u0 + j) % 2 == 0 else (upo, u0 + j - 1)
                        fw.op("pe", lambda e, ps=ps, cc=cc, j=j: e.matmul(ps[:, 0:n], diag[:, cc, j, :], usrc[:, cc, uo:uo + n], start=(j == 0), stop=(j == 30)),
                              reads=[diag, usrc], writes=[ps], sig=(j == 30))
                    fw.op("act", lambda e, ps=ps, cc=cc: e.activation(out=y32[:, cc, 0:n], in_=ps[:, 0:n], func=AF.Identity, bias=self.convbT[:, l, cc:cc + 1], scale=1.0),
                          reads=[ps, self.convbT], writes=[y32])
                    fw.op("dve", lambda e, ps=ps, cc=cc: e.tensor_scalar(out=ybf[:, cc, 0:n], in0=ps[:, 0:n], scalar1=self.convbT[:, l, cc:cc + 1], scalar2=None, op0=ALU.add),
                          reads=[ps, self.convbT], writes=[ybf])
                if cut <= 1:
                    continue
                pm = self.bank()
                for cc in range(4):
                    fw.op("pe", lambda e, cc=cc: e.matmul(pm[:, 0:n], self.ones[:, :], ybf[:, cc, 0:n], start=(cc == 0), stop=(cc == 3)), reads=[self.ones, ybf], writes=[pm], sig=(cc == 3))
                fw.op("act", lambda e: e.activation(out=mean[:, 0:n], in_=pm[:, 0:n], func=AF.Copy, scale=1.0 / 512), reads=[pm], writes=[mean])
                for cc in range(4):
                    fw.op("dve", lambda e, cc=cc: e.tensor_tensor(out=y32[:, cc, 0:n], in0=y32[:, cc, 0:n], in1=mean[:, 0:n], op=ALU.subtract), reads=[y32, mean], writes=[y32])
                fw.op("act", lambda e: e.activation(out=sq[:, :, 0:n], in_=y32[:, :, 0:n], func=AF.Square), reads=[y32], writes=[sq])
                pv = self.bank()
                for cc in range(4):
                    fw.op("pe", lambda e, cc=cc: e.matmul(pv[:, 0:n], self.ones[:, :], sq[:, cc, 0:n], start=(cc == 0), stop=(cc == 3)), reads=[self.ones, sq], writes=[pv], sig=(cc == 3))
                fw.op("act", lambda e: e.activation(out=rstd[:, 0:n], in_=pv[:, 0:n], func=AF.Sqrt, bias=self.eps[:, 0:1], scale=1.0 / 512), reads=[pv, self.eps], writes=[rstd])
                fw.op("dve", lambda e: e.reciprocal(out=rstd[:, 0:n], in_=rstd[:, 0:n]), reads=[rstd], writes=[rstd])
                if cut <= 2:
                    continue
                o = ob[bi % 2]
                for cc in range(4):
                    fw.op("dve", lambda e, cc=cc: e.scalar_tensor_tensor(out=y32[:, cc, 0:n], in0=y32[:, cc, 0:n], scalar=self.clngT[:, l, cc:cc + 1], in1=rstd[:, 0:n], op0=ALU.mult, op1=ALU.mult),
                          reads=[y32, self.clngT, rstd], writes=[y32])
                    fw.op("act", lambda e, cc=cc: e.activation(out=o[:, cc, 0:n], in_=y32[:, cc, 0:n], func=AF.Silu, bias=self.clnbT[:, l, cc:cc + 1], scale=1.0),
                          reads=[y32, self.clnbT], writes=[o])
                fw.dma("sp", self.BR.t[8:12, :, t0:t0 + n].rearrange("c p t -> p c t"), o[:, :, 0:n], reads=[o], writes=[self.BR], key=f"c_o{bi % 2}")

    def ph_hgrn(self):
        fw, l = self.fw, self.l
        NCH = T // 64
        for d in range(2):
            for hp in range(4):
                with fw.scope():
                    vtok = fw.sb([128, NT, 128], BF16, "hv")
                    fw.dma("sp", vtok[:, :, :], self.VT.t[5 + hp].rearrange("(n p) c -> p n c", p=128), reads=[self.VT], writes=[vtok], key="h_v")
                    mask = fw.sb([128, 256], F32, "hmask")
                    mname = "mask_f" if d == 0 else "mask_b"
                    fw.dma("sp", mask[:, :], self.CI[mname][:, 0:256], reads=[self.CI[mname]], writes=[mask], key="h_m")
                    maski = mask.t[:, :].bitcast(mybir.dt.int32)
                    qT = [fw.sb([64, T], BF16, f"hq{i}") for i in range(2)]
                    qpT = [fw.sb([64, T], BF16, f"hqp{i}") for i in range(2)]
                    kT = [fw.sb([64, T], BF16, f"hk{i}") for i in range(2)]
                    dec2 = fw.sb([64, 2, NCH], F32, "hdec2")
                    ktok = fw.sb([128, NT, 128], BF16, "hkt")
                    dec = fw.sb([128, NCH], F32, "hdec")
                    with fw.scope():
                        onesf = fw.sb([128, 64], F32, "h1")
                        fw.op("pool", lambda e: e.memset(onesf[:, :], 1.0), writes=[onesf])
                        zin = fw.sb([128, T], BF16, "hz")
                        qin = fw.sb([128, T], BF16, "hqi")
                        qs = fw.sb([128, T], F32, "hqs")
                        f = fw.sb([128, T], F32, "hf")
                        lf = fw.sb([128, T], F32, "hlf")
                        cum = fw.sb([128, T], F32, "hcum")
                        kk = fw.sb([128, T], F32, "hkk")
                        ex = f
                        khT = fw.sb([128, T], BF16, "hkh")
                        c3 = cum.t.rearrange("p (c s) -> p c s", s=64)
                        e3 = ex.t.rearrange("p (c s) -> p c s", s=64)
                        iend, imid = (63, 31) if d == 0 else (0, 32)
                        fw.dma("sp", qin[:, :], self.PT[26 + hp][:, :], reads=[self.PT[26 + hp]], writes=[qin], key="h_qi")
                        fw.dma("sp", zin[:, :], self.PT[30 + 4 * d + hp][:, :], reads=[self.PT[30 + 4 * d + hp]], writes=[zin], key="h_zi")
                        fw.op("act", lambda e: e.activation(out=qs[:, :], in_=qin[:, :], func=AF.Silu), reads=[qin], writes=[qs])
                        fw.op("act", lambda e: e.activation(out=f[:, :], in_=zin[:, :], func=AF.Sigmoid), reads=[zin], writes=[f])
                        li = d * 4 + hp
                        fw.op("dve", lambda e: e.tensor_scalar(out=f[:, :], in0=f[:, :], scalar1=self.omlT[:, l, li:li + 1], scalar2=self.lbT[:, l, li:li + 1], op0=ALU.mult, op1=ALU.add),
                              reads=[f, self.omlT, self.lbT], writes=[f])
                        fw.op("act", lambda e: e.activation(out=lf[:, :], in_=f[:, :], func=AF.Ln), reads=[f], writes=[lf])
                        fw.op("pool", lambda e: e.tensor_scalar(out=kk[:, :], in0=f[:, :], scalar1=-1.0, scalar2=1.0, op0=ALU.mult, op1=ALU.add), reads=[f], writes=[kk])
                        for c in range(NCH):
                            fw.op("dve", lambda e: e.tensor_tensor_scan(out=cum[:, c * 64:(c + 1) * 64], data0=onesf[:, :], data1=lf[:, c * 64:(c + 1) * 64], initial=0.0,
                                                                       op0=ALU.mult, op1=ALU.add), reads=[onesf, lf], writes=[cum], sig=(c == NCH - 1))
                        if d == 1:
                            fw.op("dve", lambda e: e.tensor_tensor(out=e3, in0=c3[:, :, 63:64].broadcast_to([128, NCH, 64]), in1=c3, op=ALU.subtract), reads=[cum], writes=[ex])
                            fw.op("dve", lambda e: e.tensor_tensor(out=cum[:, :], in0=ex[:, :], in1=lf[:, :], op=ALU.add), reads=[ex, lf], writes=[cum])
                        fw.op("act", lambda e: e.activation(out=dec[:, :], in_=c3[:, :, iend], func=AF.Exp), reads=[cum], writes=[dec])
                        for hh in range(2):
                            fw.op("dve", lambda e: e.tensor_copy(out=dec2[0:64, hh, :], in_=dec[hh * 64:(hh + 1) * 64, :]), reads=[dec], writes=[dec2])
                        fw.op("act", lambda e: e.activation(out=ex[:, :], in_=cum[:, :], func=AF.Exp), reads=[cum], writes=[ex])
                        for hh in range(2):
                            fw.op("dve", lambda e: e.scalar_tensor_tensor(out=qpT[hh][0:64, :], in0=qs[hh * 64:(hh + 1) * 64, :], scalar=0.125, in1=ex[hh * 64:(hh + 1) * 64, :], op0=ALU.mult, op1=ALU.mult),
                                  reads=[qs, ex], writes=[qpT[hh]])
                        fw.op("dve", lambda e: e.tensor_tensor(out=e3, in0=c3[:, :, iend:iend + 1].broadcast_to([128, NCH, 64]), in1=c3, op=ALU.subtract), reads=[cum], writes=[ex])
                        fw.op("act", lambda e: e.activation(out=ex[:, :], in_=ex[:, :], func=AF.Exp), reads=[ex], writes=[ex])
                        fw.op("dve", lambda e: e.tensor_tensor(out=khT[:, :], in0=kk[:, :], in1=ex[:, :], op=ALU.mult), reads=[kk, ex], writes=[khT])
                        fw.op("dve", lambda e: e.tensor_tensor(out=e3, in0=c3, in1=c3[:, :, imid:imid + 1].broadcast_to([128, NCH, 64]), op=ALU.subtract), reads=[cum], writes=[ex])
                        fw.op("act", lambda e: e.activation(out=lf[:, :], in_=ex[:, :], func=AF.Exp), reads=[ex], writes=[lf])
                        for hh in range(2):
                            fw.op("dve", lambda e: e.scalar_tensor_tensor(out=qT[hh][0:64, :], in0=qs[hh * 64:(hh + 1) * 64, :], scalar=0.125, in1=lf[hh * 64:(hh + 1) * 64, :], op0=ALU.mult, op1=ALU.mult),
                                  reads=[qs, lf], writes=[qT[hh]])
                        fw.op("act", lambda e: e.activation(out=lf[:, :], in_=ex[:, :], func=AF.Exp, scale=-1.0), reads=[ex], writes=[lf])
                        for hh in range(2):
                            fw.op("dve", lambda e: e.tensor_tensor(out=kT[hh][0:64, :], in0=kk[hh * 64:(hh + 1) * 64, :], in1=lf[hh * 64:(hh + 1) * 64, :], op=ALU.mult), reads=[kk, lf], writes=[kT[hh]])
                        import os as _os
                        hcut = int(_os.environ.get("HG_CUT", "99"))
                        for tt in range(NT if hcut > 1 else 0):
                            fw.op("pe", lambda e: e.transpose(self.psT[:, (tt % 8) * 128:(tt % 8) * 128 + 128], khT[:, tt * 128:(tt + 1) * 128], self.identb[:, :]),
                                  reads=[khT, self.identb], writes=[self.psT], sig=(tt % 8 == 7 or tt == NT - 1))
                            if tt % 8 == 7 or tt == NT - 1:
                                t8 = tt - (tt % 8)
                                nn = tt - t8 + 1
                                fw.op("act", lambda e: e.activation(out=ktok[:, t8:t8 + nn, :], in_=self.psT.t[:, 0:nn * 128].rearrange("p (a c) -> p a c", c=128), func=AF.Copy),
                                      reads=[self.psT], writes=[ktok])
                    S = fw.sb([64, 2, 64], F32, "hS")
                    Sb = [fw.sb([64, 2, 64], BF16, f"hSb{i}") for i in range(2)]
                    fw.op("dve", lambda e: e.memset(S[:, :, :], 0.0), writes=[S])
                    fw.op("dve", lambda e: e.memset(Sb[0][:, :, :], 0.0), writes=[Sb[0]])
                    attm = [fw.sb([128, 256], BF16, f"hatt{i}") for i in range(2)]
                    for a_ in attm:
                        fw.op("dve", lambda e: e.memset(a_[:, :], 0.0), writes=[a_])
                    ost = fw.sb([64, 2, T], F32, "host")
                    tiles = list(range(NT)) if d == 0 else [1, 0] + list(range(NT - 1, 1, -1))
                    if hcut <= 2:
                        tiles = []
                    sbi = 0
                    for ti, tt in enumerate(tiles):
                        am = attm[ti % 2]
                        pa = self.psb[1 + ti % 2]
                        for hh in range(2):
                            r0 = hh * 64
                            fw.op("pe", lambda e: e.matmul(pa[:, hh * 128:hh * 128 + 128], kT[hh][0:64, tt * 128:(tt + 1) * 128], qT[hh][0:64, tt * 128:(tt + 1) * 128], start=True, stop=True),
                                  reads=[kT[hh], qT[hh]], writes=[pa], sig=(hh == 1))
                        fw.op("dve", lambda e: e.copy_predicated(out=am[:, :], mask=maski, data=pa[:, 0:256]), reads=[pa, mask], writes=[am])
                        if hcut <= 3:
                            continue
                        po = self.psb[3 + ti % 2]
                        chunks = [0, 1] if d == 0 else [1, 0]
                        if hcut <= 4:
                            chunks = []
                        for hh in range(2):
                            fw.op("pe", lambda e: e.matmul(po[0:64, hh * 128:hh * 128 + 128], vtok[:, tt, hh * 64:(hh + 1) * 64], am[:, hh * 128:(hh + 1) * 128],
                                                           start=(hh == 0), stop=False, skip_group_check=True), reads=[vtok, am], writes=[po], sig=False)
                        for ci, cj in enumerate(chunks):
                            c = tt * 2 + cj
                            sb_cur = Sb[sbi % 2]
                            sb_nxt = Sb[(sbi + 1) % 2]
                            sbi += 1
                            for hh in range(2):
                                r0 = hh * 64
                                fw.op("pe", lambda e: e.matmul(po[0:64, hh * 128 + cj * 64:hh * 128 + cj * 64 + 64], sb_cur[0:64, hh, :], qpT[hh][0:64, c * 64:(c + 1) * 64],
                                                               start=False, stop=(ci == 1), skip_group_check=True), reads=[sb_cur, qpT[hh]], writes=[po], sig=(hh == 1))
                            pS = self.psb[5 + sbi % 2]
                            for hh in range(2):
                                fw.op("pe", lambda e: e.matmul(pS[0:64, hh * 64:(hh + 1) * 64], ktok[cj * 64:cj * 64 + 64, tt, hh * 64:(hh + 1) * 64], vtok[cj * 64:cj * 64 + 64, tt, hh * 64:(hh + 1) * 64],
                                                               start=(hh == 0), stop=(hh == 1), skip_group_check=True), reads=[ktok, vtok], writes=[pS], sig=(hh == 1))
                            for hh in range(2):
                                fw.op("dve", lambda e: e.scalar_tensor_tensor(out=S[0:64, hh, :], in0=S[0:64, hh, :], scalar=dec2[0:64, hh, c:c + 1], in1=pS[0:64, hh * 64:(hh + 1) * 64], op0=ALU.mult, op1=ALU.add),
                                      reads=[S, dec2, pS], writes=[S])
                            fw.op("act", lambda e: e.activation(out=sb_nxt[:, :, :], in_=S[:, :, :], func=AF.Copy), reads=[S], writes=[sb_nxt])
                        fw.op("act", lambda e: e.activation(out=ost[:, :, tt * 128:(tt + 1) * 128], in_=po.t[0:64, 0:256].rearrange("p (a c) -> p a c", c=128), func=AF.Copy),
                              reads=[po], writes=[ost])
                    fw.dma("sp", self.OD[d].t[2 * hp:2 * hp + 2].rearrange("h e t -> e h t"), ost[:, :, :], reads=[ost], writes=[self.OD[d]], key="h_o")
        with fw.scope():
            of = [fw.sb([64, T], F32, f"hof{i}") for i in range(2)]
            obw = [fw.sb([64, T], F32, f"hobw{i}") for i in range(2)]
            gt = [fw.sb([64, T], BF16, f"hgt{i}") for i in range(2)]
            sq = fw.sb([64, 512], BF16, "hsq")
            rstd = fw.sb([64, 512], F32, "hrstd")
            sg = fw.sb([64, 512], F32, "hsg")
            ob = [fw.sb([64, T], BF16, f"hob{i}") for i in range(2)]
            for h in range(8):
                a, b, g, o = of[h % 2], obw[h % 2], gt[h % 2], ob[h % 2]
                fw.dma("sp", a[:, :], self.OD[0].t[h], reads=[self.OD[0]], writes=[a], key=f"hn_a{h % 2}")
                fw.dma("sp", b[:, :], self.OD[1].t[h], reads=[self.OD[1]], writes=[b], key=f"hn_b{h % 2}")
                fw.dma("sp", g[:, :], self.PT[42 + h // 2].t[(h % 2) * 64:(h % 2) * 64 + 64, :], reads=[self.PT[42 + h // 2]], writes=[g], key=f"hn_g{h % 2}")
                fw.op("pool", lambda e, a=a, b=b: e.tensor_tensor(out=a[:, :], in0=a[:, :], in1=b[:, :], op=ALU.add), reads=[a, b], writes=[a])
                for (t0, n, ic) in BLK:
                    fw.op("act", lambda e, a=a: e.activation(out=sq[:, 0:n], in_=a[:, t0:t0 + n], func=AF.Square), reads=[a], writes=[sq])
                    ps = self.bank()
                    fw.op("pe", lambda e, ps=ps: e.matmul(ps[0:64, 0:n], self.ones[0:64, 0:64], sq[0:64, 0:n], start=True, stop=True), reads=[self.ones, sq], writes=[ps])
                    fw.op("act", lambda e, ps=ps: e.activation(out=rstd[:, 0:n], in_=ps[0:64, 0:n], func=AF.Sqrt, bias=self.eps[0:64, 0:1], scale=1.0 / 64), reads=[ps, self.eps], writes=[rstd])
                    fw.op("dve", lambda e: e.reciprocal(out=rstd[:, 0:n], in_=rstd[:, 0:n]), reads=[rstd], writes=[rstd])
                    fw.op("act", lambda e, g=g: e.activation(out=sg[:, 0:n], in_=g[:, t0:t0 + n], func=AF.Silu), reads=[g], writes=[sg])
                    fw.op("dve", lambda e: e.scalar_tensor_tensor(out=rstd[:, 0:n], in0=rstd[:, 0:n], scalar=self.hgT[:, l:l + 1], in1=sg[:, 0:n], op0=ALU.mult, op1=ALU.mult),
                          reads=[rstd, self.hgT, sg], writes=[rstd])
                    fw.op("dve", lambda e, a=a, o=o: e.tensor_tensor(out=o[:, t0:t0 + n], in0=a[:, t0:t0 + n], in1=rstd[:, 0:n], op=ALU.mult), reads=[a, rstd], writes=[o])
                fw.dma("sp", self.BR.t[12 + h // 2, (h % 2) * 64:(h % 2) * 64 + 64, :], o[:, :], reads=[o], writes=[self.BR], key=f"hn_o{h % 2}")

    def ph_merge(self):
        fw, l = self.fw, self.l
        with fw.scope():
            wb = fw.sb([128, 16, D], BF16, "wbr")
            wo = fw.sb([128, 8, D], BF16, "wout")
            for i in range(4):
                fw.dma("pool", wb[:, i * 4:(i + 1) * 4, :], self.I["w_branch"].t[l, i].rearrange("(k p) c -> p k c", p=128), reads=[self.I["w_branch"]], writes=[wb], key="m_wb")
            fw.dma("pool", wo[:, :, :], self.I["w_out"].t[l].rearrange("(k p) c -> p k c", p=128), reads=[self.I["w_out"]], writes=[wo], key="m_wo")
            brs = [fw.sb([128, 16, 512], BF16, f"mbr{i}") for i in range(1)]
            gts = [fw.sb([128, 32, 512], BF16, f"mgt{i}") for i in range(1)]
            xbs = [fw.sb([128, 8, 512], F32, f"mxb{i}") for i in range(1)]
            mg = fw.sb([128, 512], F32, "mmg")
            tmp = fw.sb([128, 512], F32, "mtmp")
            mgb = fw.sb([128, 8, 512], BF16, "mmgb")
            xo = [fw.sb([128, 8, 512], F32, f"mxo{i}") for i in range(2)]
            src = self.xsrc()
            for bi, (t0, n, ic) in enumerate(self.qblocks()):
                br, gt, xb, xn = brs[0], gts[0], xbs[0], xo[bi % 2]
                fw.dma("sp", br[:, :, 0:n], self.BR.t[:, :, t0:t0 + n].rearrange("c p t -> p c t"), reads=[self.BR], writes=[br], key="m_br")
                fw.dma("sp", gt[:, :, 0:n], self.PTall.t[:, :, t0:t0 + n].rearrange("c p t -> p c t"), reads=[self.PTall], writes=[gt], key="m_gt")
                fw.dma("sp", xb[:, :, 0:n], src.t.rearrange("(k p) t -> p k t", p=128)[:, :, t0:t0 + n], reads=[src], writes=[xb], key="m_xb")
                for oc in range(8):
                    for i in range(4):
                        ps = self.bank()
                        for k in range(4):
                            fw.op("pe", lambda e, ps=ps, i=i, k=k, oc=oc: e.matmul(ps[:, 0:n], wb[:, i * 4 + k, oc * 128:(oc + 1) * 128], br[:, i * 4 + k, 0:n], start=(k == 0), stop=(k == 3)),
                                  reads=[wb, br], writes=[ps], sig=(k == 3))
                        if i == 0:
                            fw.op("dve", lambda e, ps=ps, oc=oc: e.tensor_tensor(out=mg[:, 0:n], in0=ps[:, 0:n], in1=gt[:, oc, 0:n], op=ALU.mult), reads=[ps, gt], writes=[mg])
                        else:
                            fw.op("dve", lambda e, ps=ps, oc=oc, i=i: e.tensor_tensor(out=tmp[:, 0:n], in0=ps[:, 0:n], in1=gt[:, i * 8 + oc, 0:n], op=ALU.mult), reads=[ps, gt], writes=[tmp])
                            if i < 3:
                                fw.op("pool", lambda e: e.tensor_tensor(out=mg[:, 0:n], in0=mg[:, 0:n], in1=tmp[:, 0:n], op=ALU.add), reads=[mg, tmp], writes=[mg])
                            else:
                                fw.op("pool", lambda e, oc=oc: e.tensor_tensor(out=mgb[:, oc, 0:n], in0=mg[:, 0:n], in1=tmp[:, 0:n], op=ALU.add), reads=[mg, tmp], writes=[mgb])
                for o2 in range(8):
                    ps = self.bank()
                    for k in range(8):
                        fw.op("pe", lambda e, ps=ps, k=k, o2=o2: e.matmul(ps[:, 0:n], wo[:, k, o2 * 128:(o2 + 1) * 128], mgb[:, k, 0:n], start=(k == 0), stop=(k == 7)),
                              reads=[wo, mgb], writes=[ps], sig=(k == 7))
                    fw.op("dve", lambda e, ps=ps, o2=o2: e.scalar_tensor_tensor(out=xn[:, o2, 0:n], in0=ps[:, 0:n], scalar=self.modt[:, 16 + o2, ic:ic + 1], in1=xb[:, o2, 0:n],
                                                                         op0=ALU.mult, op1=ALU.add), reads=[ps, self.modt, xb], writes=[xn])
                fw.dma("sp", self.xs.t.rearrange("(k p) t -> p k t", p=128)[:, :, t0:t0 + n], xn[:, :, 0:n], reads=[xn], writes=[self.xs], key=f"m_xo{bi % 2}")
        self.x_written = True

    def ph_ffn(self):
        fw, l = self.fw, self.l
        moe = (l % 2 == 1)
        li = l // 2
        JG = 512
        NJG = DFF // JG
        blocks = self.qblocks()
        halves = [blocks[:len(blocks) - 4], blocks[len(blocks) - 4:]]
        for half in halves:
            if not half:
                continue
            hb0 = half[0][0]
            hlen = sum(b[1] for b in half)
            with fw.scope():
                hT = fw.sb([128, 8, hlen], BF16, "fh")
                with fw.scope():
                    xbs = [fw.sb([128, 8, 512], F32, f"fxb{i}") for i in range(2)]
                    sq = fw.sb([128, 8, 512], BF16, "fsq")
                    rstd = fw.sb([128, 512], F32, "frstd")
                    tmp = fw.sb([128, 8, 512], F32, "ftmp")
                    h32 = fw.sb([128, 8, 512], F32, "fh32") if moe else None
                    if moe:
                        lg = fw.sb([128, 8], F32, "flg")
                        m1 = fw.sb([128, 1], F32, "fm1")
                        m2 = fw.sb([128, 1], F32, "fm2")
                        k1 = fw.sb([128, 8], F32, "fk1")
                        k2 = fw.sb([128, 8], F32, "fk2")
                        l2 = fw.sb([128, 8], F32, "fl2")
                        w1 = fw.sb([128, 1], F32, "fw1")
                        w2 = fw.sb([128, 1], F32, "fw2")
                        cmb = fw.sb([128, 8], F32, "fcmb")
                        cbm = fw.sb([128, 8, 128], F32, "fcbm")
                        cbo = [fw.sb([128, 8, 512], BF16, f"fcbo{i}") for i in range(2)]
                    for bi, (t0, n, ic) in enumerate(half):
                        self.norm_block(1, t0, n, ic, hT, t0 - hb0, xbs[bi % 2], sq, rstd, tmp, h32)
                        if not moe:
                            continue
                        co = cbo[bi % 2]
                        for ti in range(n // 128):
                            ps = self.bank()
                            for k in range(8):
                                fw.op("pe", lambda e, ps=ps, k=k, ti=ti: e.matmul(ps[:, 0:8], h32[:, k, ti * 128:(ti + 1) * 128], self.routerT[:, li, k, :], start=(k == 0), stop=(k == 7)),
                                      reads=[h32, self.routerT], writes=[ps], sig=(k == 7))
                            fw.op("dve", lambda e, ps=ps: e.tensor_copy(out=lg[:, :], in_=ps[:, 0:8]), reads=[ps], writes=[lg])
                            fw.op("dve", lambda e: e.reduce_max(out=m1[:, :], in_=lg[:, :], axis=AX.X), reads=[lg], writes=[m1])
                            fw.op("dve", lambda e: e.tensor_scalar(out=k1[:, :], in0=lg[:, :], scalar1=m1[:, 0:1], scalar2=None, op0=ALU.is_ge), reads=[lg, m1], writes=[k1])
                            fw.op("dve", lambda e: e.scalar_tensor_tensor(out=l2[:, :], in0=k1[:, :], scalar=-1e30, in1=lg[:, :], op0=ALU.mult, op1=ALU.add), reads=[k1, lg], writes=[l2])
                            fw.op("dve", lambda e: e.reduce_max(out=m2[:, :], in_=l2[:, :], axis=AX.X), reads=[l2], writes=[m2])
                            fw.op("dve", lambda e: e.tensor_scalar(out=k2[:, :], in0=l2[:, :], scalar1=m2[:, 0:1], scalar2=None, op0=ALU.is_ge), reads=[l2, m2], writes=[k2])
                            fw.op("dve", lambda e: e.tensor_tensor(out=w2[:, :], in0=m2[:, :], in1=m1[:, :], op=ALU.subtract), reads=[m1, m2], writes=[w2])
                            fw.op("act", lambda e: e.activation(out=w2[:, :], in_=w2[:, :], func=AF.Exp), reads=[w2], writes=[w2])
                            fw.op("dve", lambda e: e.tensor_scalar(out=w1[:, :], in0=w2[:, :], scalar1=1.0, scalar2=None, op0=ALU.add), reads=[w2], writes=[w1])
                            fw.op("dve", lambda e: e.reciprocal(out=w1[:, :], in_=w1[:, :]), reads=[w1], writes=[w1])
                            fw.op("dve", lambda e: e.tensor_scalar(out=w2[:, :], in0=w1[:, :], scalar1=-1.0, scalar2=1.0, op0=ALU.mult, op1=ALU.add), reads=[w1], writes=[w2])
                            fw.op("dve", lambda e: e.tensor_scalar(out=cmb[:, :], in0=k1[:, :], scalar1=w1[:, 0:1], scalar2=None, op0=ALU.mult), reads=[k1, w1], writes=[cmb])
                            fw.op("dve", lambda e: e.scalar_tensor_tensor(out=cmb[:, :], in0=k2[:, :], scalar=w2[:, 0:1], in1=cmb[:, :], op0=ALU.mult, op1=ALU.add), reads=[k2, w2, cmb], writes=[cmb])
                            fw.op("dve", lambda e: e.tensor_copy(out=cbm[:, :, :], in_=cmb.t[:, :].unsqueeze(2).broadcast_to([128, 8, 128])), reads=[cmb], writes=[cbm])
                            for eh in range(2):
                                pc = self.bank()
                                for e4 in range(4):
                                    ex = eh * 4 + e4
                                    fw.op("pe", lambda e, pc=pc, e4=e4, ex=ex: e.matmul(pc[:, e4 * 128:(e4 + 1) * 128], cbm[:, ex, :], self.identf[:, :], start=True, stop=True),
                                          reads=[cbm, self.identf], writes=[pc], sig=(e4 == 3))
                                fw.op("act", lambda e, pc=pc, eh=eh, ti=ti, co=co: e.activation(out=co[:, eh * 4:eh * 4 + 4, ti * 128:(ti + 1) * 128],
                                                                                     in_=pc.t.rearrange("p (a c) -> p a c", c=128), func=AF.Copy), reads=[pc], writes=[co])
                        fw.dma("sp", self.CB.t[:, :, t0:t0 + n].rearrange("e p t -> p e t"), co[:, :, 0:n], reads=[co], writes=[self.CB], key=f"f_cb{bi % 2}")
                acc = fw.sb([128, 8, hlen], F32, "facc")
                self._ffn_experts(moe, li, half, hb0, hT, acc)
                self._ffn_resid(half, hb0, acc)

    def _ffn_experts(self, moe, li, half, hb0, hT, acc):
        fw, l = self.fw, self.l
        JG = 512
        NJG = DFF // JG
        with fw.scope():
            if True:
                w1s = [fw.sb([128, 8, JG], BF16, f"fw1_{i}") for i in range(2)]
                w3s = [fw.sb([128, 8, JG], BF16, f"fw3_{i}") for i in range(2)]
                w2s = [fw.sb([128, JG // 128, D], BF16, f"fw2_{i}") for i in range(2)]
                sa = [fw.sb([128, 512], BF16, f"fsa{i}") for i in range(2)]
                gb = [fw.sb([128, JG // 128, 512], BF16, f"fgb{i}") for i in range(2)]
                cbl = [fw.sb([128, 512], BF16, f"fcl{i}") for i in range(2)]
                ng = 0
                nb = 0
                first_acc = True
                for ex in range(NEXP if moe else 1):
                    for jg in range(NJG):
                        wa, wc, wd = w1s[ng % 2], w3s[ng % 2], w2s[ng % 2]
                        if moe:
                            s1, s3, s2 = self.I["moe_w1"].t[li, ex], self.I["moe_w3"].t[li, ex], self.I["moe_w2"].t[li, ex]
                            r1, r3, r2 = self.I["moe_w1"], self.I["moe_w3"], self.I["moe_w2"]
                        else:
                            s1, s3, s2 = self.I["ffn_w1"].t[li], self.I["ffn_w3"].t[li], self.I["ffn_w2"].t[li]
                            r1, r3, r2 = self.I["ffn_w1"], self.I["ffn_w3"], self.I["ffn_w2"]
                        fw.dma("pool", wa[:, :, :], s1.rearrange("(k p) c -> p k c", p=128)[:, :, jg * JG:(jg + 1) * JG], reads=[r1], writes=[wa], key=f"f_w1{ng % 2}")
                        fw.dma("pool", wc[:, :, :], s3.rearrange("(k p) c -> p k c", p=128)[:, :, jg * JG:(jg + 1) * JG], reads=[r3], writes=[wc], key=f"f_w3{ng % 2}")
                        fw.dma("pool", wd[:, :, :], s2[jg * JG:(jg + 1) * JG, :].rearrange("(k p) c -> p k c", p=128), reads=[r2], writes=[wd], key=f"f_w2{ng % 2}")
                        ng += 1
                        for (t0, n, ic) in half:
                            c0 = t0 - hb0
                            g = gb[nb % 2]
                            cl = cbl[nb % 2]
                            nb += 1
                            if moe:
                                fw.dma("sp", cl[:, 0:n], self.CB.t[ex, :, t0:t0 + n], reads=[self.CB], writes=[cl], key=f"f_cl{nb % 2}")
                            for jc in range(JG // 128):
                                p1 = self.psb[1 + (jc % 2) * 2]
                                p3 = self.psb[2 + (jc % 2) * 2]
                                for k in range(8):
                                    fw.op("pe", lambda e, p1=p1, k=k, jc=jc: e.matmul(p1[:, 0:n], wa[:, k, jc * 128:(jc + 1) * 128], hT[:, k, c0:c0 + n], start=(k == 0), stop=(k == 7)),
                                          reads=[wa, hT], writes=[p1], sig=(k == 7))
                                for k in range(8):
                                    fw.op("pe", lambda e, p3=p3, k=k, jc=jc: e.matmul(p3[:, 0:n], wc[:, k, jc * 128:(jc + 1) * 128], hT[:, k, c0:c0 + n], start=(k == 0), stop=(k == 7)),
                                          reads=[wc, hT], writes=[p3], sig=(k == 7))
                                s = sa[jc % 2]
                                fw.op("act", lambda e, p1=p1, s=s: e.activation(out=s[:, 0:n], in_=p1[:, 0:n], func=AF.Silu), reads=[p1], writes=[s])
                                fw.op("dve", lambda e, p3=p3, s=s, g=g, jc=jc: e.tensor_tensor(out=g[:, jc, 0:n], in0=p3[:, 0:n], in1=s[:, 0:n], op=ALU.mult), reads=[p3, s], writes=[g])
                                if moe:
                                    fw.op("pool", lambda e, g=g, jc=jc, cl=cl: e.tensor_tensor(out=g[:, jc, 0:n], in0=g[:, jc, 0:n], in1=cl[:, 0:n], op=ALU.mult), reads=[g, cl], writes=[g])
                            for o in range(8):
                                pf = self.psb[5 + (o % 2)]
                                for jc in range(JG // 128):
                                    fw.op("pe", lambda e, pf=pf, jc=jc, o=o, g=g: e.matmul(pf[:, 0:n], wd[:, jc, o * 128:(o + 1) * 128], g[:, jc, 0:n], start=(jc == 0), stop=(jc == JG // 128 - 1)),
                                          reads=[wd, g], writes=[pf], sig=(jc == JG // 128 - 1))
                                if first_acc:
                                    fw.op("act", lambda e, pf=pf, o=o: e.activation(out=acc[:, o, c0:c0 + n], in_=pf[:, 0:n], func=AF.Copy), reads=[pf], writes=[acc])
                                else:
                                    fw.op("dve", lambda e, pf=pf, o=o: e.tensor_tensor(out=acc[:, o, c0:c0 + n], in0=acc[:, o, c0:c0 + n], in1=pf[:, 0:n], op=ALU.add), reads=[pf, acc], writes=[acc])
                        first_acc = False

    def _ffn_resid(self, half, hb0, acc):
        fw, l = self.fw, self.l
        with fw.scope():
            if True:
                xbs = [fw.sb([128, 8, 512], F32, f"fxr{i}") for i in range(2)]
                for bi, (t0, n, ic) in enumerate(half):
                    c0 = t0 - hb0
                    xb = xbs[bi % 2]
                    fw.dma("sp", xb[:, :, 0:n], self.xs.t.rearrange("(k p) t -> p k t", p=128)[:, :, t0:t0 + n], reads=[self.xs], writes=[xb], key=f"f_xr{bi % 2}")
                    for k in range(8):
                        fw.op("dve", lambda e, k=k: e.scalar_tensor_tensor(out=xb[:, k, 0:n], in0=acc[:, k, c0:c0 + n], scalar=self.modt[:, 40 + k, ic:ic + 1], in1=xb[:, k, 0:n],
                                                                          op0=ALU.mult, op1=ALU.add), reads=[acc, self.modt, xb], writes=[xb])
                    fw.dma("sp", self.xs.t.rearrange("(k p) t -> p k t", p=128)[:, :, t0:t0 + n], xb[:, :, 0:n], reads=[xb], writes=[self.xs], key=f"f_xw{bi % 2}")


def host_inputs(inp, b):
    f = np.float32
    def colT(v):
        v = np.asarray(v, f)
        sh = v.shape
        v = v.reshape(sh[:-1] + (sh[-1] // 128, 128))
        return np.ascontiguousarray(np.moveaxis(v, -1, 0))
    m = {}
    m["xT0"] = np.ascontiguousarray(np.concatenate([inp["ctx"][b], inp["x"][b]], axis=0).T.astype(f))
    m["cvec"] = np.ascontiguousarray(np.stack([colT(inp["c"][b]), colT(inp["c_ctx"])], axis=-1))
    m["ada_w"] = np.asarray(inp["ada_w"], f)
    m["ada_bT"] = colT(inp["ada_b"])
    m["g1T"] = colT(inp["norm1_g"])
    m["g2T"] = colT(inp["norm2_g"])
    m["w_in"] = np.asarray(inp["w_in"], f)
    qkg = np.asarray(inp["qk_norm_g"], f)
    m["qkgT"] = np.ascontiguousarray(np.tile(np.transpose(qkg, (2, 0, 1)), (2, 1, 1)))
    m["lamB"] = np.ascontiguousarray(np.broadcast_to(np.asarray(inp["diff_lambda"], f).reshape(1, DEPTH, 256), (128, DEPTH, 256)))
    m["sublnT"] = np.ascontiguousarray(np.asarray(inp["diff_subln_g"], f).T)
    cw = np.asarray(inp["conv_w"], f)
    m["convwT"] = np.ascontiguousarray(np.transpose(cw.reshape(DEPTH, 31, 4, 128), (3, 0, 2, 1)))
    m["convbT"] = colT(inp["conv_b"])
    m["clngT"] = colT(inp["conv_ln_g"])
    m["clnbT"] = colT(inp["conv_ln_b"])
    m["lblT"] = colT(inp["hgrn_lb_logits"])
    m["hgT"] = np.ascontiguousarray(np.asarray(inp["hgrn_norm_g"], f).T)
    m["w_branch"] = np.asarray(inp["w_branch"], f)
    m["w_out"] = np.asarray(inp["w_out"], f)
    for k in ("ffn_w1", "ffn_w3", "ffn_w2", "moe_w1", "moe_w3", "moe_w2"):
        m[k] = np.asarray(inp[k], f)
    r = np.asarray(inp["moe_router"], f)
    m["routerT"] = np.ascontiguousarray(np.transpose(r.reshape(2, 8, 128, 8), (2, 0, 1, 3)))
    for k, v in _const_tables().items():
        m["c_" + k] = v
    return m


_CACHE = {}


def kernel(**inputs):
    if "nc" not in _CACHE:
        _CACHE["nc"] = MK().build()
    nc = _CACHE["nc"]
    in_maps = [host_inputs(inputs, b) for b in range(8)]
    res = run_bass_kernel_spmd(nc, in_maps, core_ids=list(range(8)))
    out = np.stack([np.ascontiguousarray(res.results[b]["xs"][:, NCTX:].T) for b in range(8)], axis=0)
    return out.astype(np.float32)
```

```python
import math
from contextlib import ExitStack, contextmanager
import numpy as np
import ml_dtypes
import concourse.bass as bass
import concourse.mybir as mybir
from concourse.bass_utils import run_bass_kernel_spmd

F32 = mybir.dt.float32
BF16 = mybir.dt.bfloat16
AF = mybir.ActivationFunctionType
ALU = mybir.AluOpType
AX = mybir.AxisListType

D = 1024
SEQ = 4096
NCTX = 256
T = SEQ + NCTX
NT = T // 128
DEPTH = 4
D_IN = 9984
NOC = D_IN // 128
DFF = 3584
NEXP = 8
EPS = 1e-6
BLK = [(0, 256, 1)] + [(256 + 512 * i, 512, 0) for i in range(8)]
VCH = {5: 0, 14: 1, 15: 2, 16: 3, 17: 4, 38: 5, 39: 6, 40: 7, 41: 8}
QKCH = [0, 1, 2, 3, 4, 6, 7, 8, 9, 10, 11, 12, 13]
UPAD = 15
ULEN = UPAD + 256 + UPAD + UPAD + 4096 + UPAD
UOFF = [UPAD, UPAD + 256 + 2 * UPAD]


class Buf:
    __slots__ = ("t", "name", "key", "w", "r", "psum")

    def __init__(self, t, name, key=None):
        self.psum = False
        self.t = t
        self.name = name
        self.key = key or name
        self.w = None
        self.r = {}

    def __getitem__(self, idx):
        return self.t[idx]


class FW:
    def __init__(self, nc, root):
        self.nc = nc
        self.root = root
        self.stack = root
        self.engs = {"pe": nc.tensor, "act": nc.scalar, "dve": nc.vector, "pool": nc.gpsimd, "sp": nc.sync}
        self.sem = {k: root.enter_context(nc.semaphore("s_" + k)) for k in self.engs}
        self.cnt = {k: 0 for k in self.engs}
        self.seen = {k: {} for k in self.engs}
        self.dsem = {}
        self.nbuf = 0
        self.ninst = 0

    @contextmanager
    def scope(self):
        old = self.stack
        with ExitStack() as s:
            self.stack = s
            try:
                yield
            finally:
                self.barrier()
                self.stack = old

    def sb(self, shape, dt, name=None):
        self.nbuf += 1
        key = name or 'sb'
        name = f"{key}_{self.nbuf}"
        t = self.stack.enter_context(self.nc.sbuf_tensor(name, list(shape), dt))
        return Buf(t, name, key)

    def ps(self, shape, dt=F32, name=None):
        self.nbuf += 1
        name = f"{name or 'ps'}_{self.nbuf}"
        t = self.root.enter_context(self.nc.psum_tensor(name, list(shape), dt))
        b = Buf(t, name)
        b.psum = True
        return b

    def dram(self, name, shape, dt, kind="Internal"):
        t = self.nc.dram_tensor(name, list(shape), dt, kind=kind)
        return Buf(t.ap(), name)

    def _semof(self, key):
        return self.sem[key] if key in self.sem else self.dsem[key][0]

    def _wait(self, E, ev):
        if ev is None:
            return
        key, val = ev
        if key not in self.sem:
            val = 16 * self.dsem[key][1]
        if key == "pe" and E == "pe":
            return
        if key == E and val > self.cnt[E]:
            return
        if self.seen[E].get(key, 0) >= val:
            return
        self.seen[E][key] = val
        self.engs[E].wait_ge(self._semof(key), val)
        self.ninst += 1

    def _deps(self, E, reads, writes):
        for b in reads:
            self._wait(E, b.w)
            if b.psum:
                for k, v in b.r.items():
                    if k != E:
                        self._wait(E, (k, v))
        for b in writes:
            self._wait(E, b.w)
            for k, v in b.r.items():
                self._wait(E, (k, v))

    def _record(self, ev, reads, writes):
        k, v = ev
        for b in reads:
            if b.r.get(k, 0) < v:
                b.r[k] = v
        for b in writes:
            b.w = ev
            b.r = {}

    def op(self, E, fn, reads=(), writes=(), sig=True):
        self._deps(E, reads, writes)
        ins = fn(self.engs[E])
        self.ninst += 1
        if sig:
            self.cnt[E] += 1
            ins.then_inc(self.sem[E], 1)
            ev = (E, self.cnt[E])
        else:
            ev = (E, self.cnt[E] + 1)
        self._record(ev, reads, writes)
        return ins

    def dma(self, Q, out, in_, reads=(), writes=(), key=None, **kw):
        self._deps(Q, reads, writes)
        if key is None:
            key = "d_" + (writes[0].key if writes else reads[0].key)
        if key not in self.dsem:
            s = self.root.enter_context(self.nc.semaphore("q_" + str(len(self.dsem))))
            self.dsem[key] = [s, 0]
        ent = self.dsem[key]
        ent[1] += 1
        ins = self.engs[Q].dma_start(out=out, in_=in_, **kw)
        ins.then_inc(ent[0], 16)
        self.ninst += 1
        self._record((key, 16 * ent[1]), reads, writes)
        return ins

    def barrier(self, engines=("pe", "act", "dve", "pool", "sp")):
        for E in engines:
            for k in ("pe", "act", "dve", "pool", "sp"):
                if k != E and self.cnt[k]:
                    self._wait(E, (k, self.cnt[k]))
            for key, (s, c) in self.dsem.items():
                if c:
                    self._wait(E, (key, 16 * c))


def _const_tables():
    c = {}
    c["ones"] = np.ones((128, 128), np.float32)
    bd = np.zeros((128, 128), np.float32)
    bd[:64, :64] = 1
    bd[64:, 64:] = 1
    c["bd64"] = bd
    c["ident"] = np.eye(128, dtype=np.float32)
    R = np.zeros((128, 128), np.float32)
    for p in range(128):
        d = p % 64
        j = d % 32
        if j < 16:
            R[p + 16, p] = -1.0
        else:
            R[p - 16, p] = 1.0
    c["rot"] = R
    inv_freq = (10000.0 ** (-np.arange(0, 32, 2, dtype=np.float32) / 32)).astype(np.float32)
    tl = np.arange(SEQ)
    row = (tl // 64).astype(np.float32)
    col = (tl % 64).astype(np.float32)
    cos = np.ones((128, T), np.float32)
    sin = np.zeros((128, T), np.float32)
    for p in range(128):
        d = p % 64
        pos = row if d < 32 else col
        f = inv_freq[(d % 32) % 16]
        ang = (pos * f).astype(np.float32)
        cos[p, NCTX:] = np.cos(ang)
        sin[p, NCTX:] = np.sin(ang)
    c["cos"] = cos
    c["sin"] = sin
    s = np.arange(128)[:, None]
    t = np.arange(128)[None, :]
    same = (s // 64) == (t // 64)
    c["mask_f"] = np.tile((same & (s <= t)).astype(np.float32), (1, 4))
    c["mask_b"] = np.tile((same & (s >= t)).astype(np.float32), (1, 4))
    return c


CONST_SPECS = [("ones", [128, 128]), ("bd64", [128, 128]), ("ident", [128, 128]), ("rot", [128, 128]),
               ("cos", [128, T]), ("sin", [128, T]), ("mask_f", [128, 512]), ("mask_b", [128, 512])]

IN_SPECS = [
    ("xT0", [D, T]), ("cvec", [128, 8, 2]), ("ada_w", [DEPTH, D, 6 * D]), ("ada_bT", [128, DEPTH, 48]),
    ("g1T", [128, DEPTH, 8]), ("g2T", [128, DEPTH, 8]), ("w_in", [DEPTH, D, D_IN]),
    ("qkgT", [128, DEPTH, 4]), ("lamB", [128, DEPTH, 256]), ("sublnT", [128, DEPTH]),
    ("convwT", [128, DEPTH, 4, 31]), ("convbT", [128, DEPTH, 4]), ("clngT", [128, DEPTH, 4]),
    ("clnbT", [128, DEPTH, 4]), ("lblT", [128, DEPTH, 2, 4]), ("hgT", [64, DEPTH]),
    ("w_branch", [DEPTH, 4, 512, D]), ("w_out", [DEPTH, D, D]),
    ("ffn_w1", [2, D, DFF]), ("ffn_w3", [2, D, DFF]), ("ffn_w2", [2, DFF, D]),
    ("routerT", [128, 2, 8, 8]), ("moe_w1", [2, NEXP, D, DFF]), ("moe_w3", [2, NEXP, D, DFF]),
    ("moe_w2", [2, NEXP, DFF, D]),
]


class MK:
    def __init__(self, n_layers=DEPTH, debug=(), stop_after=None, ext_in=()):
        self.n_layers = n_layers
        self.debug = set(debug)
        self.ext_in = set(ext_in)
        self.stop_after = stop_after
        self.nc = bass.Bass("TRN2", target_bir_lowering=False)
        self.rr = 0

    def scratch(self, name, shape, dt):
        kind = "Internal"
        if name in self.debug:
            kind = "ExternalOutput"
        if name in self.ext_in:
            kind = "ExternalInput"
        return self.fw.dram(name, shape, dt, kind=kind)

    def build(self):
        nc = self.nc
        with ExitStack() as root:
            fw = self.fw = FW(nc, root)
            big = ("ada_w", "w_in", "w_branch", "w_out", "ffn_w1", "ffn_w3", "ffn_w2", "moe_w1", "moe_w3", "moe_w2")
            tiny = getattr(self, "tiny", False)
            self.I = {n: fw.dram(n, ([1] * len(s) if (tiny and n in big) else s), F32, kind="ExternalInput") for n, s in IN_SPECS}
            self.CI = {n: fw.dram("c_" + n, s, F32, kind="ExternalInput") for n, s in CONST_SPECS}
            self.xs = fw.dram("xs", [D, T], F32, kind="ExternalOutput")
            self.PT = [self.scratch(f"PT{i}", [128, T], BF16) for i in range(NOC)]
            self.PTall = self.scratch("PTg", [32, 128, T], BF16)
            self.VT = self.scratch("VT", [9, T, 128], BF16)
            self.QK = {oc: self.scratch(f"QK{oc}", [128, T], BF16) for oc in QKCH}
            self.BR = self.scratch("BR", [16, 128, T], BF16)
            self.OD = [self.scratch(f"OD{d}", [8, 64, T], F32) for d in range(2)]
            self.CB = self.scratch("CB", [NEXP, 128, T], BF16)
            self.psb = [fw.ps([128, 512], F32, f"bank{i}") for i in range(7)]
            self.psT = fw.ps([128, 1024], BF16, "bankT")
            self.ones = fw.sb([128, 128], BF16, "ones")
            self.bd64 = fw.sb([128, 128], BF16, "bd64")
            self.identb = fw.sb([128, 128], BF16, "identb")
            self.identf = fw.sb([128, 128], F32, "identf")
            self.rot = fw.sb([128, 128], BF16, "rot")
            for nm, b in (("ones", self.ones), ("bd64", self.bd64), ("ident", self.identb), ("rot", self.rot)):
                fw.dma("pool", b[:, :], self.CI[nm][:, :], reads=[self.CI[nm]], writes=[b])
            fw.dma("sp", self.identf[:, :], self.CI["ident"][:, :], reads=[self.CI["ident"]], writes=[self.identf])
            self.eps = fw.sb([128, 1], F32, "eps")
            fw.op("dve", lambda e: e.memset(self.eps[:, :], EPS), writes=[self.eps])
            self.small_params()
            self.modt = fw.sb([128, 48, 2], F32, "mod")
            self.gs = fw.sb([128, 2, 8, 2], F32, "gs")
            for l in (getattr(self, "layers", None) or range(self.n_layers)):
                self.layer(l)
                if self.stop_after is not None and self.stop_after[0] == l and self.done:
                    break
            fw.barrier(engines=("sp",))
        return nc

    def small_params(self):
        fw, I = self.fw, self.I
        def ld(name, shape):
            b = fw.sb(shape, F32, name)
            src = I[name]
            fw.dma("sp", b.t[tuple(slice(None) for _ in shape)], src.t[tuple(slice(None) for _ in shape)], reads=[src], writes=[b])
            return b
        self.cvec = ld("cvec", [128, 8, 2])
        self.ada_bT = ld("ada_bT", [128, DEPTH, 48])
        self.g1T = ld("g1T", [128, DEPTH, 8])
        self.g2T = ld("g2T", [128, DEPTH, 8])
        self.qkgT = ld("qkgT", [128, DEPTH, 4])
        self.lamB = ld("lamB", [128, DEPTH, 256])
        self.sublnT = ld("sublnT", [128, DEPTH])
        self.convwT = ld("convwT", [128, DEPTH, 4, 31])
        self.convbT = ld("convbT", [128, DEPTH, 4])
        self.clngT = ld("clngT", [128, DEPTH, 4])
        self.clnbT = ld("clnbT", [128, DEPTH, 4])
        self.lblT = ld("lblT", [128, DEPTH, 2, 4])
        self.hgT = ld("hgT", [64, DEPTH])
        self.routerT = ld("routerT", [128, 2, 8, 8])
        self.siluc = fw.sb([128, 8, 2], BF16, "siluc")
        fw.op("act", lambda e: e.activation(out=self.siluc[:, :, :], in_=self.cvec[:, :, :], func=AF.Silu),
              reads=[self.cvec], writes=[self.siluc])
        ex = fw.sb([128, DEPTH, 8], F32, "lbex")
        fw.op("act", lambda e: e.activation(out=ex[:, :, :], in_=self.lblT.t.rearrange("p l d c -> p l (d c)"), func=AF.Exp),
              reads=[self.lblT], writes=[ex])
        ssum = fw.sb([128, 8], F32, "lbsum")
        fw.op("dve", lambda e: e.tensor_tensor(out=ssum[:, :], in0=ex[:, 0, :], in1=ex[:, 1, :], op=ALU.add), reads=[ex], writes=[ssum])
        for l in range(2, DEPTH):
            fw.op("dve", lambda e, l=l: e.tensor_tensor(out=ssum[:, :], in0=ssum[:, :], in1=ex[:, l, :], op=ALU.add), reads=[ex, ssum], writes=[ssum])
        fw.op("dve", lambda e: e.reciprocal(out=ssum[:, :], in_=ssum[:, :]), reads=[ssum], writes=[ssum])
        self.lbT = fw.sb([128, DEPTH, 8], F32, "lbT")
        self.omlT = fw.sb([128, DEPTH, 8], F32, "omlT")
        fw.op("dve", lambda e: e.memset(self.lbT[:, 0, :], 0.0), writes=[self.lbT])
        for l in range(1, DEPTH):
            fw.op("dve", lambda e, l=l: e.tensor_tensor(out=ex[:, l, :], in0=ex[:, l, :], in1=ssum[:, :], op=ALU.mult), reads=[ex, ssum], writes=[ex])
            fw.op("dve", lambda e, l=l: e.tensor_tensor(out=self.lbT[:, l, :], in0=self.lbT[:, l - 1, :], in1=ex[:, l, :], op=ALU.add),
                  reads=[ex, self.lbT], writes=[self.lbT])
        fw.op("dve", lambda e: e.tensor_scalar(out=self.omlT[:, :, :], in0=self.lbT[:, :, :], scalar1=-1.0, scalar2=1.0, op0=ALU.mult, op1=ALU.add),
              reads=[self.lbT], writes=[self.omlT])
        self.neglam = fw.sb([128, DEPTH], F32, "neglam")
        self.subg = fw.sb([128, DEPTH], F32, "subg")
        pr = fw.sb([128, DEPTH, 2, 64], F32, "lampr")
        s12 = fw.sb([128, DEPTH, 2], F32, "lams")
        lam4 = self.lamB.t.rearrange("p l (i d) -> p l i d", i=4)
        for l in range(DEPTH):
            for j in range(2):
                fw.op("dve", lambda e, l=l, j=j: e.tensor_tensor(out=pr[:, l, j, :], in0=lam4[:, l, 2 * j, :], in1=lam4[:, l, 2 * j + 1, :], op=ALU.mult),
                      reads=[self.lamB], writes=[pr])
                fw.op("dve", lambda e, l=l, j=j: e.reduce_sum(out=s12[:, l, j:j + 1], in_=pr[:, l, j, :], axis=AX.X), reads=[pr], writes=[s12])
        fw.op("act", lambda e: e.activation(out=s12[:, :, :], in_=s12[:, :, :], func=AF.Exp), reads=[s12], writes=[s12])
        for l in range(DEPTH):
            li = 0.8 - 0.6 * math.exp(-0.3 * l)
            fw.op("dve", lambda e, l=l, li=li: e.scalar_tensor_tensor(out=self.neglam[:, l:l + 1], in0=s12[:, l, 1:2], scalar=-li, in1=s12[:, l, 0:1],
                                                                     op0=ALU.add, op1=ALU.subtract), reads=[s12], writes=[self.neglam])
            fw.op("dve", lambda e, l=l, li=li: e.tensor_scalar(out=self.subg[:, l:l + 1], in0=self.sublnT[:, l:l + 1], scalar1=1.0 - li, scalar2=None, op0=ALU.mult),
                  reads=[self.sublnT], writes=[self.subg])

    def bank(self):
        self.rr = (self.rr + 1) % 7
        return self.psb[self.rr]

    def layer(self, l):
        self.done = False
        need_ctx = l < DEPTH - 1
        self.l = l
        self.need_ctx = need_ctx
        steps = [self.ph_mod, self.ph_inproj, self.ph_qk, self.ph_gqa, self.ph_diff, self.ph_conv, self.ph_hgrn,
                 self.ph_merge, self.ph_ffn]
        for i, s in enumerate(steps):
            if getattr(self, "only", None) is not None and i not in self.only:
                continue
            s()
            if self.stop_after is not None and self.stop_after == (l, i):
                self.done = True
                return

    def xsrc(self):
        return self.I["xT0"] if (self.l == 0 and not self.x_written) else self.xs

    def ph_mod(self):
        fw, l = self.fw, self.l
        self.x_written = (l > 0)
        with fw.scope():
            ps = self.psb[0]
            wts = [fw.sb([128, 8, 512], BF16, f"adaw{i}") for i in range(2)]
            aw = self.I["ada_w"].t[l].rearrange("(k p) c -> p k c", p=128)
            for g in range(12):
                w = wts[g % 2]
                fw.dma("pool", w[:, :, :], aw[:, :, g * 512:(g + 1) * 512], reads=[self.I["ada_w"]], writes=[w], key=f"adaw{g % 2}")
                for cc in range(4):
                    j = g * 4 + cc
                    for k in range(8):
                        fw.op("pe", lambda e, w=w, cc=cc, k=k, j=j: e.matmul(ps[:, 2 * j:2 * j + 2], w[:, k, cc * 128:(cc + 1) * 128], self.siluc[:, k, :],
                                                                       start=(k == 0), stop=(k == 7)),
                              reads=[w, self.siluc], writes=[ps], sig=(k == 7))
            mod = self.modt
            for i in range(2):
                fw.op("dve", lambda e, i=i: e.tensor_tensor(out=mod[:, :, i], in0=ps.t[:, 0:96].rearrange("p (j i) -> p j i", i=2)[:, :, i],
                                                            in1=self.ada_bT[:, l, :], op=ALU.add), reads=[ps, self.ada_bT], writes=[mod])
            for s, (gT, c0) in enumerate(((self.g1T, 8), (self.g2T, 32))):
                for i in range(2):
                    fw.op("dve", lambda e, s=s, gT=gT, c0=c0, i=i: e.scalar_tensor_tensor(out=self.gs[:, s, :, i], in0=mod[:, c0:c0 + 8, i], scalar=1.0, in1=gT[:, l, :],
                                                                                     op0=ALU.add, op1=ALU.mult), reads=[mod, gT], writes=[self.gs])

    def norm_block(self, s, t0, n, ic, hT, hcol, xb, sq, rstd, tmp, h32=None):
        fw = self.fw
        src = self.xsrc()
        shc = 0 if s == 0 else 24
        fw.dma("sp", xb[:, :, 0:n], src.t.rearrange("(k p) t -> p k t", p=128)[:, :, t0:t0 + n], reads=[src], writes=[xb])
        fw.op("act", lambda e: e.activation(out=sq[:, :, 0:n], in_=xb[:, :, 0:n], func=AF.Square), reads=[xb], writes=[sq])
        ps = self.bank()
        for k in range(8):
            fw.op("pe", lambda e, k=k: e.matmul(ps[:, 0:n], self.ones[:, :], sq[:, k, 0:n], start=(k == 0), stop=(k == 7)),
                  reads=[self.ones, sq], writes=[ps], sig=(k == 7))
        fw.op("act", lambda e: e.activation(out=rstd[:, 0:n], in_=ps[:, 0:n], func=AF.Sqrt, bias=self.eps[:, 0:1], scale=1.0 / D), reads=[ps, self.eps], writes=[rstd])
        fw.op("dve", lambda e: e.reciprocal(out=rstd[:, 0:n], in_=rstd[:, 0:n]), reads=[rstd], writes=[rstd])
        for k in range(8):
            fw.op("dve", lambda e, k=k: e.scalar_tensor_tensor(out=tmp[:, k, 0:n], in0=xb[:, k, 0:n], scalar=self.gs[:, s, k, ic:ic + 1], in1=rstd[:, 0:n],
                                                              op0=ALU.mult, op1=ALU.mult), reads=[xb, self.gs, rstd], writes=[tmp])
            fw.op("act", lambda e, k=k: e.activation(out=hT[:, k, hcol:hcol + n], in_=tmp[:, k, 0:n], func=AF.Identity, bias=self.modt[:, shc + k, ic:ic + 1], scale=1.0),
                  reads=[tmp, self.modt], writes=[hT])
            if h32 is not None:
                fw.op("pool", lambda e, k=k: e.tensor_scalar(out=h32[:, k, 0:n], in0=tmp[:, k, 0:n], scalar1=self.modt[:, shc + k, ic:ic + 1], scalar2=None, op0=ALU.add),
                      reads=[tmp, self.modt], writes=[h32])

    def ph_inproj(self):
        fw, l = self.fw, self.l
        with fw.scope():
            hT = fw.sb([128, 8, T], BF16, "hT")
            with fw.scope():
                xbs = [fw.sb([128, 8, 512], F32, f"xb{i}") for i in range(2)]
                sq = fw.sb([128, 8, 512], BF16, "sq")
                rstd = fw.sb([128, 512], F32, "rstd")
                tmp = fw.sb([128, 8, 512], F32, "tmp")
                for bi, (t0, n, ic) in enumerate(BLK):
                    self.norm_block(0, t0, n, ic, hT, t0, xbs[bi % 2], sq, rstd, tmp)
            wts = [fw.sb([128, 8, 512], BF16, f"win{i}") for i in range(2)]
            stg = [fw.sb([128, T], BF16, f"stg{i}") for i in range(2)]
            vst = [fw.sb([128, NT, 128], BF16, f"vst{i}") for i in range(2)]
            wv = self.I["w_in"].t[l].rearrange("(k p) c -> p k c", p=128)
            ns = 0
            for g in range(20):
                w = wts[g % 2]
                gc = min(512, D_IN - g * 512)
                fw.dma("pool", w[:, :, 0:gc], wv[:, :, g * 512:g * 512 + gc], reads=[self.I["w_in"]], writes=[w], key=f"win{g % 2}")
                for cc in range(gc // 128):
                    oc = g * 4 + cc
                    if oc in VCH:
                        v = vst[VCH[oc] % 2]
                        for tg in range(0, NT, 4):
                            ps = self.bank()
                            nt_ = min(4, NT - tg)
                            for ti in range(nt_):
                                tt = tg + ti
                                for k in range(8):
                                    fw.op("pe", lambda e, k=k, tt=tt, ti=ti, ps=ps, w=w, cc=cc: e.matmul(
                                        ps[:, ti * 128:(ti + 1) * 128], hT[:, k, tt * 128:(tt + 1) * 128], w[:, k, cc * 128:(cc + 1) * 128],
                                        start=(k == 0), stop=(k == 7)), reads=[hT, w], writes=[ps], sig=(k == 7))
                            fw.op("dve", lambda e, ps=ps, v=v, tg=tg, nt_=nt_: e.tensor_copy(out=v[:, tg:tg + nt_, :], in_=ps.t[:, 0:nt_ * 128].rearrange("p (a c) -> p a c", c=128)),
                                  reads=[ps], writes=[v])
                        fw.dma("sp", self.VT.t[VCH[oc]].rearrange("(n p) c -> p n c", p=128), v[:, :, :], reads=[v], writes=[self.VT], key="vt_st")
                        continue
                    so = stg[ns % 2]
                    ns += 1
                    for bi, (t0, n, ic) in enumerate(BLK):
                        ps = self.bank()
                        for k in range(8):
                            fw.op("pe", lambda e, k=k, ps=ps, w=w, cc=cc, t0=t0, n=n: e.matmul(ps[:, 0:n], w[:, k, cc * 128:(cc + 1) * 128], hT[:, k, t0:t0 + n],
                                                                                   start=(k == 0), stop=(k == 7)), reads=[hT, w], writes=[ps], sig=(k == 7))
                        if oc >= 46:
                            fw.op("act", lambda e, ps=ps, so=so, t0=t0, n=n: e.activation(out=so[:, t0:t0 + n], in_=ps[:, 0:n], func=AF.Sigmoid), reads=[ps], writes=[so])
                        elif bi % 2 == 0:
                            fw.op("dve", lambda e, ps=ps, so=so, t0=t0, n=n: e.tensor_copy(out=so[:, t0:t0 + n], in_=ps[:, 0:n]), reads=[ps], writes=[so])
                        else:
                            fw.op("act", lambda e, ps=ps, so=so, t0=t0, n=n: e.activation(out=so[:, t0:t0 + n], in_=ps[:, 0:n], func=AF.Copy), reads=[ps], writes=[so])
                    if oc >= 46:
                        fw.dma("sp", self.PTall.t[oc - 46], so[:, :], reads=[so], writes=[self.PTall], key="ptg_st")
                    else:
                        fw.dma("sp", self.PT[oc][:, :], so[:, :], reads=[so], writes=[self.PT[oc]], key=f"pt_st{ns % 2}")

    def ph_qk(self):
        fw, l = self.fw, self.l
        with fw.scope():
            cos = fw.sb([128, T], F32, "cos")
            sin = fw.sb([128, T], F32, "sin")
            fw.dma("sp", cos[:, :], self.CI["cos"][:, :], reads=[self.CI["cos"]], writes=[cos])
            fw.dma("sp", sin[:, :], self.CI["sin"][:, :], reads=[self.CI["sin"]], writes=[sin])
            qin = [fw.sb([128, T], BF16, f"qin{i}") for i in range(2)]
            qout = [fw.sb([128, T], BF16, f"qout{i}") for i in range(2)]
            sq = [fw.sb([128, 512], BF16, f"qsq{i}") for i in range(2)]
            rstd = [fw.sb([128, 512], F32, f"qrstd{i}") for i in range(2)]
            qn = [fw.sb([128, 512], BF16, f"qn{i}") for i in range(2)]
            t1 = [fw.sb([128, 512], F32, f"qt1{i}") for i in range(2)]
            t2 = [fw.sb([128, 512], F32, f"qt2{i}") for i in range(2)]
            it = 0
            for ci, oc in enumerate(QKCH):
                gi = 0 if oc < 4 else 1 if oc == 4 else 2 if oc < 10 else 3
                qi, qo = qin[ci % 2], qout[ci % 2]
                fw.dma("sp", qi[:, :], self.PT[oc][:, :], reads=[self.PT[oc]], writes=[qi], key=f"qk_ld{ci % 2}")
                for (t0, n, ic) in BLK:
                    i2 = it % 2
                    it += 1
                    fw.op("act", lambda e, qi=qi, i2=i2, t0=t0, n=n: e.activation(out=sq[i2][:, 0:n], in_=qi[:, t0:t0 + n], func=AF.Square), reads=[qi], writes=[sq[i2]])
                    ps = self.bank()
                    fw.op("pe", lambda e, ps=ps, i2=i2, n=n: e.matmul(ps[:, 0:n], self.bd64[:, :], sq[i2][:, 0:n], start=True, stop=True), reads=[self.bd64, sq[i2]], writes=[ps])
                    fw.op("act", lambda e, ps=ps, i2=i2, n=n: e.activation(out=rstd[i2][:, 0:n], in_=ps[:, 0:n], func=AF.Sqrt, bias=self.eps[:, 0:1], scale=1.0 / 64),
                          reads=[ps, self.eps], writes=[rstd[i2]])
                    fw.op("dve", lambda e, i2=i2, n=n: e.reciprocal(out=rstd[i2][:, 0:n], in_=rstd[i2][:, 0:n]), reads=[rstd[i2]], writes=[rstd[i2]])
                    fw.op("dve", lambda e, qi=qi, i2=i2, t0=t0, n=n, gi=gi: e.scalar_tensor_tensor(out=qn[i2][:, 0:n], in0=qi[:, t0:t0 + n], scalar=self.qkgT[:, l, gi:gi + 1],
                                                                                          in1=rstd[i2][:, 0:n], op0=ALU.mult, op1=ALU.mult),
                          reads=[qi, self.qkgT, rstd[i2]], writes=[qn[i2]])
                    ps2 = self.bank()
                    fw.op("pe", lambda e, ps2=ps2, i2=i2, n=n: e.matmul(ps2[:, 0:n], self.rot[:, :], qn[i2][:, 0:n], start=True, stop=True), reads=[self.rot, qn[i2]], writes=[ps2])
                    fw.op("pool", lambda e, i2=i2, t0=t0, n=n: e.tensor_tensor(out=t1[i2][:, 0:n], in0=qn[i2][:, 0:n], in1=cos[:, t0:t0 + n], op=ALU.mult),
                          reads=[qn[i2], cos], writes=[t1[i2]])
                    fw.op("dve", lambda e, ps2=ps2, i2=i2, t0=t0, n=n: e.tensor_tensor(out=t2[i2][:, 0:n], in0=ps2[:, 0:n], in1=sin[:, t0:t0 + n], op=ALU.mult),
                          reads=[ps2, sin], writes=[t2[i2]])
                    fw.op("dve", lambda e, qo=qo, i2=i2, t0=t0, n=n: e.tensor_tensor(out=qo[:, t0:t0 + n], in0=t1[i2][:, 0:n], in1=t2[i2][:, 0:n], op=ALU.add),
                          reads=[t1[i2], t2[i2]], writes=[qo])
                fw.dma("sp", self.QK[oc][:, :], qo[:, :], reads=[qo], writes=[self.QK[oc]], key=f"qk_st{ci % 2}")

    def attend(self, qh, kh, vaug, lsep, accs, qblocks, pbufs, finish, sbanks=None, LA=2, G=2):
        fw = self.fw
        sbanks = sbanks or self.psb[0:3]
        assert len(sbanks) >= (LA + 1) * G and len(pbufs) >= (LA + 2) * G
        for (t0, n, ic) in qblocks:
            kts = list(range(2)) if ic else list(range(NT))
            NK = len(kts)
            pend = []
            for g0 in range(0, NK, G):
                grp = []
                for idx in range(g0, min(g0 + G, NK)):
                    self.rs = (getattr(self, "rs", 0) + 1) % len(sbanks)
                    self.rp = (getattr(self, "rp", 0) + 1) % len(pbufs)
                    grp.append((idx, kts[idx], sbanks[self.rs], pbufs[self.rp]))
                fw._deps("pe", [kh, qh], [x[2] for x in reversed(grp)])
                for (idx, kt, ps, pb) in grp:
                    fw.op("pe", lambda e: e.matmul(ps[:, 0:n], kh[0:64, kt * 128:(kt + 1) * 128], qh[0:64, t0:t0 + n], start=True, stop=True),
                          reads=[kh, qh], writes=[ps])
                for (idx, kt, ps, pb) in grp:
                    fw.op("act", lambda e: e.activation(out=pb[:, 0:n], in_=ps[:, 0:n], func=AF.Exp, scale=0.125), reads=[ps], writes=[pb])
                pend.append(grp)
                if len(pend) > LA:
                    self._pvg(pend.pop(0), accs, vaug, lsep, n, NK)
            while pend:
                self._pvg(pend.pop(0), accs, vaug, lsep, n, NK)
            finish(t0, n, ic)

    def _pvg(self, grp, accs, vaug, lsep, n, NK):
        fw = self.fw
        fw._deps("pe", [x[3] for x in reversed(grp)] + [vaug], [])
        for (idx, kt, ps, pb) in grp:
            first, last = idx == 0, idx == NK - 1
            fw.op("pe", lambda e: e.matmul(accs[0][:, 0:n], vaug[:, kt, :], pb[:, 0:n], start=first, stop=last), reads=[vaug, pb], writes=[accs[0]], sig=(last and not lsep))
            if lsep:
                fw.op("pe", lambda e: e.matmul(accs[1][:, 0:n], self.ones[:, :], pb[:, 0:n], start=first, stop=last), reads=[self.ones, pb], writes=[accs[1]], sig=last)

    def bank_s(self):
        self.rs = (getattr(self, "rs", 0) + 1) % 3
        return self.psb[self.rs]

    def qblocks(self):
        return BLK if self.need_ctx else BLK[1:]

    def ph_gqa(self):
        fw, l = self.fw, self.l
        with fw.scope():
            qh = [fw.sb([64, T], BF16, f"gq{i}") for i in range(2)]
            kh = [fw.sb([64, T], BF16, f"gk{i}") for i in range(2)]
            vaug = [fw.sb([128, NT, 128], BF16, f"gv{i}") for i in range(2)]
            pbufs = [fw.sb([128, 512], BF16, f"gp{i}") for i in range(8)]
            rl = fw.sb([64, 512], F32, "grl")
            ob = [fw.sb([64, 512], BF16, f"gob{i}") for i in range(2)]
            for kv in range(2):
                fw.op("pool", lambda e, kv=kv: e.memset(vaug[kv][:, :, 64:128], 1.0), writes=[vaug[kv]])
                fw.dma("sp", vaug[kv][:, :, 0:64], self.VT.t[0].rearrange("(n p) c -> p n c", p=128)[:, :, kv * 64:(kv + 1) * 64],
                       reads=[self.VT], writes=[vaug[kv]], key=f"g_v{kv}")
                fw.dma("sp", kh[kv][:, :], self.QK[4].t[kv * 64:(kv + 1) * 64, :], reads=[self.QK[4]], writes=[kh[kv]], key=f"g_k{kv}")
            acc = self.psb[3]
            cnt = [0]
            for h in range(8):
                q = qh[h % 2]
                fw.dma("sp", q[:, :], self.QK[h // 2].t[(h % 2) * 64:(h % 2) * 64 + 64, :], reads=[self.QK[h // 2]], writes=[q], key=f"g_q{h % 2}")

                def fin(t0, n, ic, h=h):
                    o = ob[cnt[0] % 2]
                    cnt[0] += 1
                    fw.op("dve", lambda e: e.reciprocal(out=rl[0:64, 0:n], in_=acc[64:128, 0:n]), reads=[acc], writes=[rl])
                    fw.op("dve", lambda e: e.tensor_tensor(out=o[0:64, 0:n], in0=acc[0:64, 0:n], in1=rl[0:64, 0:n], op=ALU.mult), reads=[acc, rl], writes=[o])
                    fw.dma("sp", self.BR.t[h // 2, (h % 2) * 64:(h % 2) * 64 + 64, t0:t0 + n], o[0:64, 0:n], reads=[o], writes=[self.BR], key=f"g_o{cnt[0] % 2}")
                self.attend(q, kh[h // 4], vaug[h // 4], False, [acc], self.qblocks(), pbufs, fin,
                            sbanks=[self.psb[0], self.psb[1], self.psb[2], self.psb[4], self.psb[5], self.psb[6]], LA=2, G=2)

    def ph_diff(self):
        fw, l = self.fw, self.l
        with fw.scope():
            qh = [fw.sb([64, T], BF16, f"dq{i}") for i in range(2)]
            kh = [fw.sb([64, T], BF16, f"dk{i}") for i in range(2)]
            vv = [fw.sb([128, NT, 128], BF16, f"dv{i}") for i in range(2)]
            pbufs = [fw.sb([128, 512], BF16, f"dp{i}") for i in range(6)]
            r0 = fw.sb([128, 512], F32, "dr0")
            a0 = fw.sb([128, 512], F32, "da0")
            a1 = fw.sb([128, 512], F32, "da1")
            sq = fw.sb([128, 512], BF16, "dsq")
            rstd = fw.sb([128, 512], F32, "drstd")
            ob = [fw.sb([128, 512], BF16, f"dob{i}") for i in range(2)]
            accs = [self.psb[3], self.psb[4]]
            dsb = [self.psb[0], self.psb[1], self.psb[2], self.psb[5], self.psb[6]]
            cnt = [0]
            for h in range(4):
                v = vv[h % 2]
                fw.dma("sp", v[:, :, :], self.VT.t[1 + h].rearrange("(n p) c -> p n c", p=128), reads=[self.VT], writes=[v], key=f"d_v{h % 2}")
                for (t0, n, ic) in self.qblocks():
                    for i in range(2):
                        m = h * 2 + i
                        q, k = qh[i], kh[i]
                        if t0 == self.qblocks()[0][0]:
                            fw.dma("sp", q[:, :], self.QK[6 + m // 2].t[(m % 2) * 64:(m % 2) * 64 + 64, :], reads=[self.QK[6 + m // 2]], writes=[q], key=f"d_q{i}")
                            fw.dma("sp", k[:, :], self.QK[10 + m // 2].t[(m % 2) * 64:(m % 2) * 64 + 64, :], reads=[self.QK[10 + m // 2]], writes=[k], key=f"d_k{i}")
                        self.attend(q, k, v, True, accs, [(t0, n, ic)], pbufs, lambda *a: None, sbanks=dsb, LA=1, G=2)
                        ai = a0 if i == 0 else a1
                        fw.op("dve", lambda e: e.reciprocal(out=r0[:, 0:n], in_=accs[1][:, 0:n]), reads=[accs[1]], writes=[r0])
                        fw.op("dve", lambda e: e.tensor_tensor(out=ai[:, 0:n], in0=accs[0][:, 0:n], in1=r0[:, 0:n], op=ALU.mult), reads=[accs[0], r0], writes=[ai])
                    o = ob[cnt[0] % 2]
                    cnt[0] += 1
                    fw.op("dve", lambda e: e.scalar_tensor_tensor(out=a0[:, 0:n], in0=a1[:, 0:n], scalar=self.neglam[:, l:l + 1], in1=a0[:, 0:n], op0=ALU.mult, op1=ALU.add),
                          reads=[a1, a0, self.neglam], writes=[a0])
                    fw.op("act", lambda e: e.activation(out=sq[:, 0:n], in_=a0[:, 0:n], func=AF.Square), reads=[a0], writes=[sq])
                    ps = self.psb[0]
                    fw.op("pe", lambda e: e.matmul(ps[:, 0:n], self.ones[:, :], sq[:, 0:n], start=True, stop=True), reads=[self.ones, sq], writes=[ps])
                    fw.op("act", lambda e: e.activation(out=rstd[:, 0:n], in_=ps[:, 0:n], func=AF.Sqrt, bias=self.eps[:, 0:1], scale=1.0 / 128), reads=[ps, self.eps], writes=[rstd])
                    fw.op("dve", lambda e: e.reciprocal(out=rstd[:, 0:n], in_=rstd[:, 0:n]), reads=[rstd], writes=[rstd])
                    fw.op("dve", lambda e: e.scalar_tensor_tensor(out=o[:, 0:n], in0=a0[:, 0:n], scalar=self.subg[:, l:l + 1], in1=rstd[:, 0:n], op0=ALU.mult, op1=ALU.mult),
                          reads=[a0, self.subg, rstd], writes=[o])
                    fw.dma("sp", self.BR.t[4 + h, :, t0:t0 + n], o[:, 0:n], reads=[o], writes=[self.BR], key=f"d_o{cnt[0] % 2}")

    def ph_conv(self):
        fw, l = self.fw, self.l
        with fw.scope():
            upad = fw.sb([128, 4, ULEN], BF16, "upad")
            upo = fw.sb([128, 4, ULEN], BF16, "upo")
            for (a_, b_) in ((0, UOFF[0]), (UOFF[0] + 256, UOFF[1]), (UOFF[1] + 4096, ULEN)):
                fw.op("dve", lambda e: e.memset(upad[:, :, a_:b_], 0.0), writes=[upad])
                fw.op("dve", lambda e: e.memset(upo[:, :, max(a_ - 1, 0):b_ - 1 if b_ < ULEN else ULEN], 0.0), writes=[upo])
            diag = fw.sb([128, 4, 31, 128], BF16, "diag")
            ab = [fw.sb([128, T], BF16, f"ca{i}") for i in range(2)]
            gb = [fw.sb([128, T], BF16, f"cg{i}") for i in range(2)]
            for cc in range(4):
                a, g = ab[cc % 2], gb[cc % 2]
                fw.dma("sp", a[:, :], self.PT[18 + cc][:, :], reads=[self.PT[18 + cc]], writes=[a], key=f"c_a{cc % 2}")
                fw.dma("sp", g[:, :], self.PT[22 + cc][:, :], reads=[self.PT[22 + cc]], writes=[g], key=f"c_g{cc % 2}")
                fw.op("act", lambda e, g=g: e.activation(out=g[:, :], in_=g[:, :], func=AF.Sigmoid), reads=[g], writes=[g])
                fw.op("dve", lambda e, a=a, g=g, cc=cc: e.tensor_tensor(out=upad[:, cc, UOFF[0]:UOFF[0] + 256], in0=a[:, 0:256], in1=g[:, 0:256], op=ALU.mult), reads=[a, g], writes=[upad])
                fw.op("dve", lambda e, a=a, g=g, cc=cc: e.tensor_tensor(out=upad[:, cc, UOFF[1]:UOFF[1] + 4096], in0=a[:, 256:T], in1=g[:, 256:T], op=ALU.mult), reads=[a, g], writes=[upad])
                fw.op("pool", lambda e, a=a, g=g, cc=cc: e.tensor_tensor(out=upo[:, cc, UOFF[0] - 1:UOFF[0] - 1 + 256], in0=a[:, 0:256], in1=g[:, 0:256], op=ALU.mult), reads=[a, g], writes=[upo])
                fw.op("pool", lambda e, a=a, g=g, cc=cc: e.tensor_tensor(out=upo[:, cc, UOFF[1] - 1:UOFF[1] - 1 + 4096], in0=a[:, 256:T], in1=g[:, 256:T], op=ALU.mult), reads=[a, g], writes=[upo])
                for j in range(31):
                    fw.op("dve", lambda e, cc=cc, j=j: e.tensor_scalar(out=diag[:, cc, j, :], in0=self.identf[:, :], scalar1=self.convwT[:, l, cc, j:j + 1], scalar2=None, op0=ALU.mult),
                          reads=[self.identf, self.convwT], writes=[diag])
            import os as _os
            cut = int(_os.environ.get("CONV_CUT", "99"))
            if cut <= 0:
                return
            y32 = fw.sb([128, 4, 512], F32, "cy32")
            ybf = fw.sb([128, 4, 512], BF16, "cybf")
            mean = fw.sb([128, 512], F32, "cmean")
            sq = fw.sb([128, 4, 512], BF16, "csq")
            rstd = fw.sb([128, 512], F32, "crstd")
            ob = [fw.sb([128, 4, 512], BF16, f"cob{i}") for i in range(2)]
            for bi, (t0, n, ic) in enumerate(BLK):
                u0 = (UOFF[0] + t0 - UPAD) if ic else (UOFF[1] + (t0 - 256) - UPAD)
                for cc in range(4):
                    ps = self.bank()
                    for j in range(31):
                        usrc, uo = (upad, u0 + j) if (u0 + j) % 2 == 0 else (upo, u0 + j - 1)
                        fw.op("pe", lambda e, ps=ps, cc=cc, j=j: e.matmul(ps[:, 0:n], diag[:, cc, j, :], usrc[:, cc, uo:uo + n], start=(j == 0), stop=(j == 30)),
                              reads=[diag, usrc], writes=[ps], sig=(j == 30))
                    fw.op("act", lambda e, ps=ps, cc=cc: e.activation(out=y32[:, cc, 0:n], in_=ps[:, 0:n], func=AF.Identity, bias=self.convbT[:, l, cc:cc + 1], scale=1.0),
                          reads=[ps, self.convbT], writes=[y32])
                    fw.op("dve", lambda e, ps=ps, cc=cc: e.tensor_scalar(out=ybf[:, cc, 0:n], in0=ps[:, 0:n], scalar1=self.convbT[:, l, cc:cc + 1], scalar2=None, op0=ALU.add),
                          reads=[ps, self.convbT], writes=[ybf])
                if cut <= 1:
                    continue
                pm = self.bank()
                for cc in range(4):
                    fw.op("pe", lambda e, cc=cc: e.matmul(pm[:, 0:n], self.ones[:, :], ybf[:, cc, 0:n], start=(cc == 0), stop=(cc == 3)), reads=[self.ones, ybf], writes=[pm], sig=(cc == 3))
                fw.op("act", lambda e: e.activation(out=mean[:, 0:n], in_=pm[:, 0:n], func=AF.Copy, scale=1.0 / 512), reads=[pm], writes=[mean])
                for cc in range(4):
                    fw.op("dve", lambda e, cc=cc: e.tensor_tensor(out=y32[:, cc, 0:n], in0=y32[:, cc, 0:n], in1=mean[:, 0:n], op=ALU.subtract), reads=[y32, mean], writes=[y32])
                fw.op("act", lambda e: e.activation(out=sq[:, :, 0:n], in_=y32[:, :, 0:n], func=AF.Square), reads=[y32], writes=[sq])
                pv = self.bank()
                for cc in range(4):
                    fw.op("pe", lambda e, cc=cc: e.matmul(pv[:, 0:n], self.ones[:, :], sq[:, cc, 0:n], start=(cc == 0), stop=(cc == 3)), reads=[self.ones, sq], writes=[pv], sig=(cc == 3))
                fw.op("act", lambda e: e.activation(out=rstd[:, 0:n], in_=pv[:, 0:n], func=AF.Sqrt, bias=self.eps[:, 0:1], scale=1.0 / 512), reads=[pv, self.eps], writes=[rstd])
                fw.op("dve", lambda e: e.reciprocal(out=rstd[:, 0:n], in_=rstd[:, 0:n]), reads=[rstd], writes=[rstd])
                if cut <= 2:
                    continue
                o = ob[bi % 2]
                for cc in range(4):
                    fw.op("dve", lambda e, cc=cc: e.scalar_tensor_tensor(out=y32[:, cc, 0:n], in0=y32[:, cc, 0:n], scalar=self.clngT[:, l, cc:cc + 1], in1=rstd[:, 0:n], op0=ALU.mult, op1=ALU.mult),
                          reads=[y32, self.clngT, rstd], writes=[y32])
                    fw.op("act", lambda e, cc=cc: e.activation(out=o[:, cc, 0:n], in_=y32[:, cc, 0:n], func=AF.Silu, bias=self.clnbT[:, l, cc:cc + 1], scale=1.0),
                          reads=[y32, self.clnbT], writes=[o])
                fw.dma("sp", self.BR.t[8:12, :, t0:t0 + n].rearrange("c p t -> p c t"), o[:, :, 0:n], reads=[o], writes=[self.BR], key=f"c_o{bi % 2}")

    def ph_hgrn(self):
        fw, l = self.fw, self.l
        NCH = T // 64
        for d in range(2):
            for hp in range(4):
                with fw.scope():
                    vtok = fw.sb([128, NT, 128], BF16, "hv")
                    fw.dma("sp", vtok[:, :, :], self.VT.t[5 + hp].rearrange("(n p) c -> p n c", p=128), reads=[self.VT], writes=[vtok], key="h_v")
                    mask = fw.sb([128, 256], F32, "hmask")
                    mname = "mask_f" if d == 0 else "mask_b"
                    fw.dma("sp", mask[:, :], self.CI[mname][:, 0:256], reads=[self.CI[mname]], writes=[mask], key="h_m")
                    maski = mask.t[:, :].bitcast(mybir.dt.int32)
                    qT = [fw.sb([64, T], BF16, f"hq{i}") for i in range(2)]
                    qpT = [fw.sb([64, T], BF16, f"hqp{i}") for i in range(2)]
                    kT = [fw.sb([64, T], BF16, f"hk{i}") for i in range(2)]
                    dec2 = fw.sb([64, 2, NCH], F32, "hdec2")
                    ktok = fw.sb([128, NT, 128], BF16, "hkt")
                    dec = fw.sb([128, NCH], F32, "hdec")
                    with fw.scope():
                        onesf = fw.sb([128, 64], F32, "h1")
                        fw.op("pool", lambda e: e.memset(onesf[:, :], 1.0), writes=[onesf])
                        zin = fw.sb([128, T], BF16, "hz")
                        qin = fw.sb([128, T], BF16, "hqi")
                        qs = fw.sb([128, T], F32, "hqs")
                        f = fw.sb([128, T], F32, "hf")
                        lf = fw.sb([128, T], F32, "hlf")
                        cum = fw.sb([128, T], F32, "hcum")
                        kk = fw.sb([128, T], F32, "hkk")
                        ex = f
                        khT = fw.sb([128, T], BF16, "hkh")
                        c3 = cum.t.rearrange("p (c s) -> p c s", s=64)
                        e3 = ex.t.rearrange("p (c s) -> p c s", s=64)
                        iend, imid = (63, 31) if d == 0 else (0, 32)
                        fw.dma("sp", qin[:, :], self.PT[26 + hp][:, :], reads=[self.PT[26 + hp]], writes=[qin], key="h_qi")
                        fw.dma("sp", zin[:, :], self.PT[30 + 4 * d + hp][:, :], reads=[self.PT[30 + 4 * d + hp]], writes=[zin], key="h_zi")
                        fw.op("act", lambda e: e.activation(out=qs[:, :], in_=qin[:, :], func=AF.Silu), reads=[qin], writes=[qs])
                        fw.op("act", lambda e: e.activation(out=f[:, :], in_=zin[:, :], func=AF.Sigmoid), reads=[zin], writes=[f])
                        li = d * 4 + hp
                        fw.op("dve", lambda e: e.tensor_scalar(out=f[:, :], in0=f[:, :], scalar1=self.omlT[:, l, li:li + 1], scalar2=self.lbT[:, l, li:li + 1], op0=ALU.mult, op1=ALU.add),
                              reads=[f, self.omlT, self.lbT], writes=[f])
                        fw.op("act", lambda e: e.activation(out=lf[:, :], in_=f[:, :], func=AF.Ln), reads=[f], writes=[lf])
                        fw.op("pool", lambda e: e.tensor_scalar(out=kk[:, :], in0=f[:, :], scalar1=-1.0, scalar2=1.0, op0=ALU.mult, op1=ALU.add), reads=[f], writes=[kk])
                        for c in range(NCH):
                            fw.op("dve", lambda e: e.tensor_tensor_scan(out=cum[:, c * 64:(c + 1) * 64], data0=onesf[:, :], data1=lf[:, c * 64:(c + 1) * 64], initial=0.0,
                                                                       op0=ALU.mult, op1=ALU.add), reads=[onesf, lf], writes=[cum], sig=(c == NCH - 1))
                        if d == 1:
                            fw.op("dve", lambda e: e.tensor_tensor(out=e3, in0=c3[:, :, 63:64].broadcast_to([128, NCH, 64]), in1=c3, op=ALU.subtract), reads=[cum], writes=[ex])
                            fw.op("dve", lambda e: e.tensor_tensor(out=cum[:, :], in0=ex[:, :], in1=lf[:, :], op=ALU.add), reads=[ex, lf], writes=[cum])
                        fw.op("act", lambda e: e.activation(out=dec[:, :], in_=c3[:, :, iend], func=AF.Exp), reads=[cum], writes=[dec])
                        for hh in range(2):
                            fw.op("dve", lambda e: e.tensor_copy(out=dec2[0:64, hh, :], in_=dec[hh * 64:(hh + 1) * 64, :]), reads=[dec], writes=[dec2])
                        fw.op("act", lambda e: e.activation(out=ex[:, :], in_=cum[:, :], func=AF.Exp), reads=[cum], writes=[ex])
                        for hh in range(2):
                            fw.op("dve", lambda e: e.scalar_tensor_tensor(out=qpT[hh][0:64, :], in0=qs[hh * 64:(hh + 1) * 64, :], scalar=0.125, in1=ex[hh * 64:(hh + 1) * 64, :], op0=ALU.mult, op1=ALU.mult),
                                  reads=[qs, ex], writes=[qpT[hh]])
                        fw.op("dve", lambda e: e.tensor_tensor(out=e3, in0=c3[:, :, iend:iend + 1].broadcast_to([128, NCH, 64]), in1=c3, op=ALU.subtract), reads=[cum], writes=[ex])
                        fw.op("act", lambda e: e.activation(out=ex[:, :], in_=ex[:, :], func=AF.Exp), reads=[ex], writes=[ex])
                        fw.op("dve", lambda e: e.tensor_tensor(out=khT[:, :], in0=kk[:, :], in1=ex[:, :], op=ALU.mult), reads=[kk, ex], writes=[khT])
                        fw.op("dve", lambda e: e.tensor_tensor(out=e3, in0=c3, in1=c3[:, :, imid:imid + 1].broadcast_to([128, NCH, 64]), op=ALU.subtract), reads=[cum], writes=[ex])
                        fw.op("act", lambda e: e.activation(out=lf[:, :], in_=ex[:, :], func=AF.Exp), reads=[ex], writes=[lf])
                        for hh in range(2):
                            fw.op("dve", lambda e: e.scalar_tensor_tensor(out=qT[hh][0:64, :], in0=qs[hh * 64:(hh + 1) * 64, :], scalar=0.125, in1=lf[hh * 64:(hh + 1) * 64, :], op0=ALU.mult, op1=ALU.mult),
                                  reads=[qs, lf], writes=[qT[hh]])
                        fw.op("act", lambda e: e.activation(out=lf[:, :], in_=ex[:, :], func=AF.Exp, scale=-1.0), reads=[ex], writes=[lf])
                        for hh in range(2):
                            fw.op("dve", lambda e: e.tensor_tensor(out=kT[hh][0:64, :], in0=kk[hh * 64:(hh + 1) * 64, :], in1=lf[hh * 64:(hh + 1) * 64, :], op=ALU.mult), reads=[kk, lf], writes=[kT[hh]])
                        import os as _os
                        hcut = int(_os.environ.get("HG_CUT", "99"))
                        for tt in range(NT if hcut > 1 else 0):
                            fw.op("pe", lambda e: e.transpose(self.psT[:, (tt % 8) * 128:(tt % 8) * 128 + 128], khT[:, tt * 128:(tt + 1) * 128], self.identb[:, :]),
                                  reads=[khT, self.identb], writes=[self.psT], sig=(tt % 8 == 7 or tt == NT - 1))
                            if tt % 8 == 7 or tt == NT - 1:
                                t8 = tt - (tt % 8)
                                nn = tt - t8 + 1
                                fw.op("act", lambda e: e.activation(out=ktok[:, t8:t8 + nn, :], in_=self.psT.t[:, 0:nn * 128].rearrange("p (a c) -> p a c", c=128), func=AF.Copy),
                                      reads=[self.psT], writes=[ktok])
                    S = fw.sb([64, 2, 64], F32, "hS")
                    Sb = [fw.sb([64, 2, 64], BF16, f"hSb{i}") for i in range(2)]
                    fw.op("dve", lambda e: e.memset(S[:, :, :], 0.0), writes=[S])
                    fw.op("dve", lambda e: e.memset(Sb[0][:, :, :], 0.0), writes=[Sb[0]])
                    attm = [fw.sb([128, 256], BF16, f"hatt{i}") for i in range(2)]
                    for a_ in attm:
                        fw.op("dve", lambda e: e.memset(a_[:, :], 0.0), writes=[a_])
                    ost = fw.sb([64, 2, T], F32, "host")
                    tiles = list(range(NT)) if d == 0 else [1, 0] + list(range(NT - 1, 1, -1))
                    if hcut <= 2:
                        tiles = []
                    sbi = 0
                    for ti, tt in enumerate(tiles):
                        am = attm[ti % 2]
                        pa = self.psb[1 + ti % 2]
                        for hh in range(2):
                            r0 = hh * 64
                            fw.op("pe", lambda e: e.matmul(pa[:, hh * 128:hh * 128 + 128], kT[hh][0:64, tt * 128:(tt + 1) * 128], qT[hh][0:64, tt * 128:(tt + 1) * 128], start=True, stop=True),
                                  reads=[kT[hh], qT[hh]], writes=[pa], sig=(hh == 1))
                        fw.op("dve", lambda e: e.copy_predicated(out=am[:, :], mask=maski, data=pa[:, 0:256]), reads=[pa, mask], writes=[am])
                        if hcut <= 3:
                            continue
                        po = self.psb[3 + ti % 2]
                        chunks = [0, 1] if d == 0 else [1, 0]
                        if hcut <= 4:
                            chunks = []
                        for hh in range(2):
                            fw.op("pe", lambda e: e.matmul(po[0:64, hh * 128:hh * 128 + 128], vtok[:, tt, hh * 64:(hh + 1) * 64], am[:, hh * 128:(hh + 1) * 128],
                                                           start=(hh == 0), stop=False, skip_group_check=True), reads=[vtok, am], writes=[po], sig=False)
                        for ci, cj in enumerate(chunks):
                            c = tt * 2 + cj
                            sb_cur = Sb[sbi % 2]
                            sb_nxt = Sb[(sbi + 1) % 2]
                            sbi += 1
                            for hh in range(2):
                                r0 = hh * 64
                                fw.op("pe", lambda e: e.matmul(po[0:64, hh * 128 + cj * 64:hh * 128 + cj * 64 + 64], sb_cur[0:64, hh, :], qpT[hh][0:64, c * 64:(c + 1) * 64],
                                                               start=False, stop=(ci == 1), skip_group_check=True), reads=[sb_cur, qpT[hh]], writes=[po], sig=(hh == 1))
                            pS = self.psb[5 + sbi % 2]
                            for hh in range(2):
                                fw.op("pe", lambda e: e.matmul(pS[0:64, hh * 64:(hh + 1) * 64], ktok[cj * 64:cj * 64 + 64, tt, hh * 64:(hh + 1) * 64], vtok[cj * 64:cj * 64 + 64, tt, hh * 64:(hh + 1) * 64],
                                                               start=(hh == 0), stop=(hh == 1), skip_group_check=True), reads=[ktok, vtok], writes=[pS], sig=(hh == 1))
                            for hh in range(2):
                                fw.op("dve", lambda e: e.scalar_tensor_tensor(out=S[0:64, hh, :], in0=S[0:64, hh, :], scalar=dec2[0:64, hh, c:c + 1], in1=pS[0:64, hh * 64:(hh + 1) * 64], op0=ALU.mult, op1=ALU.add),
                                      reads=[S, dec2, pS], writes=[S])
                            fw.op("act", lambda e: e.activation(out=sb_nxt[:, :, :], in_=S[:, :, :], func=AF.Copy), reads=[S], writes=[sb_nxt])
                        fw.op("act", lambda e: e.activation(out=ost[:, :, tt * 128:(tt + 1) * 128], in_=po.t[0:64, 0:256].rearrange("p (a c) -> p a c", c=128), func=AF.Copy),
                              reads=[po], writes=[ost])
                    fw.dma("sp", self.OD[d].t[2 * hp:2 * hp + 2].rearrange("h e t -> e h t"), ost[:, :, :], reads=[ost], writes=[self.OD[d]], key="h_o")
        with fw.scope():
            of = [fw.sb([64, T], F32, f"hof{i}") for i in range(2)]
            obw = [fw.sb([64, T], F32, f"hobw{i}") for i in range(2)]
            gt = [fw.sb([64, T], BF16, f"hgt{i}") for i in range(2)]
            sq = fw.sb([64, 512], BF16, "hsq")
            rstd = fw.sb([64, 512], F32, "hrstd")
            sg = fw.sb([64, 512], F32, "hsg")
            ob = [fw.sb([64, T], BF16, f"hob{i}") for i in range(2)]
            for h in range(8):
                a, b, g, o = of[h % 2], obw[h % 2], gt[h % 2], ob[h % 2]
                fw.dma("sp", a[:, :], self.OD[0].t[h], reads=[self.OD[0]], writes=[a], key=f"hn_a{h % 2}")
                fw.dma("sp", b[:, :], self.OD[1].t[h], reads=[self.OD[1]], writes=[b], key=f"hn_b{h % 2}")
                fw.dma("sp", g[:, :], self.PT[42 + h // 2].t[(h % 2) * 64:(h % 2) * 64 + 64, :], reads=[self.PT[42 + h // 2]], writes=[g], key=f"hn_g{h % 2}")
                fw.op("pool", lambda e, a=a, b=b: e.tensor_tensor(out=a[:, :], in0=a[:, :], in1=b[:, :], op=ALU.add), reads=[a, b], writes=[a])
                for (t0, n, ic) in BLK:
                    fw.op("act", lambda e, a=a: e.activation(out=sq[:, 0:n], in_=a[:, t0:t0 + n], func=AF.Square), reads=[a], writes=[sq])
                    ps = self.bank()
                    fw.op("pe", lambda e, ps=ps: e.matmul(ps[0:64, 0:n], self.ones[0:64, 0:64], sq[0:64, 0:n], start=True, stop=True), reads=[self.ones, sq], writes=[ps])
                    fw.op("act", lambda e, ps=ps: e.activation(out=rstd[:, 0:n], in_=ps[0:64, 0:n], func=AF.Sqrt, bias=self.eps[0:64, 0:1], scale=1.0 / 64), reads=[ps, self.eps], writes=[rstd])
                    fw.op("dve", lambda e: e.reciprocal(out=rstd[:, 0:n], in_=rstd[:, 0:n]), reads=[rstd], writes=[rstd])
                    fw.op("act", lambda e, g=g: e.activation(out=sg[:, 0:n], in_=g[:, t0:t0 + n], func=AF.Silu), reads=[g], writes=[sg])
                    fw.op("dve", lambda e: e.scalar_tensor_tensor(out=rstd[:, 0:n], in0=rstd[:, 0:n], scalar=self.hgT[:, l:l + 1], in1=sg[:, 0:n], op0=ALU.mult, op1=ALU.mult),
                          reads=[rstd, self.hgT, sg], writes=[rstd])
                    fw.op("dve", lambda e, a=a, o=o: e.tensor_tensor(out=o[:, t0:t0 + n], in0=a[:, t0:t0 + n], in1=rstd[:, 0:n], op=ALU.mult), reads=[a, rstd], writes=[o])
                fw.dma("sp", self.BR.t[12 + h // 2, (h % 2) * 64:(h % 2) * 64 + 64, :], o[:, :], reads=[o], writes=[self.BR], key=f"hn_o{h % 2}")

    def ph_merge(self):
        fw, l = self.fw, self.l
        with fw.scope():
            wb = fw.sb([128, 16, D], BF16, "wbr")
            wo = fw.sb([128, 8, D], BF16, "wout")
            for i in range(4):
                fw.dma("pool", wb[:, i * 4:(i + 1) * 4, :], self.I["w_branch"].t[l, i].rearrange("(k p) c -> p k c", p=128), reads=[self.I["w_branch"]], writes=[wb], key="m_wb")
            fw.dma("pool", wo[:, :, :], self.I["w_out"].t[l].rearrange("(k p) c -> p k c", p=128), reads=[self.I["w_out"]], writes=[wo], key="m_wo")
            brs = [fw.sb([128, 16, 512], BF16, f"mbr{i}") for i in range(1)]
            gts = [fw.sb([128, 32, 512], BF16, f"mgt{i}") for i in range(1)]
            xbs = [fw.sb([128, 8, 512], F32, f"mxb{i}") for i in range(1)]
            mg = fw.sb([128, 512], F32, "mmg")
            tmp = fw.sb([128, 512], F32, "mtmp")
            mgb = fw.sb([128, 8, 512], BF16, "mmgb")
            xo = [fw.sb([128, 8, 512], F32, f"mxo{i}") for i in range(2)]
            src = self.xsrc()
            for bi, (t0, n, ic) in enumerate(self.qblocks()):
                br, gt, xb, xn = brs[0], gts[0], xbs[0], xo[bi % 2]
                fw.dma("sp", br[:, :, 0:n], self.BR.t[:, :, t0:t0 + n].rearrange("c p t -> p c t"), reads=[self.BR], writes=[br], key="m_br")
                fw.dma("sp", gt[:, :, 0:n], self.PTall.t[:, :, t0:t0 + n].rearrange("c p t -> p c t"), reads=[self.PTall], writes=[gt], key="m_gt")
                fw.dma("sp", xb[:, :, 0:n], src.t.rearrange("(k p) t -> p k t", p=128)[:, :, t0:t0 + n], reads=[src], writes=[xb], key="m_xb")
                for oc in range(8):
                    for i in range(4):
                        ps = self.bank()
                        for k in range(4):
                            fw.op("pe", lambda e, ps=ps, i=i, k=k, oc=oc: e.matmul(ps[:, 0:n], wb[:, i * 4 + k, oc * 128:(oc + 1) * 128], br[:, i * 4 + k, 0:n], start=(k == 0), stop=(k == 3)),
                                  reads=[wb, br], writes=[ps], sig=(k == 3))
                        if i == 0:
                            fw.op("dve", lambda e, ps=ps, oc=oc: e.tensor_tensor(out=mg[:, 0:n], in0=ps[:, 0:n], in1=gt[:, oc, 0:n], op=ALU.mult), reads=[ps, gt], writes=[mg])
                        else:
                            fw.op("dve", lambda e, ps=ps, oc=oc, i=i: e.tensor_tensor(out=tmp[:, 0:n], in0=ps[:, 0:n], in1=gt[:, i * 8 + oc, 0:n], op=ALU.mult), reads=[ps, gt], writes=[tmp])
                            if i < 3:
                                fw.op("pool", lambda e: e.tensor_tensor(out=mg[:, 0:n], in0=mg[:, 0:n], in1=tmp[:, 0:n], op=ALU.add), reads=[mg, tmp], writes=[mg])
                            else:
                                fw.op("pool", lambda e, oc=oc: e.tensor_tensor(out=mgb[:, oc, 0:n], in0=mg[:, 0:n], in1=tmp[:, 0:n], op=ALU.add), reads=[mg, tmp], writes=[mgb])
                for o2 in range(8):
                    ps = self.bank()
                    for k in range(8):
                        fw.op("pe", lambda e, ps=ps, k=k, o2=o2: e.matmul(ps[:, 0:n], wo[:, k, o2 * 128:(o2 + 1) * 128], mgb[:, k, 0:n], start=(k == 0), stop=(k == 7)),
                              reads=[wo, mgb], writes=[ps], sig=(k == 7))
                    fw.op("dve", lambda e, ps=ps, o2=o2: e.scalar_tensor_tensor(out=xn[:, o2, 0:n], in0=ps[:, 0:n], scalar=self.modt[:, 16 + o2, ic:ic + 1], in1=xb[:, o2, 0:n],
                                                                         op0=ALU.mult, op1=ALU.add), reads=[ps, self.modt, xb], writes=[xn])
                fw.dma("sp", self.xs.t.rearrange("(k p) t -> p k t", p=128)[:, :, t0:t0 + n], xn[:, :, 0:n], reads=[xn], writes=[self.xs], key=f"m_xo{bi % 2}")
        self.x_written = True

    def ph_ffn(self):
        fw, l = self.fw, self.l
        moe = (l % 2 == 1)
        li = l // 2
        JG = 512
        NJG = DFF // JG
        blocks = self.qblocks()
        halves = [blocks[:len(blocks) - 4], blocks[len(blocks) - 4:]]
        for half in halves:
            if not half:
                continue
            hb0 = half[0][0]
            hlen = sum(b[1] for b in half)
            with fw.scope():
                hT = fw.sb([128, 8, hlen], BF16, "fh")
                with fw.scope():
                    xbs = [fw.sb([128, 8, 512], F32, f"fxb{i}") for i in range(2)]
                    sq = fw.sb([128, 8, 512], BF16, "fsq")
                    rstd = fw.sb([128, 512], F32, "frstd")
                    tmp = fw.sb([128, 8, 512], F32, "ftmp")
                    h32 = fw.sb([128, 8, 512], F32, "fh32") if moe else None
                    if moe:
                        lg = fw.sb([128, 8], F32, "flg")
                        m1 = fw.sb([128, 1], F32, "fm1")
                        m2 = fw.sb([128, 1], F32, "fm2")
                        k1 = fw.sb([128, 8], F32, "fk1")
                        k2 = fw.sb([128, 8], F32, "fk2")
                        l2 = fw.sb([128, 8], F32, "fl2")
                        w1 = fw.sb([128, 1], F32, "fw1")
                        w2 = fw.sb([128, 1], F32, "fw2")
                        cmb = fw.sb([128, 8], F32, "fcmb")
                        cbm = fw.sb([128, 8, 128], F32, "fcbm")
                        cbo = [fw.sb([128, 8, 512], BF16, f"fcbo{i}") for i in range(2)]
                    for bi, (t0, n, ic) in enumerate(half):
                        self.norm_block(1, t0, n, ic, hT, t0 - hb0, xbs[bi % 2], sq, rstd, tmp, h32)
                        if not moe:
                            continue
                        co = cbo[bi % 2]
                        for ti in range(n // 128):
                            ps = self.bank()
                            for k in range(8):
                                fw.op("pe", lambda e, ps=ps, k=k, ti=ti: e.matmul(ps[:, 0:8], h32[:, k, ti * 128:(ti + 1) * 128], self.routerT[:, li, k, :], start=(k == 0), stop=(k == 7)),
                                      reads=[h32, self.routerT], writes=[ps], sig=(k == 7))
                            fw.op("dve", lambda e, ps=ps: e.tensor_copy(out=lg[:, :], in_=ps[:, 0:8]), reads=[ps], writes=[lg])
                            fw.op("dve", lambda e: e.reduce_max(out=m1[:, :], in_=lg[:, :], axis=AX.X), reads=[lg], writes=[m1])
                            fw.op("dve", lambda e: e.tensor_scalar(out=k1[:, :], in0=lg[:, :], scalar1=m1[:, 0:1], scalar2=None, op0=ALU.is_ge), reads=[lg, m1], writes=[k1])
                            fw.op("dve", lambda e: e.scalar_tensor_tensor(out=l2[:, :], in0=k1[:, :], scalar=-1e30, in1=lg[:, :], op0=ALU.mult, op1=ALU.add), reads=[k1, lg], writes=[l2])
                            fw.op("dve", lambda e: e.reduce_max(out=m2[:, :], in_=l2[:, :], axis=AX.X), reads=[l2], writes=[m2])
                            fw.op("dve", lambda e: e.tensor_scalar(out=k2[:, :], in0=l2[:, :], scalar1=m2[:, 0:1], scalar2=None, op0=ALU.is_ge), reads=[l2, m2], writes=[k2])
                            fw.op("dve", lambda e: e.tensor_tensor(out=w2[:, :], in0=m2[:, :], in1=m1[:, :], op=ALU.subtract), reads=[m1, m2], writes=[w2])
                            fw.op("act", lambda e: e.activation(out=w2[:, :], in_=w2[:, :], func=AF.Exp), reads=[w2], writes=[w2])
                            fw.op("dve", lambda e: e.tensor_scalar(out=w1[:, :], in0=w2[:, :], scalar1=1.0, scalar2=None, op0=ALU.add), reads=[w2], writes=[w1])
                            fw.op("dve", lambda e: e.reciprocal(out=w1[:, :], in_=w1[:, :]), reads=[w1], writes=[w1])
                            fw.op("dve", lambda e: e.tensor_scalar(out=w2[:, :], in0=w1[:, :], scalar1=-1.0, scalar2=1.0, op0=ALU.mult, op1=ALU.add), reads=[w1], writes=[w2])
                            fw.op("dve", lambda e: e.tensor_scalar(out=cmb[:, :], in0=k1[:, :], scalar1=w1[:, 0:1], scalar2=None, op0=ALU.mult), reads=[k1, w1], writes=[cmb])
                            fw.op("dve", lambda e: e.scalar_tensor_tensor(out=cmb[:, :], in0=k2[:, :], scalar=w2[:, 0:1], in1=cmb[:, :], op0=ALU.mult, op1=ALU.add), reads=[k2, w2, cmb], writes=[cmb])
                            fw.op("dve", lambda e: e.tensor_copy(out=cbm[:, :, :], in_=cmb.t[:, :].unsqueeze(2).broadcast_to([128, 8, 128])), reads=[cmb], writes=[cbm])
                            for eh in range(2):
                                pc = self.bank()
                                for e4 in range(4):
                                    ex = eh * 4 + e4
                                    fw.op("pe", lambda e, pc=pc, e4=e4, ex=ex: e.matmul(pc[:, e4 * 128:(e4 + 1) * 128], cbm[:, ex, :], self.identf[:, :], start=True, stop=True),
                                          reads=[cbm, self.identf], writes=[pc], sig=(e4 == 3))
                                fw.op("act", lambda e, pc=pc, eh=eh, ti=ti, co=co: e.activation(out=co[:, eh * 4:eh * 4 + 4, ti * 128:(ti + 1) * 128],
                                                                                     in_=pc.t.rearrange("p (a c) -> p a c", c=128), func=AF.Copy), reads=[pc], writes=[co])
                        fw.dma("sp", self.CB.t[:, :, t0:t0 + n].rearrange("e p t -> p e t"), co[:, :, 0:n], reads=[co], writes=[self.CB], key=f"f_cb{bi % 2}")
                acc = fw.sb([128, 8, hlen], F32, "facc")
                self._ffn_experts(moe, li, half, hb0, hT, acc)
                self._ffn_resid(half, hb0, acc)

    def _ffn_experts(self, moe, li, half, hb0, hT, acc):
        fw, l = self.fw, self.l
        JG = 512
        NJG = DFF // JG
        with fw.scope():
            if True:
                w1s = [fw.sb([128, 8, JG], BF16, f"fw1_{i}") for i in range(2)]
                w3s = [fw.sb([128, 8, JG], BF16, f"fw3_{i}") for i in range(2)]
                w2s = [fw.sb([128, JG // 128, D], BF16, f"fw2_{i}") for i in range(2)]
                sa = [fw.sb([128, 512], BF16, f"fsa{i}") for i in range(2)]
                gb = [fw.sb([128, JG // 128, 512], BF16, f"fgb{i}") for i in range(2)]
                cbl = [fw.sb([128, 512], BF16, f"fcl{i}") for i in range(2)]
                ng = 0
                nb = 0
                first_acc = True
                for ex in range(NEXP if moe else 1):
                    for jg in range(NJG):
                        wa, wc, wd = w1s[ng % 2], w3s[ng % 2], w2s[ng % 2]
                        if moe:
                            s1, s3, s2 = self.I["moe_w1"].t[li, ex], self.I["moe_w3"].t[li, ex], self.I["moe_w2"].t[li, ex]
                            r1, r3, r2 = self.I["moe_w1"], self.I["moe_w3"], self.I["moe_w2"]
                        else:
                            s1, s3, s2 = self.I["ffn_w1"].t[li], self.I["ffn_w3"].t[li], self.I["ffn_w2"].t[li]
                            r1, r3, r2 = self.I["ffn_w1"], self.I["ffn_w3"], self.I["ffn_w2"]
                        fw.dma("pool", wa[:, :, :], s1.rearrange("(k p) c -> p k c", p=128)[:, :, jg * JG:(jg + 1) * JG], reads=[r1], writes=[wa], key=f"f_w1{ng % 2}")
                        fw.dma("pool", wc[:, :, :], s3.rearrange("(k p) c -> p k c", p=128)[:, :, jg * JG:(jg + 1) * JG], reads=[r3], writes=[wc], key=f"f_w3{ng % 2}")
                        fw.dma("pool", wd[:, :, :], s2[jg * JG:(jg + 1) * JG, :].rearrange("(k p) c -> p k c", p=128), reads=[r2], writes=[wd], key=f"f_w2{ng % 2}")
                        ng += 1
                        for (t0, n, ic) in half:
                            c0 = t0 - hb0
                            g = gb[nb % 2]
                            cl = cbl[nb % 2]
                            nb += 1
                            if moe:
                                fw.dma("sp", cl[:, 0:n], self.CB.t[ex, :, t0:t0 + n], reads=[self.CB], writes=[cl], key=f"f_cl{nb % 2}")
                            for jc in range(JG // 128):
                                p1 = self.psb[1 + (jc % 2) * 2]
                                p3 = self.psb[2 + (jc % 2) * 2]
                                for k in range(8):
                                    fw.op("pe", lambda e, p1=p1, k=k, jc=jc: e.matmul(p1[:, 0:n], wa[:, k, jc * 128:(jc + 1) * 128], hT[:, k, c0:c0 + n], start=(k == 0), stop=(k == 7)),
                                          reads=[wa, hT], writes=[p1], sig=(k == 7))
                                for k in range(8):
                                    fw.op("pe", lambda e, p3=p3, k=k, jc=jc: e.matmul(p3[:, 0:n], wc[:, k, jc * 128:(jc + 1) * 128], hT[:, k, c0:c0 + n], start=(k == 0), stop=(k == 7)),
                                          reads=[wc, hT], writes=[p3], sig=(k == 7))
                                s = sa[jc % 2]
                                fw.op("act", lambda e, p1=p1, s=s: e.activation(out=s[:, 0:n], in_=p1[:, 0:n], func=AF.Silu), reads=[p1], writes=[s])
                                fw.op("dve", lambda e, p3=p3, s=s, g=g, jc=jc: e.tensor_tensor(out=g[:, jc, 0:n], in0=p3[:, 0:n], in1=s[:, 0:n], op=ALU.mult), reads=[p3, s], writes=[g])
                                if moe:
                                    fw.op("pool", lambda e, g=g, jc=jc, cl=cl: e.tensor_tensor(out=g[:, jc, 0:n], in0=g[:, jc, 0:n], in1=cl[:, 0:n], op=ALU.mult), reads=[g, cl], writes=[g])
                            for o in range(8):
                                pf = self.psb[5 + (o % 2)]
                                for jc in range(JG // 128):
                                    fw.op("pe", lambda e, pf=pf, jc=jc, o=o, g=g: e.matmul(pf[:, 0:n], wd[:, jc, o * 128:(o + 1) * 128], g[:, jc, 0:n], start=(jc == 0), stop=(jc == JG // 128 - 1)),
                                          reads=[wd, g], writes=[pf], sig=(jc == JG // 128 - 1))
                                if first_acc:
                                    fw.op("act", lambda e, pf=pf, o=o: e.activation(out=acc[:, o, c0:c0 + n], in_=pf[:, 0:n], func=AF.Copy), reads=[pf], writes=[acc])
                                else:
                                    fw.op("dve", lambda e, pf=pf, o=o: e.tensor_tensor(out=acc[:, o, c0:c0 + n], in0=acc[:, o, c0:c0 + n], in1=pf[:, 0:n], op=ALU.add), reads=[pf, acc], writes=[acc])
                        first_acc = False

    def _ffn_resid(self, half, hb0, acc):
        fw, l = self.fw, self.l
        with fw.scope():
            if True:
                xbs = [fw.sb([128, 8, 512], F32, f"fxr{i}") for i in range(2)]
                for bi, (t0, n, ic) in enumerate(half):
                    c0 = t0 - hb0
                    xb = xbs[bi % 2]
                    fw.dma("sp", xb[:, :, 0:n], self.xs.t.rearrange("(k p) t -> p k t", p=128)[:, :, t0:t0 + n], reads=[self.xs], writes=[xb], key=f"f_xr{bi % 2}")
                    for k in range(8):
                        fw.op("dve", lambda e, k=k: e.scalar_tensor_tensor(out=xb[:, k, 0:n], in0=acc[:, k, c0:c0 + n], scalar=self.modt[:, 40 + k, ic:ic + 1], in1=xb[:, k, 0:n],
                                                                          op0=ALU.mult, op1=ALU.add), reads=[acc, self.modt, xb], writes=[xb])
                    fw.dma("sp", self.xs.t.rearrange("(k p) t -> p k t", p=128)[:, :, t0:t0 + n], xb[:, :, 0:n], reads=[xb], writes=[self.xs], key=f"f_xw{bi % 2}")


def host_inputs(inp, b):
    f = np.float32
    def colT(v):
        v = np.asarray(v, f)
        sh = v.shape
        v = v.reshape(sh[:-1] + (sh[-1] // 128, 128))
        return np.ascontiguousarray(np.moveaxis(v, -1, 0))
    m = {}
    m["xT0"] = np.ascontiguousarray(np.concatenate([inp["ctx"][b], inp["x"][b]], axis=0).T.astype(f))
    m["cvec"] = np.ascontiguousarray(np.stack([colT(inp["c"][b]), colT(inp["c_ctx"])], axis=-1))
    m["ada_w"] = np.asarray(inp["ada_w"], f)
    m["ada_bT"] = colT(inp["ada_b"])
    m["g1T"] = colT(inp["norm1_g"])
    m["g2T"] = colT(inp["norm2_g"])
    m["w_in"] = np.asarray(inp["w_in"], f)
    qkg = np.asarray(inp["qk_norm_g"], f)
    m["qkgT"] = np.ascontiguousarray(np.tile(np.transpose(qkg, (2, 0, 1)), (2, 1, 1)))
    m["lamB"] = np.ascontiguousarray(np.broadcast_to(np.asarray(inp["diff_lambda"], f).reshape(1, DEPTH, 256), (128, DEPTH, 256)))
    m["sublnT"] = np.ascontiguousarray(np.asarray(inp["diff_subln_g"], f).T)
    cw = np.asarray(inp["conv_w"], f)
    m["convwT"] = np.ascontiguousarray(np.transpose(cw.reshape(DEPTH, 31, 4, 128), (3, 0, 2, 1)))
    m["convbT"] = colT(inp["conv_b"])
    m["clngT"] = colT(inp["conv_ln_g"])
    m["clnbT"] = colT(inp["conv_ln_b"])
    m["lblT"] = colT(inp["hgrn_lb_logits"])
    m["hgT"] = np.ascontiguousarray(np.asarray(inp["hgrn_norm_g"], f).T)
    m["w_branch"] = np.asarray(inp["w_branch"], f)
    m["w_out"] = np.asarray(inp["w_out"], f)
    for k in ("ffn_w1", "ffn_w3", "ffn_w2", "moe_w1", "moe_w3", "moe_w2"):
        m[k] = np.asarray(inp[k], f)
    r = np.asarray(inp["moe_router"], f)
    m["routerT"] = np.ascontiguousarray(np.transpose(r.reshape(2, 8, 128, 8), (2, 0, 1, 3)))
    for k, v in _const_tables().items():
        m["c_" + k] = v
    return m


_CACHE = {}


def kernel(**inputs):
    if "nc" not in _CACHE:
        _CACHE["nc"] = MK().build()
    nc = _CACHE["nc"]
    in_maps = [host_inputs(inputs, b) for b in range(8)]
    res = run_bass_kernel_spmd(nc, in_maps, core_ids=list(range(8)))
    out = np.stack([np.ascontiguousarray(res.results[b]["xs"][:, NCTX:].T) for b in range(8)], axis=0)
    return out.astype(np.float32)
```

```python
import math
from contextlib import ExitStack, contextmanager
import numpy as np
import ml_dtypes
import concourse.bass as bass
import concourse.mybir as mybir
from concourse.bass_utils import run_bass_kernel_spmd

F32 = mybir.dt.float32
BF16 = mybir.dt.bfloat16
AF = mybir.ActivationFunctionType
ALU = mybir.AluOpType
AX = mybir.AxisListType

D = 1024
SEQ = 4096
NCTX = 256
T = SEQ + NCTX
NT = T // 128
DEPTH = 4
D_IN = 9984
NOC = D_IN // 128
DFF = 3584
NEXP = 8
EPS = 1e-6
BLK = [(0, 256, 1)] + [(256 + 512 * i, 512, 0) for i in range(8)]
VCH = {5: 0, 14: 1, 15: 2, 16: 3, 17: 4, 38: 5, 39: 6, 40: 7, 41: 8}
QKCH = [0, 1, 2, 3, 4, 6, 7, 8, 9, 10, 11, 12, 13]
UPAD = 15
ULEN = UPAD + 256 + UPAD + UPAD + 4096 + UPAD
UOFF = [UPAD, UPAD + 256 + 2 * UPAD]


class Buf:
    __slots__ = ("t", "name", "key", "w", "r", "psum")

    def __init__(self, t, name, key=None):
        self.psum = False
        self.t = t
        self.name = name
        self.key = key or name
        self.w = None
        self.r = {}

    def __getitem__(self, idx):
        return self.t[idx]


class FW:
    def __init__(self, nc, root):
        self.nc = nc
        self.root = root
        self.stack = root
        self.engs = {"pe": nc.tensor, "act": nc.scalar, "dve": nc.vector, "pool": nc.gpsimd, "sp": nc.sync}
        self.sem = {k: root.enter_context(nc.semaphore("s_" + k)) for k in self.engs}
        self.cnt = {k: 0 for k in self.engs}
        self.seen = {k: {} for k in self.engs}
        self.dsem = {}
        self.nbuf = 0
        self.ninst = 0

    @contextmanager
    def scope(self):
        old = self.stack
        with ExitStack() as s:
            self.stack = s
            try:
                yield
            finally:
                self.barrier()
                self.stack = old

    def sb(self, shape, dt, name=None):
        self.nbuf += 1
        key = name or 'sb'
        name = f"{key}_{self.nbuf}"
        t = self.stack.enter_context(self.nc.sbuf_tensor(name, list(shape), dt))
        return Buf(t, name, key)

    def ps(self, shape, dt=F32, name=None):
        self.nbuf += 1
        name = f"{name or 'ps'}_{self.nbuf}"
        t = self.root.enter_context(self.nc.psum_tensor(name, list(shape), dt))
        b = Buf(t, name)
        b.psum = True
        return b

    def dram(self, name, shape, dt, kind="Internal"):
        t = self.nc.dram_tensor(name, list(shape), dt, kind=kind)
        return Buf(t.ap(), name)

    def _semof(self, key):
        return self.sem[key] if key in self.sem else self.dsem[key][0]

    def _wait(self, E, ev):
        if ev is None:
            return
        key, val = ev
        if key not in self.sem:
            val = 16 * self.dsem[key][1]
        if key == "pe" and E == "pe":
            return
        if key == E and val > self.cnt[E]:
            return
        if self.seen[E].get(key, 0) >= val:
            return
        self.seen[E][key] = val
        self.engs[E].wait_ge(self._semof(key), val)
        self.ninst += 1

    def _deps(self, E, reads, writes):
        for b in reads:
            self._wait(E, b.w)
            if b.psum:
                for k, v in b.r.items():
                    if k != E:
                        self._wait(E, (k, v))
        for b in writes:
            self._wait(E, b.w)
            for k, v in b.r.items():
                self._wait(E, (k, v))

    def _record(self, ev, reads, writes):
        k, v = ev
        for b in reads:
            if b.r.get(k, 0) < v:
                b.r[k] = v
        for b in writes:
            b.w = ev
            b.r = {}

    def op(self, E, fn, reads=(), writes=(), sig=True):
        self._deps(E, reads, writes)
        ins = fn(self.engs[E])
        self.ninst += 1
        if sig:
            self.cnt[E] += 1
            ins.then_inc(self.sem[E], 1)
            ev = (E, self.cnt[E])
        else:
            ev = (E, self.cnt[E] + 1)
        self._record(ev, reads, writes)
        return ins

    def dma(self, Q, out, in_, reads=(), writes=(), key=None, **kw):
        self._deps(Q, reads, writes)
        if key is None:
            key = "d_" + (writes[0].key if writes else reads[0].key)
        if key not in self.dsem:
            s = self.root.enter_context(self.nc.semaphore("q_" + str(len(self.dsem))))
            self.dsem[key] = [s, 0]
        ent = self.dsem[key]
        ent[1] += 1
        ins = self.engs[Q].dma_start(out=out, in_=in_, **kw)
        ins.then_inc(ent[0], 16)
        self.ninst += 1
        self._record((key, 16 * ent[1]), reads, writes)
        return ins

    def barrier(self, engines=("pe", "act", "dve", "pool", "sp")):
        for E in engines:
            for k in ("pe", "act", "dve", "pool", "sp"):
                if k != E and self.cnt[k]:
                    self._wait(E, (k, self.cnt[k]))
            for key, (s, c) in self.dsem.items():
                if c:
                    self._wait(E, (key, 16 * c))


def _const_tables():
    c = {}
    c["ones"] = np.ones((128, 128), np.float32)
    bd = np.zeros((128, 128), np.float32)
    bd[:64, :64] = 1
    bd[64:, 64:] = 1
    c["bd64"] = bd
    c["ident"] = np.eye(128, dtype=np.float32)
    R = np.zeros((128, 128), np.float32)
    for p in range(128):
        d = p % 64
        j = d % 32
        if j < 16:
            R[p + 16, p] = -1.0
        else:
            R[p - 16, p] = 1.0
    c["rot"] = R
    inv_freq = (10000.0 ** (-np.arange(0, 32, 2, dtype=np.float32) / 32)).astype(np.float32)
    tl = np.arange(SEQ)
    row = (tl // 64).astype(np.float32)
    col = (tl % 64).astype(np.float32)
    cos = np.ones((128, T), np.float32)
    sin = np.zeros((128, T), np.float32)
    for p in range(128):
        d = p % 64
        pos = row if d < 32 else col
        f = inv_freq[(d % 32) % 16]
        ang = (pos * f).astype(np.float32)
        cos[p, NCTX:] = np.cos(ang)
        sin[p, NCTX:] = np.sin(ang)
    c["cos"] = cos
    c["sin"] = sin
    s = np.arange(128)[:, None]
    t = np.arange(128)[None, :]
    same = (s // 64) == (t // 64)
    c["mask_f"] = np.tile((same & (s <= t)).astype(np.float32), (1, 4))
    c["mask_b"] = np.tile((same & (s >= t)).astype(np.float32), (1, 4))
    return c


CONST_SPECS = [("ones", [128, 128]), ("bd64", [128, 128]), ("ident", [128, 128]), ("rot", [128, 128]),
               ("cos", [128, T]), ("sin", [128, T]), ("mask_f", [128, 512]), ("mask_b", [128, 512])]

IN_SPECS = [
    ("xT0", [D, T]), ("cvec", [128, 8, 2]), ("ada_w", [DEPTH, D, 6 * D]), ("ada_bT", [128, DEPTH, 48]),
    ("g1T", [128, DEPTH, 8]), ("g2T", [128, DEPTH, 8]), ("w_in", [DEPTH, D, D_IN]),
    ("qkgT", [128, DEPTH, 4]), ("lamB", [128, DEPTH, 256]), ("sublnT", [128, DEPTH]),
    ("convwT", [128, DEPTH, 4, 31]), ("convbT", [128, DEPTH, 4]), ("clngT", [128, DEPTH, 4]),
    ("clnbT", [128, DEPTH, 4]), ("lblT", [128, DEPTH, 2, 4]), ("hgT", [64, DEPTH]),
    ("w_branch", [DEPTH, 4, 512, D]), ("w_out", [DEPTH, D, D]),
    ("ffn_w1", [2, D, DFF]), ("ffn_w3", [2, D, DFF]), ("ffn_w2", [2, DFF, D]),
    ("routerT", [128, 2, 8, 8]), ("moe_w1", [2, NEXP, D, DFF]), ("moe_w3", [2, NEXP, D, DFF]),
    ("moe_w2", [2, NEXP, DFF, D]),
]


class MK:
    def __init__(self, n_layers=DEPTH, debug=(), stop_after=None, ext_in=()):
        self.n_layers = n_layers
        self.debug = set(debug)
        self.ext_in = set(ext_in)
        self.stop_after = stop_after
        self.nc = bass.Bass("TRN2", target_bir_lowering=False)
        self.rr = 0

    def scratch(self, name, shape, dt):
        kind = "Internal"
        if name in self.debug:
            kind = "ExternalOutput"
        if name in self.ext_in:
            kind = "ExternalInput"
        return self.fw.dram(name, shape, dt, kind=kind)

    def build(self):
        nc = self.nc
        with ExitStack() as root:
            fw = self.fw = FW(nc, root)
            big = ("ada_w", "w_in", "w_branch", "w_out", "ffn_w1", "ffn_w3", "ffn_w2", "moe_w1", "moe_w3", "moe_w2")
            tiny = getattr(self, "tiny", False)
            self.I = {n: fw.dram(n, ([1] * len(s) if (tiny and n in big) else s), F32, kind="ExternalInput") for n, s in IN_SPECS}
            self.CI = {n: fw.dram("c_" + n, s, F32, kind="ExternalInput") for n, s in CONST_SPECS}
            self.xs = fw.dram("xs", [D, T], F32, kind="ExternalOutput")
            self.PT = [self.scratch(f"PT{i}", [128, T], BF16) for i in range(NOC)]
            self.PTall = self.scratch("PTg", [32, 128, T], BF16)
            self.VT = self.scratch("VT", [9, T, 128], BF16)
            self.QK = {oc: self.scratch(f"QK{oc}", [128, T], BF16) for oc in QKCH}
            self.BR = self.scratch("BR", [16, 128, T], BF16)
            self.OD = [self.scratch(f"OD{d}", [8, 64, T], F32) for d in range(2)]
            self.CB = self.scratch("CB", [NEXP, 128, T], BF16)
            self.psb = [fw.ps([128, 512], F32, f"bank{i}") for i in range(7)]
            self.psT = fw.ps([128, 1024], BF16, "bankT")
            self.ones = fw.sb([128, 128], BF16, "ones")
            self.bd64 = fw.sb([128, 128], BF16, "bd64")
            self.identb = fw.sb([128, 128], BF16, "identb")
            self.identf = fw.sb([128, 128], F32, "identf")
            self.rot = fw.sb([128, 128], BF16, "rot")
            for nm, b in (("ones", self.ones), ("bd64", self.bd64), ("ident", self.identb), ("rot", self.rot)):
                fw.dma("pool", b[:, :], self.CI[nm][:, :], reads=[self.CI[nm]], writes=[b])
            fw.dma("sp", self.identf[:, :], self.CI["ident"][:, :], reads=[self.CI["ident"]], writes=[self.identf])
            self.eps = fw.sb([128, 1], F32, "eps")
            fw.op("dve", lambda e: e.memset(self.eps[:, :], EPS), writes=[self.eps])
            self.small_params()
            self.modt = fw.sb([128, 48, 2], F32, "mod")
            self.gs = fw.sb([128, 2, 8, 2], F32, "gs")
            for l in (getattr(self, "layers", None) or range(self.n_layers)):
                self.layer(l)
                if self.stop_after is not None and self.stop_after[0] == l and self.done:
                    break
            fw.barrier(engines=("sp",))
        return nc

    def small_params(self):
        fw, I = self.fw, self.I
        def ld(name, shape):
            b = fw.sb(shape, F32, name)
            src = I[name]
            fw.dma("sp", b.t[tuple(slice(None) for _ in shape)], src.t[tuple(slice(None) for _ in shape)], reads=[src], writes=[b])
            return b
        self.cvec = ld("cvec", [128, 8, 2])
        self.ada_bT = ld("ada_bT", [128, DEPTH, 48])
        self.g1T = ld("g1T", [128, DEPTH, 8])
        self.g2T = ld("g2T", [128, DEPTH, 8])
        self.qkgT = ld("qkgT", [128, DEPTH, 4])
        self.lamB = ld("lamB", [128, DEPTH, 256])
        self.sublnT = ld("sublnT", [128, DEPTH])
        self.convwT = ld("convwT", [128, DEPTH, 4, 31])
        self.convbT = ld("convbT", [128, DEPTH, 4])
        self.clngT = ld("clngT", [128, DEPTH, 4])
        self.clnbT = ld("clnbT", [128, DEPTH, 4])
        self.lblT = ld("lblT", [128, DEPTH, 2, 4])
        self.hgT = ld("hgT", [64, DEPTH])
        self.routerT = ld("routerT", [128, 2, 8, 8])
        self.siluc = fw.sb([128, 8, 2], BF16, "siluc")
        fw.op("act", lambda e: e.activation(out=self.siluc[:, :, :], in_=self.cvec[:, :, :], func=AF.Silu),
              reads=[self.cvec], writes=[self.siluc])
        ex = fw.sb([128, DEPTH, 8], F32, "lbex")
        fw.op("act", lambda e: e.activation(out=ex[:, :, :], in_=self.lblT.t.rearrange("p l d c -> p l (d c)"), func=AF.Exp),
              reads=[self.lblT], writes=[ex])
        ssum = fw.sb([128, 8], F32, "lbsum")
        fw.op("dve", lambda e: e.tensor_tensor(out=ssum[:, :], in0=ex[:, 0, :], in1=ex[:, 1, :], op=ALU.add), reads=[ex], writes=[ssum])
        for l in range(2, DEPTH):
            fw.op("dve", lambda e, l=l: e.tensor_tensor(out=ssum[:, :], in0=ssum[:, :], in1=ex[:, l, :], op=ALU.add), reads=[ex, ssum], writes=[ssum])
        fw.op("dve", lambda e: e.reciprocal(out=ssum[:, :], in_=ssum[:, :]), reads=[ssum], writes=[ssum])
        self.lbT = fw.sb([128, DEPTH, 8], F32, "lbT")
        self.omlT = fw.sb([128, DEPTH, 8], F32, "omlT")
        fw.op("dve", lambda e: e.memset(self.lbT[:, 0, :], 0.0), writes=[self.lbT])
        for l in range(1, DEPTH):
            fw.op("dve", lambda e, l=l: e.tensor_tensor(out=ex[:, l, :], in0=ex[:, l, :], in1=ssum[:, :], op=ALU.mult), reads=[ex, ssum], writes=[ex])
            fw.op("dve", lambda e, l=l: e.tensor_tensor(out=self.lbT[:, l, :], in0=self.lbT[:, l - 1, :], in1=ex[:, l, :], op=ALU.add),
                  reads=[ex, self.lbT], writes=[self.lbT])
        fw.op("dve", lambda e: e.tensor_scalar(out=self.omlT[:, :, :], in0=self.lbT[:, :, :], scalar1=-1.0, scalar2=1.0, op0=ALU.mult, op1=ALU.add),
              reads=[self.lbT], writes=[self.omlT])
        self.neglam = fw.sb([128, DEPTH], F32, "neglam")
        self.subg = fw.sb([128, DEPTH], F32, "subg")
        pr = fw.sb([128, DEPTH, 2, 64], F32, "lampr")
        s12 = fw.sb([128, DEPTH, 2], F32, "lams")
        lam4 = self.lamB.t.rearrange("p l (i d) -> p l i d", i=4)
        for l in range(DEPTH):
            for j in range(2):
                fw.op("dve", lambda e, l=l, j=j: e.tensor_tensor(out=pr[:, l, j, :], in0=lam4[:, l, 2 * j, :], in1=lam4[:, l, 2 * j + 1, :], op=ALU.mult),
                      reads=[self.lamB], writes=[pr])
                fw.op("dve", lambda e, l=l, j=j: e.reduce_sum(out=s12[:, l, j:j + 1], in_=pr[:, l, j, :], axis=AX.X), reads=[pr], writes=[s12])
        fw.op("act", lambda e: e.activation(out=s12[:, :, :], in_=s12[:, :, :], func=AF.Exp), reads=[s12], writes=[s12])
        for l in range(DEPTH):
            li = 0.8 - 0.6 * math.exp(-0.3 * l)
            fw.op("dve", lambda e, l=l, li=li: e.scalar_tensor_tensor(out=self.neglam[:, l:l + 1], in0=s12[:, l, 1:2], scalar=-li, in1=s12[:, l, 0:1],
                                                                     op0=ALU.add, op1=ALU.subtract), reads=[s12], writes=[self.neglam])
            fw.op("dve", lambda e, l=l, li=li: e.tensor_scalar(out=self.subg[:, l:l + 1], in0=self.sublnT[:, l:l + 1], scalar1=1.0 - li, scalar2=None, op0=ALU.mult),
                  reads=[self.sublnT], writes=[self.subg])

    def bank(self):
        self.rr = (self.rr + 1) % 7
        return self.psb[self.rr]

    def layer(self, l):
        self.done = False
        need_ctx = l < DEPTH - 1
        self.l = l
        self.need_ctx = need_ctx
        steps = [self.ph_mod, self.ph_inproj, self.ph_qk, self.ph_gqa, self.ph_diff, self.ph_conv, self.ph_hgrn,
                 self.ph_merge, self.ph_ffn]
        for i, s in enumerate(steps):
            if getattr(self, "only", None) is not None and i not in self.only:
                continue
            s()
            if self.stop_after is not None and self.stop_after == (l, i):
                self.done = True
                return

    def xsrc(self):
        return self.I["xT0"] if (self.l == 0 and not self.x_written) else self.xs

    def ph_mod(self):
        fw, l = self.fw, self.l
        self.x_written = (l > 0)
        with fw.scope():
            ps = self.psb[0]
            wts = [fw.sb([128, 8, 512], BF16, f"adaw{i}") for i in range(2)]
            aw = self.I["ada_w"].t[l].rearrange("(k p) c -> p k c", p=128)
            for g in range(12):
                w = wts[g % 2]
                fw.dma("pool", w[:, :, :], aw[:, :, g * 512:(g + 1) * 512], reads=[self.I["ada_w"]], writes=[w], key=f"adaw{g % 2}")
                for cc in range(4):
                    j = g * 4 + cc
                    for k in range(8):
                        fw.op("pe", lambda e, w=w, cc=cc, k=k, j=j: e.matmul(ps[:, 2 * j:2 * j + 2], w[:, k, cc * 128:(cc + 1) * 128], self.siluc[:, k, :],
                                                                       start=(k == 0), stop=(k == 7)),
                              reads=[w, self.siluc], writes=[ps], sig=(k == 7))
            mod = self.modt
            for i in range(2):
                fw.op("dve", lambda e, i=i: e.tensor_tensor(out=mod[:, :, i], in0=ps.t[:, 0:96].rearrange("p (j i) -> p j i", i=2)[:, :, i],
                                                            in1=self.ada_bT[:, l, :], op=ALU.add), reads=[ps, self.ada_bT], writes=[mod])
            for s, (gT, c0) in enumerate(((self.g1T, 8), (self.g2T, 32))):
                for i in range(2):
                    fw.op("dve", lambda e, s=s, gT=gT, c0=c0, i=i: e.scalar_tensor_tensor(out=self.gs[:, s, :, i], in0=mod[:, c0:c0 + 8, i], scalar=1.0, in1=gT[:, l, :],
                                                                                     op0=ALU.add, op1=ALU.mult), reads=[mod, gT], writes=[self.gs])

    def norm_block(self, s, t0, n, ic, hT, hcol, xb, sq, rstd, tmp, h32=None):
        fw = self.fw
        src = self.xsrc()
        shc = 0 if s == 0 else 24
        fw.dma("sp", xb[:, :, 0:n], src.t.rearrange("(k p) t -> p k t", p=128)[:, :, t0:t0 + n], reads=[src], writes=[xb])
        fw.op("act", lambda e: e.activation(out=sq[:, :, 0:n], in_=xb[:, :, 0:n], func=AF.Square), reads=[xb], writes=[sq])
        ps = self.bank()
        for k in range(8):
            fw.op("pe", lambda e, k=k: e.matmul(ps[:, 0:n], self.ones[:, :], sq[:, k, 0:n], start=(k == 0), stop=(k == 7)),
                  reads=[self.ones, sq], writes=[ps], sig=(k == 7))
        fw.op("act", lambda e: e.activation(out=rstd[:, 0:n], in_=ps[:, 0:n], func=AF.Sqrt, bias=self.eps[:, 0:1], scale=1.0 / D), reads=[ps, self.eps], writes=[rstd])
        fw.op("dve", lambda e: e.reciprocal(out=rstd[:, 0:n], in_=rstd[:, 0:n]), reads=[rstd], writes=[rstd])
        for k in range(8):
            fw.op("dve", lambda e, k=k: e.scalar_tensor_tensor(out=tmp[:, k, 0:n], in0=xb[:, k, 0:n], scalar=self.gs[:, s, k, ic:ic + 1], in1=rstd[:, 0:n],
                                                              op0=ALU.mult, op1=ALU.mult), reads=[xb, self.gs, rstd], writes=[tmp])
            fw.op("act", lambda e, k=k: e.activation(out=hT[:, k, hcol:hcol + n], in_=tmp[:, k, 0:n], func=AF.Identity, bias=self.modt[:, shc + k, ic:ic + 1], scale=1.0),
                  reads=[tmp, self.modt], writes=[hT])
            if h32 is not None:
                fw.op("pool", lambda e, k=k: e.tensor_scalar(out=h32[:, k, 0:n], in0=tmp[:, k, 0:n], scalar1=self.modt[:, shc + k, ic:ic + 1], scalar2=None, op0=ALU.add),
                      reads=[tmp, self.modt], writes=[h32])

    def ph_inproj(self):
        fw, l = self.fw, self.l
        with fw.scope():
            hT = fw.sb([128, 8, T], BF16, "hT")
            with fw.scope():
                xbs = [fw.sb([128, 8, 512], F32, f"xb{i}") for i in range(2)]
                sq = fw.sb([128, 8, 512], BF16, "sq")
                rstd = fw.sb([128, 512], F32, "rstd")
                tmp = fw.sb([128, 8, 512], F32, "tmp")
                for bi, (t0, n, ic) in enumerate(BLK):
                    self.norm_block(0, t0, n, ic, hT, t0, xbs[bi % 2], sq, rstd, tmp)
            wts = [fw.sb([128, 8, 512], BF16, f"win{i}") for i in range(3)]
            stg = [fw.sb([128, T], BF16, f"stg{i}") for i in range(2)]
            vst = [fw.sb([128, NT, 128], BF16, f"vst{i}") for i in range(2)]
            wv = self.I["w_in"].t[l].rearrange("(k p) c -> p k c", p=128)
            ns = 0
            for g in range(20):
                w = wts[g % 3]
                gc = min(512, D_IN - g * 512)
                fw.dma("pool", w[:, :, 0:gc], wv[:, :, g * 512:g * 512 + gc], reads=[self.I["w_in"]], writes=[w], key=f"win{g % 3}")
                for cc in range(gc // 128):
                    oc = g * 4 + cc
                    if oc in VCH:
                        v = vst[VCH[oc] % 2]
                        for tg in range(0, NT, 4):
                            ps = self.bank()
                            nt_ = min(4, NT - tg)
                            for ti in range(nt_):
                                tt = tg + ti
                                for k in range(8):
                                    fw.op("pe", lambda e, k=k, tt=tt, ti=ti, ps=ps, w=w, cc=cc: e.matmul(
                                        ps[:, ti * 128:(ti + 1) * 128], hT[:, k, tt * 128:(tt + 1) * 128], w[:, k, cc * 128:(cc + 1) * 128],
                                        start=(k == 0), stop=(k == 7)), reads=[hT, w], writes=[ps], sig=(k == 7))
                            fw.op("dve", lambda e, ps=ps, v=v, tg=tg, nt_=nt_: e.tensor_copy(out=v[:, tg:tg + nt_, :], in_=ps.t[:, 0:nt_ * 128].rearrange("p (a c) -> p a c", c=128)),
                                  reads=[ps], writes=[v])
                        fw.dma("sp", self.VT.t[VCH[oc]].rearrange("(n p) c -> p n c", p=128), v[:, :, :], reads=[v], writes=[self.VT], key="vt_st")
                        continue
                    so = stg[ns % 2]
                    ns += 1
                    for bi, (t0, n, ic) in enumerate(BLK):
                        ps = self.bank()
                        for k in range(8):
                            fw.op("pe", lambda e, k=k, ps=ps, w=w, cc=cc, t0=t0, n=n: e.matmul(ps[:, 0:n], w[:, k, cc * 128:(cc + 1) * 128], hT[:, k, t0:t0 + n],
                                                                                   start=(k == 0), stop=(k == 7)), reads=[hT, w], writes=[ps], sig=(k == 7))
                        if oc >= 46:
                            fw.op("act", lambda e, ps=ps, so=so, t0=t0, n=n: e.activation(out=so[:, t0:t0 + n], in_=ps[:, 0:n], func=AF.Sigmoid), reads=[ps], writes=[so])
                        elif bi % 2 == 0:
                            fw.op("dve", lambda e, ps=ps, so=so, t0=t0, n=n: e.tensor_copy(out=so[:, t0:t0 + n], in_=ps[:, 0:n]), reads=[ps], writes=[so])
                        else:
                            fw.op("act", lambda e, ps=ps, so=so, t0=t0, n=n: e.activation(out=so[:, t0:t0 + n], in_=ps[:, 0:n], func=AF.Copy), reads=[ps], writes=[so])
                    if oc >= 46:
                        fw.dma("sp", self.PTall.t[oc - 46], so[:, :], reads=[so], writes=[self.PTall], key="ptg_st")
                    else:
                        fw.dma("sp", self.PT[oc][:, :], so[:, :], reads=[so], writes=[self.PT[oc]], key=f"pt_st{ns % 2}")

    def ph_qk(self):
        fw, l = self.fw, self.l
        with fw.scope():
            cos = fw.sb([128, T], F32, "cos")
            sin = fw.sb([128, T], F32, "sin")
            fw.dma("sp", cos[:, :], self.CI["cos"][:, :], reads=[self.CI["cos"]], writes=[cos])
            fw.dma("sp", sin[:, :], self.CI["sin"][:, :], reads=[self.CI["sin"]], writes=[sin])
            qin = [fw.sb([128, T], BF16, f"qin{i}") for i in range(2)]
            qout = [fw.sb([128, T], BF16, f"qout{i}") for i in range(2)]
            sq = [fw.sb([128, 512], BF16, f"qsq{i}") for i in range(2)]
            rstd = [fw.sb([128, 512], F32, f"qrstd{i}") for i in range(2)]
            qn = [fw.sb([128, 512], BF16, f"qn{i}") for i in range(2)]
            t1 = [fw.sb([128, 512], F32, f"qt1{i}") for i in range(2)]
            t2 = [fw.sb([128, 512], F32, f"qt2{i}") for i in range(2)]
            it = 0
            for ci, oc in enumerate(QKCH):
                gi = 0 if oc < 4 else 1 if oc == 4 else 2 if oc < 10 else 3
                qi, qo = qin[ci % 2], qout[ci % 2]
                fw.dma("sp", qi[:, :], self.PT[oc][:, :], reads=[self.PT[oc]], writes=[qi], key=f"qk_ld{ci % 2}")
                for (t0, n, ic) in BLK:
                    i2 = it % 2
                    it += 1
                    fw.op("act", lambda e, qi=qi, i2=i2, t0=t0, n=n: e.activation(out=sq[i2][:, 0:n], in_=qi[:, t0:t0 + n], func=AF.Square), reads=[qi], writes=[sq[i2]])
                    ps = self.bank()
                    fw.op("pe", lambda e, ps=ps, i2=i2, n=n: e.matmul(ps[:, 0:n], self.bd64[:, :], sq[i2][:, 0:n], start=True, stop=True), reads=[self.bd64, sq[i2]], writes=[ps])
                    fw.op("act", lambda e, ps=ps, i2=i2, n=n: e.activation(out=rstd[i2][:, 0:n], in_=ps[:, 0:n], func=AF.Sqrt, bias=self.eps[:, 0:1], scale=1.0 / 64),
                          reads=[ps, self.eps], writes=[rstd[i2]])
                    fw.op("dve", lambda e, i2=i2, n=n: e.reciprocal(out=rstd[i2][:, 0:n], in_=rstd[i2][:, 0:n]), reads=[rstd[i2]], writes=[rstd[i2]])
                    fw.op("dve", lambda e, qi=qi, i2=i2, t0=t0, n=n, gi=gi: e.scalar_tensor_tensor(out=qn[i2][:, 0:n], in0=qi[:, t0:t0 + n], scalar=self.qkgT[:, l, gi:gi + 1],
                                                                                          in1=rstd[i2][:, 0:n], op0=ALU.mult, op1=ALU.mult),
                          reads=[qi, self.qkgT, rstd[i2]], writes=[qn[i2]])
                    ps2 = self.bank()
                    fw.op("pe", lambda e, ps2=ps2, i2=i2, n=n: e.matmul(ps2[:, 0:n], self.rot[:, :], qn[i2][:, 0:n], start=True, stop=True), reads=[self.rot, qn[i2]], writes=[ps2])
                    fw.op("pool", lambda e, i2=i2, t0=t0, n=n: e.tensor_tensor(out=t1[i2][:, 0:n], in0=qn[i2][:, 0:n], in1=cos[:, t0:t0 + n], op=ALU.mult),
                          reads=[qn[i2], cos], writes=[t1[i2]])
                    fw.op("dve", lambda e, ps2=ps2, i2=i2, t0=t0, n=n: e.tensor_tensor(out=t2[i2][:, 0:n], in0=ps2[:, 0:n], in1=sin[:, t0:t0 + n], op=ALU.mult),
                          reads=[ps2, sin], writes=[t2[i2]])
                    fw.op("dve", lambda e, qo=qo, i2=i2, t0=t0, n=n: e.tensor_tensor(out=qo[:, t0:t0 + n], in0=t1[i2][:, 0:n], in1=t2[i2][:, 0:n], op=ALU.add),
                          reads=[t1[i2], t2[i2]], writes=[qo])
                fw.dma("sp", self.QK[oc][:, :], qo[:, :], reads=[qo], writes=[self.QK[oc]], key=f"qk_st{ci % 2}")

    def attend(self, qh, kh, vaug, lsep, accs, qblocks, pbufs, finish, sbanks=None, LA=2, G=2):
        fw = self.fw
        sbanks = sbanks or self.psb[0:3]
        assert len(sbanks) >= (LA + 1) * G and len(pbufs) >= (LA + 2) * G
        for (t0, n, ic) in qblocks:
            kts = list(range(2)) if ic else list(range(NT))
            NK = len(kts)
            pend = []
            for g0 in range(0, NK, G):
                grp = []
                for idx in range(g0, min(g0 + G, NK)):
                    self.rs = (getattr(self, "rs", 0) + 1) % len(sbanks)
                    self.rp = (getattr(self, "rp", 0) + 1) % len(pbufs)
                    grp.append((idx, kts[idx], sbanks[self.rs], pbufs[self.rp]))
                fw._deps("pe", [kh, qh], [x[2] for x in reversed(grp)])
                for (idx, kt, ps, pb) in grp:
                    fw.op("pe", lambda e: e.matmul(ps[:, 0:n], kh[0:64, kt * 128:(kt + 1) * 128], qh[0:64, t0:t0 + n], start=True, stop=True),
                          reads=[kh, qh], writes=[ps])
                for (idx, kt, ps, pb) in grp:
                    fw.op("act", lambda e: e.activation(out=pb[:, 0:n], in_=ps[:, 0:n], func=AF.Exp, scale=0.125), reads=[ps], writes=[pb])
                pend.append(grp)
                if len(pend) > LA:
                    self._pvg(pend.pop(0), accs, vaug, lsep, n, NK)
            while pend:
                self._pvg(pend.pop(0), accs, vaug, lsep, n, NK)
            finish(t0, n, ic)

    def _pvg(self, grp, accs, vaug, lsep, n, NK):
        fw = self.fw
        fw._deps("pe", [x[3] for x in reversed(grp)] + [vaug], [])
        for (idx, kt, ps, pb) in grp:
            first, last = idx == 0, idx == NK - 1
            fw.op("pe", lambda e: e.matmul(accs[0][:, 0:n], vaug[:, kt, :], pb[:, 0:n], start=first, stop=last), reads=[vaug, pb], writes=[accs[0]], sig=(last and not lsep))
            if lsep:
                fw.op("pe", lambda e: e.matmul(accs[1][:, 0:n], self.ones[:, :], pb[:, 0:n], start=first, stop=last), reads=[self.ones, pb], writes=[accs[1]], sig=last)

    def bank_s(self):
        self.rs = (getattr(self, "rs", 0) + 1) % 3
        return self.psb[self.rs]

    def qblocks(self):
        return BLK if self.need_ctx else BLK[1:]

    def ph_gqa(self):
        fw, l = self.fw, self.l
        with fw.scope():
            qh = [fw.sb([64, T], BF16, f"gq{i}") for i in range(2)]
            kh = [fw.sb([64, T], BF16, f"gk{i}") for i in range(2)]
            vaug = [fw.sb([128, NT, 128], BF16, f"gv{i}") for i in range(2)]
            pbufs = [fw.sb([128, 512], BF16, f"gp{i}") for i in range(8)]
            rl = fw.sb([64, 512], F32, "grl")
            ob = [fw.sb([64, 512], BF16, f"gob{i}") for i in range(2)]
            for kv in range(2):
                fw.op("pool", lambda e, kv=kv: e.memset(vaug[kv][:, :, 64:128], 1.0), writes=[vaug[kv]])
                fw.dma("sp", vaug[kv][:, :, 0:64], self.VT.t[0].rearrange("(n p) c -> p n c", p=128)[:, :, kv * 64:(kv + 1) * 64],
                       reads=[self.VT], writes=[vaug[kv]], key=f"g_v{kv}")
                fw.dma("sp", kh[kv][:, :], self.QK[4].t[kv * 64:(kv + 1) * 64, :], reads=[self.QK[4]], writes=[kh[kv]], key=f"g_k{kv}")
            acc = self.psb[3]
            cnt = [0]
            for h in range(8):
                q = qh[h % 2]
                fw.dma("sp", q[:, :], self.QK[h // 2].t[(h % 2) * 64:(h % 2) * 64 + 64, :], reads=[self.QK[h // 2]], writes=[q], key=f"g_q{h % 2}")

                def fin(t0, n, ic, h=h):
                    o = ob[cnt[0] % 2]
                    cnt[0] += 1
                    fw.op("dve", lambda e: e.reciprocal(out=rl[0:64, 0:n], in_=acc[64:128, 0:n]), reads=[acc], writes=[rl])
                    fw.op("dve", lambda e: e.tensor_tensor(out=o[0:64, 0:n], in0=acc[0:64, 0:n], in1=rl[0:64, 0:n], op=ALU.mult), reads=[acc, rl], writes=[o])
                    fw.dma("sp", self.BR.t[h // 2, (h % 2) * 64:(h % 2) * 64 + 64, t0:t0 + n], o[0:64, 0:n], reads=[o], writes=[self.BR], key=f"g_o{cnt[0] % 2}")
                self.attend(q, kh[h // 4], vaug[h // 4], False, [acc], self.qblocks(), pbufs, fin,
                            sbanks=[self.psb[0], self.psb[1], self.psb[2], self.psb[4], self.psb[5], self.psb[6]], LA=2, G=2)

    def ph_diff(self):
        fw, l = self.fw, self.l
        with fw.scope():
            qh = [fw.sb([64, T], BF16, f"dq{i}") for i in range(2)]
            kh = [fw.sb([64, T], BF16, f"dk{i}") for i in range(2)]
            vv = [fw.sb([128, NT, 128], BF16, f"dv{i}") for i in range(2)]
            pbufs = [fw.sb([128, 512], BF16, f"dp{i}") for i in range(6)]
            r0 = fw.sb([128, 512], F32, "dr0")
            a0 = fw.sb([128, 512], F32, "da0")
            a1 = fw.sb([128, 512], F32, "da1")
            sq = fw.sb([128, 512], BF16, "dsq")
            rstd = fw.sb([128, 512], F32, "drstd")
            ob = [fw.sb([128, 512], BF16, f"dob{i}") for i in range(2)]
            accs = [self.psb[3], self.psb[4]]
            dsb = [self.psb[0], self.psb[1], self.psb[2], self.psb[5], self.psb[6]]
            cnt = [0]
            for h in range(4):
                v = vv[h % 2]
                fw.dma("sp", v[:, :, :], self.VT.t[1 + h].rearrange("(n p) c -> p n c", p=128), reads=[self.VT], writes=[v], key=f"d_v{h % 2}")
                for (t0, n, ic) in self.qblocks():
                    for i in range(2):
                        m = h * 2 + i
                        q, k = qh[i], kh[i]
                        if t0 == self.qblocks()[0][0]:
                            fw.dma("sp", q[:, :], self.QK[6 + m // 2].t[(m % 2) * 64:(m % 2) * 64 + 64, :], reads=[self.QK[6 + m // 2]], writes=[q], key=f"d_q{i}")
                            fw.dma("sp", k[:, :], self.QK[10 + m // 2].t[(m % 2) * 64:(m % 2) * 64 + 64, :], reads=[self.QK[10 + m // 2]], writes=[k], key=f"d_k{i}")
                        self.attend(q, k, v, True, accs, [(t0, n, ic)], pbufs, lambda *a: None, sbanks=dsb, LA=1, G=2)
                        ai = a0 if i == 0 else a1
                        fw.op("dve", lambda e: e.reciprocal(out=r0[:, 0:n], in_=accs[1][:, 0:n]), reads=[accs[1]], writes=[r0])
                        fw.op("dve", lambda e: e.tensor_tensor(out=ai[:, 0:n], in0=accs[0][:, 0:n], in1=r0[:, 0:n], op=ALU.mult), reads=[accs[0], r0], writes=[ai])
                    o = ob[cnt[0] % 2]
                    cnt[0] += 1
                    fw.op("dve", lambda e: e.scalar_tensor_tensor(out=a0[:, 0:n], in0=a1[:, 0:n], scalar=self.neglam[:, l:l + 1], in1=a0[:, 0:n], op0=ALU.mult, op1=ALU.add),
                          reads=[a1, a0, self.neglam], writes=[a0])
                    fw.op("act", lambda e: e.activation(out=sq[:, 0:n], in_=a0[:, 0:n], func=AF.Square), reads=[a0], writes=[sq])
                    ps = self.psb[0]
                    fw.op("pe", lambda e: e.matmul(ps[:, 0:n], self.ones[:, :], sq[:, 0:n], start=True, stop=True), reads=[self.ones, sq], writes=[ps])
                    fw.op("act", lambda e: e.activation(out=rstd[:, 0:n], in_=ps[:, 0:n], func=AF.Sqrt, bias=self.eps[:, 0:1], scale=1.0 / 128), reads=[ps, self.eps], writes=[rstd])
                    fw.op("dve", lambda e: e.reciprocal(out=rstd[:, 0:n], in_=rstd[:, 0:n]), reads=[rstd], writes=[rstd])
                    fw.op("dve", lambda e: e.scalar_tensor_tensor(out=o[:, 0:n], in0=a0[:, 0:n], scalar=self.subg[:, l:l + 1], in1=rstd[:, 0:n], op0=ALU.mult, op1=ALU.mult),
                          reads=[a0, self.subg, rstd], writes=[o])
                    fw.dma("sp", self.BR.t[4 + h, :, t0:t0 + n], o[:, 0:n], reads=[o], writes=[self.BR], key=f"d_o{cnt[0] % 2}")

    def ph_conv(self):
        fw, l = self.fw, self.l
        with fw.scope():
            upad = fw.sb([128, 4, ULEN], BF16, "upad")
            upo = fw.sb([128, 4, ULEN], BF16, "upo")
            for (a_, b_) in ((0, UOFF[0]), (UOFF[0] + 256, UOFF[1]), (UOFF[1] + 4096, ULEN)):
                fw.op("dve", lambda e: e.memset(upad[:, :, a_:b_], 0.0), writes=[upad])
                fw.op("dve", lambda e: e.memset(upo[:, :, max(a_ - 1, 0):b_ - 1 if b_ < ULEN else ULEN], 0.0), writes=[upo])
            diag = fw.sb([128, 4, 31, 128], BF16, "diag")
            ab = [fw.sb([128, T], BF16, f"ca{i}") for i in range(2)]
            gb = [fw.sb([128, T], BF16, f"cg{i}") for i in range(2)]
            for cc in range(4):
                a, g = ab[cc % 2], gb[cc % 2]
                fw.dma("sp", a[:, :], self.PT[18 + cc][:, :], reads=[self.PT[18 + cc]], writes=[a], key=f"c_a{cc % 2}")
                fw.dma("sp", g[:, :], self.PT[22 + cc][:, :], reads=[self.PT[22 + cc]], writes=[g], key=f"c_g{cc % 2}")
                fw.op("act", lambda e, g=g: e.activation(out=g[:, :], in_=g[:, :], func=AF.Sigmoid), reads=[g], writes=[g])
                fw.op("dve", lambda e, a=a, g=g, cc=cc: e.tensor_tensor(out=upad[:, cc, UOFF[0]:UOFF[0] + 256], in0=a[:, 0:256], in1=g[:, 0:256], op=ALU.mult), reads=[a, g], writes=[upad])
                fw.op("dve", lambda e, a=a, g=g, cc=cc: e.tensor_tensor(out=upad[:, cc, UOFF[1]:UOFF[1] + 4096], in0=a[:, 256:T], in1=g[:, 256:T], op=ALU.mult), reads=[a, g], writes=[upad])
                fw.op("pool", lambda e, a=a, g=g, cc=cc: e.tensor_tensor(out=upo[:, cc, UOFF[0] - 1:UOFF[0] - 1 + 256], in0=a[:, 0:256], in1=g[:, 0:256], op=ALU.mult), reads=[a, g], writes=[upo])
                fw.op("pool", lambda e, a=a, g=g, cc=cc: e.tensor_tensor(out=upo[:, cc, UOFF[1] - 1:UOFF[1] - 1 + 4096], in0=a[:, 256:T], in1=g[:, 256:T], op=ALU.mult), reads=[a, g], writes=[upo])
                for j in range(31):
                    fw.op("dve", lambda e, cc=cc, j=j: e.tensor_scalar(out=diag[:, cc, j, :], in0=self.identf[:, :], scalar1=self.convwT[:, l, cc, j:j + 1], scalar2=None, op0=ALU.mult),
                          reads=[self.identf, self.convwT], writes=[diag])
            import os as _os
            cut = int(_os.environ.get("CONV_CUT", "99"))
            if cut <= 0:
                return
            y32 = fw.sb([128, 4, 512], F32, "cy32")
            ybf = fw.sb([128, 4, 512], BF16, "cybf")
            mean = fw.sb([128, 512], F32, "cmean")
            sq = fw.sb([128, 4, 512], BF16, "csq")
            rstd = fw.sb([128, 512], F32, "crstd")
            ob = [fw.sb([128, 4, 512], BF16, f"cob{i}") for i in range(2)]
            for bi, (t0, n, ic) in enumerate(BLK):
                u0 = (UOFF[0] + t0 - UPAD) if ic else (UOFF[1] + (t0 - 256) - UPAD)
                for cc in range(4):
                    ps = self.bank()
                    for j in range(31):
                        usrc, uo = (upad, u0 + j) if (u0 + j) % 2 == 0 else (upo, u0 + j - 1)
                        fw.op("pe", lambda e, ps=ps, cc=cc, j=j: e.matmul(ps[:, 0:n], diag[:, cc, j, :], usrc[:, cc, uo:uo + n], start=(j == 0), stop=(j == 30)),
                              reads=[diag, usrc], writes=[ps], sig=(j == 30))
                    fw.op("act", lambda e, ps=ps, cc=cc: e.activation(out=y32[:, cc, 0:n], in_=ps[:, 0:n], func=AF.Identity, bias=self.convbT[:, l, cc:cc + 1], scale=1.0),
                          reads=[ps, self.convbT], writes=[y32])
                    fw.op("dve", lambda e, ps=ps, cc=cc: e.tensor_scalar(out=ybf[:, cc, 0:n], in0=ps[:, 0:n], scalar1=self.convbT[:, l, cc:cc + 1], scalar2=None, op0=ALU.add),
                          reads=[ps, self.convbT], writes=[ybf])
                if cut <= 1:
                    continue
                pm = self.bank()
                for cc in range(4):
                    fw.op("pe", lambda e, cc=cc: e.matmul(pm[:, 0:n], self.ones[:, :], ybf[:, cc, 0:n], start=(cc == 0), stop=(cc == 3)), reads=[self.ones, ybf], writes=[pm], sig=(cc == 3))
                fw.op("act", lambda e: e.activation(out=mean[:, 0:n], in_=pm[:, 0:n], func=AF.Copy, scale=1.0 / 512), reads=[pm], writes=[mean])
                for cc in range(4):
                    fw.op("dve", lambda e, cc=cc: e.tensor_tensor(out=y32[:, cc, 0:n], in0=y32[:, cc, 0:n], in1=mean[:, 0:n], op=ALU.subtract), reads=[y32, mean], writes=[y32])
                fw.op("act", lambda e: e.activation(out=sq[:, :, 0:n], in_=y32[:, :, 0:n], func=AF.Square), reads=[y32], writes=[sq])
                pv = self.bank()
                for cc in range(4):
                    fw.op("pe", lambda e, cc=cc: e.matmul(pv[:, 0:n], self.ones[:, :], sq[:, cc, 0:n], start=(cc == 0), stop=(cc == 3)), reads=[self.ones, sq], writes=[pv], sig=(cc == 3))
                fw.op("act", lambda e: e.activation(out=rstd[:, 0:n], in_=pv[:, 0:n], func=AF.Sqrt, bias=self.eps[:, 0:1], scale=1.0 / 512), reads=[pv, self.eps], writes=[rstd])
                fw.op("dve", lambda e: e.reciprocal(out=rstd[:, 0:n], in_=rstd[:, 0:n]), reads=[rstd], writes=[rstd])
                if cut <= 2:
                    continue
                o = ob[bi % 2]
                for cc in range(4):
                    fw.op("dve", lambda e, cc=cc: e.scalar_tensor_tensor(out=y32[:, cc, 0:n], in0=y32[:, cc, 0:n], scalar=self.clngT[:, l, cc:cc + 1], in1=rstd[:, 0:n], op0=ALU.mult, op1=ALU.mult),
                          reads=[y32, self.clngT, rstd], writes=[y32])
                    fw.op("act", lambda e, cc=cc: e.activation(out=o[:, cc, 0:n], in_=y32[:, cc, 0:n], func=AF.Silu, bias=self.clnbT[:, l, cc:cc + 1], scale=1.0),
                          reads=[y32, self.clnbT], writes=[o])
                fw.dma("sp", self.BR.t[8:12, :, t0:t0 + n].rearrange("c p t -> p c t"), o[:, :, 0:n], reads=[o], writes=[self.BR], key=f"c_o{bi % 2}")

    def ph_hgrn(self):
        fw, l = self.fw, self.l
        NCH = T // 64
        for d in range(2):
            for hp in range(4):
                with fw.scope():
                    vtok = fw.sb([128, NT, 128], BF16, "hv")
                    fw.dma("sp", vtok[:, :, :], self.VT.t[5 + hp].rearrange("(n p) c -> p n c", p=128), reads=[self.VT], writes=[vtok], key="h_v")
                    mask = fw.sb([128, 256], F32, "hmask")
                    mname = "mask_f" if d == 0 else "mask_b"
                    fw.dma("sp", mask[:, :], self.CI[mname][:, 0:256], reads=[self.CI[mname]], writes=[mask], key="h_m")
                    maski = mask.t[:, :].bitcast(mybir.dt.int32)
                    qT = [fw.sb([64, T], BF16, f"hq{i}") for i in range(2)]
                    qpT = [fw.sb([64, T], BF16, f"hqp{i}") for i in range(2)]
                    kT = [fw.sb([64, T], BF16, f"hk{i}") for i in range(2)]
                    dec2 = fw.sb([64, 2, NCH], F32, "hdec2")
                    ktok = fw.sb([128, NT, 128], BF16, "hkt")
                    dec = fw.sb([128, NCH], F32, "hdec")
                    with fw.scope():
                        onesf = fw.sb([128, 64], F32, "h1")
                        fw.op("pool", lambda e: e.memset(onesf[:, :], 1.0), writes=[onesf])
                        zin = fw.sb([128, T], BF16, "hz")
                        qin = fw.sb([128, T], BF16, "hqi")
                        qs = fw.sb([128, T], F32, "hqs")
                        f = fw.sb([128, T], F32, "hf")
                        lf = fw.sb([128, T], F32, "hlf")
                        cum = fw.sb([128, T], F32, "hcum")
                        kk = fw.sb([128, T], F32, "hkk")
                        ex = f
                        khT = fw.sb([128, T], BF16, "hkh")
                        c3 = cum.t.rearrange("p (c s) -> p c s", s=64)
                        e3 = ex.t.rearrange("p (c s) -> p c s", s=64)
                        iend, imid = (63, 31) if d == 0 else (0, 32)
                        fw.dma("sp", qin[:, :], self.PT[26 + hp][:, :], reads=[self.PT[26 + hp]], writes=[qin], key="h_qi")
                        fw.dma("sp", zin[:, :], self.PT[30 + 4 * d + hp][:, :], reads=[self.PT[30 + 4 * d + hp]], writes=[zin], key="h_zi")
                        fw.op("act", lambda e: e.activation(out=qs[:, :], in_=qin[:, :], func=AF.Silu), reads=[qin], writes=[qs])
                        fw.op("act", lambda e: e.activation(out=f[:, :], in_=zin[:, :], func=AF.Sigmoid), reads=[zin], writes=[f])
                        li = d * 4 + hp
                        fw.op("dve", lambda e: e.tensor_scalar(out=f[:, :], in0=f[:, :], scalar1=self.omlT[:, l, li:li + 1], scalar2=self.lbT[:, l, li:li + 1], op0=ALU.mult, op1=ALU.add),
                              reads=[f, self.omlT, self.lbT], writes=[f])
                        fw.op("act", lambda e: e.activation(out=lf[:, :], in_=f[:, :], func=AF.Ln), reads=[f], writes=[lf])
                        fw.op("pool", lambda e: e.tensor_scalar(out=kk[:, :], in0=f[:, :], scalar1=-1.0, scalar2=1.0, op0=ALU.mult, op1=ALU.add), reads=[f], writes=[kk])
                        for c in range(NCH):
                            fw.op("dve", lambda e: e.tensor_tensor_scan(out=cum[:, c * 64:(c + 1) * 64], data0=onesf[:, :], data1=lf[:, c * 64:(c + 1) * 64], initial=0.0,
                                                                       op0=ALU.mult, op1=ALU.add), reads=[onesf, lf], writes=[cum], sig=(c == NCH - 1))
                        if d == 1:
                            fw.op("dve", lambda e: e.tensor_tensor(out=e3, in0=c3[:, :, 63:64].broadcast_to([128, NCH, 64]), in1=c3, op=ALU.subtract), reads=[cum], writes=[ex])
                            fw.op("dve", lambda e: e.tensor_tensor(out=cum[:, :], in0=ex[:, :], in1=lf[:, :], op=ALU.add), reads=[ex, lf], writes=[cum])
                        fw.op("act", lambda e: e.activation(out=dec[:, :], in_=c3[:, :, iend], func=AF.Exp), reads=[cum], writes=[dec])
                        for hh in range(2):
                            fw.op("dve", lambda e: e.tensor_copy(out=dec2[0:64, hh, :], in_=dec[hh * 64:(hh + 1) * 64, :]), reads=[dec], writes=[dec2])
                        fw.op("act", lambda e: e.activation(out=ex[:, :], in_=cum[:, :], func=AF.Exp), reads=[cum], writes=[ex])
                        for hh in range(2):
                            fw.op("dve", lambda e: e.scalar_tensor_tensor(out=qpT[hh][0:64, :], in0=qs[hh * 64:(hh + 1) * 64, :], scalar=0.125, in1=ex[hh * 64:(hh + 1) * 64, :], op0=ALU.mult, op1=ALU.mult),
                                  reads=[qs, ex], writes=[qpT[hh]])
                        fw.op("dve", lambda e: e.tensor_tensor(out=e3, in0=c3[:, :, iend:iend + 1].broadcast_to([128, NCH, 64]), in1=c3, op=ALU.subtract), reads=[cum], writes=[ex])
                        fw.op("act", lambda e: e.activation(out=ex[:, :], in_=ex[:, :], func=AF.Exp), reads=[ex], writes=[ex])
                        fw.op("dve", lambda e: e.tensor_tensor(out=khT[:, :], in0=kk[:, :], in1=ex[:, :], op=ALU.mult), reads=[kk, ex], writes=[khT])
                        fw.op("dve", lambda e: e.tensor_tensor(out=e3, in0=c3, in1=c3[:, :, imid:imid + 1].broadcast_to([128, NCH, 64]), op=ALU.subtract), reads=[cum], writes=[ex])
                        fw.op("act", lambda e: e.activation(out=lf[:, :], in_=ex[:, :], func=AF.Exp), reads=[ex], writes=[lf])
                        for hh in range(2):
                            fw.op("dve", lambda e: e.scalar_tensor_tensor(out=qT[hh][0:64, :], in0=qs[hh * 64:(hh + 1) * 64, :], scalar=0.125, in1=lf[hh * 64:(hh + 1) * 64, :], op0=ALU.mult, op1=ALU.mult),
                                  reads=[qs, lf], writes=[qT[hh]])
                        fw.op("act", lambda e: e.activation(out=lf[:, :], in_=ex[:, :], func=AF.Exp, scale=-1.0), reads=[ex], writes=[lf])
                        for hh in range(2):
                            fw.op("dve", lambda e: e.tensor_tensor(out=kT[hh][0:64, :], in0=kk[hh * 64:(hh + 1) * 64, :], in1=lf[hh * 64:(hh + 1) * 64, :], op=ALU.mult), reads=[kk, lf], writes=[kT[hh]])
                        import os as _os
                        hcut = int(_os.environ.get("HG_CUT", "99"))
                        for tt in range(NT if hcut > 1 else 0):
                            fw.op("pe", lambda e: e.transpose(self.psT[:, (tt % 8) * 128:(tt % 8) * 128 + 128], khT[:, tt * 128:(tt + 1) * 128], self.identb[:, :]),
                                  reads=[khT, self.identb], writes=[self.psT], sig=(tt % 8 == 7 or tt == NT - 1))
                            if tt % 8 == 7 or tt == NT - 1:
                                t8 = tt - (tt % 8)
                                nn = tt - t8 + 1
                                fw.op("act", lambda e: e.activation(out=ktok[:, t8:t8 + nn, :], in_=self.psT.t[:, 0:nn * 128].rearrange("p (a c) -> p a c", c=128), func=AF.Copy),
                                      reads=[self.psT], writes=[ktok])
                    S = fw.sb([64, 2, 64], F32, "hS")
                    Sb = [fw.sb([64, 2, 64], BF16, f"hSb{i}") for i in range(2)]
                    fw.op("dve", lambda e: e.memset(S[:, :, :], 0.0), writes=[S])
                    fw.op("dve", lambda e: e.memset(Sb[0][:, :, :], 0.0), writes=[Sb[0]])
                    attm = [fw.sb([128, 256], BF16, f"hatt{i}") for i in range(2)]
                    for a_ in attm:
                        fw.op("dve", lambda e: e.memset(a_[:, :], 0.0), writes=[a_])
                    ost = fw.sb([64, 2, T], F32, "host")
                    tiles = list(range(NT)) if d == 0 else [1, 0] + list(range(NT - 1, 1, -1))
                    if hcut <= 2:
                        tiles = []
                    sbi = 0
                    for ti, tt in enumerate(tiles):
                        am = attm[ti % 2]
                        pa = self.psb[1 + ti % 2]
                        for hh in range(2):
                            r0 = hh * 64
                            fw.op("pe", lambda e: e.matmul(pa[:, hh * 128:hh * 128 + 128], kT[hh][0:64, tt * 128:(tt + 1) * 128], qT[hh][0:64, tt * 128:(tt + 1) * 128], start=True, stop=True),
                                  reads=[kT[hh], qT[hh]], writes=[pa], sig=(hh == 1))
                        fw.op("dve", lambda e: e.copy_predicated(out=am[:, :], mask=maski, data=pa[:, 0:256]), reads=[pa, mask], writes=[am])
                        if hcut <= 3:
                            continue
                        po = self.psb[3 + ti % 2]
                        chunks = [0, 1] if d == 0 else [1, 0]
                        if hcut <= 4:
                            chunks = []
                        for hh in range(2):
                            fw.op("pe", lambda e: e.matmul(po[0:64, hh * 128:hh * 128 + 128], vtok[:, tt, hh * 64:(hh + 1) * 64], am[:, hh * 128:(hh + 1) * 128],
                                                           start=(hh == 0), stop=False, skip_group_check=True), reads=[vtok, am], writes=[po], sig=False)
                        for ci, cj in enumerate(chunks):
                            c = tt * 2 + cj
                            sb_cur = Sb[sbi % 2]
                            sb_nxt = Sb[(sbi + 1) % 2]
                            sbi += 1
                            for hh in range(2):
                                r0 = hh * 64
                                fw.op("pe", lambda e: e.matmul(po[0:64, hh * 128 + cj * 64:hh * 128 + cj * 64 + 64], sb_cur[0:64, hh, :], qpT[hh][0:64, c * 64:(c + 1) * 64],
                                                               start=False, stop=(ci == 1), skip_group_check=True), reads=[sb_cur, qpT[hh]], writes=[po], sig=(hh == 1))
                            pS = self.psb[5 + sbi % 2]
                            for hh in range(2):
                                fw.op("pe", lambda e: e.matmul(pS[0:64, hh * 64:(hh + 1) * 64], ktok[cj * 64:cj * 64 + 64, tt, hh * 64:(hh + 1) * 64], vtok[cj * 64:cj * 64 + 64, tt, hh * 64:(hh + 1) * 64],
                                                               start=(hh == 0), stop=(hh == 1), skip_group_check=True), reads=[ktok, vtok], writes=[pS], sig=(hh == 1))
                            for hh in range(2):
                                fw.op("dve", lambda e: e.scalar_tensor_tensor(out=S[0:64, hh, :], in0=S[0:64, hh, :], scalar=dec2[0:64, hh, c:c + 1], in1=pS[0:64, hh * 64:(hh + 1) * 64], op0=ALU.mult, op1=ALU.add),
                                      reads=[S, dec2, pS], writes=[S])
                            fw.op("act", lambda e: e.activation(out=sb_nxt[:, :, :], in_=S[:, :, :], func=AF.Copy), reads=[S], writes=[sb_nxt])
                        fw.op("act", lambda e: e.activation(out=ost[:, :, tt * 128:(tt + 1) * 128], in_=po.t[0:64, 0:256].rearrange("p (a c) -> p a c", c=128), func=AF.Copy),
                              reads=[po], writes=[ost])
                    fw.dma("sp", self.OD[d].t[2 * hp:2 * hp + 2].rearrange("h e t -> e h t"), ost[:, :, :], reads=[ost], writes=[self.OD[d]], key="h_o")
        with fw.scope():
            of = [fw.sb([64, T], F32, f"hof{i}") for i in range(2)]
            obw = [fw.sb([64, T], F32, f"hobw{i}") for i in range(2)]
            gt = [fw.sb([64, T], BF16, f"hgt{i}") for i in range(2)]
            sq = fw.sb([64, 512], BF16, "hsq")
            rstd = fw.sb([64, 512], F32, "hrstd")
            sg = fw.sb([64, 512], F32, "hsg")
            ob = [fw.sb([64, T], BF16, f"hob{i}") for i in range(2)]
            for h in range(8):
                a, b, g, o = of[h % 2], obw[h % 2], gt[h % 2], ob[h % 2]
                fw.dma("sp", a[:, :], self.OD[0].t[h], reads=[self.OD[0]], writes=[a], key=f"hn_a{h % 2}")
                fw.dma("sp", b[:, :], self.OD[1].t[h], reads=[self.OD[1]], writes=[b], key=f"hn_b{h % 2}")
                fw.dma("sp", g[:, :], self.PT[42 + h // 2].t[(h % 2) * 64:(h % 2) * 64 + 64, :], reads=[self.PT[42 + h // 2]], writes=[g], key=f"hn_g{h % 2}")
                fw.op("pool", lambda e, a=a, b=b: e.tensor_tensor(out=a[:, :], in0=a[:, :], in1=b[:, :], op=ALU.add), reads=[a, b], writes=[a])
                for (t0, n, ic) in BLK:
                    fw.op("act", lambda e, a=a: e.activation(out=sq[:, 0:n], in_=a[:, t0:t0 + n], func=AF.Square), reads=[a], writes=[sq])
                    ps = self.bank()
                    fw.op("pe", lambda e, ps=ps: e.matmul(ps[0:64, 0:n], self.ones[0:64, 0:64], sq[0:64, 0:n], start=True, stop=True), reads=[self.ones, sq], writes=[ps])
                    fw.op("act", lambda e, ps=ps: e.activation(out=rstd[:, 0:n], in_=ps[0:64, 0:n], func=AF.Sqrt, bias=self.eps[0:64, 0:1], scale=1.0 / 64), reads=[ps, self.eps], writes=[rstd])
                    fw.op("dve", lambda e: e.reciprocal(out=rstd[:, 0:n], in_=rstd[:, 0:n]), reads=[rstd], writes=[rstd])
                    fw.op("act", lambda e, g=g: e.activation(out=sg[:, 0:n], in_=g[:, t0:t0 + n], func=AF.Silu), reads=[g], writes=[sg])
                    fw.op("dve", lambda e: e.scalar_tensor_tensor(out=rstd[:, 0:n], in0=rstd[:, 0:n], scalar=self.hgT[:, l:l + 1], in1=sg[:, 0:n], op0=ALU.mult, op1=ALU.mult),
                          reads=[rstd, self.hgT, sg], writes=[rstd])
                    fw.op("dve", lambda e, a=a, o=o: e.tensor_tensor(out=o[:, t0:t0 + n], in0=a[:, t0:t0 + n], in1=rstd[:, 0:n], op=ALU.mult), reads=[a, rstd], writes=[o])
                fw.dma("sp", self.BR.t[12 + h // 2, (h % 2) * 64:(h % 2) * 64 + 64, :], o[:, :], reads=[o], writes=[self.BR], key=f"hn_o{h % 2}")

    def ph_merge(self):
        fw, l = self.fw, self.l
        with fw.scope():
            wb = fw.sb([128, 16, D], BF16, "wbr")
            wo = fw.sb([128, 8, D], BF16, "wout")
            for i in range(4):
                fw.dma("pool", wb[:, i * 4:(i + 1) * 4, :], self.I["w_branch"].t[l, i].rearrange("(k p) c -> p k c", p=128), reads=[self.I["w_branch"]], writes=[wb], key="m_wb")
            fw.dma("pool", wo[:, :, :], self.I["w_out"].t[l].rearrange("(k p) c -> p k c", p=128), reads=[self.I["w_out"]], writes=[wo], key="m_wo")
            brs = [fw.sb([128, 16, 512], BF16, f"mbr{i}") for i in range(1)]
            gts = [fw.sb([128, 32, 512], BF16, f"mgt{i}") for i in range(1)]
            xbs = [fw.sb([128, 8, 512], F32, f"mxb{i}") for i in range(1)]
            mg = fw.sb([128, 512], F32, "mmg")
            tmp = fw.sb([128, 512], F32, "mtmp")
            mgb = fw.sb([128, 8, 512], BF16, "mmgb")
            xo = [fw.sb([128, 8, 512], F32, f"mxo{i}") for i in range(2)]
            src = self.xsrc()
            for bi, (t0, n, ic) in enumerate(self.qblocks()):
                br, gt, xb, xn = brs[0], gts[0], xbs[0], xo[bi % 2]
                fw.dma("sp", br[:, :, 0:n], self.BR.t[:, :, t0:t0 + n].rearrange("c p t -> p c t"), reads=[self.BR], writes=[br], key="m_br")
                fw.dma("sp", gt[:, :, 0:n], self.PTall.t[:, :, t0:t0 + n].rearrange("c p t -> p c t"), reads=[self.PTall], writes=[gt], key="m_gt")
                fw.dma("sp", xb[:, :, 0:n], src.t.rearrange("(k p) t -> p k t", p=128)[:, :, t0:t0 + n], reads=[src], writes=[xb], key="m_xb")
                for oc in range(8):
                    for i in range(4):
                        ps = self.bank()
                        for k in range(4):
                            fw.op("pe", lambda e, ps=ps, i=i, k=k, oc=oc: e.matmul(ps[:, 0:n], wb[:, i * 4 + k, oc * 128:(oc + 1) * 128], br[:, i * 4 + k, 0:n], start=(k == 0), stop=(k == 3)),
                                  reads=[wb, br], writes=[ps], sig=(k == 3))
                        if i == 0:
                            fw.op("dve", lambda e, ps=ps, oc=oc: e.tensor_tensor(out=mg[:, 0:n], in0=ps[:, 0:n], in1=gt[:, oc, 0:n], op=ALU.mult), reads=[ps, gt], writes=[mg])
                        else:
                            fw.op("dve", lambda e, ps=ps, oc=oc, i=i: e.tensor_tensor(out=tmp[:, 0:n], in0=ps[:, 0:n], in1=gt[:, i * 8 + oc, 0:n], op=ALU.mult), reads=[ps, gt], writes=[tmp])
                            if i < 3:
                                fw.op("pool", lambda e: e.tensor_tensor(out=mg[:, 0:n], in0=mg[:, 0:n], in1=tmp[:, 0:n], op=ALU.add), reads=[mg, tmp], writes=[mg])
                            else:
                                fw.op("pool", lambda e, oc=oc: e.tensor_tensor(out=mgb[:, oc, 0:n], in0=mg[:, 0:n], in1=tmp[:, 0:n], op=ALU.add), reads=[mg, tmp], writes=[mgb])
                for o2 in range(8):
                    ps = self.bank()
                    for k in range(8):
                        fw.op("pe", lambda e, ps=ps, k=k, o2=o2: e.matmul(ps[:, 0:n], wo[:, k, o2 * 128:(o2 + 1) * 128], mgb[:, k, 0:n], start=(k == 0), stop=(k == 7)),
                              reads=[wo, mgb], writes=[ps], sig=(k == 7))
                    fw.op("dve", lambda e, ps=ps, o2=o2: e.scalar_tensor_tensor(out=xn[:, o2, 0:n], in0=ps[:, 0:n], scalar=self.modt[:, 16 + o2, ic:ic + 1], in1=xb[:, o2, 0:n],
                                                                         op0=ALU.mult, op1=ALU.add), reads=[ps, self.modt, xb], writes=[xn])
                fw.dma("sp", self.xs.t.rearrange("(k p) t -> p k t", p=128)[:, :, t0:t0 + n], xn[:, :, 0:n], reads=[xn], writes=[self.xs], key=f"m_xo{bi % 2}")
        self.x_written = True

    def ph_ffn(self):
        fw, l = self.fw, self.l
        moe = (l % 2 == 1)
        li = l // 2
        JG = 512
        NJG = DFF // JG
        blocks = self.qblocks()
        halves = [blocks[:len(blocks) - 4], blocks[len(blocks) - 4:]]
        for half in halves:
            if not half:
                continue
            hb0 = half[0][0]
            hlen = sum(b[1] for b in half)
            with fw.scope():
                hT = fw.sb([128, 8, hlen], BF16, "fh")
                with fw.scope():
                    xbs = [fw.sb([128, 8, 512], F32, f"fxb{i}") for i in range(2)]
                    sq = fw.sb([128, 8, 512], BF16, "fsq")
                    rstd = fw.sb([128, 512], F32, "frstd")
                    tmp = fw.sb([128, 8, 512], F32, "ftmp")
                    h32 = fw.sb([128, 8, 512], F32, "fh32") if moe else None
                    if moe:
                        lg = fw.sb([128, 8], F32, "flg")
                        m1 = fw.sb([128, 1], F32, "fm1")
                        m2 = fw.sb([128, 1], F32, "fm2")
                        k1 = fw.sb([128, 8], F32, "fk1")
                        k2 = fw.sb([128, 8], F32, "fk2")
                        l2 = fw.sb([128, 8], F32, "fl2")
                        w1 = fw.sb([128, 1], F32, "fw1")
                        w2 = fw.sb([128, 1], F32, "fw2")
                        cmb = fw.sb([128, 8], F32, "fcmb")
                        cbm = fw.sb([128, 8, 128], F32, "fcbm")
                        cbo = [fw.sb([128, 8, 512], BF16, f"fcbo{i}") for i in range(2)]
                    for bi, (t0, n, ic) in enumerate(half):
                        self.norm_block(1, t0, n, ic, hT, t0 - hb0, xbs[bi % 2], sq, rstd, tmp, h32)
                        if not moe:
                            continue
                        co = cbo[bi % 2]
                        for ti in range(n // 128):
                            ps = self.bank()
                            for k in range(8):
                                fw.op("pe", lambda e, ps=ps, k=k, ti=ti: e.matmul(ps[:, 0:8], h32[:, k, ti * 128:(ti + 1) * 128], self.routerT[:, li, k, :], start=(k == 0), stop=(k == 7)),
                                      reads=[h32, self.routerT], writes=[ps], sig=(k == 7))
                            fw.op("dve", lambda e, ps=ps: e.tensor_copy(out=lg[:, :], in_=ps[:, 0:8]), reads=[ps], writes=[lg])
                            fw.op("dve", lambda e: e.reduce_max(out=m1[:, :], in_=lg[:, :], axis=AX.X), reads=[lg], writes=[m1])
                            fw.op("dve", lambda e: e.tensor_scalar(out=k1[:, :], in0=lg[:, :], scalar1=m1[:, 0:1], scalar2=None, op0=ALU.is_ge), reads=[lg, m1], writes=[k1])
                            fw.op("dve", lambda e: e.scalar_tensor_tensor(out=l2[:, :], in0=k1[:, :], scalar=-1e30, in1=lg[:, :], op0=ALU.mult, op1=ALU.add), reads=[k1, lg], writes=[l2])
                            fw.op("dve", lambda e: e.reduce_max(out=m2[:, :], in_=l2[:, :], axis=AX.X), reads=[l2], writes=[m2])
                            fw.op("dve", lambda e: e.tensor_scalar(out=k2[:, :], in0=l2[:, :], scalar1=m2[:, 0:1], scalar2=None, op0=ALU.is_ge), reads=[l2, m2], writes=[k2])
                            fw.op("dve", lambda e: e.tensor_tensor(out=w2[:, :], in0=m2[:, :], in1=m1[:, :], op=ALU.subtract), reads=[m1, m2], writes=[w2])
                            fw.op("act", lambda e: e.activation(out=w2[:, :], in_=w2[:, :], func=AF.Exp), reads=[w2], writes=[w2])
                            fw.op("dve", lambda e: e.tensor_scalar(out=w1[:, :], in0=w2[:, :], scalar1=1.0, scalar2=None, op0=ALU.add), reads=[w2], writes=[w1])
                            fw.op("dve", lambda e: e.reciprocal(out=w1[:, :], in_=w1[:, :]), reads=[w1], writes=[w1])
                            fw.op("dve", lambda e: e.tensor_scalar(out=w2[:, :], in0=w1[:, :], scalar1=-1.0, scalar2=1.0, op0=ALU.mult, op1=ALU.add), reads=[w1], writes=[w2])
                            fw.op("dve", lambda e: e.tensor_scalar(out=cmb[:, :], in0=k1[:, :], scalar1=w1[:, 0:1], scalar2=None, op0=ALU.mult), reads=[k1, w1], writes=[cmb])
                            fw.op("dve", lambda e: e.scalar_tensor_tensor(out=cmb[:, :], in0=k2[:, :], scalar=w2[:, 0:1], in1=cmb[:, :], op0=ALU.mult, op1=ALU.add), reads=[k2, w2, cmb], writes=[cmb])
                            fw.op("dve", lambda e: e.tensor_copy(out=cbm[:, :, :], in_=cmb.t[:, :].unsqueeze(2).broadcast_to([128, 8, 128])), reads=[cmb], writes=[cbm])
                            for eh in range(2):
                                pc = self.bank()
                                for e4 in range(4):
                                    ex = eh * 4 + e4
                                    fw.op("pe", lambda e, pc=pc, e4=e4, ex=ex: e.matmul(pc[:, e4 * 128:(e4 + 1) * 128], cbm[:, ex, :], self.identf[:, :], start=True, stop=True),
                                          reads=[cbm, self.identf], writes=[pc], sig=(e4 == 3))
                                fw.op("act", lambda e, pc=pc, eh=eh, ti=ti, co=co: e.activation(out=co[:, eh * 4:eh * 4 + 4, ti * 128:(ti + 1) * 128],
                                                                                     in_=pc.t.rearrange("p (a c) -> p a c", c=128), func=AF.Copy), reads=[pc], writes=[co])
                        fw.dma("sp", self.CB.t[:, :, t0:t0 + n].rearrange("e p t -> p e t"), co[:, :, 0:n], reads=[co], writes=[self.CB], key=f"f_cb{bi % 2}")
                acc = fw.sb([128, 8, hlen], F32, "facc")
                self._ffn_experts(moe, li, half, hb0, hT, acc)
                self._ffn_resid(half, hb0, acc)

    def _ffn_experts(self, moe, li, half, hb0, hT, acc):
        fw, l = self.fw, self.l
        JG = 512
        NJG = DFF // JG
        with fw.scope():
            if True:
                w1s = [fw.sb([128, 8, JG], BF16, f"fw1_{i}") for i in range(2)]
                w3s = [fw.sb([128, 8, JG], BF16, f"fw3_{i}") for i in range(2)]
                w2s = [fw.sb([128, JG // 128, D], BF16, f"fw2_{i}") for i in range(2)]
                sa = [fw.sb([128, 512], BF16, f"fsa{i}") for i in range(2)]
                gb = [fw.sb([128, JG // 128, 512], BF16, f"fgb{i}") for i in range(2)]
                cbl = [fw.sb([128, 512], BF16, f"fcl{i}") for i in range(2)]
                items = []
                ng = 0
                for ex in range(NEXP if moe else 1):
                    for jg in range(NJG):
                        for bi_, (t0, n, ic) in enumerate(half):
                            items.append((ex, jg, ng, bi_ == 0, t0, n))
                        ng += 1
                NJC = JG // 128

                def stage_a(it, slot):
                    ex, jg, gi, firstb, t0, n = it
                    wa, wc, wd = w1s[gi % 2], w3s[gi % 2], w2s[gi % 2]
                    if firstb:
                        if moe:
                            s1, s3, s2 = self.I["moe_w1"].t[li, ex], self.I["moe_w3"].t[li, ex], self.I["moe_w2"].t[li, ex]
                            r1, r3, r2 = self.I["moe_w1"], self.I["moe_w3"], self.I["moe_w2"]
                        else:
                            s1, s3, s2 = self.I["ffn_w1"].t[li], self.I["ffn_w3"].t[li], self.I["ffn_w2"].t[li]
                            r1, r3, r2 = self.I["ffn_w1"], self.I["ffn_w3"], self.I["ffn_w2"]
                        fw.dma("pool", wa[:, :, :], s1.rearrange("(k p) c -> p k c", p=128)[:, :, jg * JG:(jg + 1) * JG], reads=[r1], writes=[wa], key=f"f_w1{gi % 2}")
                        fw.dma("pool", wc[:, :, :], s3.rearrange("(k p) c -> p k c", p=128)[:, :, jg * JG:(jg + 1) * JG], reads=[r3], writes=[wc], key=f"f_w3{gi % 2}")
                        fw.dma("pool", wd[:, :, :], s2[jg * JG:(jg + 1) * JG, :].rearrange("(k p) c -> p k c", p=128), reads=[r2], writes=[wd], key=f"f_w2{gi % 2}")
                    c0 = t0 - hb0
                    g = gb[slot % 2]
                    cl = cbl[slot % 2]
                    if moe:
                        fw.dma("sp", cl[:, 0:n], self.CB.t[ex, :, t0:t0 + n], reads=[self.CB], writes=[cl], key=f"f_cl{slot % 2}")
                    for jc in range(NJC):
                        p1 = self.psb[1 + (jc % 2) * 2]
                        p3 = self.psb[2 + (jc % 2) * 2]
                        for k in range(8):
                            fw.op("pe", lambda e: e.matmul(p1[:, 0:n], wa[:, k, jc * 128:(jc + 1) * 128], hT[:, k, c0:c0 + n], start=(k == 0), stop=(k == 7)),
                                  reads=[wa, hT], writes=[p1], sig=(k == 7))
                        for k in range(8):
                            fw.op("pe", lambda e: e.matmul(p3[:, 0:n], wc[:, k, jc * 128:(jc + 1) * 128], hT[:, k, c0:c0 + n], start=(k == 0), stop=(k == 7)),
                                  reads=[wc, hT], writes=[p3], sig=(k == 7))
                        sx = sa[jc % 2]
                        fw.op("act", lambda e: e.activation(out=sx[:, 0:n], in_=p1[:, 0:n], func=AF.Silu), reads=[p1], writes=[sx])
                        fw.op("dve", lambda e: e.tensor_tensor(out=g[:, jc, 0:n], in0=p3[:, 0:n], in1=sx[:, 0:n], op=ALU.mult), reads=[p3, sx], writes=[g])
                        if moe:
                            fw.op("pool", lambda e: e.tensor_tensor(out=g[:, jc, 0:n], in0=g[:, jc, 0:n], in1=cl[:, 0:n], op=ALU.mult), reads=[g, cl], writes=[g])

                def stage_w(it, slot):
                    ex, jg, gi, firstb, t0, n = it
                    wd = w2s[gi % 2]
                    c0 = t0 - hb0
                    g = gb[slot % 2]
                    for o in range(8):
                        pf = self.psb[5 + (o % 2)]
                        for jc in range(NJC):
                            fw.op("pe", lambda e: e.matmul(pf[:, 0:n], wd[:, jc, o * 128:(o + 1) * 128], g[:, jc, 0:n], start=(jc == 0), stop=(jc == NJC - 1)),
                                  reads=[wd, g], writes=[pf], sig=(jc == NJC - 1))
                        if gi == 0:
                            fw.op("act", lambda e: e.activation(out=acc[:, o, c0:c0 + n], in_=pf[:, 0:n], func=AF.Copy), reads=[pf], writes=[acc])
                        else:
                            fw.op("dve", lambda e: e.tensor_tensor(out=acc[:, o, c0:c0 + n], in0=acc[:, o, c0:c0 + n], in1=pf[:, 0:n], op=ALU.add), reads=[pf, acc], writes=[acc])

                for i, it in enumerate(items):
                    stage_a(it, i)
                    if i > 0:
                        stage_w(items[i - 1], i - 1)
                stage_w(items[-1], len(items) - 1)

    def _ffn_resid(self, half, hb0, acc):
        fw, l = self.fw, self.l
        with fw.scope():
            if True:
                xbs = [fw.sb([128, 8, 512], F32, f"fxr{i}") for i in range(2)]
                for bi, (t0, n, ic) in enumerate(half):
                    c0 = t0 - hb0
                    xb = xbs[bi % 2]
                    fw.dma("sp", xb[:, :, 0:n], self.xs.t.rearrange("(k p) t -> p k t", p=128)[:, :, t0:t0 + n], reads=[self.xs], writes=[xb], key=f"f_xr{bi % 2}")
                    for k in range(8):
                        fw.op("dve", lambda e, k=k: e.scalar_tensor_tensor(out=xb[:, k, 0:n], in0=acc[:, k, c0:c0 + n], scalar=self.modt[:, 40 + k, ic:ic + 1], in1=xb[:, k, 0:n],
                                                                          op0=ALU.mult, op1=ALU.add), reads=[acc, self.modt, xb], writes=[xb])
                    fw.dma("sp", self.xs.t.rearrange("(k p) t -> p k t", p=128)[:, :, t0:t0 + n], xb[:, :, 0:n], reads=[xb], writes=[self.xs], key=f"f_xw{bi % 2}")


def host_inputs(inp, b):
    f = np.float32
    def colT(v):
        v = np.asarray(v, f)
        sh = v.shape
        v = v.reshape(sh[:-1] + (sh[-1] // 128, 128))
        return np.ascontiguousarray(np.moveaxis(v, -1, 0))
    m = {}
    m["xT0"] = np.ascontiguousarray(np.concatenate([inp["ctx"][b], inp["x"][b]], axis=0).T.astype(f))
    m["cvec"] = np.ascontiguousarray(np.stack([colT(inp["c"][b]), colT(inp["c_ctx"])], axis=-1))
    m["ada_w"] = np.asarray(inp["ada_w"], f)
    m["ada_bT"] = colT(inp["ada_b"])
    m["g1T"] = colT(inp["norm1_g"])
    m["g2T"] = colT(inp["norm2_g"])
    m["w_in"] = np.asarray(inp["w_in"], f)
    qkg = np.asarray(inp["qk_norm_g"], f)
    m["qkgT"] = np.ascontiguousarray(np.tile(np.transpose(qkg, (2, 0, 1)), (2, 1, 1)))
    m["lamB"] = np.ascontiguousarray(np.broadcast_to(np.asarray(inp["diff_lambda"], f).reshape(1, DEPTH, 256), (128, DEPTH, 256)))
    m["sublnT"] = np.ascontiguousarray(np.asarray(inp["diff_subln_g"], f).T)
    cw = np.asarray(inp["conv_w"], f)
    m["convwT"] = np.ascontiguousarray(np.transpose(cw.reshape(DEPTH, 31, 4, 128), (3, 0, 2, 1)))
    m["convbT"] = colT(inp["conv_b"])
    m["clngT"] = colT(inp["conv_ln_g"])
    m["clnbT"] = colT(inp["conv_ln_b"])
    m["lblT"] = colT(inp["hgrn_lb_logits"])
    m["hgT"] = np.ascontiguousarray(np.asarray(inp["hgrn_norm_g"], f).T)
    m["w_branch"] = np.asarray(inp["w_branch"], f)
    m["w_out"] = np.asarray(inp["w_out"], f)
    for k in ("ffn_w1", "ffn_w3", "ffn_w2", "moe_w1", "moe_w3", "moe_w2"):
        m[k] = np.asarray(inp[k], f)
    r = np.asarray(inp["moe_router"], f)
    m["routerT"] = np.ascontiguousarray(np.transpose(r.reshape(2, 8, 128, 8), (2, 0, 1, 3)))
    for k, v in _const_tables().items():
        m["c_" + k] = v
    return m


_CACHE = {}


def kernel(**inputs):
    if "nc" not in _CACHE:
        _CACHE["nc"] = MK().build()
    nc = _CACHE["nc"]
    in_maps = [host_inputs(inputs, b) for b in range(8)]
    res = run_bass_kernel_spmd(nc, in_maps, core_ids=list(range(8)))
    out = np.stack([np.ascontiguousarray(res.results[b]["xs"][:, NCTX:].T) for b in range(8)], axis=0)
    return out.astype(np.float32)
```

```python
import math
from contextlib import ExitStack, contextmanager
import numpy as np
import ml_dtypes
import concourse.bass as bass
import concourse.mybir as mybir
from concourse.bass_utils import run_bass_kernel_spmd

F32 = mybir.dt.float32
BF16 = mybir.dt.bfloat16
AF = mybir.ActivationFunctionType
ALU = mybir.AluOpType
AX = mybir.AxisListType

D = 1024
SEQ = 4096
NCTX = 256
T = SEQ + NCTX
NT = T // 128
DEPTH = 4
D_IN = 9984
NOC = D_IN // 128
DFF = 3584
NEXP = 8
EPS = 1e-6
BLK = [(0, 256, 1)] + [(256 + 512 * i, 512, 0) for i in range(8)]
VCH = {5: 0, 14: 1, 15: 2, 16: 3, 17: 4, 38: 5, 39: 6, 40: 7, 41: 8}
QKCH = [0, 1, 2, 3, 4, 6, 7, 8, 9, 10, 11, 12, 13]
UPAD = 15
ULEN = UPAD + 256 + UPAD + UPAD + 4096 + UPAD
UOFF = [UPAD, UPAD + 256 + 2 * UPAD]


class Buf:
    __slots__ = ("t", "name", "key", "w", "r", "psum")

    def __init__(self, t, name, key=None):
        self.psum = False
        self.t = t
        self.name = name
        self.key = key or name
        self.w = None
        self.r = {}

    def __getitem__(self, idx):
        return self.t[idx]


class FW:
    def __init__(self, nc, root):
        self.nc = nc
        self.root = root
        self.stack = root
        self.engs = {"pe": nc.tensor, "act": nc.scalar, "dve": nc.vector, "pool": nc.gpsimd, "sp": nc.sync}
        self.sem = {k: root.enter_context(nc.semaphore("s_" + k)) for k in self.engs}
        self.cnt = {k: 0 for k in self.engs}
        self.seen = {k: {} for k in self.engs}
        self.dsem = {}
        self.nbuf = 0
        self.ninst = 0

    @contextmanager
    def scope(self):
        old = self.stack
        with ExitStack() as s:
            self.stack = s
            try:
                yield
            finally:
                self.barrier()
                self.stack = old

    def sb(self, shape, dt, name=None):
        self.nbuf += 1
        key = name or 'sb'
        name = f"{key}_{self.nbuf}"
        t = self.stack.enter_context(self.nc.sbuf_tensor(name, list(shape), dt))
        return Buf(t, name, key)

    def ps(self, shape, dt=F32, name=None):
        self.nbuf += 1
        name = f"{name or 'ps'}_{self.nbuf}"
        t = self.root.enter_context(self.nc.psum_tensor(name, list(shape), dt))
        b = Buf(t, name)
        b.psum = True
        return b

    def dram(self, name, shape, dt, kind="Internal"):
        t = self.nc.dram_tensor(name, list(shape), dt, kind=kind)
        return Buf(t.ap(), name)

    def _semof(self, key):
        return self.sem[key] if key in self.sem else self.dsem[key][0]

    def _wait(self, E, ev):
        if ev is None:
            return
        key, val = ev
        if key not in self.sem:
            val = 16 * self.dsem[key][1]
        if key == "pe" and E == "pe":
            return
        if key == E and val > self.cnt[E]:
            return
        if self.seen[E].get(key, 0) >= val:
            return
        self.seen[E][key] = val
        self.engs[E].wait_ge(self._semof(key), val)
        self.ninst += 1

    def _deps(self, E, reads, writes):
        for b in reads:
            self._wait(E, b.w)
            if b.psum:
                for k, v in b.r.items():
                    if k != E:
                        self._wait(E, (k, v))
        for b in writes:
            self._wait(E, b.w)
            for k, v in b.r.items():
                self._wait(E, (k, v))

    def _record(self, ev, reads, writes):
        k, v = ev
        for b in reads:
            if b.r.get(k, 0) < v:
                b.r[k] = v
        for b in writes:
            b.w = ev
            b.r = {}

    def op(self, E, fn, reads=(), writes=(), sig=True):
        self._deps(E, reads, writes)
        ins = fn(self.engs[E])
        self.ninst += 1
        if sig:
            self.cnt[E] += 1
            ins.then_inc(self.sem[E], 1)
            ev = (E, self.cnt[E])
        else:
            ev = (E, self.cnt[E] + 1)
        self._record(ev, reads, writes)
        return ins

    def dma(self, Q, out, in_, reads=(), writes=(), key=None, **kw):
        self._deps(Q, reads, writes)
        if key is None:
            key = "d_" + (writes[0].key if writes else reads[0].key)
        if key not in self.dsem:
            s = self.root.enter_context(self.nc.semaphore("q_" + str(len(self.dsem))))
            self.dsem[key] = [s, 0]
        ent = self.dsem[key]
        ent[1] += 1
        ins = self.engs[Q].dma_start(out=out, in_=in_, **kw)
        ins.then_inc(ent[0], 16)
        self.ninst += 1
        self._record((key, 16 * ent[1]), reads, writes)
        return ins

    def barrier(self, engines=("pe", "act", "dve", "pool", "sp")):
        for E in engines:
            for k in ("pe", "act", "dve", "pool", "sp"):
                if k != E and self.cnt[k]:
                    self._wait(E, (k, self.cnt[k]))
            for key, (s, c) in self.dsem.items():
                if c:
                    self._wait(E, (key, 16 * c))


def _const_tables():
    c = {}
    c["ones"] = np.ones((128, 128), np.float32)
    bd = np.zeros((128, 128), np.float32)
    bd[:64, :64] = 1
    bd[64:, 64:] = 1
    c["bd64"] = bd
    c["ident"] = np.eye(128, dtype=np.float32)
    R = np.zeros((128, 128), np.float32)
    for p in range(128):
        d = p % 64
        j = d % 32
        if j < 16:
            R[p + 16, p] = -1.0
        else:
            R[p - 16, p] = 1.0
    c["rot"] = R
    inv_freq = (10000.0 ** (-np.arange(0, 32, 2, dtype=np.float32) / 32)).astype(np.float32)
    tl = np.arange(SEQ)
    row = (tl // 64).astype(np.float32)
    col = (tl % 64).astype(np.float32)
    cos = np.ones((128, T), np.float32)
    sin = np.zeros((128, T), np.float32)
    for p in range(128):
        d = p % 64
        pos = row if d < 32 else col
        f = inv_freq[(d % 32) % 16]
        ang = (pos * f).astype(np.float32)
        cos[p, NCTX:] = np.cos(ang)
        sin[p, NCTX:] = np.sin(ang)
    c["cos"] = cos
    c["sin"] = sin
    s = np.arange(128)[:, None]
    t = np.arange(128)[None, :]
    same = (s // 64) == (t // 64)
    c["mask_f"] = np.tile((same & (s <= t)).astype(np.float32), (1, 4))
    c["mask_b"] = np.tile((same & (s >= t)).astype(np.float32), (1, 4))
    return c


CONST_SPECS = [("ones", [128, 128]), ("bd64", [128, 128]), ("ident", [128, 128]), ("rot", [128, 128]),
               ("cos", [128, T]), ("sin", [128, T]), ("mask_f", [128, 512]), ("mask_b", [128, 512])]

IN_SPECS = [
    ("xT0", [D, T]), ("cvec", [128, 8, 2]), ("ada_w", [DEPTH, D, 6 * D]), ("ada_bT", [128, DEPTH, 48]),
    ("g1T", [128, DEPTH, 8]), ("g2T", [128, DEPTH, 8]), ("w_in", [DEPTH, D, D_IN]),
    ("qkgT", [128, DEPTH, 4]), ("lamB", [128, DEPTH, 256]), ("sublnT", [128, DEPTH]),
    ("convwT", [128, DEPTH, 4, 31]), ("convbT", [128, DEPTH, 4]), ("clngT", [128, DEPTH, 4]),
    ("clnbT", [128, DEPTH, 4]), ("lblT", [128, DEPTH, 2, 4]), ("hgT", [64, DEPTH]),
    ("w_branch", [DEPTH, 4, 512, D]), ("w_out", [DEPTH, D, D]),
    ("ffn_w1", [2, D, DFF]), ("ffn_w3", [2, D, DFF]), ("ffn_w2", [2, DFF, D]),
    ("routerT", [128, 2, 8, 8]), ("moe_w1", [2, NEXP, D, DFF]), ("moe_w3", [2, NEXP, D, DFF]),
    ("moe_w2", [2, NEXP, DFF, D]),
]


class MK:
    def __init__(self, n_layers=DEPTH, debug=(), stop_after=None, ext_in=()):
        self.n_layers = n_layers
        self.debug = set(debug)
        self.ext_in = set(ext_in)
        self.stop_after = stop_after
        self.nc = bass.Bass("TRN2", target_bir_lowering=False)
        self.rr = 0

    def scratch(self, name, shape, dt):
        kind = "Internal"
        if name in self.debug:
            kind = "ExternalOutput"
        if name in self.ext_in:
            kind = "ExternalInput"
        return self.fw.dram(name, shape, dt, kind=kind)

    def build(self):
        nc = self.nc
        with ExitStack() as root:
            fw = self.fw = FW(nc, root)
            big = ("ada_w", "w_in", "w_branch", "w_out", "ffn_w1", "ffn_w3", "ffn_w2", "moe_w1", "moe_w3", "moe_w2")
            tiny = getattr(self, "tiny", False)
            self.I = {n: fw.dram(n, ([1] * len(s) if (tiny and n in big) else s), F32, kind="ExternalInput") for n, s in IN_SPECS}
            self.CI = {n: fw.dram("c_" + n, s, F32, kind="ExternalInput") for n, s in CONST_SPECS}
            self.xs = fw.dram("xs", [D, T], F32, kind="ExternalOutput")
            self.PT = [self.scratch(f"PT{i}", [128, T], BF16) for i in range(NOC)]
            self.PTall = self.scratch("PTg", [32, 128, T], BF16)
            self.VT = self.scratch("VT", [9, T, 128], BF16)
            self.QK = {oc: self.scratch(f"QK{oc}", [128, T], BF16) for oc in QKCH}
            self.BR = self.scratch("BR", [16, 128, T], BF16)
            self.OD = [self.scratch(f"OD{d}", [8, 64, T], F32) for d in range(2)]
            self.CB = self.scratch("CB", [NEXP, 128, T], BF16)
            self.psb = [fw.ps([128, 512], F32, f"bank{i}") for i in range(7)]
            self.psT = fw.ps([128, 1024], BF16, "bankT")
            self.ones = fw.sb([128, 128], BF16, "ones")
            self.bd64 = fw.sb([128, 128], BF16, "bd64")
            self.identb = fw.sb([128, 128], BF16, "identb")
            self.identf = fw.sb([128, 128], F32, "identf")
            self.rot = fw.sb([128, 128], BF16, "rot")
            for nm, b in (("ones", self.ones), ("bd64", self.bd64), ("ident", self.identb), ("rot", self.rot)):
                fw.dma("pool", b[:, :], self.CI[nm][:, :], reads=[self.CI[nm]], writes=[b])
            fw.dma("sp", self.identf[:, :], self.CI["ident"][:, :], reads=[self.CI["ident"]], writes=[self.identf])
            self.eps = fw.sb([128, 1], F32, "eps")
            fw.op("dve", lambda e: e.memset(self.eps[:, :], EPS), writes=[self.eps])
            self.small_params()
            self.modt = fw.sb([128, 48, 2], F32, "mod")
            self.gs = fw.sb([128, 2, 8, 2], F32, "gs")
            for l in (getattr(self, "layers", None) or range(self.n_layers)):
                self.layer(l)
                if self.stop_after is not None and self.stop_after[0] == l and self.done:
                    break
            fw.barrier(engines=("sp",))
        return nc

    def small_params(self):
        fw, I = self.fw, self.I
        def ld(name, shape):
            b = fw.sb(shape, F32, name)
            src = I[name]
            fw.dma("sp", b.t[tuple(slice(None) for _ in shape)], src.t[tuple(slice(None) for _ in shape)], reads=[src], writes=[b])
            return b
        self.cvec = ld("cvec", [128, 8, 2])
        self.ada_bT = ld("ada_bT", [128, DEPTH, 48])
        self.g1T = ld("g1T", [128, DEPTH, 8])
        self.g2T = ld("g2T", [128, DEPTH, 8])
        self.qkgT = ld("qkgT", [128, DEPTH, 4])
        self.lamB = ld("lamB", [128, DEPTH, 256])
        self.sublnT = ld("sublnT", [128, DEPTH])
        self.convwT = ld("convwT", [128, DEPTH, 4, 31])
        self.convbT = ld("convbT", [128, DEPTH, 4])
        self.clngT = ld("clngT", [128, DEPTH, 4])
        self.clnbT = ld("clnbT", [128, DEPTH, 4])
        self.lblT = ld("lblT", [128, DEPTH, 2, 4])
        self.hgT = ld("hgT", [64, DEPTH])
        self.routerT = ld("routerT", [128, 2, 8, 8])
        self.siluc = fw.sb([128, 8, 2], BF16, "siluc")
        fw.op("act", lambda e: e.activation(out=self.siluc[:, :, :], in_=self.cvec[:, :, :], func=AF.Silu),
              reads=[self.cvec], writes=[self.siluc])
        ex = fw.sb([128, DEPTH, 8], F32, "lbex")
        fw.op("act", lambda e: e.activation(out=ex[:, :, :], in_=self.lblT.t.rearrange("p l d c -> p l (d c)"), func=AF.Exp),
              reads=[self.lblT], writes=[ex])
        ssum = fw.sb([128, 8], F32, "lbsum")
        fw.op("dve", lambda e: e.tensor_tensor(out=ssum[:, :], in0=ex[:, 0, :], in1=ex[:, 1, :], op=ALU.add), reads=[ex], writes=[ssum])
        for l in range(2, DEPTH):
            fw.op("dve", lambda e, l=l: e.tensor_tensor(out=ssum[:, :], in0=ssum[:, :], in1=ex[:, l, :], op=ALU.add), reads=[ex, ssum], writes=[ssum])
        fw.op("dve", lambda e: e.reciprocal(out=ssum[:, :], in_=ssum[:, :]), reads=[ssum], writes=[ssum])
        self.lbT = fw.sb([128, DEPTH, 8], F32, "lbT")
        self.omlT = fw.sb([128, DEPTH, 8], F32, "omlT")
        fw.op("dve", lambda e: e.memset(self.lbT[:, 0, :], 0.0), writes=[self.lbT])
        for l in range(1, DEPTH):
            fw.op("dve", lambda e, l=l: e.tensor_tensor(out=ex[:, l, :], in0=ex[:, l, :], in1=ssum[:, :], op=ALU.mult), reads=[ex, ssum], writes=[ex])
            fw.op("dve", lambda e, l=l: e.tensor_tensor(out=self.lbT[:, l, :], in0=self.lbT[:, l - 1, :], in1=ex[:, l, :], op=ALU.add),
                  reads=[ex, self.lbT], writes=[self.lbT])
        fw.op("dve", lambda e: e.tensor_scalar(out=self.omlT[:, :, :], in0=self.lbT[:, :, :], scalar1=-1.0, scalar2=1.0, op0=ALU.mult, op1=ALU.add),
              reads=[self.lbT], writes=[self.omlT])
        self.neglam = fw.sb([128, DEPTH], F32, "neglam")
        self.subg = fw.sb([128, DEPTH], F32, "subg")
        pr = fw.sb([128, DEPTH, 2, 64], F32, "lampr")
        s12 = fw.sb([128, DEPTH, 2], F32, "lams")
        lam4 = self.lamB.t.rearrange("p l (i d) -> p l i d", i=4)
        for l in range(DEPTH):
            for j in range(2):
                fw.op("dve", lambda e, l=l, j=j: e.tensor_tensor(out=pr[:, l, j, :], in0=lam4[:, l, 2 * j, :], in1=lam4[:, l, 2 * j + 1, :], op=ALU.mult),
                      reads=[self.lamB], writes=[pr])
                fw.op("dve", lambda e, l=l, j=j: e.reduce_sum(out=s12[:, l, j:j + 1], in_=pr[:, l, j, :], axis=AX.X), reads=[pr], writes=[s12])
        fw.op("act", lambda e: e.activation(out=s12[:, :, :], in_=s12[:, :, :], func=AF.Exp), reads=[s12], writes=[s12])
        for l in range(DEPTH):
            li = 0.8 - 0.6 * math.exp(-0.3 * l)
            fw.op("dve", lambda e, l=l, li=li: e.scalar_tensor_tensor(out=self.neglam[:, l:l + 1], in0=s12[:, l, 1:2], scalar=-li, in1=s12[:, l, 0:1],
                                                                     op0=ALU.add, op1=ALU.subtract), reads=[s12], writes=[self.neglam])
            fw.op("dve", lambda e, l=l, li=li: e.tensor_scalar(out=self.subg[:, l:l + 1], in0=self.sublnT[:, l:l + 1], scalar1=1.0 - li, scalar2=None, op0=ALU.mult),
                  reads=[self.sublnT], writes=[self.subg])

    def bank(self):
        self.rr = (self.rr + 1) % 7
        return self.psb[self.rr]

    def layer(self, l):
        self.done = False
        need_ctx = l < DEPTH - 1
        self.l = l
        self.need_ctx = need_ctx
        steps = [self.ph_mod, self.ph_inproj, self.ph_qk, self.ph_gqa, self.ph_diff, self.ph_conv, self.ph_hgrn,
                 self.ph_merge, self.ph_ffn]
        for i, s in enumerate(steps):
            if getattr(self, "only", None) is not None and i not in self.only:
                continue
            s()
            if self.stop_after is not None and self.stop_after == (l, i):
                self.done = True
                return

    def xsrc(self):
        return self.I["xT0"] if (self.l == 0 and not self.x_written) else self.xs

    def ph_mod(self):
        fw, l = self.fw, self.l
        self.x_written = (l > 0)
        with fw.scope():
            ps = self.psb[0]
            wts = [fw.sb([128, 8, 512], BF16, f"adaw{i}") for i in range(2)]
            aw = self.I["ada_w"].t[l].rearrange("(k p) c -> p k c", p=128)
            for g in range(12):
                w = wts[g % 2]
                fw.dma("pool", w[:, :, :], aw[:, :, g * 512:(g + 1) * 512], reads=[self.I["ada_w"]], writes=[w], key=f"adaw{g % 2}")
                for cc in range(4):
                    j = g * 4 + cc
                    for k in range(8):
                        fw.op("pe", lambda e, w=w, cc=cc, k=k, j=j: e.matmul(ps[:, 2 * j:2 * j + 2], w[:, k, cc * 128:(cc + 1) * 128], self.siluc[:, k, :],
                                                                       start=(k == 0), stop=(k == 7)),
                              reads=[w, self.siluc], writes=[ps], sig=(k == 7))
            mod = self.modt
            for i in range(2):
                fw.op("dve", lambda e, i=i: e.tensor_tensor(out=mod[:, :, i], in0=ps.t[:, 0:96].rearrange("p (j i) -> p j i", i=2)[:, :, i],
                                                            in1=self.ada_bT[:, l, :], op=ALU.add), reads=[ps, self.ada_bT], writes=[mod])
            for s, (gT, c0) in enumerate(((self.g1T, 8), (self.g2T, 32))):
                for i in range(2):
                    fw.op("dve", lambda e, s=s, gT=gT, c0=c0, i=i: e.scalar_tensor_tensor(out=self.gs[:, s, :, i], in0=mod[:, c0:c0 + 8, i], scalar=1.0, in1=gT[:, l, :],
                                                                                     op0=ALU.add, op1=ALU.mult), reads=[mod, gT], writes=[self.gs])

    def norm_block(self, s, t0, n, ic, hT, hcol, xb, sq, rstd, tmp, h32=None):
        fw = self.fw
        src = self.xsrc()
        shc = 0 if s == 0 else 24
        fw.dma("sp", xb[:, :, 0:n], src.t.rearrange("(k p) t -> p k t", p=128)[:, :, t0:t0 + n], reads=[src], writes=[xb])
        fw.op("act", lambda e: e.activation(out=sq[:, :, 0:n], in_=xb[:, :, 0:n], func=AF.Square), reads=[xb], writes=[sq])
        ps = self.bank()
        for k in range(8):
            fw.op("pe", lambda e, k=k: e.matmul(ps[:, 0:n], self.ones[:, :], sq[:, k, 0:n], start=(k == 0), stop=(k == 7)),
                  reads=[self.ones, sq], writes=[ps], sig=(k == 7))
        fw.op("act", lambda e: e.activation(out=rstd[:, 0:n], in_=ps[:, 0:n], func=AF.Sqrt, bias=self.eps[:, 0:1], scale=1.0 / D), reads=[ps, self.eps], writes=[rstd])
        fw.op("dve", lambda e: e.reciprocal(out=rstd[:, 0:n], in_=rstd[:, 0:n]), reads=[rstd], writes=[rstd])
        for k in range(8):
            fw.op("dve", lambda e, k=k: e.scalar_tensor_tensor(out=tmp[:, k, 0:n], in0=xb[:, k, 0:n], scalar=self.gs[:, s, k, ic:ic + 1], in1=rstd[:, 0:n],
                                                              op0=ALU.mult, op1=ALU.mult), reads=[xb, self.gs, rstd], writes=[tmp])
            fw.op("act", lambda e, k=k: e.activation(out=hT[:, k, hcol:hcol + n], in_=tmp[:, k, 0:n], func=AF.Identity, bias=self.modt[:, shc + k, ic:ic + 1], scale=1.0),
                  reads=[tmp, self.modt], writes=[hT])
            if h32 is not None:
                fw.op("pool", lambda e, k=k: e.tensor_scalar(out=h32[:, k, 0:n], in0=tmp[:, k, 0:n], scalar1=self.modt[:, shc + k, ic:ic + 1], scalar2=None, op0=ALU.add),
                      reads=[tmp, self.modt], writes=[h32])

    def ph_inproj(self):
        fw, l = self.fw, self.l
        with fw.scope():
            hT = fw.sb([128, 8, T], BF16, "hT")
            with fw.scope():
                xbs = [fw.sb([128, 8, 512], F32, f"xb{i}") for i in range(2)]
                sq = fw.sb([128, 8, 512], BF16, "sq")
                rstd = fw.sb([128, 512], F32, "rstd")
                tmp = fw.sb([128, 8, 512], F32, "tmp")
                for bi, (t0, n, ic) in enumerate(BLK):
                    self.norm_block(0, t0, n, ic, hT, t0, xbs[bi % 2], sq, rstd, tmp)
            wts = [fw.sb([128, 8, 512], BF16, f"win{i}") for i in range(3)]
            stg = [fw.sb([128, T], BF16, f"stg{i}") for i in range(2)]
            vst = [fw.sb([128, NT, 128], BF16, f"vst{i}") for i in range(2)]
            wv = self.I["w_in"].t[l].rearrange("(k p) c -> p k c", p=128)
            ns = 0
            for g in range(20):
                w = wts[g % 3]
                gc = min(512, D_IN - g * 512)
                fw.dma("pool", w[:, :, 0:gc], wv[:, :, g * 512:g * 512 + gc], reads=[self.I["w_in"]], writes=[w], key=f"win{g % 3}")
                for cc in range(gc // 128):
                    oc = g * 4 + cc
                    if oc in VCH:
                        v = vst[VCH[oc] % 2]
                        for tg in range(0, NT, 4):
                            ps = self.bank()
                            nt_ = min(4, NT - tg)
                            for ti in range(nt_):
                                tt = tg + ti
                                for k in range(8):
                                    fw.op("pe", lambda e, k=k, tt=tt, ti=ti, ps=ps, w=w, cc=cc: e.matmul(
                                        ps[:, ti * 128:(ti + 1) * 128], hT[:, k, tt * 128:(tt + 1) * 128], w[:, k, cc * 128:(cc + 1) * 128],
                                        start=(k == 0), stop=(k == 7)), reads=[hT, w], writes=[ps], sig=(k == 7))
                            fw.op("dve", lambda e, ps=ps, v=v, tg=tg, nt_=nt_: e.tensor_copy(out=v[:, tg:tg + nt_, :], in_=ps.t[:, 0:nt_ * 128].rearrange("p (a c) -> p a c", c=128)),
                                  reads=[ps], writes=[v])
                        fw.dma("sp", self.VT.t[VCH[oc]].rearrange("(n p) c -> p n c", p=128), v[:, :, :], reads=[v], writes=[self.VT], key="vt_st")
                        continue
                    so = stg[ns % 2]
                    ns += 1
                    for bi, (t0, n, ic) in enumerate(BLK):
                        ps = self.bank()
                        for k in range(8):
                            fw.op("pe", lambda e, k=k, ps=ps, w=w, cc=cc, t0=t0, n=n: e.matmul(ps[:, 0:n], w[:, k, cc * 128:(cc + 1) * 128], hT[:, k, t0:t0 + n],
                                                                                   start=(k == 0), stop=(k == 7)), reads=[hT, w], writes=[ps], sig=(k == 7))
                        if oc >= 46:
                            fw.op("act", lambda e, ps=ps, so=so, t0=t0, n=n: e.activation(out=so[:, t0:t0 + n], in_=ps[:, 0:n], func=AF.Sigmoid), reads=[ps], writes=[so])
                        elif bi % 2 == 0:
                            fw.op("dve", lambda e, ps=ps, so=so, t0=t0, n=n: e.tensor_copy(out=so[:, t0:t0 + n], in_=ps[:, 0:n]), reads=[ps], writes=[so])
                        else:
                            fw.op("act", lambda e, ps=ps, so=so, t0=t0, n=n: e.activation(out=so[:, t0:t0 + n], in_=ps[:, 0:n], func=AF.Copy), reads=[ps], writes=[so])
                    if oc >= 46:
                        fw.dma("sp", self.PTall.t[oc - 46], so[:, :], reads=[so], writes=[self.PTall], key="ptg_st")
                    else:
                        fw.dma("sp", self.PT[oc][:, :], so[:, :], reads=[so], writes=[self.PT[oc]], key=f"pt_st{ns % 2}")

    def ph_qk(self):
        fw, l = self.fw, self.l
        with fw.scope():
            cos = fw.sb([128, T], F32, "cos")
            sin = fw.sb([128, T], F32, "sin")
            fw.dma("sp", cos[:, :], self.CI["cos"][:, :], reads=[self.CI["cos"]], writes=[cos])
            fw.dma("sp", sin[:, :], self.CI["sin"][:, :], reads=[self.CI["sin"]], writes=[sin])
            qin = [fw.sb([128, T], BF16, f"qin{i}") for i in range(2)]
            qout = [fw.sb([128, T], BF16, f"qout{i}") for i in range(2)]
            sq = [fw.sb([128, 512], BF16, f"qsq{i}") for i in range(2)]
            rstd = [fw.sb([128, 512], F32, f"qrstd{i}") for i in range(2)]
            qn = [fw.sb([128, 512], BF16, f"qn{i}") for i in range(2)]
            t1 = [fw.sb([128, 512], F32, f"qt1{i}") for i in range(2)]
            t2 = [fw.sb([128, 512], F32, f"qt2{i}") for i in range(2)]
            it = 0
            for ci, oc in enumerate(QKCH):
                gi = 0 if oc < 4 else 1 if oc == 4 else 2 if oc < 10 else 3
                qi, qo = qin[ci % 2], qout[ci % 2]
                fw.dma("sp", qi[:, :], self.PT[oc][:, :], reads=[self.PT[oc]], writes=[qi], key=f"qk_ld{ci % 2}")
                for (t0, n, ic) in BLK:
                    i2 = it % 2
                    it += 1
                    fw.op("pool", lambda e, qi=qi, i2=i2, t0=t0, n=n: e.tensor_tensor(out=sq[i2][:, 0:n], in0=qi[:, t0:t0 + n], in1=qi[:, t0:t0 + n], op=ALU.mult), reads=[qi], writes=[sq[i2]])
                    ps = self.bank()
                    fw.op("pe", lambda e, ps=ps, i2=i2, n=n: e.matmul(ps[:, 0:n], self.bd64[:, :], sq[i2][:, 0:n], start=True, stop=True), reads=[self.bd64, sq[i2]], writes=[ps])
                    fw.op("act", lambda e, ps=ps, i2=i2, n=n: e.activation(out=rstd[i2][:, 0:n], in_=ps[:, 0:n], func=AF.Ln, bias=self.eps[:, 0:1], scale=1.0 / 64),
                          reads=[ps, self.eps], writes=[rstd[i2]])
                    fw.op("act", lambda e, i2=i2, n=n: e.activation(out=rstd[i2][:, 0:n], in_=rstd[i2][:, 0:n], func=AF.Exp, scale=-0.5), reads=[rstd[i2]], writes=[rstd[i2]])
                    fw.op("dve", lambda e, qi=qi, i2=i2, t0=t0, n=n, gi=gi: e.scalar_tensor_tensor(out=qn[i2][:, 0:n], in0=qi[:, t0:t0 + n], scalar=self.qkgT[:, l, gi:gi + 1],
                                                                                          in1=rstd[i2][:, 0:n], op0=ALU.mult, op1=ALU.mult),
                          reads=[qi, self.qkgT, rstd[i2]], writes=[qn[i2]])
                    ps2 = self.bank()
                    fw.op("pe", lambda e, ps2=ps2, i2=i2, n=n: e.matmul(ps2[:, 0:n], self.rot[:, :], qn[i2][:, 0:n], start=True, stop=True), reads=[self.rot, qn[i2]], writes=[ps2])
                    fw.op("pool", lambda e, i2=i2, t0=t0, n=n: e.tensor_tensor(out=t1[i2][:, 0:n], in0=qn[i2][:, 0:n], in1=cos[:, t0:t0 + n], op=ALU.mult),
                          reads=[qn[i2], cos], writes=[t1[i2]])
                    fw.op("dve", lambda e, ps2=ps2, i2=i2, t0=t0, n=n: e.tensor_tensor(out=t2[i2][:, 0:n], in0=ps2[:, 0:n], in1=sin[:, t0:t0 + n], op=ALU.mult),
                          reads=[ps2, sin], writes=[t2[i2]])
                    fw.op("dve", lambda e, qo=qo, i2=i2, t0=t0, n=n: e.tensor_tensor(out=qo[:, t0:t0 + n], in0=t1[i2][:, 0:n], in1=t2[i2][:, 0:n], op=ALU.add),
                          reads=[t1[i2], t2[i2]], writes=[qo])
                fw.dma("sp", self.QK[oc][:, :], qo[:, :], reads=[qo], writes=[self.QK[oc]], key=f"qk_st{ci % 2}")

    def attend(self, qh, kh, vaug, lsep, accs, qblocks, pbufs, finish, sbanks=None, LA=2, G=2):
        fw = self.fw
        sbanks = sbanks or self.psb[0:3]
        assert len(sbanks) >= (LA + 1) * G and len(pbufs) >= (LA + 2) * G
        for (t0, n, ic) in qblocks:
            kts = list(range(2)) if ic else list(range(NT))
            NK = len(kts)
            pend = []
            for g0 in range(0, NK, G):
                grp = []
                for idx in range(g0, min(g0 + G, NK)):
                    self.rs = (getattr(self, "rs", 0) + 1) % len(sbanks)
                    self.rp = (getattr(self, "rp", 0) + 1) % len(pbufs)
                    grp.append((idx, kts[idx], sbanks[self.rs], pbufs[self.rp]))
                fw._deps("pe", [kh, qh], [x[2] for x in reversed(grp)])
                for (idx, kt, ps, pb) in grp:
                    fw.op("pe", lambda e: e.matmul(ps[:, 0:n], kh[0:64, kt * 128:(kt + 1) * 128], qh[0:64, t0:t0 + n], start=True, stop=True),
                          reads=[kh, qh], writes=[ps])
                for (idx, kt, ps, pb) in grp:
                    fw.op("act", lambda e: e.activation(out=pb[:, 0:n], in_=ps[:, 0:n], func=AF.Exp, scale=0.125), reads=[ps], writes=[pb])
                pend.append(grp)
                if len(pend) > LA:
                    self._pvg(pend.pop(0), accs, vaug, lsep, n, NK)
            while pend:
                self._pvg(pend.pop(0), accs, vaug, lsep, n, NK)
            finish(t0, n, ic)

    def _pvg(self, grp, accs, vaug, lsep, n, NK):
        fw = self.fw
        fw._deps("pe", [x[3] for x in reversed(grp)] + [vaug], [])
        for (idx, kt, ps, pb) in grp:
            first, last = idx == 0, idx == NK - 1
            fw.op("pe", lambda e: e.matmul(accs[0][:, 0:n], vaug[:, kt, :], pb[:, 0:n], start=first, stop=last), reads=[vaug, pb], writes=[accs[0]], sig=(last and not lsep))
            if lsep:
                fw.op("pe", lambda e: e.matmul(accs[1][:, 0:n], self.ones[:, :], pb[:, 0:n], start=first, stop=last), reads=[self.ones, pb], writes=[accs[1]], sig=last)

    def bank_s(self):
        self.rs = (getattr(self, "rs", 0) + 1) % 3
        return self.psb[self.rs]

    def qblocks(self):
        return BLK if self.need_ctx else BLK[1:]

    def ph_gqa(self):
        fw, l = self.fw, self.l
        with fw.scope():
            qh = [fw.sb([64, T], BF16, f"gq{i}") for i in range(2)]
            kh = [fw.sb([64, T], BF16, f"gk{i}") for i in range(2)]
            vaug = [fw.sb([128, NT, 128], BF16, f"gv{i}") for i in range(2)]
            pbufs = [fw.sb([128, 512], BF16, f"gp{i}") for i in range(9)]
            rl = fw.sb([64, 512], F32, "grl")
            ob = [fw.sb([64, 512], BF16, f"gob{i}") for i in range(2)]
            for kv in range(2):
                fw.op("pool", lambda e, kv=kv: e.memset(vaug[kv][:, :, 64:128], 1.0), writes=[vaug[kv]])
                fw.dma("sp", vaug[kv][:, :, 0:64], self.VT.t[0].rearrange("(n p) c -> p n c", p=128)[:, :, kv * 64:(kv + 1) * 64],
                       reads=[self.VT], writes=[vaug[kv]], key=f"g_v{kv}")
                fw.dma("sp", kh[kv][:, :], self.QK[4].t[kv * 64:(kv + 1) * 64, :], reads=[self.QK[4]], writes=[kh[kv]], key=f"g_k{kv}")
            acc = self.psb[3]
            cnt = [0]
            for h in range(8):
                q = qh[h % 2]
                fw.dma("sp", q[:, :], self.QK[h // 2].t[(h % 2) * 64:(h % 2) * 64 + 64, :], reads=[self.QK[h // 2]], writes=[q], key=f"g_q{h % 2}")

                def fin(t0, n, ic, h=h):
                    o = ob[cnt[0] % 2]
                    cnt[0] += 1
                    fw.op("dve", lambda e: e.reciprocal(out=rl[0:64, 0:n], in_=acc[64:128, 0:n]), reads=[acc], writes=[rl])
                    fw.op("dve", lambda e: e.tensor_tensor(out=o[0:64, 0:n], in0=acc[0:64, 0:n], in1=rl[0:64, 0:n], op=ALU.mult), reads=[acc, rl], writes=[o])
                    fw.dma("sp", self.BR.t[h // 2, (h % 2) * 64:(h % 2) * 64 + 64, t0:t0 + n], o[0:64, 0:n], reads=[o], writes=[self.BR], key=f"g_o{cnt[0] % 2}")
                self.attend(q, kh[h // 4], vaug[h // 4], False, [acc], self.qblocks(), pbufs, fin,
                            sbanks=[self.psb[0], self.psb[1], self.psb[2], self.psb[4], self.psb[5], self.psb[6]], LA=1, G=3)

    def ph_diff(self):
        fw, l = self.fw, self.l
        with fw.scope():
            qh = [fw.sb([64, T], BF16, f"dq{i}") for i in range(2)]
            kh = [fw.sb([64, T], BF16, f"dk{i}") for i in range(2)]
            vv = [fw.sb([128, NT, 128], BF16, f"dv{i}") for i in range(2)]
            pbufs = [fw.sb([128, 512], BF16, f"dp{i}") for i in range(6)]
            r0 = fw.sb([128, 512], F32, "dr0")
            a0 = fw.sb([128, 512], F32, "da0")
            a1 = fw.sb([128, 512], F32, "da1")
            sq = fw.sb([128, 512], BF16, "dsq")
            rstd = fw.sb([128, 512], F32, "drstd")
            ob = [fw.sb([128, 512], BF16, f"dob{i}") for i in range(2)]
            accs = [self.psb[3], self.psb[4]]
            dsb = [self.psb[0], self.psb[1], self.psb[2], self.psb[5], self.psb[6]]
            cnt = [0]
            for h in range(4):
                v = vv[h % 2]
                fw.dma("sp", v[:, :, :], self.VT.t[1 + h].rearrange("(n p) c -> p n c", p=128), reads=[self.VT], writes=[v], key=f"d_v{h % 2}")
                for (t0, n, ic) in self.qblocks():
                    for i in range(2):
                        m = h * 2 + i
                        q, k = qh[i], kh[i]
                        if t0 == self.qblocks()[0][0]:
                            fw.dma("sp", q[:, :], self.QK[6 + m // 2].t[(m % 2) * 64:(m % 2) * 64 + 64, :], reads=[self.QK[6 + m // 2]], writes=[q], key=f"d_q{i}")
                            fw.dma("sp", k[:, :], self.QK[10 + m // 2].t[(m % 2) * 64:(m % 2) * 64 + 64, :], reads=[self.QK[10 + m // 2]], writes=[k], key=f"d_k{i}")
                        self.attend(q, k, v, True, accs, [(t0, n, ic)], pbufs, lambda *a: None, sbanks=dsb, LA=1, G=2)
                        ai = a0 if i == 0 else a1
                        fw.op("act", lambda e: e.activation(out=r0[:, 0:n], in_=accs[1][:, 0:n], func=AF.Ln), reads=[accs[1]], writes=[r0])
                        fw.op("act", lambda e: e.activation(out=r0[:, 0:n], in_=r0[:, 0:n], func=AF.Exp, scale=-1.0), reads=[r0], writes=[r0])
                        fw.op("dve", lambda e: e.tensor_tensor(out=ai[:, 0:n], in0=accs[0][:, 0:n], in1=r0[:, 0:n], op=ALU.mult), reads=[accs[0], r0], writes=[ai])
                    o = ob[cnt[0] % 2]
                    cnt[0] += 1
                    fw.op("dve", lambda e: e.scalar_tensor_tensor(out=a0[:, 0:n], in0=a1[:, 0:n], scalar=self.neglam[:, l:l + 1], in1=a0[:, 0:n], op0=ALU.mult, op1=ALU.add),
                          reads=[a1, a0, self.neglam], writes=[a0])
                    fw.op("pool", lambda e: e.tensor_tensor(out=sq[:, 0:n], in0=a0[:, 0:n], in1=a0[:, 0:n], op=ALU.mult), reads=[a0], writes=[sq])
                    ps = self.psb[0]
                    fw.op("pe", lambda e: e.matmul(ps[:, 0:n], self.ones[:, :], sq[:, 0:n], start=True, stop=True), reads=[self.ones, sq], writes=[ps])
                    fw.op("act", lambda e: e.activation(out=rstd[:, 0:n], in_=ps[:, 0:n], func=AF.Ln, bias=self.eps[:, 0:1], scale=1.0 / 128), reads=[ps, self.eps], writes=[rstd])
                    fw.op("act", lambda e: e.activation(out=rstd[:, 0:n], in_=rstd[:, 0:n], func=AF.Exp, scale=-0.5), reads=[rstd], writes=[rstd])
                    fw.op("dve", lambda e: e.scalar_tensor_tensor(out=o[:, 0:n], in0=a0[:, 0:n], scalar=self.subg[:, l:l + 1], in1=rstd[:, 0:n], op0=ALU.mult, op1=ALU.mult),
                          reads=[a0, self.subg, rstd], writes=[o])
                    fw.dma("sp", self.BR.t[4 + h, :, t0:t0 + n], o[:, 0:n], reads=[o], writes=[self.BR], key=f"d_o{cnt[0] % 2}")

    def ph_conv(self):
        fw, l = self.fw, self.l
        with fw.scope():
            upad = fw.sb([128, 4, ULEN], BF16, "upad")
            upo = fw.sb([128, 4, ULEN], BF16, "upo")
            for (a_, b_) in ((0, UOFF[0]), (UOFF[0] + 256, UOFF[1]), (UOFF[1] + 4096, ULEN)):
                fw.op("dve", lambda e: e.memset(upad[:, :, a_:b_], 0.0), writes=[upad])
                fw.op("dve", lambda e: e.memset(upo[:, :, max(a_ - 1, 0):b_ - 1 if b_ < ULEN else ULEN], 0.0), writes=[upo])
            diag = fw.sb([128, 4, 31, 128], BF16, "diag")
            ab = [fw.sb([128, T], BF16, f"ca{i}") for i in range(2)]
            gb = [fw.sb([128, T], BF16, f"cg{i}") for i in range(2)]
            for cc in range(4):
                a, g = ab[cc % 2], gb[cc % 2]
                fw.dma("sp", a[:, :], self.PT[18 + cc][:, :], reads=[self.PT[18 + cc]], writes=[a], key=f"c_a{cc % 2}")
                fw.dma("sp", g[:, :], self.PT[22 + cc][:, :], reads=[self.PT[22 + cc]], writes=[g], key=f"c_g{cc % 2}")
                fw.op("act", lambda e, g=g: e.activation(out=g[:, :], in_=g[:, :], func=AF.Sigmoid), reads=[g], writes=[g])
                fw.op("dve", lambda e, a=a, g=g, cc=cc: e.tensor_tensor(out=upad[:, cc, UOFF[0]:UOFF[0] + 256], in0=a[:, 0:256], in1=g[:, 0:256], op=ALU.mult), reads=[a, g], writes=[upad])
                fw.op("dve", lambda e, a=a, g=g, cc=cc: e.tensor_tensor(out=upad[:, cc, UOFF[1]:UOFF[1] + 4096], in0=a[:, 256:T], in1=g[:, 256:T], op=ALU.mult), reads=[a, g], writes=[upad])
                fw.op("pool", lambda e, a=a, g=g, cc=cc: e.tensor_tensor(out=upo[:, cc, UOFF[0] - 1:UOFF[0] - 1 + 256], in0=a[:, 0:256], in1=g[:, 0:256], op=ALU.mult), reads=[a, g], writes=[upo])
                fw.op("pool", lambda e, a=a, g=g, cc=cc: e.tensor_tensor(out=upo[:, cc, UOFF[1] - 1:UOFF[1] - 1 + 4096], in0=a[:, 256:T], in1=g[:, 256:T], op=ALU.mult), reads=[a, g], writes=[upo])
                for j in range(31):
                    fw.op("dve", lambda e, cc=cc, j=j: e.tensor_scalar(out=diag[:, cc, j, :], in0=self.identf[:, :], scalar1=self.convwT[:, l, cc, j:j + 1], scalar2=None, op0=ALU.mult),
                          reads=[self.identf, self.convwT], writes=[diag])
            import os as _os
            cut = int(_os.environ.get("CONV_CUT", "99"))
            if cut <= 0:
                return
            y32 = fw.sb([128, 4, 512], F32, "cy32")
            ybf = fw.sb([128, 4, 512], BF16, "cybf")
            mean = fw.sb([128, 512], F32, "cmean")
            sq = fw.sb([128, 4, 512], BF16, "csq")
            rstd = fw.sb([128, 512], F32, "crstd")
            ob = [fw.sb([128, 4, 512], BF16, f"cob{i}") for i in range(2)]
            for bi, (t0, n, ic) in enumerate(BLK):
                u0 = (UOFF[0] + t0 - UPAD) if ic else (UOFF[1] + (t0 - 256) - UPAD)
                for cc in range(4):
                    ps = self.bank()
                    for j in range(31):
                        usrc, uo = (upad, u0 + j) if (u0 + j) % 2 == 0 else (upo, u0 + j - 1)
                        fw.op("pe", lambda e, ps=ps, cc=cc, j=j: e.matmul(ps[:, 0:n], diag[:, cc, j, :], usrc[:, cc, uo:uo + n], start=(j == 0), stop=(j == 30)),
                              reads=[diag, usrc], writes=[ps], sig=(j == 30))
                    fw.op("act", lambda e, ps=ps, cc=cc: e.activation(out=y32[:, cc, 0:n], in_=ps[:, 0:n], func=AF.Identity, bias=self.convbT[:, l, cc:cc + 1], scale=1.0),
                          reads=[ps, self.convbT], writes=[y32])
                    fw.op("dve", lambda e, ps=ps, cc=cc: e.tensor_scalar(out=ybf[:, cc, 0:n], in0=ps[:, 0:n], scalar1=self.convbT[:, l, cc:cc + 1], scalar2=None, op0=ALU.add),
                          reads=[ps, self.convbT], writes=[ybf])
                if cut <= 1:
                    continue
                pm = self.bank()
                for cc in range(4):
                    fw.op("pe", lambda e, cc=cc: e.matmul(pm[:, 0:n], self.ones[:, :], ybf[:, cc, 0:n], start=(cc == 0), stop=(cc == 3)), reads=[self.ones, ybf], writes=[pm], sig=(cc == 3))
                fw.op("act", lambda e: e.activation(out=mean[:, 0:n], in_=pm[:, 0:n], func=AF.Copy, scale=1.0 / 512), reads=[pm], writes=[mean])
                for cc in range(4):
                    fw.op("dve", lambda e, cc=cc: e.tensor_tensor(out=y32[:, cc, 0:n], in0=y32[:, cc, 0:n], in1=mean[:, 0:n], op=ALU.subtract), reads=[y32, mean], writes=[y32])
                fw.op("act", lambda e: e.activation(out=sq[:, :, 0:n], in_=y32[:, :, 0:n], func=AF.Square), reads=[y32], writes=[sq])
                pv = self.bank()
                for cc in range(4):
                    fw.op("pe", lambda e, cc=cc: e.matmul(pv[:, 0:n], self.ones[:, :], sq[:, cc, 0:n], start=(cc == 0), stop=(cc == 3)), reads=[self.ones, sq], writes=[pv], sig=(cc == 3))
                fw.op("act", lambda e: e.activation(out=rstd[:, 0:n], in_=pv[:, 0:n], func=AF.Sqrt, bias=self.eps[:, 0:1], scale=1.0 / 512), reads=[pv, self.eps], writes=[rstd])
                fw.op("dve", lambda e: e.reciprocal(out=rstd[:, 0:n], in_=rstd[:, 0:n]), reads=[rstd], writes=[rstd])
                if cut <= 2:
                    continue
                o = ob[bi % 2]
                for cc in range(4):
                    fw.op("dve", lambda e, cc=cc: e.scalar_tensor_tensor(out=y32[:, cc, 0:n], in0=y32[:, cc, 0:n], scalar=self.clngT[:, l, cc:cc + 1], in1=rstd[:, 0:n], op0=ALU.mult, op1=ALU.mult),
                          reads=[y32, self.clngT, rstd], writes=[y32])
                    fw.op("act", lambda e, cc=cc: e.activation(out=o[:, cc, 0:n], in_=y32[:, cc, 0:n], func=AF.Silu, bias=self.clnbT[:, l, cc:cc + 1], scale=1.0),
                          reads=[y32, self.clnbT], writes=[o])
                fw.dma("sp", self.BR.t[8:12, :, t0:t0 + n].rearrange("c p t -> p c t"), o[:, :, 0:n], reads=[o], writes=[self.BR], key=f"c_o{bi % 2}")

    def ph_hgrn(self):
        fw, l = self.fw, self.l
        NCH = T // 64
        for d in range(2):
            for hp in range(4):
                with fw.scope():
                    vtok = fw.sb([128, NT, 128], BF16, "hv")
                    fw.dma("sp", vtok[:, :, :], self.VT.t[5 + hp].rearrange("(n p) c -> p n c", p=128), reads=[self.VT], writes=[vtok], key="h_v")
                    mask = fw.sb([128, 256], F32, "hmask")
                    mname = "mask_f" if d == 0 else "mask_b"
                    fw.dma("sp", mask[:, :], self.CI[mname][:, 0:256], reads=[self.CI[mname]], writes=[mask], key="h_m")
                    maski = mask.t[:, :].bitcast(mybir.dt.int32)
                    qT = [fw.sb([64, T], BF16, f"hq{i}") for i in range(2)]
                    qpT = [fw.sb([64, T], BF16, f"hqp{i}") for i in range(2)]
                    kT = [fw.sb([64, T], BF16, f"hk{i}") for i in range(2)]
                    dec2 = fw.sb([64, 2, NCH], F32, "hdec2")
                    ktok = fw.sb([128, NT, 128], BF16, "hkt")
                    dec = fw.sb([128, NCH], F32, "hdec")
                    with fw.scope():
                        onesf = fw.sb([128, 64], F32, "h1")
                        fw.op("pool", lambda e: e.memset(onesf[:, :], 1.0), writes=[onesf])
                        zin = fw.sb([128, T], BF16, "hz")
                        qin = fw.sb([128, T], BF16, "hqi")
                        qs = fw.sb([128, T], F32, "hqs")
                        f = fw.sb([128, T], F32, "hf")
                        lf = fw.sb([128, T], F32, "hlf")
                        cum = fw.sb([128, T], F32, "hcum")
                        kk = fw.sb([128, T], F32, "hkk")
                        ex = f
                        khT = fw.sb([128, T], BF16, "hkh")
                        c3 = cum.t.rearrange("p (c s) -> p c s", s=64)
                        e3 = ex.t.rearrange("p (c s) -> p c s", s=64)
                        iend, imid = (63, 31) if d == 0 else (0, 32)
                        fw.dma("sp", qin[:, :], self.PT[26 + hp][:, :], reads=[self.PT[26 + hp]], writes=[qin], key="h_qi")
                        fw.dma("sp", zin[:, :], self.PT[30 + 4 * d + hp][:, :], reads=[self.PT[30 + 4 * d + hp]], writes=[zin], key="h_zi")
                        fw.op("act", lambda e: e.activation(out=qs[:, :], in_=qin[:, :], func=AF.Silu), reads=[qin], writes=[qs])
                        fw.op("act", lambda e: e.activation(out=f[:, :], in_=zin[:, :], func=AF.Sigmoid), reads=[zin], writes=[f])
                        li = d * 4 + hp
                        fw.op("dve", lambda e: e.tensor_scalar(out=f[:, :], in0=f[:, :], scalar1=self.omlT[:, l, li:li + 1], scalar2=self.lbT[:, l, li:li + 1], op0=ALU.mult, op1=ALU.add),
                              reads=[f, self.omlT, self.lbT], writes=[f])
                        fw.op("act", lambda e: e.activation(out=lf[:, :], in_=f[:, :], func=AF.Ln), reads=[f], writes=[lf])
                        fw.op("pool", lambda e: e.tensor_scalar(out=kk[:, :], in0=f[:, :], scalar1=-1.0, scalar2=1.0, op0=ALU.mult, op1=ALU.add), reads=[f], writes=[kk])
                        for c in range(NCH):
                            fw.op("dve", lambda e: e.tensor_tensor_scan(out=cum[:, c * 64:(c + 1) * 64], data0=onesf[:, :], data1=lf[:, c * 64:(c + 1) * 64], initial=0.0,
                                                                       op0=ALU.mult, op1=ALU.add), reads=[onesf, lf], writes=[cum], sig=(c == NCH - 1))
                        if d == 1:
                            fw.op("dve", lambda e: e.tensor_tensor(out=e3, in0=c3[:, :, 63:64].broadcast_to([128, NCH, 64]), in1=c3, op=ALU.subtract), reads=[cum], writes=[ex])
                            fw.op("dve", lambda e: e.tensor_tensor(out=cum[:, :], in0=ex[:, :], in1=lf[:, :], op=ALU.add), reads=[ex, lf], writes=[cum])
                        fw.op("act", lambda e: e.activation(out=dec[:, :], in_=c3[:, :, iend], func=AF.Exp), reads=[cum], writes=[dec])
                        for hh in range(2):
                            fw.op("dve", lambda e: e.tensor_copy(out=dec2[0:64, hh, :], in_=dec[hh * 64:(hh + 1) * 64, :]), reads=[dec], writes=[dec2])
                        fw.op("act", lambda e: e.activation(out=ex[:, :], in_=cum[:, :], func=AF.Exp), reads=[cum], writes=[ex])
                        for hh in range(2):
                            fw.op("dve", lambda e: e.scalar_tensor_tensor(out=qpT[hh][0:64, :], in0=qs[hh * 64:(hh + 1) * 64, :], scalar=0.125, in1=ex[hh * 64:(hh + 1) * 64, :], op0=ALU.mult, op1=ALU.mult),
                                  reads=[qs, ex], writes=[qpT[hh]])
                        fw.op("dve", lambda e: e.tensor_tensor(out=e3, in0=c3[:, :, iend:iend + 1].broadcast_to([128, NCH, 64]), in1=c3, op=ALU.subtract), reads=[cum], writes=[ex])
                        fw.op("act", lambda e: e.activation(out=ex[:, :], in_=ex[:, :], func=AF.Exp), reads=[ex], writes=[ex])
                        fw.op("dve", lambda e: e.tensor_tensor(out=khT[:, :], in0=kk[:, :], in1=ex[:, :], op=ALU.mult), reads=[kk, ex], writes=[khT])
                        fw.op("dve", lambda e: e.tensor_tensor(out=e3, in0=c3, in1=c3[:, :, imid:imid + 1].broadcast_to([128, NCH, 64]), op=ALU.subtract), reads=[cum], writes=[ex])
                        fw.op("act", lambda e: e.activation(out=lf[:, :], in_=ex[:, :], func=AF.Exp), reads=[ex], writes=[lf])
                        for hh in range(2):
                            fw.op("dve", lambda e: e.scalar_tensor_tensor(out=qT[hh][0:64, :], in0=qs[hh * 64:(hh + 1) * 64, :], scalar=0.125, in1=lf[hh * 64:(hh + 1) * 64, :], op0=ALU.mult, op1=ALU.mult),
                                  reads=[qs, lf], writes=[qT[hh]])
                        fw.op("act", lambda e: e.activation(out=lf[:, :], in_=ex[:, :], func=AF.Exp, scale=-1.0), reads=[ex], writes=[lf])
                        for hh in range(2):
                            fw.op("dve", lambda e: e.tensor_tensor(out=kT[hh][0:64, :], in0=kk[hh * 64:(hh + 1) * 64, :], in1=lf[hh * 64:(hh + 1) * 64, :], op=ALU.mult), reads=[kk, lf], writes=[kT[hh]])
                        import os as _os
                        hcut = int(_os.environ.get("HG_CUT", "99"))
                        for tt in range(NT if hcut > 1 else 0):
                            fw.op("pe", lambda e: e.transpose(self.psT[:, (tt % 8) * 128:(tt % 8) * 128 + 128], khT[:, tt * 128:(tt + 1) * 128], self.identb[:, :]),
                                  reads=[khT, self.identb], writes=[self.psT], sig=(tt % 8 == 7 or tt == NT - 1))
                            if tt % 8 == 7 or tt == NT - 1:
                                t8 = tt - (tt % 8)
                                nn = tt - t8 + 1
                                fw.op("act", lambda e: e.activation(out=ktok[:, t8:t8 + nn, :], in_=self.psT.t[:, 0:nn * 128].rearrange("p (a c) -> p a c", c=128), func=AF.Copy),
                                      reads=[self.psT], writes=[ktok])
                    S = fw.sb([64, 2, 64], F32, "hS")
                    Sb = [fw.sb([64, 2, 64], BF16, f"hSb{i}") for i in range(2)]
                    fw.op("dve", lambda e: e.memset(S[:, :, :], 0.0), writes=[S])
                    fw.op("dve", lambda e: e.memset(Sb[0][:, :, :], 0.0), writes=[Sb[0]])
                    attm = [fw.sb([128, 256], BF16, f"hatt{i}") for i in range(2)]
                    for a_ in attm:
                        fw.op("dve", lambda e: e.memset(a_[:, :], 0.0), writes=[a_])
                    ost = fw.sb([64, 2, T], F32, "host")
                    tiles = list(range(NT)) if d == 0 else [1, 0] + list(range(NT - 1, 1, -1))
                    if hcut <= 2:
                        tiles = []
                    sbi = 0
                    for ti, tt in enumerate(tiles):
                        am = attm[ti % 2]
                        pa = self.psb[1 + ti % 2]
                        for hh in range(2):
                            r0 = hh * 64
                            fw.op("pe", lambda e: e.matmul(pa[:, hh * 128:hh * 128 + 128], kT[hh][0:64, tt * 128:(tt + 1) * 128], qT[hh][0:64, tt * 128:(tt + 1) * 128], start=True, stop=True),
                                  reads=[kT[hh], qT[hh]], writes=[pa], sig=(hh == 1))
                        fw.op("dve", lambda e: e.copy_predicated(out=am[:, :], mask=maski, data=pa[:, 0:256]), reads=[pa, mask], writes=[am])
                        if hcut <= 3:
                            continue
                        po = self.psb[3 + ti % 2]
                        chunks = [0, 1] if d == 0 else [1, 0]
                        if hcut <= 4:
                            chunks = []
                        for hh in range(2):
                            fw.op("pe", lambda e: e.matmul(po[0:64, hh * 128:hh * 128 + 128], vtok[:, tt, hh * 64:(hh + 1) * 64], am[:, hh * 128:(hh + 1) * 128],
                                                           start=(hh == 0), stop=False, skip_group_check=True), reads=[vtok, am], writes=[po], sig=False)
                        for ci, cj in enumerate(chunks):
                            c = tt * 2 + cj
                            sb_cur = Sb[sbi % 2]
                            sb_nxt = Sb[(sbi + 1) % 2]
                            sbi += 1
                            for hh in range(2):
                                r0 = hh * 64
                                fw.op("pe", lambda e: e.matmul(po[0:64, hh * 128 + cj * 64:hh * 128 + cj * 64 + 64], sb_cur[0:64, hh, :], qpT[hh][0:64, c * 64:(c + 1) * 64],
                                                               start=False, stop=(ci == 1), skip_group_check=True), reads=[sb_cur, qpT[hh]], writes=[po], sig=(hh == 1))
                            pS = self.psb[5 + sbi % 2]
                            for hh in range(2):
                                fw.op("pe", lambda e: e.matmul(pS[0:64, hh * 64:(hh + 1) * 64], ktok[cj * 64:cj * 64 + 64, tt, hh * 64:(hh + 1) * 64], vtok[cj * 64:cj * 64 + 64, tt, hh * 64:(hh + 1) * 64],
                                                               start=(hh == 0), stop=(hh == 1), skip_group_check=True), reads=[ktok, vtok], writes=[pS], sig=(hh == 1))
                            for hh in range(2):
                                fw.op("dve", lambda e: e.scalar_tensor_tensor(out=S[0:64, hh, :], in0=S[0:64, hh, :], scalar=dec2[0:64, hh, c:c + 1], in1=pS[0:64, hh * 64:(hh + 1) * 64], op0=ALU.mult, op1=ALU.add),
                                      reads=[S, dec2, pS], writes=[S])
                            fw.op("act", lambda e: e.activation(out=sb_nxt[:, :, :], in_=S[:, :, :], func=AF.Copy), reads=[S], writes=[sb_nxt])
                        fw.op("act", lambda e: e.activation(out=ost[:, :, tt * 128:(tt + 1) * 128], in_=po.t[0:64, 0:256].rearrange("p (a c) -> p a c", c=128), func=AF.Copy),
                              reads=[po], writes=[ost])
                    fw.dma("sp", self.OD[d].t[2 * hp:2 * hp + 2].rearrange("h e t -> e h t"), ost[:, :, :], reads=[ost], writes=[self.OD[d]], key="h_o")
        with fw.scope():
            of = [fw.sb([64, T], F32, f"hof{i}") for i in range(2)]
            obw = [fw.sb([64, T], F32, f"hobw{i}") for i in range(2)]
            gt = [fw.sb([64, T], BF16, f"hgt{i}") for i in range(2)]
            sq = fw.sb([64, 512], BF16, "hsq")
            rstd = fw.sb([64, 512], F32, "hrstd")
            sg = fw.sb([64, 512], F32, "hsg")
            ob = [fw.sb([64, T], BF16, f"hob{i}") for i in range(2)]
            for h in range(8):
                a, b, g, o = of[h % 2], obw[h % 2], gt[h % 2], ob[h % 2]
                fw.dma("sp", a[:, :], self.OD[0].t[h], reads=[self.OD[0]], writes=[a], key=f"hn_a{h % 2}")
                fw.dma("sp", b[:, :], self.OD[1].t[h], reads=[self.OD[1]], writes=[b], key=f"hn_b{h % 2}")
                fw.dma("sp", g[:, :], self.PT[42 + h // 2].t[(h % 2) * 64:(h % 2) * 64 + 64, :], reads=[self.PT[42 + h // 2]], writes=[g], key=f"hn_g{h % 2}")
                fw.op("pool", lambda e, a=a, b=b: e.tensor_tensor(out=a[:, :], in0=a[:, :], in1=b[:, :], op=ALU.add), reads=[a, b], writes=[a])
                for (t0, n, ic) in BLK:
                    fw.op("act", lambda e, a=a: e.activation(out=sq[:, 0:n], in_=a[:, t0:t0 + n], func=AF.Square), reads=[a], writes=[sq])
                    ps = self.bank()
                    fw.op("pe", lambda e, ps=ps: e.matmul(ps[0:64, 0:n], self.ones[0:64, 0:64], sq[0:64, 0:n], start=True, stop=True), reads=[self.ones, sq], writes=[ps])
                    fw.op("act", lambda e, ps=ps: e.activation(out=rstd[:, 0:n], in_=ps[0:64, 0:n], func=AF.Sqrt, bias=self.eps[0:64, 0:1], scale=1.0 / 64), reads=[ps, self.eps], writes=[rstd])
                    fw.op("dve", lambda e: e.reciprocal(out=rstd[:, 0:n], in_=rstd[:, 0:n]), reads=[rstd], writes=[rstd])
                    fw.op("act", lambda e, g=g: e.activation(out=sg[:, 0:n], in_=g[:, t0:t0 + n], func=AF.Silu), reads=[g], writes=[sg])
                    fw.op("dve", lambda e: e.scalar_tensor_tensor(out=rstd[:, 0:n], in0=rstd[:, 0:n], scalar=self.hgT[:, l:l + 1], in1=sg[:, 0:n], op0=ALU.mult, op1=ALU.mult),
                          reads=[rstd, self.hgT, sg], writes=[rstd])
                    fw.op("dve", lambda e, a=a, o=o: e.tensor_tensor(out=o[:, t0:t0 + n], in0=a[:, t0:t0 + n], in1=rstd[:, 0:n], op=ALU.mult), reads=[a, rstd], writes=[o])
                fw.dma("sp", self.BR.t[12 + h // 2, (h % 2) * 64:(h % 2) * 64 + 64, :], o[:, :], reads=[o], writes=[self.BR], key=f"hn_o{h % 2}")

    def ph_merge(self):
        fw, l = self.fw, self.l
        with fw.scope():
            wb = fw.sb([128, 16, D], BF16, "wbr")
            wo = fw.sb([128, 8, D], BF16, "wout")
            for i in range(4):
                fw.dma("pool", wb[:, i * 4:(i + 1) * 4, :], self.I["w_branch"].t[l, i].rearrange("(k p) c -> p k c", p=128), reads=[self.I["w_branch"]], writes=[wb], key="m_wb")
            fw.dma("pool", wo[:, :, :], self.I["w_out"].t[l].rearrange("(k p) c -> p k c", p=128), reads=[self.I["w_out"]], writes=[wo], key="m_wo")
            brs = [fw.sb([128, 16, 512], BF16, f"mbr{i}") for i in range(1)]
            gts = [fw.sb([128, 32, 512], BF16, f"mgt{i}") for i in range(1)]
            xbs = [fw.sb([128, 8, 512], F32, f"mxb{i}") for i in range(1)]
            mg = fw.sb([128, 512], F32, "mmg")
            tmp = fw.sb([128, 512], F32, "mtmp")
            mgb = fw.sb([128, 8, 512], BF16, "mmgb")
            xo = [fw.sb([128, 8, 512], F32, f"mxo{i}") for i in range(2)]
            src = self.xsrc()
            for bi, (t0, n, ic) in enumerate(self.qblocks()):
                br, gt, xb, xn = brs[0], gts[0], xbs[0], xo[bi % 2]
                fw.dma("sp", br[:, :, 0:n], self.BR.t[:, :, t0:t0 + n].rearrange("c p t -> p c t"), reads=[self.BR], writes=[br], key="m_br")
                fw.dma("sp", gt[:, :, 0:n], self.PTall.t[:, :, t0:t0 + n].rearrange("c p t -> p c t"), reads=[self.PTall], writes=[gt], key="m_gt")
                fw.dma("sp", xb[:, :, 0:n], src.t.rearrange("(k p) t -> p k t", p=128)[:, :, t0:t0 + n], reads=[src], writes=[xb], key="m_xb")
                for oc in range(8):
                    for i in range(4):
                        ps = self.bank()
                        for k in range(4):
                            fw.op("pe", lambda e, ps=ps, i=i, k=k, oc=oc: e.matmul(ps[:, 0:n], wb[:, i * 4 + k, oc * 128:(oc + 1) * 128], br[:, i * 4 + k, 0:n], start=(k == 0), stop=(k == 3)),
                                  reads=[wb, br], writes=[ps], sig=(k == 3))
                        if i == 0:
                            fw.op("dve", lambda e, ps=ps, oc=oc: e.tensor_tensor(out=mg[:, 0:n], in0=ps[:, 0:n], in1=gt[:, oc, 0:n], op=ALU.mult), reads=[ps, gt], writes=[mg])
                        else:
                            fw.op("dve", lambda e, ps=ps, oc=oc, i=i: e.tensor_tensor(out=tmp[:, 0:n], in0=ps[:, 0:n], in1=gt[:, i * 8 + oc, 0:n], op=ALU.mult), reads=[ps, gt], writes=[tmp])
                            if i < 3:
                                fw.op("pool", lambda e: e.tensor_tensor(out=mg[:, 0:n], in0=mg[:, 0:n], in1=tmp[:, 0:n], op=ALU.add), reads=[mg, tmp], writes=[mg])
                            else:
                                fw.op("pool", lambda e, oc=oc: e.tensor_tensor(out=mgb[:, oc, 0:n], in0=mg[:, 0:n], in1=tmp[:, 0:n], op=ALU.add), reads=[mg, tmp], writes=[mgb])
                for o2 in range(8):
                    ps = self.bank()
                    for k in range(8):
                        fw.op("pe", lambda e, ps=ps, k=k, o2=o2: e.matmul(ps[:, 0:n], wo[:, k, o2 * 128:(o2 + 1) * 128], mgb[:, k, 0:n], start=(k == 0), stop=(k == 7)),
                              reads=[wo, mgb], writes=[ps], sig=(k == 7))
                    fw.op("dve", lambda e, ps=ps, o2=o2: e.scalar_tensor_tensor(out=xn[:, o2, 0:n], in0=ps[:, 0:n], scalar=self.modt[:, 16 + o2, ic:ic + 1], in1=xb[:, o2, 0:n],
                                                                         op0=ALU.mult, op1=ALU.add), reads=[ps, self.modt, xb], writes=[xn])
                fw.dma("sp", self.xs.t.rearrange("(k p) t -> p k t", p=128)[:, :, t0:t0 + n], xn[:, :, 0:n], reads=[xn], writes=[self.xs], key=f"m_xo{bi % 2}")
        self.x_written = True

    def ph_ffn(self):
        fw, l = self.fw, self.l
        moe = (l % 2 == 1)
        li = l // 2
        JG = 512
        NJG = DFF // JG
        blocks = self.qblocks()
        halves = [blocks[:len(blocks) - 4], blocks[len(blocks) - 4:]]
        for half in halves:
            if not half:
                continue
            hb0 = half[0][0]
            hlen = sum(b[1] for b in half)
            with fw.scope():
                hT = fw.sb([128, 8, hlen], BF16, "fh")
                with fw.scope():
                    xbs = [fw.sb([128, 8, 512], F32, f"fxb{i}") for i in range(2)]
                    sq = fw.sb([128, 8, 512], BF16, "fsq")
                    rstd = fw.sb([128, 512], F32, "frstd")
                    tmp = fw.sb([128, 8, 512], F32, "ftmp")
                    h32 = fw.sb([128, 8, 512], F32, "fh32") if moe else None
                    if moe:
                        lg = fw.sb([128, 8], F32, "flg")
                        m1 = fw.sb([128, 1], F32, "fm1")
                        m2 = fw.sb([128, 1], F32, "fm2")
                        k1 = fw.sb([128, 8], F32, "fk1")
                        k2 = fw.sb([128, 8], F32, "fk2")
                        l2 = fw.sb([128, 8], F32, "fl2")
                        w1 = fw.sb([128, 1], F32, "fw1")
                        w2 = fw.sb([128, 1], F32, "fw2")
                        cmb = fw.sb([128, 8], F32, "fcmb")
                        cbm = fw.sb([128, 8, 128], F32, "fcbm")
                        cbo = [fw.sb([128, 8, 512], BF16, f"fcbo{i}") for i in range(2)]
                    for bi, (t0, n, ic) in enumerate(half):
                        self.norm_block(1, t0, n, ic, hT, t0 - hb0, xbs[bi % 2], sq, rstd, tmp, h32)
                        if not moe:
                            continue
                        co = cbo[bi % 2]
                        for ti in range(n // 128):
                            ps = self.bank()
                            for k in range(8):
                                fw.op("pe", lambda e, ps=ps, k=k, ti=ti: e.matmul(ps[:, 0:8], h32[:, k, ti * 128:(ti + 1) * 128], self.routerT[:, li, k, :], start=(k == 0), stop=(k == 7)),
                                      reads=[h32, self.routerT], writes=[ps], sig=(k == 7))
                            fw.op("dve", lambda e, ps=ps: e.tensor_copy(out=lg[:, :], in_=ps[:, 0:8]), reads=[ps], writes=[lg])
                            fw.op("dve", lambda e: e.reduce_max(out=m1[:, :], in_=lg[:, :], axis=AX.X), reads=[lg], writes=[m1])
                            fw.op("dve", lambda e: e.tensor_scalar(out=k1[:, :], in0=lg[:, :], scalar1=m1[:, 0:1], scalar2=None, op0=ALU.is_ge), reads=[lg, m1], writes=[k1])
                            fw.op("dve", lambda e: e.scalar_tensor_tensor(out=l2[:, :], in0=k1[:, :], scalar=-1e30, in1=lg[:, :], op0=ALU.mult, op1=ALU.add), reads=[k1, lg], writes=[l2])
                            fw.op("dve", lambda e: e.reduce_max(out=m2[:, :], in_=l2[:, :], axis=AX.X), reads=[l2], writes=[m2])
                            fw.op("dve", lambda e: e.tensor_scalar(out=k2[:, :], in0=l2[:, :], scalar1=m2[:, 0:1], scalar2=None, op0=ALU.is_ge), reads=[l2, m2], writes=[k2])
                            fw.op("dve", lambda e: e.tensor_tensor(out=w2[:, :], in0=m2[:, :], in1=m1[:, :], op=ALU.subtract), reads=[m1, m2], writes=[w2])
                            fw.op("act", lambda e: e.activation(out=w2[:, :], in_=w2[:, :], func=AF.Exp), reads=[w2], writes=[w2])
                            fw.op("dve", lambda e: e.tensor_scalar(out=w1[:, :], in0=w2[:, :], scalar1=1.0, scalar2=None, op0=ALU.add), reads=[w2], writes=[w1])
                            fw.op("dve", lambda e: e.reciprocal(out=w1[:, :], in_=w1[:, :]), reads=[w1], writes=[w1])
                            fw.op("dve", lambda e: e.tensor_scalar(out=w2[:, :], in0=w1[:, :], scalar1=-1.0, scalar2=1.0, op0=ALU.mult, op1=ALU.add), reads=[w1], writes=[w2])
                            fw.op("dve", lambda e: e.tensor_scalar(out=cmb[:, :], in0=k1[:, :], scalar1=w1[:, 0:1], scalar2=None, op0=ALU.mult), reads=[k1, w1], writes=[cmb])
                            fw.op("dve", lambda e: e.scalar_tensor_tensor(out=cmb[:, :], in0=k2[:, :], scalar=w2[:, 0:1], in1=cmb[:, :], op0=ALU.mult, op1=ALU.add), reads=[k2, w2, cmb], writes=[cmb])
                            fw.op("dve", lambda e: e.tensor_copy(out=cbm[:, :, :], in_=cmb.t[:, :].unsqueeze(2).broadcast_to([128, 8, 128])), reads=[cmb], writes=[cbm])
                            for eh in range(2):
                                pc = self.bank()
                                for e4 in range(4):
                                    ex = eh * 4 + e4
                                    fw.op("pe", lambda e, pc=pc, e4=e4, ex=ex: e.matmul(pc[:, e4 * 128:(e4 + 1) * 128], cbm[:, ex, :], self.identf[:, :], start=True, stop=True),
                                          reads=[cbm, self.identf], writes=[pc], sig=(e4 == 3))
                                fw.op("act", lambda e, pc=pc, eh=eh, ti=ti, co=co: e.activation(out=co[:, eh * 4:eh * 4 + 4, ti * 128:(ti + 1) * 128],
                                                                                     in_=pc.t.rearrange("p (a c) -> p a c", c=128), func=AF.Copy), reads=[pc], writes=[co])
                        fw.dma("sp", self.CB.t[:, :, t0:t0 + n].rearrange("e p t -> p e t"), co[:, :, 0:n], reads=[co], writes=[self.CB], key=f"f_cb{bi % 2}")
                acc = fw.sb([128, 8, hlen], F32, "facc")
                self._ffn_experts(moe, li, half, hb0, hT, acc)
                self._ffn_resid(half, hb0, acc)

    def _ffn_experts(self, moe, li, half, hb0, hT, acc):
        fw, l = self.fw, self.l
        JG = 512
        NJG = DFF // JG
        with fw.scope():
            if True:
                w1s = [fw.sb([128, 8, JG], BF16, f"fw1_{i}") for i in range(2)]
                w3s = [fw.sb([128, 8, JG], BF16, f"fw3_{i}") for i in range(2)]
                w2s = [fw.sb([128, JG // 128, D], BF16, f"fw2_{i}") for i in range(2)]
                sa = [fw.sb([128, 512], BF16, f"fsa{i}") for i in range(2)]
                gb = [fw.sb([128, JG // 128, 512], BF16, f"fgb{i}") for i in range(2)]
                cbl = [fw.sb([128, 512], BF16, f"fcl{i}") for i in range(2)]
                items = []
                ng = 0
                for ex in range(NEXP if moe else 1):
                    for jg in range(NJG):
                        for bi_, (t0, n, ic) in enumerate(half):
                            items.append((ex, jg, ng, bi_ == 0, t0, n))
                        ng += 1
                NJC = JG // 128

                def stage_a(it, slot):
                    ex, jg, gi, firstb, t0, n = it
                    wa, wc, wd = w1s[gi % 2], w3s[gi % 2], w2s[gi % 2]
                    if firstb:
                        if moe:
                            s1, s3, s2 = self.I["moe_w1"].t[li, ex], self.I["moe_w3"].t[li, ex], self.I["moe_w2"].t[li, ex]
                            r1, r3, r2 = self.I["moe_w1"], self.I["moe_w3"], self.I["moe_w2"]
                        else:
                            s1, s3, s2 = self.I["ffn_w1"].t[li], self.I["ffn_w3"].t[li], self.I["ffn_w2"].t[li]
                            r1, r3, r2 = self.I["ffn_w1"], self.I["ffn_w3"], self.I["ffn_w2"]
                        fw.dma("pool", wa[:, :, :], s1.rearrange("(k p) c -> p k c", p=128)[:, :, jg * JG:(jg + 1) * JG], reads=[r1], writes=[wa], key=f"f_w1{gi % 2}")
                        fw.dma("pool", wc[:, :, :], s3.rearrange("(k p) c -> p k c", p=128)[:, :, jg * JG:(jg + 1) * JG], reads=[r3], writes=[wc], key=f"f_w3{gi % 2}")
                        fw.dma("pool", wd[:, :, :], s2[jg * JG:(jg + 1) * JG, :].rearrange("(k p) c -> p k c", p=128), reads=[r2], writes=[wd], key=f"f_w2{gi % 2}")
                    c0 = t0 - hb0
                    g = gb[slot % 2]
                    cl = cbl[slot % 2]
                    if moe:
                        fw.dma("sp", cl[:, 0:n], self.CB.t[ex, :, t0:t0 + n], reads=[self.CB], writes=[cl], key=f"f_cl{slot % 2}")
                    for jc in range(NJC):
                        p1 = self.psb[1 + (jc % 2) * 2]
                        p3 = self.psb[2 + (jc % 2) * 2]
                        for k in range(8):
                            fw.op("pe", lambda e: e.matmul(p1[:, 0:n], wa[:, k, jc * 128:(jc + 1) * 128], hT[:, k, c0:c0 + n], start=(k == 0), stop=(k == 7)),
                                  reads=[wa, hT], writes=[p1], sig=(k == 7))
                        for k in range(8):
                            fw.op("pe", lambda e: e.matmul(p3[:, 0:n], wc[:, k, jc * 128:(jc + 1) * 128], hT[:, k, c0:c0 + n], start=(k == 0), stop=(k == 7)),
                                  reads=[wc, hT], writes=[p3], sig=(k == 7))
                        sx = sa[jc % 2]
                        fw.op("act", lambda e: e.activation(out=sx[:, 0:n], in_=p1[:, 0:n], func=AF.Silu), reads=[p1], writes=[sx])
                        fw.op("dve", lambda e: e.tensor_tensor(out=g[:, jc, 0:n], in0=p3[:, 0:n], in1=sx[:, 0:n], op=ALU.mult), reads=[p3, sx], writes=[g])
                        if moe:
                            fw.op("pool", lambda e: e.tensor_tensor(out=g[:, jc, 0:n], in0=g[:, jc, 0:n], in1=cl[:, 0:n], op=ALU.mult), reads=[g, cl], writes=[g])

                def stage_w(it, slot):
                    ex, jg, gi, firstb, t0, n = it
                    wd = w2s[gi % 2]
                    c0 = t0 - hb0
                    g = gb[slot % 2]
                    for o in range(8):
                        pf = self.psb[5 + (o % 2)]
                        for jc in range(NJC):
                            fw.op("pe", lambda e: e.matmul(pf[:, 0:n], wd[:, jc, o * 128:(o + 1) * 128], g[:, jc, 0:n], start=(jc == 0), stop=(jc == NJC - 1)),
                                  reads=[wd, g], writes=[pf], sig=(jc == NJC - 1))
                        if gi == 0:
                            fw.op("act", lambda e: e.activation(out=acc[:, o, c0:c0 + n], in_=pf[:, 0:n], func=AF.Copy), reads=[pf], writes=[acc])
                        else:
                            fw.op("dve", lambda e: e.tensor_tensor(out=acc[:, o, c0:c0 + n], in0=acc[:, o, c0:c0 + n], in1=pf[:, 0:n], op=ALU.add), reads=[pf, acc], writes=[acc])

                for i, it in enumerate(items):
                    stage_a(it, i)
                    if i > 0:
                        stage_w(items[i - 1], i - 1)
                stage_w(items[-1], len(items) - 1)

    def _ffn_resid(self, half, hb0, acc):
        fw, l = self.fw, self.l
        with fw.scope():
            if True:
                xbs = [fw.sb([128, 8, 512], F32, f"fxr{i}") for i in range(2)]
                for bi, (t0, n, ic) in enumerate(half):
                    c0 = t0 - hb0
                    xb = xbs[bi % 2]
                    fw.dma("sp", xb[:, :, 0:n], self.xs.t.rearrange("(k p) t -> p k t", p=128)[:, :, t0:t0 + n], reads=[self.xs], writes=[xb], key=f"f_xr{bi % 2}")
                    for k in range(8):
                        fw.op("dve", lambda e, k=k: e.scalar_tensor_tensor(out=xb[:, k, 0:n], in0=acc[:, k, c0:c0 + n], scalar=self.modt[:, 40 + k, ic:ic + 1], in1=xb[:, k, 0:n],
                                                                          op0=ALU.mult, op1=ALU.add), reads=[acc, self.modt, xb], writes=[xb])
                    fw.dma("sp", self.xs.t.rearrange("(k p) t -> p k t", p=128)[:, :, t0:t0 + n], xb[:, :, 0:n], reads=[xb], writes=[self.xs], key=f"f_xw{bi % 2}")


def host_inputs(inp, b):
    f = np.float32
    def colT(v):
        v = np.asarray(v, f)
        sh = v.shape
        v = v.reshape(sh[:-1] + (sh[-1] // 128, 128))
        return np.ascontiguousarray(np.moveaxis(v, -1, 0))
    m = {}
    m["xT0"] = np.ascontiguousarray(np.concatenate([inp["ctx"][b], inp["x"][b]], axis=0).T.astype(f))
    m["cvec"] = np.ascontiguousarray(np.stack([colT(inp["c"][b]), colT(inp["c_ctx"])], axis=-1))
    m["ada_w"] = np.asarray(inp["ada_w"], f)
    m["ada_bT"] = colT(inp["ada_b"])
    m["g1T"] = colT(inp["norm1_g"])
    m["g2T"] = colT(inp["norm2_g"])
    m["w_in"] = np.asarray(inp["w_in"], f)
    qkg = np.asarray(inp["qk_norm_g"], f)
    m["qkgT"] = np.ascontiguousarray(np.tile(np.transpose(qkg, (2, 0, 1)), (2, 1, 1)))
    m["lamB"] = np.ascontiguousarray(np.broadcast_to(np.asarray(inp["diff_lambda"], f).reshape(1, DEPTH, 256), (128, DEPTH, 256)))
    m["sublnT"] = np.ascontiguousarray(np.asarray(inp["diff_subln_g"], f).T)
    cw = np.asarray(inp["conv_w"], f)
    m["convwT"] = np.ascontiguousarray(np.transpose(cw.reshape(DEPTH, 31, 4, 128), (3, 0, 2, 1)))
    m["convbT"] = colT(inp["conv_b"])
    m["clngT"] = colT(inp["conv_ln_g"])
    m["clnbT"] = colT(inp["conv_ln_b"])
    m["lblT"] = colT(inp["hgrn_lb_logits"])
    m["hgT"] = np.ascontiguousarray(np.asarray(inp["hgrn_norm_g"], f).T)
    m["w_branch"] = np.asarray(inp["w_branch"], f)
    m["w_out"] = np.asarray(inp["w_out"], f)
    for k in ("ffn_w1", "ffn_w3", "ffn_w2", "moe_w1", "moe_w3", "moe_w2"):
        m[k] = np.asarray(inp[k], f)
    r = np.asarray(inp["moe_router"], f)
    m["routerT"] = np.ascontiguousarray(np.transpose(r.reshape(2, 8, 128, 8), (2, 0, 1, 3)))
    for k, v in _const_tables().items():
        m["c_" + k] = v
    return m


_CACHE = {}


def kernel(**inputs):
    if "nc" not in _CACHE:
        _CACHE["nc"] = MK().build()
    nc = _CACHE["nc"]
    in_maps = [host_inputs(inputs, b) for b in range(8)]
    res = run_bass_kernel_spmd(nc, in_maps, core_ids=list(range(8)))
    out = np.stack([np.ascontiguousarray(res.results[b]["xs"][:, NCTX:].T) for b in range(8)], axis=0)
    return out.astype(np.float32)
```
